# Optimizing a Trainium2 kernel written in Bass

```python
import math
import jax, jax.numpy as jnp
from jax import lax
import numpy as np

D_MODEL = 2048
BATCH = 16
SEQ = 2048
DEPTH = 1

CHUNK = 64
N_META = 16
ATTN_HEADS = 8
ATTN_HEAD_DIM = 64
ATTN_VDIM = 2 * ATTN_HEAD_DIM
ATTN_WIDTH = ATTN_HEADS * 2 * ATTN_HEAD_DIM
Q_BLOCK = 128
SSM_WIDTH = D_MODEL // 4
SSM_GROUP = 16
SSM_GROUPS = SSM_WIDTH // SSM_GROUP
SSM_STATE = 64
DT_MIN = 1e-3
DT_MAX = 1e-1
N_EXPERT_GROUPS = 4
EXPERTS_PER_GROUP = 8
N_EXPERTS = N_EXPERT_GROUPS * EXPERTS_PER_GROUP
TOP_K = 2
D_FF_EXPERT = D_MODEL // 4
ROW_BLOCK = 128
IN_COLS = SSM_WIDTH + 3 * ATTN_WIDTH + 2 * D_MODEL
LN_EPS = 1e-5
DEEPNORM_ALPHA = (2.0 * DEPTH) ** 0.25
DEEPNORM_BETA = (8.0 * DEPTH) ** -0.25

kernel_name = "hybrid_s5_diffattn_hmoe_deepnorm"


def layer_norm(x, g, b):
    xf = x.astype(jnp.float32)
    mu = jnp.mean(xf, axis=-1, keepdims=True)
    var = jnp.mean(jnp.square(xf - mu), axis=-1, keepdims=True)
    return ((xf - mu) * lax.rsqrt(var + LN_EPS) * g + b).astype(x.dtype)


def chunk_index(pos):
    return jnp.where(pos < N_META, 0, 1 + (pos - N_META) // CHUNK)


def _ssm_combine(earlier, later):
    a1r, a1i, b1r, b1i = earlier
    a2r, a2i, b2r, b2i = later
    ar = a2r * a1r - a2i * a1i
    ai = a2r * a1i + a2i * a1r
    br = a2r * b1r - a2i * b1i + b2r
    bi = a2r * b1i + a2i * b1r + b2i
    return ar, ai, br, bi


def s5_branch(u, a_re, a_im, log_dt, b_re, b_im, c_re, c_im, d_skip, w_glu):
    bsz, L, _ = u.shape
    f32 = jnp.float32
    uf = u.astype(f32).reshape(bsz, L, SSM_GROUPS, SSM_GROUP)
    lam_re = jnp.minimum(a_re.astype(f32), -1e-4)
    lam_im = a_im.astype(f32)
    dt = jnp.exp(log_dt.astype(f32))[:, None]
    mag = jnp.exp(lam_re * dt)
    ab_re = mag * jnp.cos(lam_im * dt)
    ab_im = mag * jnp.sin(lam_im * dt)
    den = lam_re * lam_re + lam_im * lam_im
    nr = ab_re - 1.0
    ni = ab_im
    z_re = (nr * lam_re + ni * lam_im) / den
    z_im = (ni * lam_re - nr * lam_im) / den
    br32 = b_re.astype(f32)
    bi32 = b_im.astype(f32)
    bb_re = z_re[..., None] * br32 - z_im[..., None] * bi32
    bb_im = z_re[..., None] * bi32 + z_im[..., None] * br32
    bu_re = jnp.einsum('blgc,gpc->blgp', uf, bb_re)
    bu_im = jnp.einsum('blgc,gpc->blgp', uf, bb_im)
    a_re_t = jnp.broadcast_to(ab_re, (1, L, SSM_GROUPS, SSM_STATE))
    a_im_t = jnp.broadcast_to(ab_im, (1, L, SSM_GROUPS, SSM_STATE))
    _, _, s_re, s_im = lax.associative_scan(_ssm_combine, (a_re_t, a_im_t, bu_re, bu_im), axis=1)
    y = (jnp.einsum('blgp,gcp->blgc', s_re, c_re.astype(f32))
         - jnp.einsum('blgp,gcp->blgc', s_im, c_im.astype(f32)))
    y = y + d_skip.astype(f32) * uf
    y = jax.nn.gelu(y.reshape(bsz, L, SSM_WIDTH))
    y = y * jax.nn.sigmoid(y @ w_glu.astype(f32))
    return y.astype(u.dtype)


def diff_attention(q, k, v, lam, lambda_init, subln_g):
    bsz, L = q.shape[0], q.shape[1]
    nqb = -(-L // Q_BLOCK)
    lq = nqb * Q_BLOCK
    qp = jnp.pad(q, ((0, 0), (0, lq - L), (0, 0), (0, 0), (0, 0)))
    qb = qp.reshape(bsz, nqb, Q_BLOCK, ATTN_HEADS, 2, ATTN_HEAD_DIM).transpose(1, 0, 2, 3, 4, 5)
    qpos = jnp.arange(lq).reshape(nqb, Q_BLOCK)
    kchunk = chunk_index(jnp.arange(L))
    scale = ATTN_HEAD_DIM ** -0.5

    def block(args):
        qblk, pos = args
        s = jnp.einsum('bqhmd,bkhmd->bhmqk', qblk, k).astype(jnp.float32) * scale
        allowed = chunk_index(pos)[:, None] >= kchunk[None, :]
        s = jnp.where(allowed, s, -1e30)
        p = jax.nn.softmax(s, axis=-1)
        a = p[:, :, 0] - lam * p[:, :, 1]
        return jnp.einsum('bhqk,bkhe->bqhe', a.astype(v.dtype), v)

    o = lax.map(block, (qb, qpos))
    o = o.transpose(1, 0, 2, 3, 4).reshape(bsz, lq, ATTN_HEADS, ATTN_VDIM)[:, :L]
    of = o.astype(jnp.float32)
    of = of * lax.rsqrt(jnp.mean(of * of, axis=-1, keepdims=True) + LN_EPS) * subln_g * (1.0 - lambda_init)
    return of.reshape(bsz, L, ATTN_WIDTH).astype(q.dtype)


def hier_moe(h, w_rg, b_rg, w_re, b_re, w_gate, w_up, w_down):
    bsz, L, d = h.shape
    x = h.reshape(-1, d)
    T = bsz * L
    g_prob = jax.nn.softmax((x @ w_rg + b_rg).astype(jnp.float32), axis=-1)
    g_val, g_idx = lax.top_k(g_prob, 1)
    e_logits = (x @ w_re + b_re).astype(jnp.float32).reshape(T, N_EXPERT_GROUPS, EXPERTS_PER_GROUP)
    e_sel = jnp.take_along_axis(e_logits, g_idx[:, :, None], axis=1)[:, 0]
    e_val, e_idx = lax.top_k(jax.nn.softmax(e_sel, axis=-1), TOP_K)
    e_val = e_val / jnp.sum(e_val, axis=-1, keepdims=True)
    weights = g_val * e_val
    expert = g_idx * EXPERTS_PER_GROUP + e_idx

    A = T * TOP_K
    flat_e = expert.reshape(-1).astype(jnp.int32)
    flat_tok = jnp.repeat(jnp.arange(T, dtype=jnp.int32), TOP_K)
    flat_w = weights.reshape(-1)
    order = jnp.argsort(flat_e)
    se = flat_e[order]
    stok = flat_tok[order]
    sw = flat_w[order]
    counts = jnp.bincount(flat_e, length=N_EXPERTS)
    starts = jnp.cumsum(counts) - counts
    padded = (counts + ROW_BLOCK - 1) // ROW_BLOCK * ROW_BLOCK
    pad_ends = jnp.cumsum(padded)
    pad_starts = pad_ends - padded
    dest = pad_starts[se] + (jnp.arange(A) - starts[se])
    n_blocks = -(-(A + N_EXPERTS * (ROW_BLOCK - 1)) // ROW_BLOCK)
    R = n_blocks * ROW_BLOCK
    row_tok = jnp.full((R,), T, jnp.int32).at[dest].set(stok)
    row_w = jnp.zeros((R,), jnp.float32).at[dest].set(sw)
    block_exp = jnp.minimum(jnp.searchsorted(pad_ends, jnp.arange(n_blocks) * ROW_BLOCK, side='right'),
                            N_EXPERTS - 1)
    x_pad = jnp.concatenate([x, jnp.zeros((1, d), x.dtype)], axis=0)

    def run_block(args):
        toks, e = args
        xb = x_pad[toks]
        hb = jax.nn.silu(xb @ w_gate[e]) * (xb @ w_up[e])
        return hb @ w_down[e]

    y_rows = lax.map(run_block, (row_tok.reshape(n_blocks, ROW_BLOCK), block_exp))
    y_rows = y_rows.reshape(R, d) * row_w[:, None].astype(y_rows.dtype)
    y = jax.ops.segment_sum(y_rows, row_tok, num_segments=T + 1)[:T]
    return y.reshape(bsz, L, d)


def setup_inputs(seed: int = 0) -> dict:
    key = jax.random.key(seed)
    ks = iter(jax.random.split(key, 40))
    nrm = lambda shape, std: jax.random.normal(next(ks), shape, jnp.float32) * std
    Dp = DEPTH
    n_idx = jnp.arange(SSM_STATE, dtype=jnp.float32)
    a_im0 = jnp.broadcast_to(math.pi * n_idx, (Dp, SSM_GROUPS, SSM_STATE))
    return {
        "x": nrm((BATCH, SEQ, D_MODEL), 1.0),
        "meta_tokens": nrm((N_META, D_MODEL), 1.0),
        "ln_in_g": 1.0 + nrm((D_MODEL,), 0.02),
        "ln_in_b": nrm((D_MODEL,), 0.02),
        "w_in": nrm((Dp, D_MODEL, IN_COLS), D_MODEL ** -0.5),
        "ssm_a_re": -0.5 + nrm((Dp, SSM_GROUPS, SSM_STATE), 0.01),
        "ssm_a_im": a_im0 + nrm((Dp, SSM_GROUPS, SSM_STATE), 0.01),
        "ssm_log_dt": jax.random.uniform(next(ks), (Dp, SSM_GROUPS), jnp.float32,
                                         math.log(DT_MIN), math.log(DT_MAX)),
        "ssm_b_re": nrm((Dp, SSM_GROUPS, SSM_STATE, SSM_GROUP), SSM_GROUP ** -0.5),
        "ssm_b_im": nrm((Dp, SSM_GROUPS, SSM_STATE, SSM_GROUP), SSM_GROUP ** -0.5),
        "ssm_c_re": nrm((Dp, SSM_GROUPS, SSM_GROUP, SSM_STATE), SSM_STATE ** -0.5),
        "ssm_c_im": nrm((Dp, SSM_GROUPS, SSM_GROUP, SSM_STATE), SSM_STATE ** -0.5),
        "ssm_d": nrm((Dp, SSM_GROUPS, SSM_GROUP), 1.0),
        "ssm_w_glu": nrm((Dp, SSM_WIDTH, SSM_WIDTH), SSM_WIDTH ** -0.5),
        "attn_lambda_q1": nrm((Dp, ATTN_HEAD_DIM), 0.1),
        "attn_lambda_k1": nrm((Dp, ATTN_HEAD_DIM), 0.1),
        "attn_lambda_q2": nrm((Dp, ATTN_HEAD_DIM), 0.1),
        "attn_lambda_k2": nrm((Dp, ATTN_HEAD_DIM), 0.1),
        "attn_subln_g": 1.0 + nrm((Dp, ATTN_VDIM), 0.02),
        "w_br_ssm": nrm((Dp, SSM_WIDTH, D_MODEL), SSM_WIDTH ** -0.5 * DEEPNORM_BETA),
        "w_br_attn": nrm((Dp, ATTN_WIDTH, D_MODEL), ATTN_WIDTH ** -0.5 * DEEPNORM_BETA),
        "w_o": nrm((Dp, D_MODEL, D_MODEL), D_MODEL ** -0.5 * DEEPNORM_BETA),
        "ln1_g": 1.0 + nrm((Dp, D_MODEL), 0.02),
        "ln1_b": nrm((Dp, D_MODEL), 0.02),
        "router_g_w": nrm((Dp, D_MODEL, N_EXPERT_GROUPS), D_MODEL ** -0.5),
        "router_g_b": nrm((Dp, N_EXPERT_GROUPS), 0.01),
        "router_e_w": nrm((Dp, D_MODEL, N_EXPERTS), D_MODEL ** -0.5),
        "router_e_b": nrm((Dp, N_EXPERTS), 0.01),
        "exp_w_gate": nrm((Dp, N_EXPERTS, D_MODEL, D_FF_EXPERT), D_MODEL ** -0.5),
        "exp_w_up": nrm((Dp, N_EXPERTS, D_MODEL, D_FF_EXPERT), D_MODEL ** -0.5),
        "exp_w_down": nrm((Dp, N_EXPERTS, D_FF_EXPERT, D_MODEL), D_FF_EXPERT ** -0.5 * DEEPNORM_BETA),
        "ln2_g": 1.0 + nrm((Dp, D_MODEL), 0.02),
        "ln2_b": nrm((Dp, D_MODEL), 0.02),
    }


def reference(x, meta_tokens, ln_in_g, ln_in_b, w_in, ssm_a_re, ssm_a_im, ssm_log_dt,
              ssm_b_re, ssm_b_im, ssm_c_re, ssm_c_im, ssm_d, ssm_w_glu,
              attn_lambda_q1, attn_lambda_k1, attn_lambda_q2, attn_lambda_k2, attn_subln_g,
              w_br_ssm, w_br_attn, w_o, ln1_g, ln1_b,
              router_g_w, router_g_b, router_e_w, router_e_b,
              exp_w_gate, exp_w_up, exp_w_down, ln2_g, ln2_b):
    bsz = x.shape[0]
    meta = jnp.broadcast_to(meta_tokens[None].astype(x.dtype), (bsz, N_META, D_MODEL))
    h = jnp.concatenate([meta, x], axis=1)
    L = h.shape[1]
    h = layer_norm(h, ln_in_g, ln_in_b)
    splits = np.cumsum([SSM_WIDTH, ATTN_WIDTH, ATTN_WIDTH, ATTN_WIDTH, D_MODEL]).tolist()
    for l in range(DEPTH):
        lambda_init = 0.8 - 0.6 * math.exp(-0.3 * l)
        proj = h @ w_in[l]
        u_ssm, q, k, v, g_ssm, g_attn = jnp.split(proj, splits, axis=-1)
        y_ssm = s5_branch(u_ssm, ssm_a_re[l], ssm_a_im[l], ssm_log_dt[l], ssm_b_re[l], ssm_b_im[l],
                          ssm_c_re[l], ssm_c_im[l], ssm_d[l], ssm_w_glu[l])
        q = q.reshape(bsz, L, ATTN_HEADS, 2, ATTN_HEAD_DIM)
        k = k.reshape(bsz, L, ATTN_HEADS, 2, ATTN_HEAD_DIM)
        v = v.reshape(bsz, L, ATTN_HEADS, ATTN_VDIM)
        lam = (jnp.exp(jnp.sum(attn_lambda_q1[l].astype(jnp.float32) * attn_lambda_k1[l].astype(jnp.float32)))
               - jnp.exp(jnp.sum(attn_lambda_q2[l].astype(jnp.float32) * attn_lambda_k2[l].astype(jnp.float32)))
               + lambda_init)
        y_attn = diff_attention(q, k, v, lam, lambda_init, attn_subln_g[l])
        merged = (jax.nn.sigmoid(g_ssm) * (y_ssm @ w_br_ssm[l])
                  + jax.nn.sigmoid(g_attn) * (y_attn @ w_br_attn[l]))
        mix = merged @ w_o[l]
        h = layer_norm(DEEPNORM_ALPHA * h + mix, ln1_g[l], ln1_b[l])
        ffn = hier_moe(h, router_g_w[l], router_g_b[l], router_e_w[l], router_e_b[l],
                       exp_w_gate[l], exp_w_up[l], exp_w_down[l])
        h = layer_norm(DEEPNORM_ALPHA * h + ffn, ln2_g[l], ln2_b[l])
    return h[:, N_META:]
```

```python
import math
from contextlib import ExitStack

import numpy as np
import concourse.bass as bass
import concourse.mybir as mybir
from concourse.bass_utils import run_bass_kernel_spmd

F32 = mybir.dt.float32
F32R = mybir.dt.float32r
BF16 = mybir.dt.bfloat16
U32 = mybir.dt.uint32
I32 = mybir.dt.int32
AF = mybir.ActivationFunctionType
ALU = mybir.AluOpType
AX = mybir.AxisListType

D = 2048
KC = 16
SEQ = 2048
NSEQ = 2
NREAL = NSEQ * SEQ
NTOK = NREAL + 128
META0 = NREAL
IN_COLS = 7680
LN_EPS = 1e-5
ALPHA = 2.0 ** 0.25
LAMBDA_INIT = 0.2
NCORES = 8


class Buf:
    __slots__ = ("name", "w", "r")

    def __init__(self, name):
        self.name = name
        self.w = None
        self.r = {}


class Prog:
    ENG = ("pe", "dve", "act", "pool", "sp")

    def __init__(self, nc, es):
        self.nc = nc
        self.es = es
        self.ops = {e: [] for e in self.ENG}
        self.sems = {}
        self.cnt = {}
        self.seen = {e: {} for e in self.ENG}
        self.pending = {e: {} for e in self.ENG}
        self.nbuf = 0

    def reg(self, eng, val):
        if not hasattr(self, "_regs"):
            self._regs = {}
        if val not in self._regs:
            self._regs[val] = eng.to_reg(val)
        return self._regs[val]

    def buf(self, name=None):
        self.nbuf += 1
        return Buf(name or f"b{self.nbuf}")

    def bufs(self, n, name="b"):
        return [self.buf(f"{name}{i}") for i in range(n)]

    def _mksem(self, key):
        self.sems[key] = self.es.enter_context(self.nc.semaphore("s_" + key))
        self.cnt[key] = 0

    def op(self, eng, emit, reads=(), writes=(), dma=None):
        if dma:
            dma = "d_" + (writes[0].name if writes else reads[0].name)
        key = dma if dma else eng
        if key not in self.sems:
            self._mksem(key)
        waits = dict(self.pending[eng])
        self.pending[eng] = {}

        def need(tok, kind):
            k, v = tok
            if dma is None and k == eng:
                if eng == "pe" or kind != "raw":
                    return
            if v > waits.get(k, 0):
                waits[k] = v

        for b in reads:
            if b.w:
                need(b.w, "raw")
        for b in writes:
            if b.w:
                need(b.w, "waw")
            for k, v in b.r.items():
                need((k, v), "war")
        wl = []
        for k, v in waits.items():
            if self.seen[eng].get(k, 0) >= v:
                continue
            self.seen[eng][k] = v
            wl.append((k, v))
        inc = 16 if dma else 1
        self.cnt[key] += inc
        tok = (key, self.cnt[key])
        self.ops[eng].append((wl, emit, key, inc))
        for b in writes:
            b.w = tok
            b.r = {}
        for b in reads:
            if b not in writes:
                if b.r.get(key, 0) < tok[1]:
                    b.r[key] = tok[1]
        return tok

    def barrier(self):
        for e in self.ENG:
            for k, v in self.cnt.items():
                if v > self.pending[e].get(k, 0):
                    self.pending[e][k] = v

    def flush(self):
        nc = self.nc
        self.barrier()
        for e in self.ENG:
            wl = []
            for k, v in self.pending[e].items():
                if self.seen[e].get(k, 0) < v:
                    self.seen[e][k] = v
                    wl.append((k, v))
            self.pending[e] = {}
            self.ops[e].append((wl, None, None, 0))
        sems = self.sems

        def mk(lst):
            def body(eng):
                for wl, emit, key, inc in lst:
                    for k, v in wl:
                        eng.wait_ge(sems[k], v)
                    if emit is not None:
                        ins = emit(eng)
                        ins.then_inc(sems[key], inc)
            return body

        with nc.Block() as block:
            block.tensor(mk(self.ops["pe"]))
            block.vector(mk(self.ops["dve"]))
            block.scalar(mk(self.ops["act"]))
            block.gpsimd(mk(self.ops["pool"]))
            block.sync(mk(self.ops["sp"]))
        self.ops = {e: [] for e in self.ENG}
        self._regs = {}

    emit_all = flush


class Ring:
    def __init__(self, items):
        self.items = items
        self.i = 0

    def next(self):
        it = self.items[self.i % len(self.items)]
        self.i += 1
        return it


def build_program(dbg=None):
    nc = bass.Bass("TRN2", target_bir_lowering=False)
    nc.dge_precook = False
    es = ExitStack()
    P = Prog(nc, es)

    def din(name, shape, dt=F32):
        return nc.dram_tensor(name, list(shape), dt, kind="ExternalInput").ap()

    def dscr(name, shape, dt=F32):
        kind = "ExternalOutput" if (dbg and name in dbg) else "Internal"
        return nc.dram_tensor(name, list(shape), dt, kind=kind).ap()

    cur = {"es": es, "n": 0}

    def sb(name, shape, dt=F32):
        return cur["es"].enter_context(nc.sbuf_tensor(name, list(shape), dt))

    def psum_banks(n=8, width=512):
        cur["n"] += 1
        return [cur["es"].enter_context(nc.psum_tensor(f"ps{cur['n']}_{i}", [128, width], F32)) for i in range(n)]

    def begin_phase():
        cur["es"] = ExitStack()

    def end_phase():
        P.flush()
        cur["es"].close()
        cur["es"] = es

    x = din("x", [NREAL, D])
    meta = din("meta", [16, D])
    ident_d = din("ident_d", [128, 128])
    lng_in = din("ln_in_gT", [128, KC])
    lnb_in = din("ln_in_bT", [128, KC])
    w_in = din("w_in", [D, IN_COLS], F32R)
    out = nc.dram_tensor("out", [NREAL, D], F32, kind="ExternalOutput").ap()

    HT = dscr("HT", [KC, 128, NTOK], F32R)
    Usc = dscr("Usc", [4, 128, NTOK], BF16)
    Qsc = dscr("Qsc", [8, 128, NTOK], BF16)
    Ksc = dscr("Ksc", [8, 128, NTOK], BF16)
    Vsc = dscr("Vsc", [8, NTOK, 128], BF16)

    ident = sb("ident", [128, 128])
    identb = sb("identb", [128, 128], BF16)
    g_in = sb("g_in", [128, KC])
    b_in = sb("b_in", [128, KC])
    epst = sb("epst", [128, 1])
    B_ident, B_identb, B_gin, B_bin, B_eps = P.bufs(5, "c")
    P.op("sp", lambda e: e.dma_start(out=ident[:], in_=ident_d), writes=[B_ident], dma="ldc")
    P.op("sp", lambda e: e.dma_start(out=g_in[:], in_=lng_in), writes=[B_gin], dma="ldc")
    P.op("sp", lambda e: e.dma_start(out=b_in[:], in_=lnb_in), writes=[B_bin], dma="ldc")
    P.op("dve", lambda e: e.tensor_copy(out=identb[:], in_=ident[:]), reads=[B_ident], writes=[B_identb])
    P.op("dve", lambda e: e.memset(epst[:], LN_EPS), writes=[B_eps])

    P.flush()

    begin_phase()
    psb = psum_banks(8)
    PS = Ring([(psb[i], P.buf(f"ps1_{i}")) for i in range(8)])
    xt = [sb(f"xt{i}", [128, D]) for i in range(2)]
    XT = Ring([(xt[i], P.buf(f"xt{i}")) for i in range(2)])
    hT = sb("hT", [128, KC, 512])
    B_hT = P.buf("hT")
    wts = [sb(f"wt{i}", [128, 8, 512], F32R) for i in range(4)]
    WT = Ring([(wts[i], P.buf(f"wt{i}")) for i in range(4)])
    stat = [sb(f"stat{i}", [128, 4, 6]) for i in range(2)]
    mv = [sb(f"mv{i}", [128, 2]) for i in range(2)]
    rstd = [sb(f"rstd{i}", [128, 1]) for i in range(2)]
    ST = Ring([((stat[i], mv[i], rstd[i]), P.buf(f"st{i}")) for i in range(2)])
    ob = [sb(f"ob{i}", [128, 512], BF16) for i in range(3)]
    OB = Ring([(ob[i], P.buf(f"ob{i}")) for i in range(3)])
    vtm = [sb(f"vtm{i}", [128, 4, 128], BF16) for i in range(2)]
    VTM = Ring([(vtm[i], P.buf(f"vtm{i}")) for i in range(2)])

    w_in_v = w_in.rearrange("(k p) n -> p k n", p=128)

    def ln_tile_to_hT(src_ap, nrows, tcol, gT, bT, B_g, B_b):
        xa, Bx = XT.next()
        (sa, ma, ra), Bs = ST.next()
        if nrows < 128:
            P.op("dve", lambda e: e.memset(xa[:], 0.0), writes=[Bx])
        P.op("sp", lambda e: e.dma_start(out=xa[0:nrows, :], in_=src_ap), writes=[Bx], dma="ldx")
        sub = (dbg or {}).get('sub', 99)
        if sub < 2:
            return
        for q in range(4):
            P.op("dve", lambda e, q=q: e.bn_stats(out=sa[:, q, :], in_=xa[:, q * 512:(q + 1) * 512]),
                 reads=[Bx], writes=[Bs])
        P.op("dve", lambda e: e.bn_aggr(out=ma[:], in_=sa[:].rearrange("p a b -> p (a b)")), reads=[Bs], writes=[Bs])
        if sub < 3:
            return
        P.op("act", lambda e: e.activation(out=ra[:], in_=ma[:, 1:2], func=AF.Sqrt, bias=epst[:], scale=1.0),
             reads=[Bs, B_eps], writes=[Bs])
        P.op("dve", lambda e: e.reciprocal(out=ra[:], in_=ra[:]), reads=[Bs], writes=[Bs])
        if sub < 4:
            return
        P.op("dve", lambda e: e.tensor_scalar(out=xa[:], in0=xa[:], scalar1=ma[:, 0:1], scalar2=ra[:],
                                              op0=ALU.subtract, op1=ALU.mult), reads=[Bx, Bs], writes=[Bx])
        if sub < 5:
            return
        for b4 in range(4):
            pa, Bp = PS.next()
            for j in range(4):
                k = b4 * 4 + j
                P.op("pe", lambda e, k=k, j=j, pa=pa: e.transpose(out=pa[:, j * 128:(j + 1) * 128],
                                                           in_=xa[:, k * 128:(k + 1) * 128], identity=ident[:]),
                     reads=[Bx, B_ident], writes=[Bp])
            if sub < 6:
                continue
            if (dbg or {}).get('bar', 0):
                P.barrier()
            for j in range(4):
                k = b4 * 4 + j
                var = (dbg or {}).get('var', 0)
                if var == 0:
                    P.op("act", lambda e, k=k, j=j, pa=pa: e.activation(out=hT[:, k, tcol:tcol + 128].bitcast(F32R),
                                                                 in_=pa[:, j * 128:(j + 1) * 128], func=AF.Identity,
                                                                 scale=gT[:, k:k + 1], bias=bT[:, k:k + 1]),
                         reads=[Bp, B_g, B_b], writes=[B_hT])
                elif var == 3:
                    P.op("act", lambda e, k=k, j=j, pa=pa: e.activation(out=ident[:, :],
                                                                 in_=pa[:, j * 128:(j + 1) * 128], func=AF.Copy),
                         reads=[Bp, B_g, B_b], writes=[B_hT])
                elif var == 4:
                    P.op("act", lambda e, k=k, j=j, pa=pa: e.activation(out=hT[:, k, tcol:tcol + 128],
                                                                 in_=ident[:, :], func=AF.Copy),
                         reads=[Bp, B_g, B_b], writes=[B_hT])
                elif var == 1:
                    P.op("act", lambda e, k=k, j=j, pa=pa: e.activation(out=hT[:, k, tcol:tcol + 128],
                                                                 in_=pa[:, j * 128:(j + 1) * 128], func=AF.Copy),
                         reads=[Bp, B_g, B_b], writes=[B_hT])
                elif var == 2:
                    P.op("dve", lambda e, k=k, j=j, pa=pa: e.tensor_scalar(out=hT[:, k, tcol:tcol + 128],
                                                                 in0=pa[:, j * 128:(j + 1) * 128], scalar1=gT[:, k:k + 1],
                                                                 scalar2=bT[:, k:k + 1], op0=ALU.mult, op1=ALU.add),
                         reads=[Bp, B_g, B_b], writes=[B_hT])

    wcache = {}

    def proj_chunk(col0, ntok):
        base = (col0 // 512) * 512
        if wcache.get("base") != base:
            hv = []
            for hk in range(2):
                wa, Bw = WT.next()
                P.op("sp", lambda e, wa=wa, hk=hk: e.dma_start(out=wa[:], in_=w_in_v[:, hk * 8:(hk + 1) * 8, base:base + 512]), writes=[Bw], dma=1)
                hv.append((wa, Bw))
            wcache.update(base=base, hv=hv)
        hv = wcache["hv"]
        off = col0 - base
        pa, Bp = PS.next()
        for k in range(KC):
            wa, Bw = hv[k // 8]
            P.op("pe", lambda e, k=k, wa=wa: e.matmul(out=pa[:, 0:ntok], lhsT=wa[:, k % 8, off:off + 128],
                                                      rhs=hT[:, k, 0:ntok].bitcast(F32R), start=(k == 0), stop=(k == KC - 1)),
                 reads=[Bw, B_hT], writes=[Bp])
        return pa, Bp

    groups = [(g * 512, 512) for g in range(NREAL // 512)] + [(META0, 128)]
    if dbg and "ngroups" in dbg:
        groups = groups[:dbg["ngroups"]] + [groups[-1]]
    stage = (dbg or {}).get('stage', 99)
    for (tok0, ntok) in groups:
        if stage < 1:
            break
        ntile = ntok // 128
        wcache.clear()
        for t in range(ntile):
            if tok0 == META0:
                ln_tile_to_hT(meta, 16, 0, g_in, b_in, B_gin, B_bin)
            else:
                ln_tile_to_hT(x[tok0 + t * 128: tok0 + (t + 1) * 128, :], 128, t * 128, g_in, b_in, B_gin, B_bin)
        if stage >= 2:
          P.op("pool", lambda e, tok0=tok0, ntok=ntok: e.dma_start(
            out=HT[:, :, tok0:tok0 + ntok].rearrange("k p t -> p k t"), in_=hT[:, :, 0:ntok].bitcast(F32R)),
            reads=[B_hT], dma="st1")
        if stage < 3:
            continue
        plan = [("u", c, c * 128) for c in range(4)]
        if tok0 != META0:
            plan += [("q", c, 512 + c * 128) for c in range(8)]
        plan += [("k", c, 1536 + c * 128) for c in range(8)]
        plan += [("v", c, 2560 + c * 128) for c in range(8)]
        for (kind, c, col0) in plan:
            pa, Bp = proj_chunk(col0, ntok)
            oa, Bo = OB.next()
            if kind == "q":
                P.op("act", lambda e, pa=pa, oa=oa, ntok=ntok: e.activation(
                    out=oa[:, 0:ntok], in_=pa[:, 0:ntok], func=AF.Copy, scale=0.125), reads=[Bp], writes=[Bo])
            else:
                P.op("dve", lambda e, pa=pa, oa=oa, ntok=ntok: e.tensor_copy(out=oa[:, 0:ntok], in_=pa[:, 0:ntok]),
                     reads=[Bp], writes=[Bo])
            if kind != "v":
                dst = {"u": Usc, "q": Qsc, "k": Ksc}[kind]
                P.op("pool", lambda e, dst=dst, c=c, oa=oa, tok0=tok0, ntok=ntok: e.dma_start(
                    out=dst[c, :, tok0:tok0 + ntok], in_=oa[:, 0:ntok]), reads=[Bo], dma="st1")
            else:
                pt, Bpt = PS.next()
                ptb = pt[:].bitcast(BF16)
                va, Bv = VTM.next()
                for t in range(ntile):
                    P.op("pe", lambda e, t=t, oa=oa, ptb=ptb: e.transpose(
                        out=ptb[:, t * 128:(t + 1) * 128], in_=oa[:, t * 128:(t + 1) * 128], identity=identb[:]),
                        reads=[Bo, B_identb], writes=[Bpt])
                P.op("dve", lambda e, va=va, ptb=ptb, ntile=ntile: e.tensor_copy(
                    out=va[:, 0:ntile, :], in_=ptb[:, 0:ntile * 128].rearrange("p (t e) -> p t e", e=128)),
                    reads=[Bpt], writes=[Bv])
                P.op("pool", lambda e, c=c, va=va, tok0=tok0, ntile=ntile, ntok=ntok: e.dma_start(
                    out=Vsc[c, tok0:tok0 + ntok, :].rearrange("(t p) e -> p t e", p=128), in_=va[:, 0:ntile, :]),
                    reads=[Bv], dma="st1")
    end_phase()

    Yattn = dscr("Yattn", [8, 128, NTOK], F32R)
    lamv_d = din("lamv", [1, 256])
    gsub_d = din("gsub_b", [128, 128])


    YG = dscr("YG", [4, 128, NTOK], F32R)
    Yssm = dscr("Yssm", [4, 128, NTOK], F32R)
    s_are_d = din("ssm_are", [128, 16]); s_aim_d = din("ssm_aim", [128, 16]); s_ldt_d = din("ssm_ldt", [128, 16])
    s_bre_d = din("ssm_bre", [128, 16, 16]); s_bim_d = din("ssm_bim", [128, 16, 16])
    s_cre_d = din("ssm_cre", [128, 16, 16]); s_cim_d = din("ssm_cim", [128, 16, 16])
    s_d_d = din("ssm_dT", [128, 4]); wglu_d = din("ssm_wglu", [512, 512], F32R)
    TS = 1032
    tt_d = din("tt_d", [128, TS])
    PI = math.pi
    ph3 = (dbg or {}).get("ph3", 1)
    ph2 = (dbg or {}).get("ph2", 1)
    nseq2 = (dbg or {}).get("nseq2", NSEQ)
    mid = ExitStack()

    def sbm(name, shape, dt=F32):
        return mid.enter_context(nc.sbuf_tensor(name, list(shape), dt))

    if ph2:
        hpi = sbm("s_hpi", [128, 1]); B_hpi = P.buf("s_hpi")
        prm = sbm("s_prm", [128, 24, 16])
        dT = sbm("s_dT", [128, 4])
        tt = sbm("s_tt", [128, TS])
        wbt = sbm("s_wb", [128, 16, 2, 128], BF16)
        cwt = sbm("s_cw", [128, 16, 2, 128], BF16)
        begin_phase()
        yb = psum_banks(2, 512)
        YPS = Ring([(yb[i], P.buf(f"p2y{i}")) for i in range(2)])
        P.op("dve", lambda e: e.memset(hpi[:], PI / 2), writes=[B_hpi])
        NPRM = 24
        B_prm = P.buf("s_prm")
        names = ["are", "aim", "ldt", "lre", "dt", "mag", "ang", "sn", "cs", "t1", "t2", "ar", "ai", "nr", "den", "zr", "zi", "phs"]
        V = {n: prm[:, i, :] for i, n in enumerate(names)}
        bc = sb("s_bc", [128, 4, 16, 16]); B_bc = P.buf("s_bc")
        bb = sb("s_bb", [128, 4, 16, 16]); B_bb = P.buf("s_bb")
        B_dT = P.buf("s_dT")
        B_tt = P.buf("s_tt")
        P.op("sp", lambda e: e.dma_start(out=prm[:, 0, :], in_=s_are_d), writes=[B_prm], dma=1)
        P.op("sp", lambda e: e.dma_start(out=prm[:, 1, :], in_=s_aim_d), writes=[B_prm], dma=1)
        P.op("sp", lambda e: e.dma_start(out=prm[:, 2, :], in_=s_ldt_d), writes=[B_prm], dma=1)
        for i, dd in enumerate((s_bre_d, s_bim_d, s_cre_d, s_cim_d)):
            P.op("sp", lambda e, i=i, dd=dd: e.dma_start(out=bc[:, i, :, :], in_=dd), writes=[B_bc], dma=1)
        P.op("sp", lambda e: e.dma_start(out=dT[:], in_=s_d_d), writes=[B_dT], dma=1)
        P.op("sp", lambda e: e.dma_start(out=tt[:], in_=tt_d), writes=[B_tt], dma=1)

        def dv(fn, rd=(), wr=None):
            P.op("dve", fn, reads=[B_prm] + list(rd), writes=[wr or B_prm])

        def tt_(o, a, b, op):
            dv(lambda e: e.tensor_tensor(out=V[o], in0=V[a], in1=V[b], op=op))

        def ts_(o, a, s1, op0, s2=None, op1=None):
            if op1 is None:
                dv(lambda e: e.tensor_scalar(out=V[o], in0=V[a], scalar1=s1, scalar2=None, op0=op0))
            else:
                dv(lambda e: e.tensor_scalar(out=V[o], in0=V[a], scalar1=s1, scalar2=s2, op0=op0, op1=op1))

        def act_(o, a, func, scale=1.0):
            P.op("act", lambda e: e.activation(out=V[o], in_=V[a], func=func, scale=scale), reads=[B_prm], writes=[B_prm])

        ts_("lre", "are", -1e-4, ALU.min)
        act_("dt", "ldt", AF.Exp)
        tt_("t1", "lre", "dt", ALU.mult)
        act_("mag", "t1", AF.Exp)
        tt_("ang", "aim", "dt", ALU.mult)
        dv(lambda e: e.tensor_scalar(out=V["t1"].bitcast(I32), in0=V["ang"], scalar1=1.0 / (2 * PI), scalar2=None, op0=ALU.mult))
        dv(lambda e: e.tensor_copy(out=V["t2"], in_=V["t1"].bitcast(I32)))
        dv(lambda e: e.scalar_tensor_tensor(out=V["ang"], in0=V["t2"], scalar=-2 * PI, in1=V["ang"], op0=ALU.mult, op1=ALU.add))
        dv(lambda e: e.tensor_scalar(out=V["t1"], in0=V["ang"], scalar1=0.0, scalar2=2 * PI, op0=ALU.is_lt, op1=ALU.mult))
        tt_("ang", "ang", "t1", ALU.add)
        dv(lambda e: e.tensor_scalar(out=V["t1"], in0=V["ang"], scalar1=PI, scalar2=-2 * PI, op0=ALU.is_gt, op1=ALU.mult))
        tt_("t1", "t1", "ang", ALU.add)
        act_("sn", "t1", AF.Sin)
        dv(lambda e: e.tensor_scalar(out=V["t1"], in0=V["ang"], scalar1=PI / 2, scalar2=-2 * PI, op0=ALU.is_gt, op1=ALU.mult))
        dv(lambda e: e.scalar_tensor_tensor(out=V["t1"], in0=V["ang"], scalar=PI / 2, in1=V["t1"], op0=ALU.add, op1=ALU.add))
        act_("cs", "t1", AF.Sin)
        tt_("ar", "mag", "cs", ALU.mult)
        tt_("ai", "mag", "sn", ALU.mult)
        ts_("nr", "ar", -1.0, ALU.add)
        tt_("t1", "lre", "lre", ALU.mult)
        tt_("t2", "aim", "aim", ALU.mult)
        tt_("den", "t1", "t2", ALU.add)
        dv(lambda e: e.reciprocal(out=V["den"], in_=V["den"]))
        tt_("t1", "nr", "lre", ALU.mult)
        tt_("t2", "ai", "aim", ALU.mult)
        tt_("zr", "t1", "t2", ALU.add)
        tt_("zr", "zr", "den", ALU.mult)
        tt_("t1", "ai", "lre", ALU.mult)
        tt_("t2", "nr", "aim", ALU.mult)
        tt_("zi", "t1", "t2", ALU.subtract)
        tt_("zi", "zi", "den", ALU.mult)
        ts_("phs", "ang", float(TS), ALU.mult)
        zrb = V["zr"].unsqueeze(2).to_broadcast([128, 16, 16])
        zib = V["zi"].unsqueeze(2).to_broadcast([128, 16, 16])
        P.op("dve", lambda e: e.tensor_tensor(out=bb[:, 2], in0=bc[:, 0], in1=zrb, op=ALU.mult), reads=[B_prm, B_bc], writes=[B_bb])
        P.op("dve", lambda e: e.tensor_tensor(out=bb[:, 3], in0=bc[:, 1], in1=zib, op=ALU.mult), reads=[B_prm, B_bc], writes=[B_bb])
        P.op("dve", lambda e: e.tensor_tensor(out=bb[:, 0], in0=bb[:, 2], in1=bb[:, 3], op=ALU.subtract), reads=[B_bb], writes=[B_bb])
        P.op("dve", lambda e: e.tensor_tensor(out=bb[:, 2], in0=bc[:, 1], in1=zrb, op=ALU.mult), reads=[B_prm, B_bc, B_bb], writes=[B_bb])
        P.op("dve", lambda e: e.tensor_tensor(out=bb[:, 3], in0=bc[:, 0], in1=zib, op=ALU.mult), reads=[B_prm, B_bc, B_bb], writes=[B_bb])
        P.op("dve", lambda e: e.tensor_tensor(out=bb[:, 1], in0=bb[:, 2], in1=bb[:, 3], op=ALU.add), reads=[B_bb], writes=[B_bb])
        bmw = sb("s_bmw", [128, 16, 2, 128], BF16); B_bmw = P.buf("s_bmw")
        B_wb = P.buf("s_wb")
        B_cw = P.buf("s_cw")
        P.op("pool", lambda e: e.memset(bmw[:], 0.0), writes=[B_bmw])
        P.op("pool", lambda e: e.memset(cwt[:], 0.0), writes=[B_cw])
        for jm in range(4):
            for gl in range(2):
                c0 = 32 * jm + 16 * gl
                for ri in range(2):
                    P.op("dve", lambda e, jm=jm, gl=gl, ri=ri, c0=c0: e.tensor_copy(
                        out=bmw[64 * gl:64 * gl + 64, jm::4, ri, c0:c0 + 16], in_=bb[64 * gl:64 * gl + 64, ri, jm::4, :]),
                        reads=[B_bb], writes=[B_bmw])
                P.op("dve", lambda e, jm=jm, gl=gl, c0=c0: e.tensor_copy(
                    out=cwt[64 * gl:64 * gl + 64, jm::4, 0, c0:c0 + 16], in_=bc[64 * gl:64 * gl + 64, 2, jm::4, :]),
                    reads=[B_bc], writes=[B_cw])
                P.op("dve", lambda e, jm=jm, gl=gl, c0=c0: e.tensor_scalar(
                    out=cwt[64 * gl:64 * gl + 64, jm::4, 1, c0:c0 + 16], in0=bc[64 * gl:64 * gl + 64, 3, jm::4, :],
                    scalar1=-1.0, scalar2=None, op0=ALU.mult), reads=[B_bc], writes=[B_cw])
        for g4 in range(8):
            pa, Bp = YPS.next()
            pab = pa[:].bitcast(BF16)
            for i4 in range(4):
                idx = g4 * 4 + i4
                j, ri = idx // 2, idx % 2
                P.op("pe", lambda e, pab=pab, i4=i4, j=j, ri=ri: e.transpose(out=pab[:, i4 * 128:(i4 + 1) * 128], in_=bmw[:, j, ri, :],
                                                                             identity=identb[:]), reads=[B_bmw, B_identb], writes=[Bp])
            j0 = (g4 * 4) // 2
            P.op("dve", lambda e, pab=pab, j0=j0: e.tensor_copy(out=wbt[:, j0:j0 + 2, :, :].rearrange("p a b c -> p (a b c)"),
                                                                 in_=pab[:, 0:512]), reads=[Bp], writes=[B_wb])

        end_phase()

    begin_phase()
    psb = psum_banks(8)
    bankA = psb[6]; B_bankA = P.buf("p23_bankA")
    bankB = psb[7]; B_bankBt = P.buf("p23_bankB"); B_bankBy = B_bankBt
    g3 = None
    g2 = None
    if ph3:
        PS = Ring([(psb[7], B_bankBt)])
        lamv = sb("lamv_s", [1, 256]); lamt = sb("lamt", [1, 8]); ones1 = sb("ones1", [1, 128])
        neglam = sb("neglam", [128, 1]); gsub = sb("gsub", [128, 128])
        B_lam, B_neglam, B_gsub, B_ones1 = P.bufs(4, "a3c")
        P.op("sp", lambda e: e.dma_start(out=lamv[:], in_=lamv_d), writes=[B_lam], dma=1)
        P.op("sp", lambda e: e.dma_start(out=gsub[:], in_=gsub_d), writes=[B_gsub], dma=1)
        P.op("dve", lambda e: e.memset(ones1[:], 1.0), writes=[B_ones1])
        P.op("dve", lambda e: e.tensor_scalar(out=gsub[:], in0=gsub[:], scalar1=1.0 - LAMBDA_INIT, scalar2=None, op0=ALU.mult),
             reads=[B_gsub], writes=[B_gsub])
        P.op("dve", lambda e: e.tensor_tensor(out=lamv[:, 0:64], in0=lamv[:, 0:64], in1=lamv[:, 64:128], op=ALU.mult), reads=[B_lam], writes=[B_lam])
        P.op("dve", lambda e: e.tensor_tensor(out=lamv[:, 128:192], in0=lamv[:, 128:192], in1=lamv[:, 192:256], op=ALU.mult), reads=[B_lam], writes=[B_lam])
        P.op("dve", lambda e: e.tensor_reduce(out=lamt[:, 0:1], in_=lamv[:, 0:64], axis=AX.X, op=ALU.add), reads=[B_lam], writes=[B_lam])
        P.op("dve", lambda e: e.tensor_reduce(out=lamt[:, 1:2], in_=lamv[:, 128:192], axis=AX.X, op=ALU.add), reads=[B_lam], writes=[B_lam])
        P.op("act", lambda e: e.activation(out=lamt[:, 2:4], in_=lamt[:, 0:2], func=AF.Exp), reads=[B_lam], writes=[B_lam])
        P.op("dve", lambda e: e.tensor_tensor(out=lamt[:, 4:5], in0=lamt[:, 3:4], in1=lamt[:, 2:3], op=ALU.subtract), reads=[B_lam], writes=[B_lam])
        P.op("dve", lambda e: e.tensor_scalar(out=lamt[:, 5:6], in0=lamt[:, 4:5], scalar1=-LAMBDA_INIT, scalar2=None, op0=ALU.add), reads=[B_lam], writes=[B_lam])
        pa, Bp = PS.next()
        P.op("pe", lambda e, pa=pa: e.matmul(out=pa[:, 0:1], lhsT=ones1[:, :], rhs=lamt[:, 5:6], start=True, stop=True),
             reads=[B_lam, B_ones1], writes=[Bp])
        P.op("dve", lambda e, pa=pa: e.tensor_copy(out=neglam[:], in_=pa[:, 0:1]), reads=[Bp], writes=[B_neglam])

        ORING = Ring([(psb[i], P.buf(f"pso{i}")) for i in range(0, 4)])
        SRING = Ring([(psb[i], P.buf(f"pss{i}")) for i in range(4, 6)])
        TRING = Ring([(psb[7], B_bankBt)])
        kts = [sb(f"a_kt{i}", [128, 2, 128 + SEQ], BF16) for i in range(2)]
        qts = [sb(f"a_qt{i}", [128, SEQ], BF16) for i in range(2)]
        vts = [sb(f"a_vt{i}", [128, 17, 129], BF16) for i in range(2)]
        HRING = Ring([((kts[i], qts[i], vts[i]), (P.buf(f"a_kt{i}"), P.buf(f"a_qt{i}"), P.buf(f"a_vt{i}"), P.buf(f"a_vm{i}"))) for i in range(2)])
        for i in range(2):
            P.op("pool", lambda e, i=i: e.memset(vts[i][:], 1.0), writes=[HRING.items[i][1][2], HRING.items[i][1][3]])
            P.op("pool", lambda e, i=i: e.memset(vts[i][:, 16, :], 0.0), writes=[HRING.items[i][1][3]])
            P.op("pool", lambda e, i=i: e.memset(vts[i][0:16, 16, 128:129], 1.0), writes=[HRING.items[i][1][3]])
            P.op("pool", lambda e, i=i: e.memset(kts[i][:], 0.0), writes=[HRING.items[i][1][0]])
        pts = [sb(f"a_pt{i}", [128, 512], BF16) for i in range(5)]
        PTR = Ring([(pts[i], P.buf(f"a_pt{i}")) for i in range(5)])
        yTs = [sb(f"a_yT{i}", [128, SEQ], F32R) for i in range(2)]
        YTR = Ring([(yTs[i], P.buf(f"a_yT{i}")) for i in range(2)])
        eps3 = [(sb(f"a_rc{i}", [128, 4]), sb(f"a_t1{i}", [128, 128]), sb(f"a_od{i}", [128, 128]), sb(f"a_yq{i}", [128, 128]),
                 sb(f"a_jk{i}", [128, 128])) for i in range(4)]
        EPR = Ring([(eps3[i], P.bufs(5, f"a_ep{i}_")) for i in range(4)])

        def gen3():
            nseq3 = (dbg or {}).get("nseq3", NSEQ)
            nhead3 = (dbg or {}).get("nhead3", 8)
            for s_ in range(nseq3):
                for h in range(nhead3):
                    (kt, qt_, vt), (Bk, Bq, Bv, Bvm) = HRING.next()
                    r0 = s_ * SEQ
                    for m in range(2):
                        P.op("sp", lambda e, kt=kt, h=h, m=m: e.dma_start(out=kt[m * 64:(m + 1) * 64, m, 0:16], in_=Ksc[h, m * 64:(m + 1) * 64, META0:META0 + 16]), writes=[Bk], dma=1)
                        P.op("sp", lambda e, kt=kt, h=h, r0=r0, m=m: e.dma_start(out=kt[m * 64:(m + 1) * 64, m, 128:128 + SEQ], in_=Ksc[h, m * 64:(m + 1) * 64, r0:r0 + SEQ]), writes=[Bk], dma=1)
                    P.op("sp", lambda e, qt_=qt_, h=h, r0=r0: e.dma_start(out=qt_[:, :], in_=Qsc[h, :, r0:r0 + SEQ]), writes=[Bq], dma=1)
                    P.op("sp", lambda e, vt=vt, h=h, r0=r0: e.dma_start(out=vt[:, 0:16, 0:128], in_=Vsc[h, r0:r0 + SEQ, :].rearrange("(t p) e -> p t e", p=128)), writes=[Bv], dma=1)
                    P.op("sp", lambda e, vt=vt, h=h: e.dma_start(out=vt[0:16, 16, 0:128], in_=Vsc[h, META0:META0 + 16, :]), writes=[Bvm], dma=1)
                    yT, ByT = YTR.next()
                    steps = []
                    for qi in range(SEQ // 128):
                        blks = [("m", 0)] + [("r", kb) for kb in range(qi + 1)]
                        pairs = [blks[i:i + 2] for i in range(0, len(blks), 2)]
                        for pi_, pr in enumerate(pairs):
                            steps.append((qi, pi_, pr, len(pairs)))
                    LA = 2
                    pend = {}
                    oacc = {}
                    later = []

                    def emit_S(st):
                        qi, pi_, pr, npair = st
                        sp_, Bs_ = SRING.next()
                        for bj, (typ, kb) in enumerate(pr):
                            kc0 = 0 if typ == "m" else 128 + kb * 128
                            for m in range(2):
                                c0 = bj * 256 + m * 128
                                P.op("pe", lambda e, sp_=sp_, m=m, kc0=kc0, kt=kt, qt_=qt_, qi=qi, c0=c0: e.matmul(
                                    out=sp_[:, c0:c0 + 128], lhsT=kt[:, m, kc0:kc0 + 128],
                                    rhs=qt_[:, qi * 128:(qi + 1) * 128], start=True, stop=True),
                                    reads=[Bk, Bq], writes=[Bs_])
                        pt, Bpt = PTR.next()
                        w_ = 256 * len(pr)
                        P.op("act", lambda e, pt=pt, sp_=sp_, w_=w_: e.activation(out=pt[:, 0:w_], in_=sp_[:, 0:w_], func=AF.Exp),
                             reads=[Bs_], writes=[Bpt])
                        for bj, (typ, kb) in enumerate(pr):
                            if typ == "r" and kb == qi:
                                P.op("pool", lambda e, pt=pt, bj=bj: e.memset(pt[64:128, bj * 256:(bj + 1) * 256].rearrange("p (m q) -> p m q", m=2)[:, :, 0:64], 0.0),
                                     reads=[Bpt], writes=[Bpt])
                        pend[(qi, pi_)] = (pt, Bpt)

                    def emit_AV(st, now):
                        qi, pi_, pr, npair = st
                        pt, Bpt = pend.pop((qi, pi_))
                        if pi_ == 0:
                            oacc[qi] = (ORING.next(), ORING.next())
                        (o0, Bo0), (o1, Bo1) = oacc[qi]
                        for bj, (typ, kb) in enumerate(pr):
                            vidx = 16 if typ == "m" else kb
                            first = (pi_ == 0 and bj == 0)
                            last = (pi_ == npair - 1 and bj == len(pr) - 1)
                            for m, (oo, Boo) in enumerate(((o0, Bo0), (o1, Bo1))):
                                c0 = bj * 256 + m * 128
                                P.op("pe", lambda e, oo=oo, pt=pt, c0=c0, vt=vt, vidx=vidx, first=first, last=last: e.matmul(
                                    out=oo[:, 0:129], lhsT=pt[:, c0:c0 + 128], rhs=vt[:, vidx, :], start=first, stop=last),
                                    reads=[Bpt, Bv, Bvm], writes=[Boo])
                        if pi_ != npair - 1:
                            return
                        del oacc[qi]
                        (rc, t1, od, yq, jk), (Brc, Bt1, Bod, Byq, Bjk) = EPR.next()
                        P.op("dve", lambda e, rc=rc, o0=o0: e.reciprocal(out=rc[:, 0:1], in_=o0[:, 128:129]), reads=[Bo0], writes=[Brc])
                        P.op("dve", lambda e, rc=rc, o1=o1: e.reciprocal(out=rc[:, 1:2], in_=o1[:, 128:129]), reads=[Bo1], writes=[Brc])
                        P.op("dve", lambda e, rc=rc: e.tensor_scalar(out=rc[:, 2:3], in0=rc[:, 1:2], scalar1=neglam[:, 0:1], scalar2=None, op0=ALU.mult),
                             reads=[Brc, B_neglam], writes=[Brc])
                        P.op("dve", lambda e, rc=rc, t1=t1, o1=o1: e.tensor_scalar(out=t1[:], in0=o1[:, 0:128], scalar1=rc[:, 2:3], scalar2=None, op0=ALU.mult),
                             reads=[Brc, Bo1], writes=[Bt1])
                        P.op("dve", lambda e, rc=rc, t1=t1, o0=o0, od=od: e.scalar_tensor_tensor(out=od[:], in0=o0[:, 0:128], scalar=rc[:, 0:1], in1=t1[:],
                                                                                           op0=ALU.mult, op1=ALU.add),
                             reads=[Brc, Bo0, Bt1], writes=[Bod])
                        P.op("dve", lambda e, od=od, jk=jk, rc=rc: e.scalar_tensor_tensor(out=jk[:], in0=od[:], scalar=1.0, in1=od[:], op0=ALU.mult, op1=ALU.mult,
                                                                                    accum_out=rc[:, 3:4]),
                             reads=[Bod, Brc], writes=[Bjk, Brc])

                        def stage2(rc=rc, od=od, yq=yq, Brc=Brc, Bod=Bod, Byq=Byq):
                            P.op("act", lambda e: e.activation(out=rc[:, 3:4], in_=rc[:, 3:4], func=AF.Sqrt, bias=epst[:], scale=1.0 / 128.0),
                                 reads=[Brc, B_eps], writes=[Brc])
                            P.op("dve", lambda e: e.reciprocal(out=rc[:, 3:4], in_=rc[:, 3:4]), reads=[Brc], writes=[Brc])
                            P.op("dve", lambda e: e.scalar_tensor_tensor(out=yq[:], in0=od[:], scalar=rc[:, 3:4], in1=gsub[:],
                                                                         op0=ALU.mult, op1=ALU.mult),
                                 reads=[Brc, Bod, B_gsub], writes=[Byq])

                        def stage3(yq=yq, Byq=Byq, qi=qi, yT=yT, ByT=ByT):
                            tp, Btp = TRING.next()
                            P.op("pe", lambda e: e.transpose(out=tp[:, 0:128], in_=yq[:], identity=ident[:]), reads=[Byq, B_ident], writes=[Btp])
                            P.op("dve", lambda e: e.tensor_copy(out=yT[:, qi * 128:(qi + 1) * 128], in_=tp[:, 0:128]),
                                 reads=[Btp], writes=[ByT])

                        later.append((now + 3, stage2))
                        later.append((now + 6, stage3))

                    def run_later(now):
                        keep = []
                        for due, fn in later:
                            if due <= now:
                                fn()
                            else:
                                keep.append((due, fn))
                        later[:] = keep

                    nst = len(steps)
                    for i in range(nst + LA):
                        if i < nst:
                            emit_S(steps[i])
                        if i >= LA:
                            emit_AV(steps[i - LA], i)
                        run_later(i)
                        yield
                    run_later(10 ** 9)
                    P.op("pool", lambda e, yT=yT, h=h, r0=r0: e.dma_start(out=Yattn[h, :, r0:r0 + SEQ], in_=yT[:, :]), reads=[ByT], dma=1)

        g3 = gen3()
    if ph2:
        uTs = [[sb(f"s_uT{s_}_{i}", [128, TS], BF16) for i in range(2)] for s_ in range(NSEQ)]
        UTR = [Ring([(uTs[s_][i], P.buf(f"s_uT{s_}_{i}")) for i in range(2)]) for s_ in range(NSEQ)]
        wk = {n: sb("s_" + n, [128, TS]) for n in ("A1", "A2", "M1", "M2", "M3", "M4", "Z1", "Z2", "W1", "W2", "N1", "N2", "N3", "N4",
                                                   "ST", "CT", "Rt")}
        Bw = {n: P.buf("s_" + n) for n in wk}
        ygs = [sb(f"s_yg{i}", [128, TS], F32R) for i in range(1)]
        YGR = Ring([(ygs[i], P.buf(f"s_yg{i}")) for i in range(1)])
        Xs = [sb(f"s_X{s_}", [128, 4, 2, TS], BF16) for s_ in range(NSEQ)]
        B_X = [[[P.buf(f"s_X{s_}{a}{b}") for b in range(2)] for a in range(4)] for s_ in range(NSEQ)]
        carry = sb("s_carry", [128, NSEQ, 16, 2]); B_carry = P.buf("s_carry")

        def gen2():
            CT6 = lambda ap: ap.rearrange("p (a b) -> p a b", b=172)
            for seg in range(2):
                for q in range(4):
                    uT_s = []
                    for s_ in range(nseq2):
                        r0 = s_ * SEQ
                        uT, BuT = UTR[s_].next()
                        if seg == 0:
                            P.op("sp", lambda e, uT=uT, q=q: e.dma_start(out=uT[:, 0:16], in_=Usc[q, :, META0:META0 + 16]), writes=[BuT], dma=1)
                            P.op("sp", lambda e, uT=uT, q=q, r0=r0: e.dma_start(out=uT[:, 16:TS], in_=Usc[q, :, r0:r0 + TS - 16]), writes=[BuT], dma=1)
                        else:
                            P.op("sp", lambda e, uT=uT, q=q, r0=r0: e.dma_start(out=uT[:, :], in_=Usc[q, :, r0 + TS - 16:r0 + SEQ]), writes=[BuT], dma=1)
                        uT_s.append((uT, BuT))
                    for jm in range(4):
                        j = 4 * q + jm
                        phj = V["ang"][:, j:j + 1]
                        if seg == 0:
                            P.op("dve", lambda e, phj=phj: e.tensor_scalar(out=wk["A1"][:], in0=tt[:], scalar1=phj, scalar2=None, op0=ALU.mult),
                                 reads=[B_tt, B_prm], writes=[Bw["A1"]])
                        else:
                            P.op("dve", lambda e, phj=phj, j=j: e.tensor_scalar(out=wk["A1"][:], in0=tt[:], scalar1=phj, scalar2=V["phs"][:, j:j + 1],
                                                                            op0=ALU.mult, op1=ALU.add), reads=[B_tt, B_prm], writes=[Bw["A1"]])
                        P.op("dve", lambda e: e.tensor_scalar(out=wk["A2"][:].bitcast(I32), in0=wk["A1"][:], scalar1=1.0 / (2 * PI), scalar2=None, op0=ALU.mult),
                             reads=[Bw["A1"]], writes=[Bw["A2"]])
                        P.op("dve", lambda e: e.scalar_tensor_tensor(out=wk["A1"][:], in0=wk["A2"][:].bitcast(I32), scalar=-2 * PI, in1=wk["A1"][:], op0=ALU.mult, op1=ALU.add),
                             reads=[Bw["A2"], Bw["A1"]], writes=[Bw["A1"]])
                        P.op("dve", lambda e: e.tensor_scalar(out=wk["A1"][:], in0=wk["A1"][:], scalar1=-PI, scalar2=PI, op0=ALU.max, op1=ALU.min),
                             reads=[Bw["A1"]], writes=[Bw["A1"]])
                        for _ in range(3):
                            yield
                        P.op("act", lambda e: e.activation(out=wk["ST"][:], in_=wk["A1"][:], func=AF.Sin), reads=[Bw["A1"]], writes=[Bw["ST"]])
                        P.op("act", lambda e: e.activation(out=wk["A2"][:], in_=wk["A1"][:], func=AF.Abs), reads=[Bw["A1"]], writes=[Bw["A2"]])
                        P.op("act", lambda e: e.activation(out=wk["CT"][:], in_=wk["A2"][:], func=AF.Sin, scale=-1.0, bias=hpi[:]), reads=[Bw["A2"], B_hpi], writes=[Bw["CT"]])
                        P.op("pool", lambda e, j=j: e.tensor_scalar(out=wk["Rt"][:], in0=tt[:], scalar1=0.0, scalar2=V["mag"][:, j:j + 1], op0=ALU.mult, op1=ALU.add),
                             reads=[B_tt, B_prm], writes=[Bw["Rt"]])
                        yield
                        for s_ in range(nseq2):
                            uT, BuT = uT_s[s_]
                            for p6 in range(6):
                                c0 = p6 * 172
                                for ri in range(2):
                                    P.op("pe", lambda e, ri=ri, c0=c0, j=j, uT=uT: e.matmul(
                                        out=bankA[:, ri * 172:(ri + 1) * 172], lhsT=wbt[:, j, ri, :], rhs=uT[:, c0:c0 + 172],
                                        start=True, stop=True), reads=[B_wb, BuT], writes=[B_bankA])
                                bur, bui = bankA[:, 0:172], bankA[:, 172:344]
                                for (mn, src, tab) in (("M1", bur, "CT"), ("M2", bui, "ST"), ("M3", bui, "CT"), ("M4", bur, "ST")):
                                    P.op("dve", lambda e, mn=mn, src=src, tab=tab, c0=c0: e.tensor_tensor(out=wk[mn][:, c0:c0 + 172], in0=src, in1=wk[tab][:, c0:c0 + 172], op=ALU.mult),
                                         reads=[B_bankA, Bw[tab]], writes=[Bw[mn]])
                                yield
                            P.op("pool", lambda e: e.tensor_tensor(out=wk["Z1"][:], in0=wk["M1"][:], in1=wk["M2"][:], op=ALU.add),
                                 reads=[Bw["M1"], Bw["M2"]], writes=[Bw["Z1"]])
                            P.op("pool", lambda e: e.tensor_tensor(out=wk["Z2"][:], in0=wk["M3"][:], in1=wk["M4"][:], op=ALU.subtract),
                                 reads=[Bw["M3"], Bw["M4"]], writes=[Bw["Z2"]])
                            yield
                            for ri, (zn, wn) in enumerate((("Z1", "W1"), ("Z2", "W2"))):
                                if seg == 0:
                                    P.op("dve", lambda e, zn=zn, wn=wn: e.tensor_tensor_scan(out=wk[wn][:], data0=wk["Rt"][:], data1=wk[zn][:], initial=0.0,
                                                                                          op0=ALU.mult, op1=ALU.add),
                                         reads=[Bw["Rt"], Bw[zn]], writes=[Bw[wn]])
                                    P.op("dve", lambda e, wn=wn, j=j, ri=ri, s_=s_: e.tensor_copy(out=carry[:, s_, j, ri:ri + 1], in_=wk[wn][:, TS - 1:TS]),
                                         reads=[Bw[wn]], writes=[B_carry])
                                else:
                                    P.op("dve", lambda e, zn=zn, wn=wn, j=j, ri=ri, s_=s_: e.tensor_tensor_scan(out=wk[wn][:], data0=wk["Rt"][:], data1=wk[zn][:],
                                                                                                        initial=carry[:, s_, j, ri:ri + 1], op0=ALU.mult, op1=ALU.add),
                                         reads=[Bw["Rt"], Bw[zn], B_carry], writes=[Bw[wn]])
                            yield
                            P.op("dve", lambda e: e.tensor_tensor(out=wk["N1"][:], in0=wk["W1"][:], in1=wk["CT"][:], op=ALU.mult),
                                 reads=[Bw["W1"], Bw["CT"]], writes=[Bw["N1"]])
                            P.op("dve", lambda e: e.tensor_tensor(out=wk["N2"][:], in0=wk["W2"][:], in1=wk["ST"][:], op=ALU.mult),
                                 reads=[Bw["W2"], Bw["ST"]], writes=[Bw["N2"]])
                            P.op("dve", lambda e, jm=jm, s_=s_: e.tensor_tensor(out=Xs[s_][:, jm, 0, :], in0=wk["N1"][:], in1=wk["N2"][:], op=ALU.subtract),
                                 reads=[Bw["N1"], Bw["N2"]], writes=[B_X[s_][jm][0]])
                            P.op("pool", lambda e: e.tensor_tensor(out=wk["N3"][:], in0=wk["W1"][:], in1=wk["ST"][:], op=ALU.mult),
                                 reads=[Bw["W1"], Bw["ST"]], writes=[Bw["N3"]])
                            P.op("pool", lambda e: e.tensor_tensor(out=wk["N4"][:], in0=wk["W2"][:], in1=wk["CT"][:], op=ALU.mult),
                                 reads=[Bw["W2"], Bw["CT"]], writes=[Bw["N4"]])
                            P.op("pool", lambda e, jm=jm, s_=s_: e.tensor_tensor(out=Xs[s_][:, jm, 1, :], in0=wk["N3"][:], in1=wk["N4"][:], op=ALU.add),
                                 reads=[Bw["N3"], Bw["N4"]], writes=[B_X[s_][jm][1]])
                            for _ in range(3):
                                yield
                    for _ in range(3):
                        yield
                    for s_ in range(nseq2):
                        r0 = s_ * SEQ
                        uT, BuT = uT_s[s_]
                        for p3 in range(3):
                            n = 0
                            for jm in range(4):
                                for ri in range(2):
                                    P.op("pe", lambda e, jm=jm, ri=ri, q=q, p3=p3, n=n, s_=s_: e.matmul(
                                        out=bankB[:, 128:472], lhsT=cwt[:, 4 * q + jm, ri, :], rhs=Xs[s_][:, jm, ri, p3 * 344:(p3 + 1) * 344],
                                        start=(n == 0), stop=(n == 7)), reads=[B_cw, B_X[s_][jm][ri]], writes=[B_bankBy])
                                    n += 1
                            P.op("dve", lambda e, uT=uT, q=q, p3=p3: e.scalar_tensor_tensor(
                                out=wk["M1"][:, p3 * 344:(p3 + 1) * 344], in0=uT[:, p3 * 344:(p3 + 1) * 344], scalar=dT[:, q:q + 1], in1=bankB[:, 128:472],
                                op0=ALU.mult, op1=ALU.add), reads=[B_bankBy, BuT, B_dT], writes=[Bw["M1"]])
                            yield
                        yg, Byg = YGR.next()
                        P.op("pool", lambda e: e.tensor_tensor(out=wk["M2"][:], in0=wk["M1"][:], in1=wk["M1"][:], op=ALU.mult), reads=[Bw["M1"]], writes=[Bw["M2"]])
                        P.op("pool", lambda e: e.tensor_scalar(out=wk["M2"][:], in0=wk["M2"][:], scalar1=0.044715, scalar2=1.0, op0=ALU.mult, op1=ALU.add),
                             reads=[Bw["M2"]], writes=[Bw["M2"]])
                        P.op("pool", lambda e: e.tensor_tensor(out=wk["M2"][:], in0=wk["M2"][:], in1=wk["M1"][:], op=ALU.mult), reads=[Bw["M2"], Bw["M1"]], writes=[Bw["M2"]])
                        for _ in range(4):
                            yield
                        P.op("act", lambda e: e.activation(out=wk["M2"][:], in_=wk["M2"][:], func=AF.Sigmoid, scale=1.5957691216057308),
                             reads=[Bw["M2"]], writes=[Bw["M2"]])
                        P.op("pool", lambda e, yg=yg: e.tensor_tensor(out=yg[:], in0=wk["M2"][:], in1=wk["M1"][:], op=ALU.mult),
                             reads=[Bw["M2"], Bw["M1"]], writes=[Byg])
                        if seg == 0:
                            P.op("pool", lambda e, yg=yg, q=q, r0=r0: e.dma_start(out=YG[q, :, r0:r0 + TS - 16], in_=yg[:, 16:TS]), reads=[Byg], dma=1)
                        else:
                            P.op("pool", lambda e, yg=yg, q=q, r0=r0: e.dma_start(out=YG[q, :, r0 + TS - 16:r0 + SEQ], in_=yg[:, :]), reads=[Byg], dma=1)
                        yield

        g2 = gen2()
    n3_est = NSEQ * 8 * 80 + 8
    n2_est = 8 * (4 * (4 + 2 * 14) + 3 + 2 * 9)
    ratio = (n3_est / n2_est) if (g2 is not None) else 1.0
    alive3, alive2 = g3 is not None, g2 is not None
    acc = 0.0
    while alive3 or alive2:
        acc += ratio
        while acc >= 1.0 or not alive2:
            acc -= 1.0
            if alive3:
                try:
                    next(g3)
                except StopIteration:
                    alive3 = False
            if not alive3:
                acc = 0.0
                break
        if alive2:
            try:
                next(g2)
            except StopIteration:
                alive2 = False
    end_phase()
    if ph2:

        begin_phase()
        psb = psum_banks(8)
        PS = Ring([(psb[i], P.buf(f"ps2b_{i}")) for i in range(8)])
        wgl = sb("g_w", [128, 4, 512], F32R); B_wgl = P.buf("g_w")
        P.op("sp", lambda e: e.dma_start(out=wgl[:], in_=wglu_d.rearrange("(k p) n -> p k n", p=128)), writes=[B_wgl], dma=1)
        gys = [sb(f"g_y{i}", [128, 4, 512], F32R) for i in range(2)]
        GYR = Ring([(gys[i], P.buf(f"g_y{i}")) for i in range(2)])
        gsg = [sb(f"g_s{i}", [128, 512]) for i in range(2)]
        GSR = Ring([(gsg[i], P.buf(f"g_s{i}")) for i in range(2)])
        gos = [sb(f"g_o{i}", [128, 512], F32R) for i in range(2)]
        GOR = Ring([(gos[i], P.buf(f"g_o{i}")) for i in range(2)])
        for tp in range(nseq2 * SEQ // 512):
            gy, Bgy = GYR.next()
            P.op("sp", lambda e, gy=gy, tp=tp: e.dma_start(out=gy[:], in_=YG[:, :, tp * 512:(tp + 1) * 512].rearrange("k p t -> p k t")), writes=[Bgy], dma=1)
            for c in range(4):
                pa, Bp = PS.next()
                for k in range(4):
                    P.op("pe", lambda e, pa=pa, k=k, c=c, gy=gy: e.matmul(out=pa[:, :], lhsT=wgl[:, k, c * 128:(c + 1) * 128], rhs=gy[:, k, :],
                                                                       start=(k == 0), stop=(k == 3)), reads=[B_wgl, Bgy], writes=[Bp])
                sg, Bsg = GSR.next()
                go, Bgo = GOR.next()
                P.op("act", lambda e, pa=pa, sg=sg: e.activation(out=sg[:], in_=pa[:, :], func=AF.Sigmoid), reads=[Bp], writes=[Bsg])
                P.op("dve", lambda e, sg=sg, go=go, gy=gy, c=c: e.tensor_tensor(out=go[:], in0=sg[:], in1=gy[:, c, :].bitcast(F32), op=ALU.mult),
                     reads=[Bsg, Bgy], writes=[Bgo])
                P.op("pool", lambda e, go=go, c=c, tp=tp: e.dma_start(out=Yssm[c, :, tp * 512:(tp + 1) * 512], in_=go[:]), reads=[Bgo], dma=1)
        end_phase()
    mid.close()


    H1T = dscr("H1T", [KC, 128, NREAL], F32R)
    H1N = dscr("H1N", [NREAL, D])
    wbs_d = din("w_br_ssm", [512, D], F32R)
    wba_d = din("w_br_attn", [1024, D], F32R)
    wo_d = din("w_o", [D, D], F32R)
    ln1g_d = din("ln1_gT", [128, KC]); ln1b_d = din("ln1_bT", [128, KC])
    ph4 = (dbg or {}).get("ph4", 1)
    if ph4:
        begin_phase()
        psb = psum_banks(8)
        PS = Ring([(psb[i], P.buf(f"ps4_{i}")) for i in range(8)])
        g1T = sb("p4_g1", [128, KC]); b1T = sb("p4_b1", [128, KC]); B_g1, B_b1 = P.bufs(2, "p4gb")
        P.op("sp", lambda e: e.dma_start(out=g1T[:], in_=ln1g_d), writes=[B_g1], dma=1)
        P.op("sp", lambda e: e.dma_start(out=b1T[:], in_=ln1b_d), writes=[B_b1], dma=1)
        hT4 = sb("p4_hT", [128, KC, 512], F32R); B_hT4 = P.buf("p4_hT")
        mT = sb("p4_mT", [128, KC, 512], F32R); B_mT = P.buf("p4_mT")
        ysT = sb("p4_ys", [128, 4, 512], F32R); B_ysT = P.buf("p4_ys")
        yaT = sb("p4_ya", [128, 8, 512], F32R); B_yaT = P.buf("p4_ya")
        w16 = [sb(f"p4_w16_{i}", [128, 8, 512], F32R) for i in range(5)]
        W16 = Ring([(w16[i], P.buf(f"p4_w16_{i}")) for i in range(5)])
        S1t = sb("p4_S1", [128, 4, 512]); B_S1 = [P.buf(f"p4_S1_{f}") for f in range(4)]
        sgs = [sb(f"p4_sg{i}", [128, 512]) for i in range(4)]
        SG = Ring([(sgs[i], P.buf(f"p4_sg{i}")) for i in range(4)])
        xt4 = [sb(f"p4_xt{i}", [128, D]) for i in range(2)]
        XT4 = Ring([(xt4[i], P.buf(f"p4_xt{i}")) for i in range(2)])
        st4 = [(sb(f"p4_stat{i}", [128, 4, 6]), sb(f"p4_mv{i}", [128, 2]), sb(f"p4_rstd{i}", [128, 1])) for i in range(2)]
        ST4 = Ring([(st4[i], P.buf(f"p4_st{i}")) for i in range(2)])
        w_in_v4 = w_in.rearrange("(k p) n -> p k n", p=128)
        wo_v = wo_d.rearrange("(k p) n -> p k n", p=128)
        wbs_v = wbs_d.rearrange("(k p) n -> p k n", p=128)
        wba_v = wba_d.rearrange("(k p) n -> p k n", p=128)

        def mm_chain(pa, Bp, wtile, Bw, off, nk, rhs_tile, B_rhs):
            for k in range(nk):
                P.op("pe", lambda e, k=k: e.matmul(out=pa[:, 0:512], lhsT=wtile[:, k, off:off + 128], rhs=rhs_tile[:, k, :],
                                                   start=(k == 0), stop=(k == nk - 1)), reads=[Bw, B_rhs], writes=[Bp])

        ngrp4 = (dbg or {}).get("ngrp4", NREAL // 512)
        for g in range(ngrp4):
            tok0 = g * 512
            P.op("sp", lambda e, tok0=tok0: e.dma_start(out=hT4[:], in_=HT[:, :, tok0:tok0 + 512].rearrange("k p t -> p k t")), writes=[B_hT4], dma=1)
            P.op("sp", lambda e, tok0=tok0: e.dma_start(out=ysT[:], in_=Yssm[:, :, tok0:tok0 + 512].rearrange("k p t -> p k t")), writes=[B_ysT], dma=1)
            P.op("sp", lambda e, tok0=tok0: e.dma_start(out=yaT[:], in_=Yattn[:, :, tok0:tok0 + 512].rearrange("k p t -> p k t")), writes=[B_yaT], dma=1)
            def load_half(src_v, col0, k0, nk):
                wt_, Bwt_ = W16.next()
                P.op("sp", lambda e, wt_=wt_: e.dma_start(out=wt_[:, 0:nk, :], in_=src_v[:, k0:k0 + nk, col0:col0 + 512]), writes=[Bwt_], dma=1)
                return wt_, Bwt_

            def mm16(pa, Bp, halves_, f, rhs_tile, B_rhs):
                for k in range(KC):
                    wt_, Bwt_ = halves_[k // 8]
                    P.op("pe", lambda e, k=k, wt_=wt_: e.matmul(out=pa[:, 0:512], lhsT=wt_[:, k % 8, f * 128:(f + 1) * 128], rhs=rhs_tile[:, k, :],
                                                            start=(k == 0), stop=(k == KC - 1)), reads=[Bwt_, B_rhs], writes=[Bp])

            def mmk(pa, Bp, wt_, Bwt_, nk, f, rhs_tile, B_rhs):
                for k in range(nk):
                    P.op("pe", lambda e, k=k: e.matmul(out=pa[:, 0:512], lhsT=wt_[:, k, f * 128:(f + 1) * 128], rhs=rhs_tile[:, k, :],
                                                       start=(k == 0), stop=(k == nk - 1)), reads=[Bwt_, B_rhs], writes=[Bp])

            for cb in range(4):
                gsh = [load_half(w_in_v4, 3584 + cb * 512, hk * 8, 8) for hk in range(2)]
                wsb, Bwsb = load_half(wbs_v, cb * 512, 0, 4)
                for f in range(4):
                    pgs, Bpgs = PS.next(); mm16(pgs, Bpgs, gsh, f, hT4, B_hT4)
                    pbs, Bpbs = PS.next(); mmk(pbs, Bpbs, wsb, Bwsb, 4, f, ysT, B_ysT)
                    s1, Bs1 = SG.next()
                    P.op("act", lambda e, s1=s1, pgs=pgs: e.activation(out=s1[:], in_=pgs[:, :], func=AF.Sigmoid), reads=[Bpgs], writes=[Bs1])
                    P.op("dve", lambda e, s1=s1, pbs=pbs, f=f: e.tensor_tensor(out=S1t[:, f, :], in0=s1[:], in1=pbs[:, :], op=ALU.mult), reads=[Bs1, Bpbs], writes=[B_S1[f]])
                gah = [load_half(w_in_v4, 5632 + cb * 512, hk * 8, 8) for hk in range(2)]
                wab, Bwab = load_half(wba_v, cb * 512, 0, 8)
                for f in range(4):
                    c = cb * 4 + f
                    pga, Bpga = PS.next(); mm16(pga, Bpga, gah, f, hT4, B_hT4)
                    pba, Bpba = PS.next(); mmk(pba, Bpba, wab, Bwab, 8, f, yaT, B_yaT)
                    s2, Bs2 = SG.next()
                    P.op("act", lambda e, s2=s2, pga=pga: e.activation(out=s2[:], in_=pga[:, :], func=AF.Sigmoid), reads=[Bpga], writes=[Bs2])
                    P.op("dve", lambda e, s2=s2, pba=pba: e.tensor_tensor(out=s2[:], in0=s2[:], in1=pba[:, :], op=ALU.mult), reads=[Bs2, Bpba], writes=[Bs2])
                    P.op("pool", lambda e, s2=s2, c=c, f=f: e.tensor_tensor(out=mT[:, c, :], in0=S1t[:, f, :], in1=s2[:], op=ALU.add),
                         reads=[B_S1[f], Bs2], writes=[B_mT])
            for cb in range(4):
                woh = [load_half(wo_v, cb * 512, hk * 8, 8) for hk in range(2)]
                for f in range(4):
                    c = cb * 4 + f
                    pm, Bpm = PS.next(); mm16(pm, Bpm, woh, f, mT, B_mT)
                    P.op("dve", lambda e, pm=pm, c=c: e.scalar_tensor_tensor(out=hT4[:, c, :], in0=hT4[:, c, :].bitcast(F32), scalar=ALPHA,
                                                                       in1=pm[:, :], op0=ALU.mult, op1=ALU.add),
                         reads=[Bpm, B_hT4], writes=[B_hT4])
            for t in range(4):
                xa, Bx = XT4.next()
                for b4 in range(4):
                    pa, Bp = PS.next()
                    for jj in range(4):
                        k = b4 * 4 + jj
                        P.op("pe", lambda e, pa=pa, jj=jj, k=k, t=t: e.transpose(out=pa[:, jj * 128:(jj + 1) * 128],
                                                                                 in_=hT4[:, k, t * 128:(t + 1) * 128].bitcast(F32), identity=ident[:]),
                             reads=[B_hT4, B_ident], writes=[Bp])
                    P.op("act", lambda e, pa=pa, xa=xa, b4=b4: e.activation(out=xa[:, b4 * 512:(b4 + 1) * 512], in_=pa[:, :], func=AF.Copy), reads=[Bp], writes=[Bx])
                (sa, ma, ra), Bs = ST4.next()
                for qq in range(4):
                    P.op("dve", lambda e, qq=qq, sa=sa, xa=xa: e.bn_stats(out=sa[:, qq, :], in_=xa[:, qq * 512:(qq + 1) * 512]), reads=[Bx], writes=[Bs])
                P.op("dve", lambda e, sa=sa, ma=ma: e.bn_aggr(out=ma[:], in_=sa[:].rearrange("p a b -> p (a b)")), reads=[Bs], writes=[Bs])
                P.op("act", lambda e, ra=ra, ma=ma: e.activation(out=ra[:], in_=ma[:, 1:2], func=AF.Sqrt, bias=epst[:], scale=1.0), reads=[Bs, B_eps], writes=[Bs])
                P.op("dve", lambda e, ra=ra: e.reciprocal(out=ra[:], in_=ra[:]), reads=[Bs], writes=[Bs])
                P.op("dve", lambda e, xa=xa, ma=ma, ra=ra: e.tensor_scalar(out=xa[:], in0=xa[:], scalar1=ma[:, 0:1], scalar2=ra[:], op0=ALU.subtract, op1=ALU.mult),
                     reads=[Bx, Bs], writes=[Bx])
                P.op("pool", lambda e, xa=xa, tok0=tok0, t=t: e.dma_start(out=H1N[tok0 + t * 128:tok0 + (t + 1) * 128, :], in_=xa[:]), reads=[Bx], dma=1)
                for b4 in range(4):
                    pa, Bp = PS.next()
                    for jj in range(4):
                        k = b4 * 4 + jj
                        P.op("pe", lambda e, pa=pa, jj=jj, k=k, xa=xa: e.transpose(out=pa[:, jj * 128:(jj + 1) * 128], in_=xa[:, k * 128:(k + 1) * 128], identity=ident[:]),
                             reads=[Bx, B_ident], writes=[Bp])
                    for jj in range(4):
                        k = b4 * 4 + jj
                        P.op("act", lambda e, pa=pa, jj=jj, k=k, t=t: e.activation(out=mT[:, k, t * 128:(t + 1) * 128], in_=pa[:, jj * 128:(jj + 1) * 128],
                                                                              func=AF.Identity, scale=g1T[:, k:k + 1], bias=b1T[:, k:k + 1]),
                             reads=[Bp, B_g1, B_b1], writes=[B_mT])
            P.op("pool", lambda e, tok0=tok0: e.dma_start(out=H1T[:, :, tok0:tok0 + 512].rearrange("k p t -> p k t"), in_=mT[:]), reads=[B_mT], dma=1)
        end_phase()


    CAP = 512
    RTOT = 32 * CAP
    BIG = float(RTOT + 64)
    YEXP = dscr("YEXP", [RTOT, D])
    LTOK = dscr("LTOK", [RTOT, 1], I32)
    wr_d = din("router_w", [D, 36], F32R)
    rb_d = din("router_b_b", [128, 36])
    wg_d = din("exp_w_gate", [32, D, 512], F32R)
    wu_d = din("exp_w_up", [32, D, 512], F32R)
    wd_d = din("exp_w_down", [32, 512, D], F32R)
    tri_d = din("tri_d", [128, 128])
    ecp1_d = din("ecp1_d", [128, 32])
    NT = NREAL // 128
    RK = sb("RK", [128, NT, 2], I32); WK = sb("WK", [128, NT, 2]); B_RK = P.buf("RK"); B_WK = P.buf("WK")
    ph5 = (dbg or {}).get("ph5", 1)
    ngrp5 = (dbg or {}).get("ngrp5", NREAL // 512)
    if ph5:
        begin_phase()
        psb = psum_banks(8)
        PS = Ring([(psb[i], P.buf(f"ps5_{i}")) for i in range(8)])
        wr = sb("p5_wr", [128, KC, 36], F32R); rbb = sb("p5_rb", [128, 36]); B_wr, B_rb = P.bufs(2, "p5r")
        P.op("sp", lambda e: e.dma_start(out=wr[:], in_=wr_d.rearrange("(k p) n -> p k n", p=128)), writes=[B_wr], dma=1)
        P.op("sp", lambda e: e.dma_start(out=rbb[:], in_=rb_d), writes=[B_rb], dma=1)
        trif = sb("p5_trif", [128, 128]); trib = sb("p5_trib", [128, 128], BF16); onesb = sb("p5_onesb", [128, 128], BF16)
        ecp1 = sb("p5_ecp1", [128, 32]); cnt = sb("p5_cnt", [128, 32]); zer = sb("p5_zer", [128, RTOT // 128], I32)
        B_tri, B_ones, B_ecp, B_cnt, B_zer, B_ltok = P.bufs(6, "p5c")
        P.op("sp", lambda e: e.dma_start(out=trif[:], in_=tri_d), writes=[B_tri], dma=1)
        P.op("sp", lambda e: e.dma_start(out=ecp1[:], in_=ecp1_d), writes=[B_ecp], dma=1)
        P.op("dve", lambda e: e.tensor_copy(out=trib[:], in_=trif[:]), reads=[B_tri], writes=[B_tri])
        P.op("dve", lambda e: e.memset(onesb[:], 1.0), writes=[B_ones])
        P.op("dve", lambda e: e.memset(cnt[:], 0.0), writes=[B_cnt])
        P.op("dve", lambda e: e.memset(zer[:], 0), writes=[B_zer])
        P.op("dve", lambda e: e.memset(RK[:], RTOT + 64), writes=[B_RK])
        P.op("dve", lambda e: e.memset(WK[:], 0.0), writes=[B_WK])
        P.op("sp", lambda e: e.dma_start(out=LTOK.rearrange("(p a) o -> p (a o)", p=128), in_=zer[:]), reads=[B_zer], writes=[B_ltok], dma=1)
        h1T = sb("p5_h1T", [128, KC, 512], F32R); B_h1T = P.buf("p5_h1T")
        W32 = sb("p5_W32", [128, 32]); B_W32 = P.buf("p5_W32")
        maskb = sb("p5_maskb", [128, 32], BF16); B_maskb = P.buf("p5_maskb")
        toks = [sb(f"p5_tok{i}", [128, 1], I32) for i in range(2)]
        TOK = Ring([(toks[i], P.buf(f"p5_tok{i}")) for i in range(2)])
        rt = sb("p5_rt", [128, 288]); B_rt = P.buf("p5_rt")
        L = rt[:, 0:36]; gmx = rt[:, 36:37]; gex = rt[:, 40:44]; gsum = rt[:, 44:45]; gmask = rt[:, 48:52]
        esel = rt[:, 56:64]; v8 = rt[:, 64:72]; sel = rt[:, 72:80]; ex8 = rt[:, 80:88]; den = rt[:, 88:89]
        tmp32 = rt[:, 96:128]; wsel = rt[:, 128:136]; slot = rt[:, 160:192]; key = rt[:, 192:224]; okm = rt[:, 224:256]
        kv8 = rt[:, 256:264]; zz = rt[:, 264:266]; rf = rt[:, 266:268]; jk32 = rt[:, 136:160]

        def rdv(fn, rd=(), wr_=()):
            P.op("dve", fn, reads=[B_rt] + list(rd), writes=[B_rt] + list(wr_))

        for g in range(ngrp5):
            tok0 = g * 512
            P.op("sp", lambda e, tok0=tok0: e.dma_start(out=h1T[:], in_=H1T[:, :, tok0:tok0 + 512].rearrange("k p t -> p k t")), writes=[B_h1T], dma=1)
            for t in range(4):
                ti = g * 4 + t
                pa, Bp = PS.next()
                for k in range(KC):
                    P.op("pe", lambda e, pa=pa, k=k, t=t: e.matmul(out=pa[:, 0:36], lhsT=h1T[:, k, t * 128:(t + 1) * 128], rhs=wr[:, k, :],
                                                             start=(k == 0), stop=(k == KC - 1)), reads=[B_h1T, B_wr], writes=[Bp])
                P.op("dve", lambda e, pa=pa: e.tensor_tensor(out=L, in0=pa[:, 0:36], in1=rbb[:], op=ALU.add), reads=[Bp, B_rb, B_rt], writes=[B_rt])
                rdv(lambda e: e.tensor_reduce(out=gmx, in_=L[:, 0:4], axis=AX.X, op=ALU.max))
                rdv(lambda e: e.tensor_scalar(out=gex, in0=L[:, 0:4], scalar1=gmx, scalar2=None, op0=ALU.subtract))
                P.op("act", lambda e: e.activation(out=gex, in_=gex, func=AF.Exp), reads=[B_rt], writes=[B_rt])
                rdv(lambda e: e.tensor_reduce(out=gsum, in_=gex, axis=AX.X, op=ALU.add))
                rdv(lambda e: e.reciprocal(out=gsum, in_=gsum))
                rdv(lambda e: e.tensor_scalar(out=gmask, in0=L[:, 0:4], scalar1=gmx, scalar2=None, op0=ALU.is_equal))
                rdv(lambda e: e.tensor_tensor(out=tmp32.rearrange("p (g e) -> p g e", g=4), in0=L[:, 4:36].rearrange("p (g e) -> p g e", g=4),
                                              in1=gmask.unsqueeze(2).to_broadcast([128, 4, 8]), op=ALU.mult))
                rdv(lambda e: e.tensor_reduce(out=esel, in_=tmp32.rearrange("p (g e) -> p e g", g=4), axis=AX.X, op=ALU.add))
                rdv(lambda e: e.max(out=v8, in_=esel))
                rdv(lambda e: e.tensor_scalar(out=sel, in0=esel, scalar1=v8[:, 1:2], scalar2=None, op0=ALU.is_ge))
                rdv(lambda e: e.tensor_scalar(out=ex8, in0=esel, scalar1=v8[:, 0:1], scalar2=None, op0=ALU.subtract))
                P.op("act", lambda e: e.activation(out=ex8, in_=ex8, func=AF.Exp), reads=[B_rt], writes=[B_rt])
                rdv(lambda e: e.tensor_tensor(out=ex8, in0=ex8, in1=sel, op=ALU.mult))
                rdv(lambda e: e.tensor_reduce(out=den, in_=ex8, axis=AX.X, op=ALU.add))
                rdv(lambda e: e.reciprocal(out=den, in_=den))
                rdv(lambda e: e.tensor_tensor(out=den, in0=den, in1=gsum, op=ALU.mult))
                rdv(lambda e: e.tensor_scalar(out=wsel, in0=ex8, scalar1=den, scalar2=None, op0=ALU.mult))
                P.op("dve", lambda e: e.tensor_tensor(out=W32[:, :].rearrange("p (g e) -> p g e", g=4),
                                                      in0=gmask.unsqueeze(2).to_broadcast([128, 4, 8]),
                                                      in1=wsel.unsqueeze(1).to_broadcast([128, 4, 8]), op=ALU.mult),
                     reads=[B_rt], writes=[B_W32])
                P.op("dve", lambda e: e.tensor_scalar(out=maskb[:], in0=W32[:], scalar1=0.0, scalar2=None, op0=ALU.is_gt), reads=[B_W32], writes=[B_maskb])
                pc, Bpc = PS.next()
                P.op("pe", lambda e, pc=pc: e.matmul(out=pc[:, 0:32], lhsT=trib[:], rhs=maskb[:], start=True, stop=True), reads=[B_tri, B_maskb], writes=[Bpc])
                ptot, Bpt = PS.next()
                P.op("pe", lambda e, ptot=ptot: e.matmul(out=ptot[:, 0:32], lhsT=onesb[:], rhs=maskb[:], start=True, stop=True), reads=[B_ones, B_maskb], writes=[Bpt])
                rdv(lambda e, pc=pc: e.scalar_tensor_tensor(out=slot, in0=pc[:, 0:32], scalar=-1.0, in1=cnt[:], op0=ALU.add, op1=ALU.add), rd=[Bpc, B_cnt])
                P.op("dve", lambda e, ptot=ptot: e.tensor_tensor(out=cnt[:], in0=cnt[:], in1=ptot[:, 0:32], op=ALU.add), reads=[Bpt, B_cnt, B_rt], writes=[B_cnt])
                rdv(lambda e: e.tensor_scalar(out=okm, in0=slot, scalar1=float(CAP), scalar2=None, op0=ALU.is_lt))
                rdv(lambda e: e.tensor_tensor(out=okm, in0=okm, in1=maskb[:], op=ALU.mult), rd=[B_maskb])
                rdv(lambda e: e.tensor_tensor(out=key, in0=slot, in1=ecp1[:], op=ALU.add), rd=[B_ecp])
                rdv(lambda e: e.tensor_tensor(out=key, in0=key, in1=okm, op=ALU.mult))
                rdv(lambda e: e.tensor_tensor(out=tmp32, in0=W32[:], in1=okm, op=ALU.mult), rd=[B_W32])
                rdv(lambda e: e.max(out=kv8, in_=key))
                rdv(lambda e: e.tensor_scalar(out=zz, in0=kv8[:, 0:2], scalar1=0.0, scalar2=BIG, op0=ALU.is_equal, op1=ALU.mult))
                rdv(lambda e: e.scalar_tensor_tensor(out=rf, in0=kv8[:, 0:2], scalar=-1.0, in1=zz, op0=ALU.add, op1=ALU.add))
                P.op("dve", lambda e, ti=ti: e.tensor_copy(out=RK[:, ti, :], in_=rf), reads=[B_rt], writes=[B_RK])
                for kk in range(2):
                    P.op("dve", lambda e, ti=ti, kk=kk: e.scalar_tensor_tensor(out=slot, in0=key, scalar=kv8[:, kk:kk + 1], in1=tmp32,
                                                                           op0=ALU.is_equal, op1=ALU.mult, accum_out=WK[:, ti, kk:kk + 1]),
                         reads=[B_rt], writes=[B_rt, B_WK])
                tk, Btk = TOK.next()
                P.op("pool", lambda e, tk=tk, ti=ti: e.iota(tk[:], pattern=[[0, 1]], base=ti * 128, channel_multiplier=1), writes=[Btk])
                for kk in range(2):
                    P.op("pool", lambda e, tk=tk, ti=ti, kk=kk: e.indirect_dma_start(
                        out=LTOK[:, :], out_offset=bass.IndirectOffsetOnAxis(ap=RK[:, ti, kk:kk + 1], axis=0), in_=tk[:, :], in_offset=None,
                        bounds_check=P.reg(e, RTOT - 1), oob_is_err=False), reads=[B_RK, Btk], writes=[B_ltok], dma=1)
        end_phase()

        begin_phase()
        psb = psum_banks(8)
        PS = Ring([(psb[i], P.buf(f"ps5e_{i}")) for i in range(8)])
        g1T = sb("p5_g1", [128, KC]); b1T = sb("p5_b1", [128, KC]); B_g1, B_b1 = P.bufs(2, "p5gb")
        P.op("sp", lambda e: e.dma_start(out=g1T[:], in_=ln1g_d), writes=[B_g1], dma=1)
        P.op("sp", lambda e: e.dma_start(out=b1T[:], in_=ln1b_d), writes=[B_b1], dma=1)
        XTl = [sb(f"p5_XT{i}", [128, KC, CAP], F32R) for i in range(2)]
        B_XTl = [P.buf(f"p5_XT{i}") for i in range(2)]
        NB = CAP // 128
        xgs = [sb(f"p5_xg{i}", [128, D]) for i in range(3)]
        XG = Ring([(xgs[i], P.buf(f"p5_xg{i}")) for i in range(3)])
        idxs = [sb(f"p5_idx{i}", [128, 4], I32) for i in range(2)]
        IDX = Ring([(idxs[i], P.buf(f"p5_idx{i}")) for i in range(2)])
        w16 = [sb(f"p5_w16_{i}", [128, 8, 512], F32R) for i in range(4)]
        W16 = Ring([(w16[i], P.buf(f"p5_w16_{i}")) for i in range(4)])
        wdt = [sb(f"p5_wd{i}", [128, 4, 512], F32R) for i in range(2)]
        WD = Ring([(wdt[i], P.buf(f"p5_wd{i}")) for i in range(2)])
        hb = sb("p5_hb", [128, 4, CAP], F32R); Bhb = P.buf("p5_hb")
        sg4 = sb("p5_sg4", [128, 4, CAP]); Bsg4 = [P.buf(f"p5_sg4_{f}") for f in range(4)]
        yrs = [sb(f"p5_yr{i}", [128, NB, 512]) for i in range(2)]
        YR = Ring([(yrs[i], P.buf(f"p5_yr{i}")) for i in range(2)])
        nexp5 = (dbg or {}).get("nexp5", 32)

        def prep_pieces(ex, slot):
            XT_, BXT_ = XTl[slot], B_XTl[slot]
            state = {}

            def p_fetch():
                idx, Bidx = IDX.next()
                for b in range(NB):
                    P.op("sp", lambda e, idx=idx, b=b: e.dma_start(out=idx[:, b:b + 1], in_=LTOK[ex * CAP + b * 128:ex * CAP + (b + 1) * 128, :]), writes=[Bidx], dma=1)
                state["idx"] = (idx, Bidx)

            def p_gather(b):
                idx, Bidx = state["idx"]
                xg, Bxg = XG.next()
                P.op("pool", lambda e, xg=xg, idx=idx, b=b: e.indirect_dma_start(
                    out=xg[:, :], out_offset=None, in_=H1N[:, :], in_offset=bass.IndirectOffsetOnAxis(ap=idx[:, b:b + 1], axis=0),
                    bounds_check=P.reg(e, NREAL - 1), oob_is_err=False), reads=[Bidx], writes=[Bxg], dma=1)
                state[("xg", b)] = (xg, Bxg)

            def p_tr(b, half):
                if half == 0:
                    p_gather(b)
                xg, Bxg = state[("xg", b)]
                for b4 in range(half * 2, half * 2 + 2):
                    pa, Bp = PS.next()
                    for jj in range(4):
                        k = b4 * 4 + jj
                        P.op("pe", lambda e, pa=pa, jj=jj, k=k, xg=xg: e.transpose(out=pa[:, jj * 128:(jj + 1) * 128], in_=xg[:, k * 128:(k + 1) * 128], identity=ident[:]),
                             reads=[Bxg, B_ident], writes=[Bp])
                    for jj in range(4):
                        k = b4 * 4 + jj
                        P.op("act", lambda e, pa=pa, jj=jj, k=k, b=b, XT_=XT_: e.activation(out=XT_[:, k, b * 128:(b + 1) * 128], in_=pa[:, jj * 128:(jj + 1) * 128],
                                                                                  func=AF.Identity, scale=g1T[:, k:k + 1], bias=b1T[:, k:k + 1]),
                             reads=[Bp, B_g1, B_b1], writes=[BXT_])

            return [p_fetch] + [(lambda b=b, half=half: p_tr(b, half)) for b in range(NB) for half in range(2)]

        pieces = prep_pieces(0, 0)
        for pc_ in pieces:
            pc_()
        for ex in range(nexp5):
            slot = ex % 2
            XT_, BXT_ = XTl[slot], B_XTl[slot]
            nxt = prep_pieces(ex + 1, 1 - slot) if ex + 1 < nexp5 else []
            if nxt:
                nxt.pop(0)()
            halves = {}

            def load_halves(nm, wsrc, ex=ex, halves=halves):
                for hk in range(2):
                    wt_, Bwt_ = W16.next()
                    P.op("sp", lambda e, wt_=wt_, hk=hk, wsrc=wsrc: e.dma_start(
                        out=wt_[:], in_=wsrc[ex].rearrange("(k p) n -> p k n", p=128)[:, hk * 8:(hk + 1) * 8, :]), writes=[Bwt_], dma=1)
                    halves[(nm, hk)] = (wt_, Bwt_)

            load_halves("g", wg_d)
            load_halves("u", wu_d)
            for nm in ("g", "u"):
                for f in range(4):
                    pp, Bpp = PS.next()
                    for k in range(KC):
                        wt_, Bwt_ = halves[(nm, k // 8)]
                        P.op("pe", lambda e, pp=pp, k=k, f=f, wt_=wt_, XT_=XT_: e.matmul(out=pp[:, 0:CAP], lhsT=wt_[:, k % 8, f * 128:(f + 1) * 128], rhs=XT_[:, k, :],
                                                                                  start=(k == 0), stop=(k == KC - 1)), reads=[Bwt_, BXT_], writes=[Bpp])
                    if nm == "g":
                        P.op("act", lambda e, pp=pp, f=f: e.activation(out=sg4[:, f, :], in_=pp[:, 0:CAP], func=AF.Silu), reads=[Bpp], writes=[Bsg4[f]])
                    else:
                        P.op("dve", lambda e, pp=pp, f=f: e.tensor_tensor(out=hb[:, f, :], in0=sg4[:, f, :], in1=pp[:, 0:CAP], op=ALU.mult),
                             reads=[Bsg4[f], Bpp], writes=[Bhb])
                    if nxt:
                        nxt.pop(0)()
            for cg in range(4):
                wd_, Bwd = WD.next()
                P.op("sp", lambda e, wd_=wd_, ex=ex, cg=cg: e.dma_start(out=wd_[:], in_=wd_d[ex].rearrange("(k p) n -> p k n", p=128)[:, :, cg * 512:(cg + 1) * 512]),
                     writes=[Bwd], dma=1)
                yr, Byr = YR.next()
                for b in range(NB):
                    pa, Bp = PS.next()
                    for k in range(4):
                        P.op("pe", lambda e, pa=pa, k=k, b=b, wd_=wd_: e.matmul(out=pa[:, :], lhsT=hb[:, k, b * 128:(b + 1) * 128], rhs=wd_[:, k, :],
                                                                        start=(k == 0), stop=(k == 3)), reads=[Bhb, Bwd], writes=[Bp])
                    P.op("dve", lambda e, pa=pa, b=b, yr=yr: e.tensor_copy(out=yr[:, b, :], in_=pa[:, :]), reads=[Bp], writes=[Byr])
                P.op("pool", lambda e, ex=ex, cg=cg, yr=yr: e.dma_start(
                    out=YEXP[ex * CAP:(ex + 1) * CAP, cg * 512:(cg + 1) * 512].rearrange("(b p) n -> p b n", p=128), in_=yr[:]), reads=[Byr], dma=1)
            while nxt:
                nxt.pop(0)()
        end_phase()

    gb_d = [din(n, [128, D]) for n in ("ln1_g_b", "ln1_b_b", "ln2_g_b", "ln2_b_b")]
    ph6 = (dbg or {}).get("ph6", 1)
    if ph6:
        begin_phase()
        gbt = [sb(f"p6_gb{i}", [128, D]) for i in range(4)]
        B_gb = P.bufs(4, "p6gb")
        for i in range(4):
            P.op("sp", lambda e, i=i: e.dma_start(out=gbt[i][:], in_=gb_d[i]), writes=[B_gb[i]], dma=1)
        for i in range(2):
            P.op("pool", lambda e, i=i: e.tensor_scalar(out=gbt[i][:], in0=gbt[i][:], scalar1=ALPHA, scalar2=None, op0=ALU.mult), reads=[B_gb[i]], writes=[B_gb[i]])
        xs6 = [sb(f"p6_x{i}", [128, D]) for i in range(4)]
        X6 = Ring([(xs6[i], P.buf(f"p6_x{i}")) for i in range(4)])
        ys6 = [sb(f"p6_y{i}", [128, D]) for i in range(8)]
        Y6 = Ring([(ys6[i], P.buf(f"p6_y{i}")) for i in range(8)])
        for i in range(8):
            P.op("dve", lambda e, i=i: e.memset(ys6[i][:], 0.0), writes=[Y6.items[i][1]])
        st6 = [(sb(f"p6_stat{i}", [128, 4, 6]), sb(f"p6_mv{i}", [128, 2]), sb(f"p6_rstd{i}", [128, 1])) for i in range(2)]
        ST6 = Ring([(st6[i], P.buf(f"p6_st{i}")) for i in range(2)])
        ntile6 = (dbg or {}).get("ntile6", NREAL // 128)
        def p6_prep(tt6):
            xa, Bx = X6.next()
            r0 = tt6 * 128
            P.op("sp", lambda e, xa=xa, r0=r0: e.dma_start(out=xa[:], in_=H1N[r0:r0 + 128, :]), writes=[Bx], dma=1)
            ya = []
            for kk in range(2):
                yt_, Byt = Y6.next()
                P.op("pool", lambda e, yt_=yt_, tt6=tt6, kk=kk: e.indirect_dma_start(
                    out=yt_[:, :], out_offset=None, in_=YEXP[:, :], in_offset=bass.IndirectOffsetOnAxis(ap=RK[:, tt6, kk:kk + 1], axis=0),
                    bounds_check=P.reg(e, RTOT - 1), oob_is_err=False), reads=[B_RK], writes=[Byt], dma=1)
                ya.append((yt_, Byt))
            P.op("pool", lambda e, xa=xa: e.tensor_tensor(out=xa[:], in0=xa[:], in1=gbt[0][:], op=ALU.mult), reads=[Bx, B_gb[0]], writes=[Bx])
            P.op("pool", lambda e, xa=xa: e.tensor_tensor(out=xa[:], in0=xa[:], in1=gbt[1][:], op=ALU.add), reads=[Bx, B_gb[1]], writes=[Bx])
            return xa, Bx, ya

        def p6_finish(tt6, xa, Bx, ya):
            r0 = tt6 * 128
            for kk in range(2):
                yt_, Byt = ya[kk]
                P.op("dve", lambda e, xa=xa, yt_=yt_, tt6=tt6, kk=kk: e.scalar_tensor_tensor(out=xa[:], in0=yt_[:], scalar=WK[:, tt6, kk:kk + 1], in1=xa[:],
                                                                                   op0=ALU.mult, op1=ALU.add), reads=[Bx, Byt, B_WK], writes=[Bx])
            (sa, ma, ra), Bs = ST6.next()
            for qq in range(4):
                P.op("dve", lambda e, qq=qq, sa=sa, xa=xa: e.bn_stats(out=sa[:, qq, :], in_=xa[:, qq * 512:(qq + 1) * 512]), reads=[Bx], writes=[Bs])
            P.op("dve", lambda e, sa=sa, ma=ma: e.bn_aggr(out=ma[:], in_=sa[:].rearrange("p a b -> p (a b)")), reads=[Bs], writes=[Bs])
            P.op("act", lambda e, ra=ra, ma=ma: e.activation(out=ra[:], in_=ma[:, 1:2], func=AF.Sqrt, bias=epst[:], scale=1.0), reads=[Bs, B_eps], writes=[Bs])
            P.op("dve", lambda e, ra=ra: e.reciprocal(out=ra[:], in_=ra[:]), reads=[Bs], writes=[Bs])
            P.op("dve", lambda e, ma=ma, ra=ra: e.scalar_tensor_tensor(out=ma[:, 1:2], in0=ma[:, 0:1], scalar=-1.0, in1=ra[:], op0=ALU.mult, op1=ALU.mult),
                 reads=[Bs], writes=[Bs])
            P.op("act", lambda e, xa=xa, ma=ma, ra=ra: e.activation(out=xa[:], in_=xa[:], func=AF.Identity, scale=ra[:], bias=ma[:, 1:2]),
                 reads=[Bx, Bs], writes=[Bx])
            P.op("dve", lambda e, xa=xa: e.tensor_tensor(out=xa[:], in0=xa[:], in1=gbt[2][:], op=ALU.mult), reads=[Bx, B_gb[2]], writes=[Bx])
            P.op("pool", lambda e, xa=xa: e.tensor_tensor(out=xa[:], in0=xa[:], in1=gbt[3][:], op=ALU.add), reads=[Bx, B_gb[3]], writes=[Bx])
            P.op("sp", lambda e, xa=xa, r0=r0: e.dma_start(out=out[r0:r0 + 128, :], in_=xa[:]), reads=[Bx], dma=1)

        preps = {}
        DEPTH6 = 2
        for tt6 in range(min(DEPTH6, ntile6)):
            preps[tt6] = p6_prep(tt6)
        for tt6 in range(ntile6):
            if tt6 + DEPTH6 < ntile6:
                preps[tt6 + DEPTH6] = p6_prep(tt6 + DEPTH6)
            p6_finish(tt6, *preps.pop(tt6))
        end_phase()

    P.flush()
    es.close()
    return nc


def _prep_shared(inputs):
    sh = {}
    sh["meta"] = np.ascontiguousarray(inputs["meta_tokens"], dtype=np.float32)
    sh["ident_d"] = np.eye(128, dtype=np.float32)
    sh["ln_in_gT"] = np.ascontiguousarray(np.asarray(inputs["ln_in_g"], np.float32).reshape(KC, 128).T)
    sh["ln_in_bT"] = np.ascontiguousarray(np.asarray(inputs["ln_in_b"], np.float32).reshape(KC, 128).T)
    sh["w_in"] = np.ascontiguousarray(inputs["w_in"][0], dtype=np.float32)
    f = lambda k: np.asarray(inputs[k], np.float32).reshape(-1)
    sh["tri_d"] = np.triu(np.ones((128, 128), np.float32))
    sh["ecp1_d"] = np.ascontiguousarray(np.broadcast_to((np.arange(32, dtype=np.float32) * 512 + 1)[None, :], (128, 32)))
    rep = lambda a, n=128: np.ascontiguousarray(np.broadcast_to(np.asarray(a, np.float32).reshape(1, -1), (n, np.asarray(a).size)))
    sh["router_w"] = np.ascontiguousarray(np.concatenate([inputs["router_g_w"][0], inputs["router_e_w"][0]], axis=1), dtype=np.float32)
    sh["router_b_b"] = rep(np.concatenate([np.asarray(inputs["router_g_b"][0]).reshape(-1), np.asarray(inputs["router_e_b"][0]).reshape(-1)]))
    sh["exp_w_gate"] = np.ascontiguousarray(inputs["exp_w_gate"][0], dtype=np.float32)
    sh["exp_w_up"] = np.ascontiguousarray(inputs["exp_w_up"][0], dtype=np.float32)
    sh["exp_w_down"] = np.ascontiguousarray(inputs["exp_w_down"][0], dtype=np.float32)
    sh["ln1_g_b"] = rep(inputs["ln1_g"][0]); sh["ln1_b_b"] = rep(inputs["ln1_b"][0])
    sh["ln2_g_b"] = rep(inputs["ln2_g"][0]); sh["ln2_b_b"] = rep(inputs["ln2_b"][0])
    colT = lambda a: np.ascontiguousarray(np.asarray(a, np.float32).reshape(KC, 128).T)
    sh["ln1_gT"] = colT(inputs["ln1_g"][0]); sh["ln1_bT"] = colT(inputs["ln1_b"][0])
    sh["w_br_ssm"] = np.ascontiguousarray(inputs["w_br_ssm"][0], dtype=np.float32)
    sh["w_br_attn"] = np.ascontiguousarray(inputs["w_br_attn"][0], dtype=np.float32)
    sh["w_o"] = np.ascontiguousarray(inputs["w_o"][0], dtype=np.float32)
    sl = lambda a: np.ascontiguousarray(np.asarray(a, np.float32).reshape(16, 128).T)
    sh["ssm_are"] = sl(inputs["ssm_a_re"][0])
    sh["ssm_aim"] = sl(inputs["ssm_a_im"][0])
    sh["ssm_ldt"] = sl(np.repeat(np.asarray(inputs["ssm_log_dt"][0], np.float32).reshape(32, 1), 64, axis=1))
    sl3 = lambda a: np.ascontiguousarray(np.asarray(a, np.float32).reshape(16, 128, 16).transpose(1, 0, 2))
    sh["ssm_bre"] = sl3(inputs["ssm_b_re"][0])
    sh["ssm_bim"] = sl3(inputs["ssm_b_im"][0])
    sh["ssm_cre"] = sl3(np.asarray(inputs["ssm_c_re"][0]).transpose(0, 2, 1))
    sh["ssm_cim"] = sl3(np.asarray(inputs["ssm_c_im"][0]).transpose(0, 2, 1))
    sh["ssm_dT"] = np.ascontiguousarray(np.asarray(inputs["ssm_d"][0], np.float32).reshape(4, 128).T)
    sh["ssm_wglu"] = np.ascontiguousarray(inputs["ssm_w_glu"][0], dtype=np.float32)
    sh["tt_d"] = np.ascontiguousarray(np.broadcast_to(np.arange(1032, dtype=np.float32)[None, :], (128, 1032)))
    sh["lamv"] = np.concatenate([f("attn_lambda_q1"), f("attn_lambda_k1"), f("attn_lambda_q2"), f("attn_lambda_k2")]).reshape(1, 256)
    sh["gsub_b"] = np.ascontiguousarray(np.broadcast_to(f("attn_subln_g")[None, :], (128, 128)))
    return sh


def kernel(**inputs):
    x = np.asarray(inputs["x"], np.float32)
    sh = _prep_shared(inputs)
    nc = build_program()
    in_maps = []
    for c in range(NCORES):
        m = dict(sh)
        m["x"] = np.ascontiguousarray(x[c * NSEQ:(c + 1) * NSEQ].reshape(NREAL, D))
        in_maps.append(m)
    res = run_bass_kernel_spmd(nc, in_maps, core_ids=list(range(NCORES)))
    outs = [np.asarray(r["out"], np.float32).reshape(NSEQ, SEQ, D) for r in res.results]
    return np.concatenate(outs, axis=0)
```

```python
import math
from contextlib import ExitStack

import numpy as np
import concourse.bass as bass
import concourse.mybir as mybir
from concourse.bass_utils import run_bass_kernel_spmd

F32 = mybir.dt.float32
F32R = mybir.dt.float32r
BF16 = mybir.dt.bfloat16
U32 = mybir.dt.uint32
I32 = mybir.dt.int32
AF = mybir.ActivationFunctionType
ALU = mybir.AluOpType
AX = mybir.AxisListType

D = 2048
KC = 16
SEQ = 2048
NSEQ = 2
NREAL = NSEQ * SEQ
NTOK = NREAL + 128
META0 = NREAL
IN_COLS = 7680
LN_EPS = 1e-5
ALPHA = 2.0 ** 0.25
LAMBDA_INIT = 0.2
NCORES = 8


class Buf:
    __slots__ = ("name", "w", "r")

    def __init__(self, name):
        self.name = name
        self.w = None
        self.r = {}


class Prog:
    ENG = ("pe", "dve", "act", "pool", "sp")

    def __init__(self, nc, es):
        self.nc = nc
        self.es = es
        self.ops = {e: [] for e in self.ENG}
        self.sems = {}
        self.cnt = {}
        self.seen = {e: {} for e in self.ENG}
        self.pending = {e: {} for e in self.ENG}
        self.nbuf = 0

    def reg(self, eng, val):
        if not hasattr(self, "_regs"):
            self._regs = {}
        if val not in self._regs:
            self._regs[val] = eng.to_reg(val)
        return self._regs[val]

    def buf(self, name=None):
        self.nbuf += 1
        return Buf(name or f"b{self.nbuf}")

    def bufs(self, n, name="b"):
        return [self.buf(f"{name}{i}") for i in range(n)]

    def _mksem(self, key):
        self.sems[key] = self.es.enter_context(self.nc.semaphore("s_" + key))
        self.cnt[key] = 0

    def op(self, eng, emit, reads=(), writes=(), dma=None):
        if dma:
            dma = "d_" + (writes[0].name if writes else reads[0].name)
        key = dma if dma else eng
        if key not in self.sems:
            self._mksem(key)
        waits = dict(self.pending[eng])
        self.pending[eng] = {}

        def need(tok, kind):
            k, v = tok
            if dma is None and k == eng:
                if eng == "pe" or kind != "raw":
                    return
            if v > waits.get(k, 0):
                waits[k] = v

        for b in reads:
            if b.w:
                need(b.w, "raw")
        for b in writes:
            if b.w:
                need(b.w, "waw")
            for k, v in b.r.items():
                need((k, v), "war")
        wl = []
        for k, v in waits.items():
            if self.seen[eng].get(k, 0) >= v:
                continue
            self.seen[eng][k] = v
            wl.append((k, v))
        inc = 16 if dma else 1
        self.cnt[key] += inc
        tok = (key, self.cnt[key])
        self.ops[eng].append((wl, emit, key, inc))
        for b in writes:
            b.w = tok
            b.r = {}
        for b in reads:
            if b not in writes:
                if b.r.get(key, 0) < tok[1]:
                    b.r[key] = tok[1]
        return tok

    def barrier(self):
        for e in self.ENG:
            for k, v in self.cnt.items():
                if v > self.pending[e].get(k, 0):
                    self.pending[e][k] = v

    def flush(self):
        nc = self.nc
        self.barrier()
        for e in self.ENG:
            wl = []
            for k, v in self.pending[e].items():
                if self.seen[e].get(k, 0) < v:
                    self.seen[e][k] = v
                    wl.append((k, v))
            self.pending[e] = {}
            self.ops[e].append((wl, None, None, 0))
        sems = self.sems

        def mk(lst):
            def body(eng):
                for wl, emit, key, inc in lst:
                    for k, v in wl:
                        eng.wait_ge(sems[k], v)
                    if emit is not None:
                        ins = emit(eng)
                        ins.then_inc(sems[key], inc)
            return body

        with nc.Block() as block:
            block.tensor(mk(self.ops["pe"]))
            block.vector(mk(self.ops["dve"]))
            block.scalar(mk(self.ops["act"]))
            block.gpsimd(mk(self.ops["pool"]))
            block.sync(mk(self.ops["sp"]))
        self.ops = {e: [] for e in self.ENG}
        self._regs = {}

    emit_all = flush


class Ring:
    def __init__(self, items):
        self.items = items
        self.i = 0

    def next(self):
        it = self.items[self.i % len(self.items)]
        self.i += 1
        return it


def build_program(dbg=None):
    nc = bass.Bass("TRN2", target_bir_lowering=False)
    nc.dge_precook = False
    es = ExitStack()
    P = Prog(nc, es)

    def din(name, shape, dt=F32):
        return nc.dram_tensor(name, list(shape), dt, kind="ExternalInput").ap()

    def dscr(name, shape, dt=F32):
        kind = "ExternalOutput" if (dbg and name in dbg) else "Internal"
        return nc.dram_tensor(name, list(shape), dt, kind=kind).ap()

    cur = {"es": es, "n": 0}

    def sb(name, shape, dt=F32):
        return cur["es"].enter_context(nc.sbuf_tensor(name, list(shape), dt))

    def psum_banks(n=8, width=512):
        cur["n"] += 1
        return [cur["es"].enter_context(nc.psum_tensor(f"ps{cur['n']}_{i}", [128, width], F32)) for i in range(n)]

    def begin_phase():
        cur["es"] = ExitStack()

    def end_phase():
        P.flush()
        cur["es"].close()
        cur["es"] = es

    x = din("x", [NREAL, D])
    meta = din("meta", [16, D])
    ident_d = din("ident_d", [128, 128])
    lng_in = din("ln_in_gT", [128, KC])
    lnb_in = din("ln_in_bT", [128, KC])
    w_in = din("w_in", [D, IN_COLS], F32R)
    out = nc.dram_tensor("out", [NREAL, D], F32, kind="ExternalOutput").ap()

    HT = dscr("HT", [KC, 128, NTOK], F32R)
    Usc = dscr("Usc", [4, 128, NTOK], BF16)
    Qsc = dscr("Qsc", [8, 128, NTOK], BF16)
    Ksc = dscr("Ksc", [8, 128, NTOK], BF16)
    Vsc = dscr("Vsc", [8, NTOK, 128], BF16)

    ident = sb("ident", [128, 128])
    identb = sb("identb", [128, 128], BF16)
    g_in = sb("g_in", [128, KC])
    b_in = sb("b_in", [128, KC])
    epst = sb("epst", [128, 1])
    B_ident, B_identb, B_gin, B_bin, B_eps = P.bufs(5, "c")
    P.op("sp", lambda e: e.dma_start(out=ident[:], in_=ident_d), writes=[B_ident], dma="ldc")
    P.op("sp", lambda e: e.dma_start(out=g_in[:], in_=lng_in), writes=[B_gin], dma="ldc")
    P.op("sp", lambda e: e.dma_start(out=b_in[:], in_=lnb_in), writes=[B_bin], dma="ldc")
    P.op("dve", lambda e: e.tensor_copy(out=identb[:], in_=ident[:]), reads=[B_ident], writes=[B_identb])
    P.op("dve", lambda e: e.memset(epst[:], LN_EPS), writes=[B_eps])

    P.flush()

    begin_phase()
    psb = psum_banks(8)
    PS = Ring([(psb[i], P.buf(f"ps1_{i}")) for i in range(8)])
    xt = [sb(f"xt{i}", [128, D]) for i in range(2)]
    XT = Ring([(xt[i], P.buf(f"xt{i}")) for i in range(2)])
    hT = sb("hT", [128, KC, 512])
    B_hT = P.buf("hT")
    wts = [sb(f"wt{i}", [128, 8, 1024], F32R) for i in range(4)]
    WT = Ring([(wts[i], P.buf(f"wt{i}")) for i in range(4)])
    stat = [sb(f"stat{i}", [128, 4, 6]) for i in range(2)]
    mv = [sb(f"mv{i}", [128, 2]) for i in range(2)]
    rstd = [sb(f"rstd{i}", [128, 1]) for i in range(2)]
    ST = Ring([((stat[i], mv[i], rstd[i]), P.buf(f"st{i}")) for i in range(2)])
    ob = [sb(f"ob{i}", [128, 512], BF16) for i in range(3)]
    OB = Ring([(ob[i], P.buf(f"ob{i}")) for i in range(3)])
    vtm = [sb(f"vtm{i}", [128, 4, 128], BF16) for i in range(2)]
    VTM = Ring([(vtm[i], P.buf(f"vtm{i}")) for i in range(2)])

    w_in_v = w_in.rearrange("(k p) n -> p k n", p=128)

    def ln_tile_to_hT(src_ap, nrows, tcol, gT, bT, B_g, B_b):
        xa, Bx = XT.next()
        (sa, ma, ra), Bs = ST.next()
        if nrows < 128:
            P.op("dve", lambda e: e.memset(xa[:], 0.0), writes=[Bx])
        P.op("sp", lambda e: e.dma_start(out=xa[0:nrows, :], in_=src_ap), writes=[Bx], dma="ldx")
        sub = (dbg or {}).get('sub', 99)
        if sub < 2:
            return
        for q in range(4):
            P.op("dve", lambda e, q=q: e.bn_stats(out=sa[:, q, :], in_=xa[:, q * 512:(q + 1) * 512]),
                 reads=[Bx], writes=[Bs])
        P.op("dve", lambda e: e.bn_aggr(out=ma[:], in_=sa[:].rearrange("p a b -> p (a b)")), reads=[Bs], writes=[Bs])
        if sub < 3:
            return
        P.op("act", lambda e: e.activation(out=ra[:], in_=ma[:, 1:2], func=AF.Sqrt, bias=epst[:], scale=1.0),
             reads=[Bs, B_eps], writes=[Bs])
        P.op("dve", lambda e: e.reciprocal(out=ra[:], in_=ra[:]), reads=[Bs], writes=[Bs])
        if sub < 4:
            return
        P.op("dve", lambda e: e.tensor_scalar(out=xa[:], in0=xa[:], scalar1=ma[:, 0:1], scalar2=ra[:],
                                              op0=ALU.subtract, op1=ALU.mult), reads=[Bx, Bs], writes=[Bx])
        if sub < 5:
            return
        for b4 in range(4):
            pa, Bp = PS.next()
            for j in range(4):
                k = b4 * 4 + j
                P.op("pe", lambda e, k=k, j=j, pa=pa: e.transpose(out=pa[:, j * 128:(j + 1) * 128],
                                                           in_=xa[:, k * 128:(k + 1) * 128], identity=ident[:]),
                     reads=[Bx, B_ident], writes=[Bp])
            if sub < 6:
                continue
            if (dbg or {}).get('bar', 0):
                P.barrier()
            for j in range(4):
                k = b4 * 4 + j
                var = (dbg or {}).get('var', 0)
                if var == 0:
                    P.op("act", lambda e, k=k, j=j, pa=pa: e.activation(out=hT[:, k, tcol:tcol + 128].bitcast(F32R),
                                                                 in_=pa[:, j * 128:(j + 1) * 128], func=AF.Identity,
                                                                 scale=gT[:, k:k + 1], bias=bT[:, k:k + 1]),
                         reads=[Bp, B_g, B_b], writes=[B_hT])
                elif var == 3:
                    P.op("act", lambda e, k=k, j=j, pa=pa: e.activation(out=ident[:, :],
                                                                 in_=pa[:, j * 128:(j + 1) * 128], func=AF.Copy),
                         reads=[Bp, B_g, B_b], writes=[B_hT])
                elif var == 4:
                    P.op("act", lambda e, k=k, j=j, pa=pa: e.activation(out=hT[:, k, tcol:tcol + 128],
                                                                 in_=ident[:, :], func=AF.Copy),
                         reads=[Bp, B_g, B_b], writes=[B_hT])
                elif var == 1:
                    P.op("act", lambda e, k=k, j=j, pa=pa: e.activation(out=hT[:, k, tcol:tcol + 128],
                                                                 in_=pa[:, j * 128:(j + 1) * 128], func=AF.Copy),
                         reads=[Bp, B_g, B_b], writes=[B_hT])
                elif var == 2:
                    P.op("dve", lambda e, k=k, j=j, pa=pa: e.tensor_scalar(out=hT[:, k, tcol:tcol + 128],
                                                                 in0=pa[:, j * 128:(j + 1) * 128], scalar1=gT[:, k:k + 1],
                                                                 scalar2=bT[:, k:k + 1], op0=ALU.mult, op1=ALU.add),
                         reads=[Bp, B_g, B_b], writes=[B_hT])

    wcache = {}

    def proj_chunk(col0, ntok):
        base = ((col0 - 512) // 1024) * 1024 + 512 if col0 >= 512 else 0
        width = 1024 if col0 >= 512 else 512
        if wcache.get("base") != base:
            hv = []
            for hk in range(2):
                wa, Bw = WT.next()
                P.op("sp", lambda e, wa=wa, hk=hk: e.dma_start(out=wa[:, :, 0:width], in_=w_in_v[:, hk * 8:(hk + 1) * 8, base:base + width]), writes=[Bw], dma=1)
                hv.append((wa, Bw))
            wcache.update(base=base, hv=hv)
        hv = wcache["hv"]
        off = col0 - base
        pa, Bp = PS.next()
        for k in range(KC):
            wa, Bw = hv[k // 8]
            P.op("pe", lambda e, k=k, wa=wa: e.matmul(out=pa[:, 0:ntok], lhsT=wa[:, k % 8, off:off + 128],
                                                      rhs=hT[:, k, 0:ntok].bitcast(F32R), start=(k == 0), stop=(k == KC - 1)),
                 reads=[Bw, B_hT], writes=[Bp])
        return pa, Bp

    groups = [(g * 512, 512) for g in range(NREAL // 512)] + [(META0, 128)]
    if dbg and "ngroups" in dbg:
        groups = groups[:dbg["ngroups"]] + [groups[-1]]
    stage = (dbg or {}).get('stage', 99)
    for (tok0, ntok) in groups:
        if stage < 1:
            break
        ntile = ntok // 128
        wcache.clear()
        for t in range(ntile):
            if tok0 == META0:
                ln_tile_to_hT(meta, 16, 0, g_in, b_in, B_gin, B_bin)
            else:
                ln_tile_to_hT(x[tok0 + t * 128: tok0 + (t + 1) * 128, :], 128, t * 128, g_in, b_in, B_gin, B_bin)
        if stage >= 2:
          P.op("pool", lambda e, tok0=tok0, ntok=ntok: e.dma_start(
            out=HT[:, :, tok0:tok0 + ntok].rearrange("k p t -> p k t"), in_=hT[:, :, 0:ntok].bitcast(F32R)),
            reads=[B_hT], dma="st1")
        if stage < 3:
            continue
        plan = [("u", c, c * 128) for c in range(4)]
        if tok0 != META0:
            plan += [("q", c, 512 + c * 128) for c in range(8)]
        plan += [("k", c, 1536 + c * 128) for c in range(8)]
        plan += [("v", c, 2560 + c * 128) for c in range(8)]
        for (kind, c, col0) in plan:
            pa, Bp = proj_chunk(col0, ntok)
            oa, Bo = OB.next()
            if kind == "q":
                P.op("act", lambda e, pa=pa, oa=oa, ntok=ntok: e.activation(
                    out=oa[:, 0:ntok], in_=pa[:, 0:ntok], func=AF.Copy, scale=0.125), reads=[Bp], writes=[Bo])
            else:
                P.op("dve", lambda e, pa=pa, oa=oa, ntok=ntok: e.tensor_copy(out=oa[:, 0:ntok], in_=pa[:, 0:ntok]),
                     reads=[Bp], writes=[Bo])
            if kind != "v":
                dst = {"u": Usc, "q": Qsc, "k": Ksc}[kind]
                P.op("pool", lambda e, dst=dst, c=c, oa=oa, tok0=tok0, ntok=ntok: e.dma_start(
                    out=dst[c, :, tok0:tok0 + ntok], in_=oa[:, 0:ntok]), reads=[Bo], dma="st1")
            else:
                pt, Bpt = PS.next()
                ptb = pt[:].bitcast(BF16)
                va, Bv = VTM.next()
                for t in range(ntile):
                    P.op("pe", lambda e, t=t, oa=oa, ptb=ptb: e.transpose(
                        out=ptb[:, t * 128:(t + 1) * 128], in_=oa[:, t * 128:(t + 1) * 128], identity=identb[:]),
                        reads=[Bo, B_identb], writes=[Bpt])
                P.op("dve", lambda e, va=va, ptb=ptb, ntile=ntile: e.tensor_copy(
                    out=va[:, 0:ntile, :], in_=ptb[:, 0:ntile * 128].rearrange("p (t e) -> p t e", e=128)),
                    reads=[Bpt], writes=[Bv])
                P.op("pool", lambda e, c=c, va=va, tok0=tok0, ntile=ntile, ntok=ntok: e.dma_start(
                    out=Vsc[c, tok0:tok0 + ntok, :].rearrange("(t p) e -> p t e", p=128), in_=va[:, 0:ntile, :]),
                    reads=[Bv], dma="st1")
    end_phase()

    Yattn = dscr("Yattn", [8, 128, NTOK], F32R)
    lamv_d = din("lamv", [1, 256])
    gsub_d = din("gsub_b", [128, 128])


    YG = dscr("YG", [4, 128, NTOK], F32R)
    Yssm = dscr("Yssm", [4, 128, NTOK], F32R)
    s_are_d = din("ssm_are", [128, 16]); s_aim_d = din("ssm_aim", [128, 16]); s_ldt_d = din("ssm_ldt", [128, 16])
    s_bre_d = din("ssm_bre", [128, 16, 16]); s_bim_d = din("ssm_bim", [128, 16, 16])
    s_cre_d = din("ssm_cre", [128, 16, 16]); s_cim_d = din("ssm_cim", [128, 16, 16])
    s_d_d = din("ssm_dT", [128, 4]); wglu_d = din("ssm_wglu", [512, 512], F32R)
    TS = 1032
    tt_d = din("tt_d", [128, TS])
    PI = math.pi
    ph3 = (dbg or {}).get("ph3", 1)
    ph2 = (dbg or {}).get("ph2", 1)
    nseq2 = (dbg or {}).get("nseq2", NSEQ)
    mid = ExitStack()

    def sbm(name, shape, dt=F32):
        return mid.enter_context(nc.sbuf_tensor(name, list(shape), dt))

    if ph2:
        hpi = sbm("s_hpi", [128, 1]); B_hpi = P.buf("s_hpi")
        prm = sbm("s_prm", [128, 24, 16])
        dT = sbm("s_dT", [128, 4])
        tt = sbm("s_tt", [128, TS])
        wbt = sbm("s_wb", [128, 16, 2, 128], BF16)
        cwt = sbm("s_cw", [128, 16, 2, 128], BF16)
        begin_phase()
        yb = psum_banks(2, 512)
        YPS = Ring([(yb[i], P.buf(f"p2y{i}")) for i in range(2)])
        P.op("dve", lambda e: e.memset(hpi[:], PI / 2), writes=[B_hpi])
        NPRM = 24
        B_prm = P.buf("s_prm")
        names = ["are", "aim", "ldt", "lre", "dt", "mag", "ang", "sn", "cs", "t1", "t2", "ar", "ai", "nr", "den", "zr", "zi", "phs"]
        V = {n: prm[:, i, :] for i, n in enumerate(names)}
        bc = sb("s_bc", [128, 4, 16, 16]); B_bc = P.buf("s_bc")
        bb = sb("s_bb", [128, 4, 16, 16]); B_bb = P.buf("s_bb")
        B_dT = P.buf("s_dT")
        B_tt = P.buf("s_tt")
        P.op("sp", lambda e: e.dma_start(out=prm[:, 0, :], in_=s_are_d), writes=[B_prm], dma=1)
        P.op("sp", lambda e: e.dma_start(out=prm[:, 1, :], in_=s_aim_d), writes=[B_prm], dma=1)
        P.op("sp", lambda e: e.dma_start(out=prm[:, 2, :], in_=s_ldt_d), writes=[B_prm], dma=1)
        for i, dd in enumerate((s_bre_d, s_bim_d, s_cre_d, s_cim_d)):
            P.op("sp", lambda e, i=i, dd=dd: e.dma_start(out=bc[:, i, :, :], in_=dd), writes=[B_bc], dma=1)
        P.op("sp", lambda e: e.dma_start(out=dT[:], in_=s_d_d), writes=[B_dT], dma=1)
        P.op("sp", lambda e: e.dma_start(out=tt[:], in_=tt_d), writes=[B_tt], dma=1)

        def dv(fn, rd=(), wr=None):
            P.op("dve", fn, reads=[B_prm] + list(rd), writes=[wr or B_prm])

        def tt_(o, a, b, op):
            dv(lambda e: e.tensor_tensor(out=V[o], in0=V[a], in1=V[b], op=op))

        def ts_(o, a, s1, op0, s2=None, op1=None):
            if op1 is None:
                dv(lambda e: e.tensor_scalar(out=V[o], in0=V[a], scalar1=s1, scalar2=None, op0=op0))
            else:
                dv(lambda e: e.tensor_scalar(out=V[o], in0=V[a], scalar1=s1, scalar2=s2, op0=op0, op1=op1))

        def act_(o, a, func, scale=1.0):
            P.op("act", lambda e: e.activation(out=V[o], in_=V[a], func=func, scale=scale), reads=[B_prm], writes=[B_prm])

        ts_("lre", "are", -1e-4, ALU.min)
        act_("dt", "ldt", AF.Exp)
        tt_("t1", "lre", "dt", ALU.mult)
        act_("mag", "t1", AF.Exp)
        tt_("ang", "aim", "dt", ALU.mult)
        dv(lambda e: e.tensor_scalar(out=V["t1"].bitcast(I32), in0=V["ang"], scalar1=1.0 / (2 * PI), scalar2=None, op0=ALU.mult))
        dv(lambda e: e.tensor_copy(out=V["t2"], in_=V["t1"].bitcast(I32)))
        dv(lambda e: e.scalar_tensor_tensor(out=V["ang"], in0=V["t2"], scalar=-2 * PI, in1=V["ang"], op0=ALU.mult, op1=ALU.add))
        dv(lambda e: e.tensor_scalar(out=V["t1"], in0=V["ang"], scalar1=0.0, scalar2=2 * PI, op0=ALU.is_lt, op1=ALU.mult))
        tt_("ang", "ang", "t1", ALU.add)
        dv(lambda e: e.tensor_scalar(out=V["t1"], in0=V["ang"], scalar1=PI, scalar2=-2 * PI, op0=ALU.is_gt, op1=ALU.mult))
        tt_("t1", "t1", "ang", ALU.add)
        act_("sn", "t1", AF.Sin)
        dv(lambda e: e.tensor_scalar(out=V["t1"], in0=V["ang"], scalar1=PI / 2, scalar2=-2 * PI, op0=ALU.is_gt, op1=ALU.mult))
        dv(lambda e: e.scalar_tensor_tensor(out=V["t1"], in0=V["ang"], scalar=PI / 2, in1=V["t1"], op0=ALU.add, op1=ALU.add))
        act_("cs", "t1", AF.Sin)
        tt_("ar", "mag", "cs", ALU.mult)
        tt_("ai", "mag", "sn", ALU.mult)
        ts_("nr", "ar", -1.0, ALU.add)
        tt_("t1", "lre", "lre", ALU.mult)
        tt_("t2", "aim", "aim", ALU.mult)
        tt_("den", "t1", "t2", ALU.add)
        dv(lambda e: e.reciprocal(out=V["den"], in_=V["den"]))
        tt_("t1", "nr", "lre", ALU.mult)
        tt_("t2", "ai", "aim", ALU.mult)
        tt_("zr", "t1", "t2", ALU.add)
        tt_("zr", "zr", "den", ALU.mult)
        tt_("t1", "ai", "lre", ALU.mult)
        tt_("t2", "nr", "aim", ALU.mult)
        tt_("zi", "t1", "t2", ALU.subtract)
        tt_("zi", "zi", "den", ALU.mult)
        ts_("phs", "ang", float(TS), ALU.mult)
        zrb = V["zr"].unsqueeze(2).to_broadcast([128, 16, 16])
        zib = V["zi"].unsqueeze(2).to_broadcast([128, 16, 16])
        P.op("dve", lambda e: e.tensor_tensor(out=bb[:, 2], in0=bc[:, 0], in1=zrb, op=ALU.mult), reads=[B_prm, B_bc], writes=[B_bb])
        P.op("dve", lambda e: e.tensor_tensor(out=bb[:, 3], in0=bc[:, 1], in1=zib, op=ALU.mult), reads=[B_prm, B_bc], writes=[B_bb])
        P.op("dve", lambda e: e.tensor_tensor(out=bb[:, 0], in0=bb[:, 2], in1=bb[:, 3], op=ALU.subtract), reads=[B_bb], writes=[B_bb])
        P.op("dve", lambda e: e.tensor_tensor(out=bb[:, 2], in0=bc[:, 1], in1=zrb, op=ALU.mult), reads=[B_prm, B_bc, B_bb], writes=[B_bb])
        P.op("dve", lambda e: e.tensor_tensor(out=bb[:, 3], in0=bc[:, 0], in1=zib, op=ALU.mult), reads=[B_prm, B_bc, B_bb], writes=[B_bb])
        P.op("dve", lambda e: e.tensor_tensor(out=bb[:, 1], in0=bb[:, 2], in1=bb[:, 3], op=ALU.add), reads=[B_bb], writes=[B_bb])
        bmw = sb("s_bmw", [128, 16, 2, 128], BF16); B_bmw = P.buf("s_bmw")
        B_wb = P.buf("s_wb")
        B_cw = P.buf("s_cw")
        P.op("pool", lambda e: e.memset(bmw[:], 0.0), writes=[B_bmw])
        P.op("pool", lambda e: e.memset(cwt[:], 0.0), writes=[B_cw])
        for jm in range(4):
            for gl in range(2):
                c0 = 32 * jm + 16 * gl
                for ri in range(2):
                    P.op("dve", lambda e, jm=jm, gl=gl, ri=ri, c0=c0: e.tensor_copy(
                        out=bmw[64 * gl:64 * gl + 64, jm::4, ri, c0:c0 + 16], in_=bb[64 * gl:64 * gl + 64, ri, jm::4, :]),
                        reads=[B_bb], writes=[B_bmw])
                P.op("dve", lambda e, jm=jm, gl=gl, c0=c0: e.tensor_copy(
                    out=cwt[64 * gl:64 * gl + 64, jm::4, 0, c0:c0 + 16], in_=bc[64 * gl:64 * gl + 64, 2, jm::4, :]),
                    reads=[B_bc], writes=[B_cw])
                P.op("dve", lambda e, jm=jm, gl=gl, c0=c0: e.tensor_scalar(
                    out=cwt[64 * gl:64 * gl + 64, jm::4, 1, c0:c0 + 16], in0=bc[64 * gl:64 * gl + 64, 3, jm::4, :],
                    scalar1=-1.0, scalar2=None, op0=ALU.mult), reads=[B_bc], writes=[B_cw])
        for g4 in range(8):
            pa, Bp = YPS.next()
            pab = pa[:].bitcast(BF16)
            for i4 in range(4):
                idx = g4 * 4 + i4
                j, ri = idx // 2, idx % 2
                P.op("pe", lambda e, pab=pab, i4=i4, j=j, ri=ri: e.transpose(out=pab[:, i4 * 128:(i4 + 1) * 128], in_=bmw[:, j, ri, :],
                                                                             identity=identb[:]), reads=[B_bmw, B_identb], writes=[Bp])
            j0 = (g4 * 4) // 2
            P.op("dve", lambda e, pab=pab, j0=j0: e.tensor_copy(out=wbt[:, j0:j0 + 2, :, :].rearrange("p a b c -> p (a b c)"),
                                                                 in_=pab[:, 0:512]), reads=[Bp], writes=[B_wb])

        end_phase()

    begin_phase()
    psb = psum_banks(8)
    bankA = psb[6]; B_bankA = P.buf("p23_bankA")
    bankB = psb[7]; B_bankBt = P.buf("p23_bankB"); B_bankBy = B_bankBt
    g3 = None
    g2 = None
    if ph3:
        PS = Ring([(psb[7], B_bankBt)])
        lamv = sb("lamv_s", [1, 256]); lamt = sb("lamt", [1, 8]); ones1 = sb("ones1", [1, 128])
        neglam = sb("neglam", [128, 1]); gsub = sb("gsub", [128, 128])
        B_lam, B_neglam, B_gsub, B_ones1 = P.bufs(4, "a3c")
        P.op("sp", lambda e: e.dma_start(out=lamv[:], in_=lamv_d), writes=[B_lam], dma=1)
        P.op("sp", lambda e: e.dma_start(out=gsub[:], in_=gsub_d), writes=[B_gsub], dma=1)
        P.op("dve", lambda e: e.memset(ones1[:], 1.0), writes=[B_ones1])
        P.op("dve", lambda e: e.tensor_scalar(out=gsub[:], in0=gsub[:], scalar1=1.0 - LAMBDA_INIT, scalar2=None, op0=ALU.mult),
             reads=[B_gsub], writes=[B_gsub])
        P.op("dve", lambda e: e.tensor_tensor(out=lamv[:, 0:64], in0=lamv[:, 0:64], in1=lamv[:, 64:128], op=ALU.mult), reads=[B_lam], writes=[B_lam])
        P.op("dve", lambda e: e.tensor_tensor(out=lamv[:, 128:192], in0=lamv[:, 128:192], in1=lamv[:, 192:256], op=ALU.mult), reads=[B_lam], writes=[B_lam])
        P.op("dve", lambda e: e.tensor_reduce(out=lamt[:, 0:1], in_=lamv[:, 0:64], axis=AX.X, op=ALU.add), reads=[B_lam], writes=[B_lam])
        P.op("dve", lambda e: e.tensor_reduce(out=lamt[:, 1:2], in_=lamv[:, 128:192], axis=AX.X, op=ALU.add), reads=[B_lam], writes=[B_lam])
        P.op("act", lambda e: e.activation(out=lamt[:, 2:4], in_=lamt[:, 0:2], func=AF.Exp), reads=[B_lam], writes=[B_lam])
        P.op("dve", lambda e: e.tensor_tensor(out=lamt[:, 4:5], in0=lamt[:, 3:4], in1=lamt[:, 2:3], op=ALU.subtract), reads=[B_lam], writes=[B_lam])
        P.op("dve", lambda e: e.tensor_scalar(out=lamt[:, 5:6], in0=lamt[:, 4:5], scalar1=-LAMBDA_INIT, scalar2=None, op0=ALU.add), reads=[B_lam], writes=[B_lam])
        pa, Bp = PS.next()
        P.op("pe", lambda e, pa=pa: e.matmul(out=pa[:, 0:1], lhsT=ones1[:, :], rhs=lamt[:, 5:6], start=True, stop=True),
             reads=[B_lam, B_ones1], writes=[Bp])
        P.op("dve", lambda e, pa=pa: e.tensor_copy(out=neglam[:], in_=pa[:, 0:1]), reads=[Bp], writes=[B_neglam])

        ORING = Ring([(psb[i], P.buf(f"pso{i}")) for i in range(0, 4)])
        SRING = Ring([(psb[i], P.buf(f"pss{i}")) for i in range(4, 6)])
        TRING = Ring([(psb[7], B_bankBt)])
        kts = [sb(f"a_kt{i}", [128, 2, 128 + SEQ], BF16) for i in range(2)]
        qts = [sb(f"a_qt{i}", [128, SEQ], BF16) for i in range(2)]
        vts = [sb(f"a_vt{i}", [128, 17, 129], BF16) for i in range(2)]
        HRING = Ring([((kts[i], qts[i], vts[i]), (P.buf(f"a_kt{i}"), P.buf(f"a_qt{i}"), P.buf(f"a_vt{i}"), P.buf(f"a_vm{i}"))) for i in range(2)])
        for i in range(2):
            P.op("pool", lambda e, i=i: e.memset(vts[i][:], 1.0), writes=[HRING.items[i][1][2], HRING.items[i][1][3]])
            P.op("pool", lambda e, i=i: e.memset(vts[i][:, 16, :], 0.0), writes=[HRING.items[i][1][3]])
            P.op("pool", lambda e, i=i: e.memset(vts[i][0:16, 16, 128:129], 1.0), writes=[HRING.items[i][1][3]])
            P.op("pool", lambda e, i=i: e.memset(kts[i][:], 0.0), writes=[HRING.items[i][1][0]])
        pts = [sb(f"a_pt{i}", [128, 512], BF16) for i in range(5)]
        PTR = Ring([(pts[i], P.buf(f"a_pt{i}")) for i in range(5)])
        yTs = [sb(f"a_yT{i}", [128, SEQ], F32R) for i in range(2)]
        YTR = Ring([(yTs[i], P.buf(f"a_yT{i}")) for i in range(2)])
        eps3 = [(sb(f"a_rc{i}", [128, 4]), sb(f"a_t1{i}", [128, 128]), sb(f"a_od{i}", [128, 128]), sb(f"a_yq{i}", [128, 128]),
                 sb(f"a_jk{i}", [128, 128])) for i in range(4)]
        EPR = Ring([(eps3[i], P.bufs(5, f"a_ep{i}_")) for i in range(4)])

        def gen3():
            nseq3 = (dbg or {}).get("nseq3", NSEQ)
            nhead3 = (dbg or {}).get("nhead3", 8)
            for s_ in range(nseq3):
                for h in range(nhead3):
                    (kt, qt_, vt), (Bk, Bq, Bv, Bvm) = HRING.next()
                    r0 = s_ * SEQ
                    for m in range(2):
                        P.op("sp", lambda e, kt=kt, h=h, m=m: e.dma_start(out=kt[m * 64:(m + 1) * 64, m, 0:16], in_=Ksc[h, m * 64:(m + 1) * 64, META0:META0 + 16]), writes=[Bk], dma=1)
                        P.op("sp", lambda e, kt=kt, h=h, r0=r0, m=m: e.dma_start(out=kt[m * 64:(m + 1) * 64, m, 128:128 + SEQ], in_=Ksc[h, m * 64:(m + 1) * 64, r0:r0 + SEQ]), writes=[Bk], dma=1)
                    P.op("sp", lambda e, qt_=qt_, h=h, r0=r0: e.dma_start(out=qt_[:, :], in_=Qsc[h, :, r0:r0 + SEQ]), writes=[Bq], dma=1)
                    P.op("sp", lambda e, vt=vt, h=h, r0=r0: e.dma_start(out=vt[:, 0:16, 0:128], in_=Vsc[h, r0:r0 + SEQ, :].rearrange("(t p) e -> p t e", p=128)), writes=[Bv], dma=1)
                    P.op("sp", lambda e, vt=vt, h=h: e.dma_start(out=vt[0:16, 16, 0:128], in_=Vsc[h, META0:META0 + 16, :]), writes=[Bvm], dma=1)
                    yT, ByT = YTR.next()
                    steps = []
                    for qi in range(SEQ // 128):
                        blks = [("m", 0)] + [("r", kb) for kb in range(qi + 1)]
                        pairs = [blks[i:i + 2] for i in range(0, len(blks), 2)]
                        for pi_, pr in enumerate(pairs):
                            steps.append((qi, pi_, pr, len(pairs)))
                    LA = 2
                    pend = {}
                    oacc = {}
                    later = []

                    def emit_S(st):
                        qi, pi_, pr, npair = st
                        sp_, Bs_ = SRING.next()
                        for bj, (typ, kb) in enumerate(pr):
                            kc0 = 0 if typ == "m" else 128 + kb * 128
                            for m in range(2):
                                c0 = bj * 256 + m * 128
                                P.op("pe", lambda e, sp_=sp_, m=m, kc0=kc0, kt=kt, qt_=qt_, qi=qi, c0=c0: e.matmul(
                                    out=sp_[:, c0:c0 + 128], lhsT=kt[:, m, kc0:kc0 + 128],
                                    rhs=qt_[:, qi * 128:(qi + 1) * 128], start=True, stop=True),
                                    reads=[Bk, Bq], writes=[Bs_])
                        pt, Bpt = PTR.next()
                        w_ = 256 * len(pr)
                        P.op("act", lambda e, pt=pt, sp_=sp_, w_=w_: e.activation(out=pt[:, 0:w_], in_=sp_[:, 0:w_], func=AF.Exp),
                             reads=[Bs_], writes=[Bpt])
                        for bj, (typ, kb) in enumerate(pr):
                            if typ == "r" and kb == qi:
                                P.op("pool", lambda e, pt=pt, bj=bj: e.memset(pt[64:128, bj * 256:(bj + 1) * 256].rearrange("p (m q) -> p m q", m=2)[:, :, 0:64], 0.0),
                                     reads=[Bpt], writes=[Bpt])
                        pend[(qi, pi_)] = (pt, Bpt)

                    def emit_AV(st, now):
                        qi, pi_, pr, npair = st
                        pt, Bpt = pend.pop((qi, pi_))
                        if pi_ == 0:
                            oacc[qi] = (ORING.next(), ORING.next())
                        (o0, Bo0), (o1, Bo1) = oacc[qi]
                        for bj, (typ, kb) in enumerate(pr):
                            vidx = 16 if typ == "m" else kb
                            first = (pi_ == 0 and bj == 0)
                            last = (pi_ == npair - 1 and bj == len(pr) - 1)
                            for m, (oo, Boo) in enumerate(((o0, Bo0), (o1, Bo1))):
                                c0 = bj * 256 + m * 128
                                P.op("pe", lambda e, oo=oo, pt=pt, c0=c0, vt=vt, vidx=vidx, first=first, last=last: e.matmul(
                                    out=oo[:, 0:129], lhsT=pt[:, c0:c0 + 128], rhs=vt[:, vidx, :], start=first, stop=last),
                                    reads=[Bpt, Bv, Bvm], writes=[Boo])
                        if pi_ != npair - 1:
                            return
                        del oacc[qi]
                        (rc, t1, od, yq, jk), (Brc, Bt1, Bod, Byq, Bjk) = EPR.next()
                        P.op("dve", lambda e, rc=rc, o0=o0: e.reciprocal(out=rc[:, 0:1], in_=o0[:, 128:129]), reads=[Bo0], writes=[Brc])
                        P.op("dve", lambda e, rc=rc, o1=o1: e.reciprocal(out=rc[:, 1:2], in_=o1[:, 128:129]), reads=[Bo1], writes=[Brc])
                        P.op("dve", lambda e, rc=rc: e.tensor_scalar(out=rc[:, 2:3], in0=rc[:, 1:2], scalar1=neglam[:, 0:1], scalar2=None, op0=ALU.mult),
                             reads=[Brc, B_neglam], writes=[Brc])
                        P.op("dve", lambda e, rc=rc, t1=t1, o1=o1: e.tensor_scalar(out=t1[:], in0=o1[:, 0:128], scalar1=rc[:, 2:3], scalar2=None, op0=ALU.mult),
                             reads=[Brc, Bo1], writes=[Bt1])
                        P.op("dve", lambda e, rc=rc, t1=t1, o0=o0, od=od: e.scalar_tensor_tensor(out=od[:], in0=o0[:, 0:128], scalar=rc[:, 0:1], in1=t1[:],
                                                                                           op0=ALU.mult, op1=ALU.add),
                             reads=[Brc, Bo0, Bt1], writes=[Bod])
                        P.op("dve", lambda e, od=od, jk=jk, rc=rc: e.scalar_tensor_tensor(out=jk[:], in0=od[:], scalar=1.0, in1=od[:], op0=ALU.mult, op1=ALU.mult,
                                                                                    accum_out=rc[:, 3:4]),
                             reads=[Bod, Brc], writes=[Bjk, Brc])

                        def stage2(rc=rc, od=od, yq=yq, Brc=Brc, Bod=Bod, Byq=Byq):
                            P.op("act", lambda e: e.activation(out=rc[:, 3:4], in_=rc[:, 3:4], func=AF.Sqrt, bias=epst[:], scale=1.0 / 128.0),
                                 reads=[Brc, B_eps], writes=[Brc])
                            P.op("dve", lambda e: e.reciprocal(out=rc[:, 3:4], in_=rc[:, 3:4]), reads=[Brc], writes=[Brc])
                            P.op("dve", lambda e: e.scalar_tensor_tensor(out=yq[:], in0=od[:], scalar=rc[:, 3:4], in1=gsub[:],
                                                                         op0=ALU.mult, op1=ALU.mult),
                                 reads=[Brc, Bod, B_gsub], writes=[Byq])

                        def stage3(yq=yq, Byq=Byq, qi=qi, yT=yT, ByT=ByT):
                            tp, Btp = TRING.next()
                            P.op("pe", lambda e: e.transpose(out=tp[:, 0:128], in_=yq[:], identity=ident[:]), reads=[Byq, B_ident], writes=[Btp])
                            P.op("dve", lambda e: e.tensor_copy(out=yT[:, qi * 128:(qi + 1) * 128], in_=tp[:, 0:128]),
                                 reads=[Btp], writes=[ByT])

                        later.append((now + 3, stage2))
                        later.append((now + 6, stage3))

                    def run_later(now):
                        keep = []
                        for due, fn in later:
                            if due <= now:
                                fn()
                            else:
                                keep.append((due, fn))
                        later[:] = keep

                    nst = len(steps)
                    for i in range(nst + LA):
                        if i < nst:
                            emit_S(steps[i])
                        if i >= LA:
                            emit_AV(steps[i - LA], i)
                        run_later(i)
                        yield
                    run_later(10 ** 9)
                    P.op("pool", lambda e, yT=yT, h=h, r0=r0: e.dma_start(out=Yattn[h, :, r0:r0 + SEQ], in_=yT[:, :]), reads=[ByT], dma=1)

        g3 = gen3()
    if ph2:
        uTs = [[sb(f"s_uT{s_}_{i}", [128, TS], BF16) for i in range(2)] for s_ in range(NSEQ)]
        UTR = [Ring([(uTs[s_][i], P.buf(f"s_uT{s_}_{i}")) for i in range(2)]) for s_ in range(NSEQ)]
        wk = {n: sb("s_" + n, [128, TS]) for n in ("A1", "A2", "M1", "M2", "M3", "M4", "Z1", "Z2", "W1", "W2", "N1", "N2", "N3", "N4",
                                                   "ST", "CT", "Rt")}
        Bw = {n: P.buf("s_" + n) for n in wk}
        ygs = [sb(f"s_yg{i}", [128, TS], F32R) for i in range(1)]
        YGR = Ring([(ygs[i], P.buf(f"s_yg{i}")) for i in range(1)])
        Xs = [sb(f"s_X{s_}", [128, 4, 2, TS], BF16) for s_ in range(NSEQ)]
        B_X = [[[P.buf(f"s_X{s_}{a}{b}") for b in range(2)] for a in range(4)] for s_ in range(NSEQ)]
        carry = sb("s_carry", [128, NSEQ, 16, 2]); B_carry = P.buf("s_carry")

        def gen2():
            CT6 = lambda ap: ap.rearrange("p (a b) -> p a b", b=172)
            for seg in range(2):
                for q in range(4):
                    uT_s = []
                    for s_ in range(nseq2):
                        r0 = s_ * SEQ
                        uT, BuT = UTR[s_].next()
                        if seg == 0:
                            P.op("sp", lambda e, uT=uT, q=q: e.dma_start(out=uT[:, 0:16], in_=Usc[q, :, META0:META0 + 16]), writes=[BuT], dma=1)
                            P.op("sp", lambda e, uT=uT, q=q, r0=r0: e.dma_start(out=uT[:, 16:TS], in_=Usc[q, :, r0:r0 + TS - 16]), writes=[BuT], dma=1)
                        else:
                            P.op("sp", lambda e, uT=uT, q=q, r0=r0: e.dma_start(out=uT[:, :], in_=Usc[q, :, r0 + TS - 16:r0 + SEQ]), writes=[BuT], dma=1)
                        uT_s.append((uT, BuT))
                    for jm in range(4):
                        j = 4 * q + jm
                        phj = V["ang"][:, j:j + 1]
                        if seg == 0:
                            P.op("dve", lambda e, phj=phj: e.tensor_scalar(out=wk["A1"][:], in0=tt[:], scalar1=phj, scalar2=None, op0=ALU.mult),
                                 reads=[B_tt, B_prm], writes=[Bw["A1"]])
                        else:
                            P.op("dve", lambda e, phj=phj, j=j: e.tensor_scalar(out=wk["A1"][:], in0=tt[:], scalar1=phj, scalar2=V["phs"][:, j:j + 1],
                                                                            op0=ALU.mult, op1=ALU.add), reads=[B_tt, B_prm], writes=[Bw["A1"]])
                        P.op("dve", lambda e: e.tensor_scalar(out=wk["A2"][:].bitcast(I32), in0=wk["A1"][:], scalar1=1.0 / (2 * PI), scalar2=None, op0=ALU.mult),
                             reads=[Bw["A1"]], writes=[Bw["A2"]])
                        P.op("dve", lambda e: e.scalar_tensor_tensor(out=wk["A1"][:], in0=wk["A2"][:].bitcast(I32), scalar=-2 * PI, in1=wk["A1"][:], op0=ALU.mult, op1=ALU.add),
                             reads=[Bw["A2"], Bw["A1"]], writes=[Bw["A1"]])
                        P.op("dve", lambda e: e.tensor_scalar(out=wk["A1"][:], in0=wk["A1"][:], scalar1=-PI, scalar2=PI, op0=ALU.max, op1=ALU.min),
                             reads=[Bw["A1"]], writes=[Bw["A1"]])
                        yield
                        P.op("act", lambda e: e.activation(out=wk["ST"][:], in_=wk["A1"][:], func=AF.Sin), reads=[Bw["A1"]], writes=[Bw["ST"]])
                        P.op("act", lambda e: e.activation(out=wk["A2"][:], in_=wk["A1"][:], func=AF.Abs), reads=[Bw["A1"]], writes=[Bw["A2"]])
                        P.op("act", lambda e: e.activation(out=wk["CT"][:], in_=wk["A2"][:], func=AF.Sin, scale=-1.0, bias=hpi[:]), reads=[Bw["A2"], B_hpi], writes=[Bw["CT"]])
                        P.op("pool", lambda e, j=j: e.tensor_scalar(out=wk["Rt"][:], in0=tt[:], scalar1=0.0, scalar2=V["mag"][:, j:j + 1], op0=ALU.mult, op1=ALU.add),
                             reads=[B_tt, B_prm], writes=[Bw["Rt"]])
                        yield
                        for s_ in range(nseq2):
                            uT, BuT = uT_s[s_]
                            for p6 in range(6):
                                c0 = p6 * 172
                                for ri in range(2):
                                    P.op("pe", lambda e, ri=ri, c0=c0, j=j, uT=uT: e.matmul(
                                        out=bankA[:, ri * 172:(ri + 1) * 172], lhsT=wbt[:, j, ri, :], rhs=uT[:, c0:c0 + 172],
                                        start=True, stop=True), reads=[B_wb, BuT], writes=[B_bankA])
                                bur, bui = bankA[:, 0:172], bankA[:, 172:344]
                                for (mn, src, tab) in (("M1", bur, "CT"), ("M2", bui, "ST"), ("M3", bui, "CT"), ("M4", bur, "ST")):
                                    P.op("dve", lambda e, mn=mn, src=src, tab=tab, c0=c0: e.tensor_tensor(out=wk[mn][:, c0:c0 + 172], in0=src, in1=wk[tab][:, c0:c0 + 172], op=ALU.mult),
                                         reads=[B_bankA, Bw[tab]], writes=[Bw[mn]])
                                yield
                            P.op("pool", lambda e: e.tensor_tensor(out=wk["Z1"][:], in0=wk["M1"][:], in1=wk["M2"][:], op=ALU.add),
                                 reads=[Bw["M1"], Bw["M2"]], writes=[Bw["Z1"]])
                            P.op("pool", lambda e: e.tensor_tensor(out=wk["Z2"][:], in0=wk["M3"][:], in1=wk["M4"][:], op=ALU.subtract),
                                 reads=[Bw["M3"], Bw["M4"]], writes=[Bw["Z2"]])
                            yield
                            for ri, (zn, wn) in enumerate((("Z1", "W1"), ("Z2", "W2"))):
                                if seg == 0:
                                    P.op("dve", lambda e, zn=zn, wn=wn: e.tensor_tensor_scan(out=wk[wn][:], data0=wk["Rt"][:], data1=wk[zn][:], initial=0.0,
                                                                                          op0=ALU.mult, op1=ALU.add),
                                         reads=[Bw["Rt"], Bw[zn]], writes=[Bw[wn]])
                                    P.op("dve", lambda e, wn=wn, j=j, ri=ri, s_=s_: e.tensor_copy(out=carry[:, s_, j, ri:ri + 1], in_=wk[wn][:, TS - 1:TS]),
                                         reads=[Bw[wn]], writes=[B_carry])
                                else:
                                    P.op("dve", lambda e, zn=zn, wn=wn, j=j, ri=ri, s_=s_: e.tensor_tensor_scan(out=wk[wn][:], data0=wk["Rt"][:], data1=wk[zn][:],
                                                                                                        initial=carry[:, s_, j, ri:ri + 1], op0=ALU.mult, op1=ALU.add),
                                         reads=[Bw["Rt"], Bw[zn], B_carry], writes=[Bw[wn]])
                            yield
                            P.op("dve", lambda e: e.tensor_tensor(out=wk["N1"][:], in0=wk["W1"][:], in1=wk["CT"][:], op=ALU.mult),
                                 reads=[Bw["W1"], Bw["CT"]], writes=[Bw["N1"]])
                            P.op("dve", lambda e: e.tensor_tensor(out=wk["N2"][:], in0=wk["W2"][:], in1=wk["ST"][:], op=ALU.mult),
                                 reads=[Bw["W2"], Bw["ST"]], writes=[Bw["N2"]])
                            P.op("dve", lambda e, jm=jm, s_=s_: e.tensor_tensor(out=Xs[s_][:, jm, 0, :], in0=wk["N1"][:], in1=wk["N2"][:], op=ALU.subtract),
                                 reads=[Bw["N1"], Bw["N2"]], writes=[B_X[s_][jm][0]])
                            P.op("pool", lambda e: e.tensor_tensor(out=wk["N3"][:], in0=wk["W1"][:], in1=wk["ST"][:], op=ALU.mult),
                                 reads=[Bw["W1"], Bw["ST"]], writes=[Bw["N3"]])
                            P.op("pool", lambda e: e.tensor_tensor(out=wk["N4"][:], in0=wk["W2"][:], in1=wk["CT"][:], op=ALU.mult),
                                 reads=[Bw["W2"], Bw["CT"]], writes=[Bw["N4"]])
                            P.op("pool", lambda e, jm=jm, s_=s_: e.tensor_tensor(out=Xs[s_][:, jm, 1, :], in0=wk["N3"][:], in1=wk["N4"][:], op=ALU.add),
                                 reads=[Bw["N3"], Bw["N4"]], writes=[B_X[s_][jm][1]])
                            yield
                    for _ in range(3):
                        yield
                    for s_ in range(nseq2):
                        r0 = s_ * SEQ
                        uT, BuT = uT_s[s_]
                        for p3 in range(3):
                            n = 0
                            for jm in range(4):
                                for ri in range(2):
                                    P.op("pe", lambda e, jm=jm, ri=ri, q=q, p3=p3, n=n, s_=s_: e.matmul(
                                        out=bankB[:, 128:472], lhsT=cwt[:, 4 * q + jm, ri, :], rhs=Xs[s_][:, jm, ri, p3 * 344:(p3 + 1) * 344],
                                        start=(n == 0), stop=(n == 7)), reads=[B_cw, B_X[s_][jm][ri]], writes=[B_bankBy])
                                    n += 1
                            P.op("dve", lambda e, uT=uT, q=q, p3=p3: e.scalar_tensor_tensor(
                                out=wk["M1"][:, p3 * 344:(p3 + 1) * 344], in0=uT[:, p3 * 344:(p3 + 1) * 344], scalar=dT[:, q:q + 1], in1=bankB[:, 128:472],
                                op0=ALU.mult, op1=ALU.add), reads=[B_bankBy, BuT, B_dT], writes=[Bw["M1"]])
                            yield
                        yg, Byg = YGR.next()
                        P.op("pool", lambda e: e.tensor_tensor(out=wk["M2"][:], in0=wk["M1"][:], in1=wk["M1"][:], op=ALU.mult), reads=[Bw["M1"]], writes=[Bw["M2"]])
                        P.op("pool", lambda e: e.tensor_scalar(out=wk["M2"][:], in0=wk["M2"][:], scalar1=0.044715, scalar2=1.0, op0=ALU.mult, op1=ALU.add),
                             reads=[Bw["M2"]], writes=[Bw["M2"]])
                        P.op("pool", lambda e: e.tensor_tensor(out=wk["M2"][:], in0=wk["M2"][:], in1=wk["M1"][:], op=ALU.mult), reads=[Bw["M2"], Bw["M1"]], writes=[Bw["M2"]])
                        yield
                        P.op("act", lambda e: e.activation(out=wk["M2"][:], in_=wk["M2"][:], func=AF.Sigmoid, scale=1.5957691216057308),
                             reads=[Bw["M2"]], writes=[Bw["M2"]])
                        P.op("pool", lambda e, yg=yg: e.tensor_tensor(out=yg[:], in0=wk["M2"][:], in1=wk["M1"][:], op=ALU.mult),
                             reads=[Bw["M2"], Bw["M1"]], writes=[Byg])
                        if seg == 0:
                            P.op("pool", lambda e, yg=yg, q=q, r0=r0: e.dma_start(out=YG[q, :, r0:r0 + TS - 16], in_=yg[:, 16:TS]), reads=[Byg], dma=1)
                        else:
                            P.op("pool", lambda e, yg=yg, q=q, r0=r0: e.dma_start(out=YG[q, :, r0 + TS - 16:r0 + SEQ], in_=yg[:, :]), reads=[Byg], dma=1)
                        yield

        g2 = gen2()
    alive3, alive2 = g3 is not None, g2 is not None
    while alive3 or alive2:
        for _ in range(2):
            if alive3:
                try:
                    next(g3)
                except StopIteration:
                    alive3 = False
        if alive2:
            try:
                next(g2)
            except StopIteration:
                alive2 = False
    end_phase()
    if ph2:

        begin_phase()
        psb = psum_banks(8)
        PS = Ring([(psb[i], P.buf(f"ps2b_{i}")) for i in range(8)])
        wgl = sb("g_w", [128, 4, 512], F32R); B_wgl = P.buf("g_w")
        P.op("sp", lambda e: e.dma_start(out=wgl[:], in_=wglu_d.rearrange("(k p) n -> p k n", p=128)), writes=[B_wgl], dma=1)
        gys = [sb(f"g_y{i}", [128, 4, 512], F32R) for i in range(2)]
        GYR = Ring([(gys[i], P.buf(f"g_y{i}")) for i in range(2)])
        gsg = [sb(f"g_s{i}", [128, 512]) for i in range(2)]
        GSR = Ring([(gsg[i], P.buf(f"g_s{i}")) for i in range(2)])
        gos = [sb(f"g_o{i}", [128, 512], F32R) for i in range(2)]
        GOR = Ring([(gos[i], P.buf(f"g_o{i}")) for i in range(2)])
        for tp in range(nseq2 * SEQ // 512):
            gy, Bgy = GYR.next()
            P.op("sp", lambda e, gy=gy, tp=tp: e.dma_start(out=gy[:], in_=YG[:, :, tp * 512:(tp + 1) * 512].rearrange("k p t -> p k t")), writes=[Bgy], dma=1)
            for c in range(4):
                pa, Bp = PS.next()
                for k in range(4):
                    P.op("pe", lambda e, pa=pa, k=k, c=c, gy=gy: e.matmul(out=pa[:, :], lhsT=wgl[:, k, c * 128:(c + 1) * 128], rhs=gy[:, k, :],
                                                                       start=(k == 0), stop=(k == 3)), reads=[B_wgl, Bgy], writes=[Bp])
                sg, Bsg = GSR.next()
                go, Bgo = GOR.next()
                P.op("act", lambda e, pa=pa, sg=sg: e.activation(out=sg[:], in_=pa[:, :], func=AF.Sigmoid), reads=[Bp], writes=[Bsg])
                P.op("dve", lambda e, sg=sg, go=go, gy=gy, c=c: e.tensor_tensor(out=go[:], in0=sg[:], in1=gy[:, c, :].bitcast(F32), op=ALU.mult),
                     reads=[Bsg, Bgy], writes=[Bgo])
                P.op("pool", lambda e, go=go, c=c, tp=tp: e.dma_start(out=Yssm[c, :, tp * 512:(tp + 1) * 512], in_=go[:]), reads=[Bgo], dma=1)
        end_phase()
    mid.close()


    H1T = dscr("H1T", [KC, 128, NREAL], F32R)
    H1N = dscr("H1N", [NREAL, D])
    wbs_d = din("w_br_ssm", [512, D], F32R)
    wba_d = din("w_br_attn", [1024, D], F32R)
    wo_d = din("w_o", [D, D], F32R)
    ln1g_d = din("ln1_gT", [128, KC]); ln1b_d = din("ln1_bT", [128, KC])
    ph4 = (dbg or {}).get("ph4", 1)
    if ph4:
        begin_phase()
        psb = psum_banks(8)
        PS = Ring([(psb[i], P.buf(f"ps4_{i}")) for i in range(8)])
        g1T = sb("p4_g1", [128, KC]); b1T = sb("p4_b1", [128, KC]); B_g1, B_b1 = P.bufs(2, "p4gb")
        P.op("sp", lambda e: e.dma_start(out=g1T[:], in_=ln1g_d), writes=[B_g1], dma=1)
        P.op("sp", lambda e: e.dma_start(out=b1T[:], in_=ln1b_d), writes=[B_b1], dma=1)
        hT4 = sb("p4_hT", [128, KC, 512], F32R); B_hT4 = P.buf("p4_hT")
        mT = sb("p4_mT", [128, KC, 512], F32R); B_mT = P.buf("p4_mT")
        ysT = sb("p4_ys", [128, 4, 512], F32R); B_ysT = P.buf("p4_ys")
        yaT = sb("p4_ya", [128, 8, 512], F32R); B_yaT = P.buf("p4_ya")
        w16 = [sb(f"p4_w16_{i}", [128, 8, 512], F32R) for i in range(5)]
        W16 = Ring([(w16[i], P.buf(f"p4_w16_{i}")) for i in range(5)])
        S1t = sb("p4_S1", [128, 4, 512]); B_S1 = [P.buf(f"p4_S1_{f}") for f in range(4)]
        sgs = [sb(f"p4_sg{i}", [128, 512]) for i in range(4)]
        SG = Ring([(sgs[i], P.buf(f"p4_sg{i}")) for i in range(4)])
        xt4 = [sb(f"p4_xt{i}", [128, D]) for i in range(2)]
        XT4 = Ring([(xt4[i], P.buf(f"p4_xt{i}")) for i in range(2)])
        st4 = [(sb(f"p4_stat{i}", [128, 4, 6]), sb(f"p4_mv{i}", [128, 2]), sb(f"p4_rstd{i}", [128, 1])) for i in range(2)]
        ST4 = Ring([(st4[i], P.buf(f"p4_st{i}")) for i in range(2)])
        w_in_v4 = w_in.rearrange("(k p) n -> p k n", p=128)
        wo_v = wo_d.rearrange("(k p) n -> p k n", p=128)
        wbs_v = wbs_d.rearrange("(k p) n -> p k n", p=128)
        wba_v = wba_d.rearrange("(k p) n -> p k n", p=128)

        def mm_chain(pa, Bp, wtile, Bw, off, nk, rhs_tile, B_rhs):
            for k in range(nk):
                P.op("pe", lambda e, k=k: e.matmul(out=pa[:, 0:512], lhsT=wtile[:, k, off:off + 128], rhs=rhs_tile[:, k, :],
                                                   start=(k == 0), stop=(k == nk - 1)), reads=[Bw, B_rhs], writes=[Bp])

        ngrp4 = (dbg or {}).get("ngrp4", NREAL // 512)
        for g in range(ngrp4):
            tok0 = g * 512
            P.op("sp", lambda e, tok0=tok0: e.dma_start(out=hT4[:], in_=HT[:, :, tok0:tok0 + 512].rearrange("k p t -> p k t")), writes=[B_hT4], dma=1)
            P.op("sp", lambda e, tok0=tok0: e.dma_start(out=ysT[:], in_=Yssm[:, :, tok0:tok0 + 512].rearrange("k p t -> p k t")), writes=[B_ysT], dma=1)
            P.op("sp", lambda e, tok0=tok0: e.dma_start(out=yaT[:], in_=Yattn[:, :, tok0:tok0 + 512].rearrange("k p t -> p k t")), writes=[B_yaT], dma=1)
            def load_half(src_v, col0, k0, nk):
                wt_, Bwt_ = W16.next()
                P.op("sp", lambda e, wt_=wt_: e.dma_start(out=wt_[:, 0:nk, :], in_=src_v[:, k0:k0 + nk, col0:col0 + 512]), writes=[Bwt_], dma=1)
                return wt_, Bwt_

            def mm16(pa, Bp, halves_, f, rhs_tile, B_rhs):
                for k in range(KC):
                    wt_, Bwt_ = halves_[k // 8]
                    P.op("pe", lambda e, k=k, wt_=wt_: e.matmul(out=pa[:, 0:512], lhsT=wt_[:, k % 8, f * 128:(f + 1) * 128], rhs=rhs_tile[:, k, :],
                                                            start=(k == 0), stop=(k == KC - 1)), reads=[Bwt_, B_rhs], writes=[Bp])

            def mmk(pa, Bp, wt_, Bwt_, nk, f, rhs_tile, B_rhs):
                for k in range(nk):
                    P.op("pe", lambda e, k=k: e.matmul(out=pa[:, 0:512], lhsT=wt_[:, k, f * 128:(f + 1) * 128], rhs=rhs_tile[:, k, :],
                                                       start=(k == 0), stop=(k == nk - 1)), reads=[Bwt_, B_rhs], writes=[Bp])

            for cb in range(4):
                gsh = [load_half(w_in_v4, 3584 + cb * 512, hk * 8, 8) for hk in range(2)]
                wsb, Bwsb = load_half(wbs_v, cb * 512, 0, 4)
                for f in range(4):
                    pgs, Bpgs = PS.next(); mm16(pgs, Bpgs, gsh, f, hT4, B_hT4)
                    pbs, Bpbs = PS.next(); mmk(pbs, Bpbs, wsb, Bwsb, 4, f, ysT, B_ysT)
                    s1, Bs1 = SG.next()
                    P.op("act", lambda e, s1=s1, pgs=pgs: e.activation(out=s1[:], in_=pgs[:, :], func=AF.Sigmoid), reads=[Bpgs], writes=[Bs1])
                    P.op("dve", lambda e, s1=s1, pbs=pbs, f=f: e.tensor_tensor(out=S1t[:, f, :], in0=s1[:], in1=pbs[:, :], op=ALU.mult), reads=[Bs1, Bpbs], writes=[B_S1[f]])
                gah = [load_half(w_in_v4, 5632 + cb * 512, hk * 8, 8) for hk in range(2)]
                wab, Bwab = load_half(wba_v, cb * 512, 0, 8)
                for f in range(4):
                    c = cb * 4 + f
                    pga, Bpga = PS.next(); mm16(pga, Bpga, gah, f, hT4, B_hT4)
                    pba, Bpba = PS.next(); mmk(pba, Bpba, wab, Bwab, 8, f, yaT, B_yaT)
                    s2, Bs2 = SG.next()
                    P.op("act", lambda e, s2=s2, pga=pga: e.activation(out=s2[:], in_=pga[:, :], func=AF.Sigmoid), reads=[Bpga], writes=[Bs2])
                    P.op("dve", lambda e, s2=s2, pba=pba: e.tensor_tensor(out=s2[:], in0=s2[:], in1=pba[:, :], op=ALU.mult), reads=[Bs2, Bpba], writes=[Bs2])
                    P.op("pool", lambda e, s2=s2, c=c, f=f: e.tensor_tensor(out=mT[:, c, :], in0=S1t[:, f, :], in1=s2[:], op=ALU.add),
                         reads=[B_S1[f], Bs2], writes=[B_mT])
            for cb in range(4):
                woh = [load_half(wo_v, cb * 512, hk * 8, 8) for hk in range(2)]
                for f in range(4):
                    c = cb * 4 + f
                    pm, Bpm = PS.next(); mm16(pm, Bpm, woh, f, mT, B_mT)
                    P.op("dve", lambda e, pm=pm, c=c: e.scalar_tensor_tensor(out=hT4[:, c, :], in0=hT4[:, c, :].bitcast(F32), scalar=ALPHA,
                                                                       in1=pm[:, :], op0=ALU.mult, op1=ALU.add),
                         reads=[Bpm, B_hT4], writes=[B_hT4])
            for t in range(4):
                xa, Bx = XT4.next()
                for b4 in range(4):
                    pa, Bp = PS.next()
                    for jj in range(4):
                        k = b4 * 4 + jj
                        P.op("pe", lambda e, pa=pa, jj=jj, k=k, t=t: e.transpose(out=pa[:, jj * 128:(jj + 1) * 128],
                                                                                 in_=hT4[:, k, t * 128:(t + 1) * 128].bitcast(F32), identity=ident[:]),
                             reads=[B_hT4, B_ident], writes=[Bp])
                    P.op("act", lambda e, pa=pa, xa=xa, b4=b4: e.activation(out=xa[:, b4 * 512:(b4 + 1) * 512], in_=pa[:, :], func=AF.Copy), reads=[Bp], writes=[Bx])
                (sa, ma, ra), Bs = ST4.next()
                for qq in range(4):
                    P.op("dve", lambda e, qq=qq, sa=sa, xa=xa: e.bn_stats(out=sa[:, qq, :], in_=xa[:, qq * 512:(qq + 1) * 512]), reads=[Bx], writes=[Bs])
                P.op("dve", lambda e, sa=sa, ma=ma: e.bn_aggr(out=ma[:], in_=sa[:].rearrange("p a b -> p (a b)")), reads=[Bs], writes=[Bs])
                P.op("act", lambda e, ra=ra, ma=ma: e.activation(out=ra[:], in_=ma[:, 1:2], func=AF.Sqrt, bias=epst[:], scale=1.0), reads=[Bs, B_eps], writes=[Bs])
                P.op("dve", lambda e, ra=ra: e.reciprocal(out=ra[:], in_=ra[:]), reads=[Bs], writes=[Bs])
                P.op("dve", lambda e, xa=xa, ma=ma, ra=ra: e.tensor_scalar(out=xa[:], in0=xa[:], scalar1=ma[:, 0:1], scalar2=ra[:], op0=ALU.subtract, op1=ALU.mult),
                     reads=[Bx, Bs], writes=[Bx])
                P.op("pool", lambda e, xa=xa, tok0=tok0, t=t: e.dma_start(out=H1N[tok0 + t * 128:tok0 + (t + 1) * 128, :], in_=xa[:]), reads=[Bx], dma=1)
                for b4 in range(4):
                    pa, Bp = PS.next()
                    for jj in range(4):
                        k = b4 * 4 + jj
                        P.op("pe", lambda e, pa=pa, jj=jj, k=k, xa=xa: e.transpose(out=pa[:, jj * 128:(jj + 1) * 128], in_=xa[:, k * 128:(k + 1) * 128], identity=ident[:]),
                             reads=[Bx, B_ident], writes=[Bp])
                    for jj in range(4):
                        k = b4 * 4 + jj
                        P.op("act", lambda e, pa=pa, jj=jj, k=k, t=t: e.activation(out=mT[:, k, t * 128:(t + 1) * 128], in_=pa[:, jj * 128:(jj + 1) * 128],
                                                                              func=AF.Identity, scale=g1T[:, k:k + 1], bias=b1T[:, k:k + 1]),
                             reads=[Bp, B_g1, B_b1], writes=[B_mT])
            P.op("pool", lambda e, tok0=tok0: e.dma_start(out=H1T[:, :, tok0:tok0 + 512].rearrange("k p t -> p k t"), in_=mT[:]), reads=[B_mT], dma=1)
        end_phase()


    CAP = 512
    RTOT = 32 * CAP
    BIG = float(RTOT + 64)
    YEXP = dscr("YEXP", [RTOT, D])
    LTOK = dscr("LTOK", [RTOT, 1], I32)
    wr_d = din("router_w", [D, 36], F32R)
    rb_d = din("router_b_b", [128, 36])
    wg_d = din("exp_w_gate", [32, D, 512], F32R)
    wu_d = din("exp_w_up", [32, D, 512], F32R)
    wd_d = din("exp_w_down", [32, 512, D], F32R)
    tri_d = din("tri_d", [128, 128])
    ecp1_d = din("ecp1_d", [128, 32])
    NT = NREAL // 128
    RK = sb("RK", [128, NT, 2], I32); WK = sb("WK", [128, NT, 2]); B_RK = P.buf("RK"); B_WK = P.buf("WK")
    ph5 = (dbg or {}).get("ph5", 1)
    ngrp5 = (dbg or {}).get("ngrp5", NREAL // 512)
    if ph5:
        begin_phase()
        psb = psum_banks(8)
        PS = Ring([(psb[i], P.buf(f"ps5_{i}")) for i in range(8)])
        wr = sb("p5_wr", [128, KC, 36], F32R); rbb = sb("p5_rb", [128, 36]); B_wr, B_rb = P.bufs(2, "p5r")
        P.op("sp", lambda e: e.dma_start(out=wr[:], in_=wr_d.rearrange("(k p) n -> p k n", p=128)), writes=[B_wr], dma=1)
        P.op("sp", lambda e: e.dma_start(out=rbb[:], in_=rb_d), writes=[B_rb], dma=1)
        trif = sb("p5_trif", [128, 128]); trib = sb("p5_trib", [128, 128], BF16); onesb = sb("p5_onesb", [128, 128], BF16)
        ecp1 = sb("p5_ecp1", [128, 32]); cnt = sb("p5_cnt", [128, 32]); zer = sb("p5_zer", [128, RTOT // 128], I32)
        B_tri, B_ones, B_ecp, B_cnt, B_zer, B_ltok = P.bufs(6, "p5c")
        P.op("sp", lambda e: e.dma_start(out=trif[:], in_=tri_d), writes=[B_tri], dma=1)
        P.op("sp", lambda e: e.dma_start(out=ecp1[:], in_=ecp1_d), writes=[B_ecp], dma=1)
        P.op("dve", lambda e: e.tensor_copy(out=trib[:], in_=trif[:]), reads=[B_tri], writes=[B_tri])
        P.op("dve", lambda e: e.memset(onesb[:], 1.0), writes=[B_ones])
        P.op("dve", lambda e: e.memset(cnt[:], 0.0), writes=[B_cnt])
        P.op("dve", lambda e: e.memset(zer[:], 0), writes=[B_zer])
        P.op("dve", lambda e: e.memset(RK[:], RTOT + 64), writes=[B_RK])
        P.op("dve", lambda e: e.memset(WK[:], 0.0), writes=[B_WK])
        P.op("sp", lambda e: e.dma_start(out=LTOK.rearrange("(p a) o -> p (a o)", p=128), in_=zer[:]), reads=[B_zer], writes=[B_ltok], dma=1)
        h1T = sb("p5_h1T", [128, KC, 512], F32R); B_h1T = P.buf("p5_h1T")
        W32 = sb("p5_W32", [128, 32]); B_W32 = P.buf("p5_W32")
        maskb = sb("p5_maskb", [128, 32], BF16); B_maskb = P.buf("p5_maskb")
        toks = [sb(f"p5_tok{i}", [128, 1], I32) for i in range(2)]
        TOK = Ring([(toks[i], P.buf(f"p5_tok{i}")) for i in range(2)])
        rt = sb("p5_rt", [128, 288]); B_rt = P.buf("p5_rt")
        L = rt[:, 0:36]; gmx = rt[:, 36:37]; gex = rt[:, 40:44]; gsum = rt[:, 44:45]; gmask = rt[:, 48:52]
        esel = rt[:, 56:64]; v8 = rt[:, 64:72]; sel = rt[:, 72:80]; ex8 = rt[:, 80:88]; den = rt[:, 88:89]
        tmp32 = rt[:, 96:128]; wsel = rt[:, 128:136]; slot = rt[:, 160:192]; key = rt[:, 192:224]; okm = rt[:, 224:256]
        kv8 = rt[:, 256:264]; zz = rt[:, 264:266]; rf = rt[:, 266:268]; jk32 = rt[:, 136:160]

        def rdv(fn, rd=(), wr_=()):
            P.op("dve", fn, reads=[B_rt] + list(rd), writes=[B_rt] + list(wr_))

        for g in range(ngrp5):
            tok0 = g * 512
            P.op("sp", lambda e, tok0=tok0: e.dma_start(out=h1T[:], in_=H1T[:, :, tok0:tok0 + 512].rearrange("k p t -> p k t")), writes=[B_h1T], dma=1)
            for t in range(4):
                ti = g * 4 + t
                pa, Bp = PS.next()
                for k in range(KC):
                    P.op("pe", lambda e, pa=pa, k=k, t=t: e.matmul(out=pa[:, 0:36], lhsT=h1T[:, k, t * 128:(t + 1) * 128], rhs=wr[:, k, :],
                                                             start=(k == 0), stop=(k == KC - 1)), reads=[B_h1T, B_wr], writes=[Bp])
                P.op("dve", lambda e, pa=pa: e.tensor_tensor(out=L, in0=pa[:, 0:36], in1=rbb[:], op=ALU.add), reads=[Bp, B_rb, B_rt], writes=[B_rt])
                rdv(lambda e: e.tensor_reduce(out=gmx, in_=L[:, 0:4], axis=AX.X, op=ALU.max))
                rdv(lambda e: e.tensor_scalar(out=gex, in0=L[:, 0:4], scalar1=gmx, scalar2=None, op0=ALU.subtract))
                P.op("act", lambda e: e.activation(out=gex, in_=gex, func=AF.Exp), reads=[B_rt], writes=[B_rt])
                rdv(lambda e: e.tensor_reduce(out=gsum, in_=gex, axis=AX.X, op=ALU.add))
                rdv(lambda e: e.reciprocal(out=gsum, in_=gsum))
                rdv(lambda e: e.tensor_scalar(out=gmask, in0=L[:, 0:4], scalar1=gmx, scalar2=None, op0=ALU.is_equal))
                rdv(lambda e: e.tensor_tensor(out=tmp32.rearrange("p (g e) -> p g e", g=4), in0=L[:, 4:36].rearrange("p (g e) -> p g e", g=4),
                                              in1=gmask.unsqueeze(2).to_broadcast([128, 4, 8]), op=ALU.mult))
                rdv(lambda e: e.tensor_reduce(out=esel, in_=tmp32.rearrange("p (g e) -> p e g", g=4), axis=AX.X, op=ALU.add))
                rdv(lambda e: e.max(out=v8, in_=esel))
                rdv(lambda e: e.tensor_scalar(out=sel, in0=esel, scalar1=v8[:, 1:2], scalar2=None, op0=ALU.is_ge))
                rdv(lambda e: e.tensor_scalar(out=ex8, in0=esel, scalar1=v8[:, 0:1], scalar2=None, op0=ALU.subtract))
                P.op("act", lambda e: e.activation(out=ex8, in_=ex8, func=AF.Exp), reads=[B_rt], writes=[B_rt])
                rdv(lambda e: e.tensor_tensor(out=ex8, in0=ex8, in1=sel, op=ALU.mult))
                rdv(lambda e: e.tensor_reduce(out=den, in_=ex8, axis=AX.X, op=ALU.add))
                rdv(lambda e: e.reciprocal(out=den, in_=den))
                rdv(lambda e: e.tensor_tensor(out=den, in0=den, in1=gsum, op=ALU.mult))
                rdv(lambda e: e.tensor_scalar(out=wsel, in0=ex8, scalar1=den, scalar2=None, op0=ALU.mult))
                P.op("dve", lambda e: e.tensor_tensor(out=W32[:, :].rearrange("p (g e) -> p g e", g=4),
                                                      in0=gmask.unsqueeze(2).to_broadcast([128, 4, 8]),
                                                      in1=wsel.unsqueeze(1).to_broadcast([128, 4, 8]), op=ALU.mult),
                     reads=[B_rt], writes=[B_W32])
                P.op("dve", lambda e: e.tensor_scalar(out=maskb[:], in0=W32[:], scalar1=0.0, scalar2=None, op0=ALU.is_gt), reads=[B_W32], writes=[B_maskb])
                pc, Bpc = PS.next()
                P.op("pe", lambda e, pc=pc: e.matmul(out=pc[:, 0:32], lhsT=trib[:], rhs=maskb[:], start=True, stop=True), reads=[B_tri, B_maskb], writes=[Bpc])
                ptot, Bpt = PS.next()
                P.op("pe", lambda e, ptot=ptot: e.matmul(out=ptot[:, 0:32], lhsT=onesb[:], rhs=maskb[:], start=True, stop=True), reads=[B_ones, B_maskb], writes=[Bpt])
                rdv(lambda e, pc=pc: e.scalar_tensor_tensor(out=slot, in0=pc[:, 0:32], scalar=-1.0, in1=cnt[:], op0=ALU.add, op1=ALU.add), rd=[Bpc, B_cnt])
                P.op("dve", lambda e, ptot=ptot: e.tensor_tensor(out=cnt[:], in0=cnt[:], in1=ptot[:, 0:32], op=ALU.add), reads=[Bpt, B_cnt, B_rt], writes=[B_cnt])
                rdv(lambda e: e.tensor_scalar(out=okm, in0=slot, scalar1=float(CAP), scalar2=None, op0=ALU.is_lt))
                rdv(lambda e: e.tensor_tensor(out=okm, in0=okm, in1=maskb[:], op=ALU.mult), rd=[B_maskb])
                rdv(lambda e: e.tensor_tensor(out=key, in0=slot, in1=ecp1[:], op=ALU.add), rd=[B_ecp])
                rdv(lambda e: e.tensor_tensor(out=key, in0=key, in1=okm, op=ALU.mult))
                rdv(lambda e: e.tensor_tensor(out=tmp32, in0=W32[:], in1=okm, op=ALU.mult), rd=[B_W32])
                rdv(lambda e: e.max(out=kv8, in_=key))
                rdv(lambda e: e.tensor_scalar(out=zz, in0=kv8[:, 0:2], scalar1=0.0, scalar2=BIG, op0=ALU.is_equal, op1=ALU.mult))
                rdv(lambda e: e.scalar_tensor_tensor(out=rf, in0=kv8[:, 0:2], scalar=-1.0, in1=zz, op0=ALU.add, op1=ALU.add))
                P.op("dve", lambda e, ti=ti: e.tensor_copy(out=RK[:, ti, :], in_=rf), reads=[B_rt], writes=[B_RK])
                for kk in range(2):
                    P.op("dve", lambda e, ti=ti, kk=kk: e.scalar_tensor_tensor(out=slot, in0=key, scalar=kv8[:, kk:kk + 1], in1=tmp32,
                                                                           op0=ALU.is_equal, op1=ALU.mult, accum_out=WK[:, ti, kk:kk + 1]),
                         reads=[B_rt], writes=[B_rt, B_WK])
                tk, Btk = TOK.next()
                P.op("pool", lambda e, tk=tk, ti=ti: e.iota(tk[:], pattern=[[0, 1]], base=ti * 128, channel_multiplier=1), writes=[Btk])
                for kk in range(2):
                    P.op("pool", lambda e, tk=tk, ti=ti, kk=kk: e.indirect_dma_start(
                        out=LTOK[:, :], out_offset=bass.IndirectOffsetOnAxis(ap=RK[:, ti, kk:kk + 1], axis=0), in_=tk[:, :], in_offset=None,
                        bounds_check=P.reg(e, RTOT - 1), oob_is_err=False), reads=[B_RK, Btk], writes=[B_ltok], dma=1)
        end_phase()

        begin_phase()
        psb = psum_banks(8)
        PS = Ring([(psb[i], P.buf(f"ps5e_{i}")) for i in range(8)])
        g1T = sb("p5_g1", [128, KC]); b1T = sb("p5_b1", [128, KC]); B_g1, B_b1 = P.bufs(2, "p5gb")
        P.op("sp", lambda e: e.dma_start(out=g1T[:], in_=ln1g_d), writes=[B_g1], dma=1)
        P.op("sp", lambda e: e.dma_start(out=b1T[:], in_=ln1b_d), writes=[B_b1], dma=1)
        XTl = [sb(f"p5_XT{i}", [128, KC, CAP], F32R) for i in range(2)]
        B_XTl = [P.buf(f"p5_XT{i}") for i in range(2)]
        NB = CAP // 128
        xgs = [sb(f"p5_xg{i}", [128, D]) for i in range(3)]
        XG = Ring([(xgs[i], P.buf(f"p5_xg{i}")) for i in range(3)])
        idxs = [sb(f"p5_idx{i}", [128, 4], I32) for i in range(2)]
        IDX = Ring([(idxs[i], P.buf(f"p5_idx{i}")) for i in range(2)])
        w16 = [sb(f"p5_w16_{i}", [128, 8, 512], F32R) for i in range(4)]
        W16 = Ring([(w16[i], P.buf(f"p5_w16_{i}")) for i in range(4)])
        wdt = [sb(f"p5_wd{i}", [128, 4, 512], F32R) for i in range(2)]
        WD = Ring([(wdt[i], P.buf(f"p5_wd{i}")) for i in range(2)])
        hb = sb("p5_hb", [128, 4, CAP], F32R); Bhb = P.buf("p5_hb")
        sg4 = sb("p5_sg4", [128, 4, CAP]); Bsg4 = [P.buf(f"p5_sg4_{f}") for f in range(4)]
        yrs = [sb(f"p5_yr{i}", [128, NB, 512]) for i in range(2)]
        YR = Ring([(yrs[i], P.buf(f"p5_yr{i}")) for i in range(2)])
        nexp5 = (dbg or {}).get("nexp5", 32)

        def prep_pieces(ex, slot):
            XT_, BXT_ = XTl[slot], B_XTl[slot]
            state = {}

            def p_fetch():
                idx, Bidx = IDX.next()
                for b in range(NB):
                    P.op("sp", lambda e, idx=idx, b=b: e.dma_start(out=idx[:, b:b + 1], in_=LTOK[ex * CAP + b * 128:ex * CAP + (b + 1) * 128, :]), writes=[Bidx], dma=1)
                state["idx"] = (idx, Bidx)

            def p_gather(b):
                idx, Bidx = state["idx"]
                xg, Bxg = XG.next()
                P.op("pool", lambda e, xg=xg, idx=idx, b=b: e.indirect_dma_start(
                    out=xg[:, :], out_offset=None, in_=H1N[:, :], in_offset=bass.IndirectOffsetOnAxis(ap=idx[:, b:b + 1], axis=0),
                    bounds_check=P.reg(e, NREAL - 1), oob_is_err=False), reads=[Bidx], writes=[Bxg], dma=1)
                state[("xg", b)] = (xg, Bxg)

            def p_tr(b, half):
                if half == 0:
                    p_gather(b)
                xg, Bxg = state[("xg", b)]
                for b4 in range(half * 2, half * 2 + 2):
                    pa, Bp = PS.next()
                    for jj in range(4):
                        k = b4 * 4 + jj
                        P.op("pe", lambda e, pa=pa, jj=jj, k=k, xg=xg: e.transpose(out=pa[:, jj * 128:(jj + 1) * 128], in_=xg[:, k * 128:(k + 1) * 128], identity=ident[:]),
                             reads=[Bxg, B_ident], writes=[Bp])
                    for jj in range(4):
                        k = b4 * 4 + jj
                        P.op("act", lambda e, pa=pa, jj=jj, k=k, b=b, XT_=XT_: e.activation(out=XT_[:, k, b * 128:(b + 1) * 128], in_=pa[:, jj * 128:(jj + 1) * 128],
                                                                                  func=AF.Identity, scale=g1T[:, k:k + 1], bias=b1T[:, k:k + 1]),
                             reads=[Bp, B_g1, B_b1], writes=[BXT_])

            return [p_fetch] + [(lambda b=b, half=half: p_tr(b, half)) for b in range(NB) for half in range(2)]

        pieces = prep_pieces(0, 0)
        for pc_ in pieces:
            pc_()
        for ex in range(nexp5):
            slot = ex % 2
            XT_, BXT_ = XTl[slot], B_XTl[slot]
            nxt = prep_pieces(ex + 1, 1 - slot) if ex + 1 < nexp5 else []
            if nxt:
                nxt.pop(0)()
            halves = {}

            def load_halves(nm, wsrc, ex=ex, halves=halves):
                for hk in range(2):
                    wt_, Bwt_ = W16.next()
                    P.op("sp", lambda e, wt_=wt_, hk=hk, wsrc=wsrc: e.dma_start(
                        out=wt_[:], in_=wsrc[ex].rearrange("(k p) n -> p k n", p=128)[:, hk * 8:(hk + 1) * 8, :]), writes=[Bwt_], dma=1)
                    halves[(nm, hk)] = (wt_, Bwt_)

            load_halves("g", wg_d)
            load_halves("u", wu_d)
            for nm in ("g", "u"):
                for f in range(4):
                    pp, Bpp = PS.next()
                    for k in range(KC):
                        wt_, Bwt_ = halves[(nm, k // 8)]
                        P.op("pe", lambda e, pp=pp, k=k, f=f, wt_=wt_, XT_=XT_: e.matmul(out=pp[:, 0:CAP], lhsT=wt_[:, k % 8, f * 128:(f + 1) * 128], rhs=XT_[:, k, :],
                                                                                  start=(k == 0), stop=(k == KC - 1)), reads=[Bwt_, BXT_], writes=[Bpp])
                    if nm == "g":
                        P.op("act", lambda e, pp=pp, f=f: e.activation(out=sg4[:, f, :], in_=pp[:, 0:CAP], func=AF.Silu), reads=[Bpp], writes=[Bsg4[f]])
                    else:
                        P.op("dve", lambda e, pp=pp, f=f: e.tensor_tensor(out=hb[:, f, :], in0=sg4[:, f, :], in1=pp[:, 0:CAP], op=ALU.mult),
                             reads=[Bsg4[f], Bpp], writes=[Bhb])
                    if nxt:
                        nxt.pop(0)()
            for cg in range(4):
                wd_, Bwd = WD.next()
                P.op("sp", lambda e, wd_=wd_, ex=ex, cg=cg: e.dma_start(out=wd_[:], in_=wd_d[ex].rearrange("(k p) n -> p k n", p=128)[:, :, cg * 512:(cg + 1) * 512]),
                     writes=[Bwd], dma=1)
                yr, Byr = YR.next()
                for b in range(NB):
                    pa, Bp = PS.next()
                    for k in range(4):
                        P.op("pe", lambda e, pa=pa, k=k, b=b, wd_=wd_: e.matmul(out=pa[:, :], lhsT=hb[:, k, b * 128:(b + 1) * 128], rhs=wd_[:, k, :],
                                                                        start=(k == 0), stop=(k == 3)), reads=[Bhb, Bwd], writes=[Bp])
                    P.op("dve", lambda e, pa=pa, b=b, yr=yr: e.tensor_copy(out=yr[:, b, :], in_=pa[:, :]), reads=[Bp], writes=[Byr])
                P.op("pool", lambda e, ex=ex, cg=cg, yr=yr: e.dma_start(
                    out=YEXP[ex * CAP:(ex + 1) * CAP, cg * 512:(cg + 1) * 512].rearrange("(b p) n -> p b n", p=128), in_=yr[:]), reads=[Byr], dma=1)
            while nxt:
                nxt.pop(0)()
        end_phase()

    gb_d = [din(n, [128, D]) for n in ("ln1_g_b", "ln1_b_b", "ln2_g_b", "ln2_b_b")]
    ph6 = (dbg or {}).get("ph6", 1)
    if ph6:
        begin_phase()
        gbt = [sb(f"p6_gb{i}", [128, D]) for i in range(4)]
        B_gb = P.bufs(4, "p6gb")
        for i in range(4):
            P.op("sp", lambda e, i=i: e.dma_start(out=gbt[i][:], in_=gb_d[i]), writes=[B_gb[i]], dma=1)
        for i in range(2):
            P.op("pool", lambda e, i=i: e.tensor_scalar(out=gbt[i][:], in0=gbt[i][:], scalar1=ALPHA, scalar2=None, op0=ALU.mult), reads=[B_gb[i]], writes=[B_gb[i]])
        xs6 = [sb(f"p6_x{i}", [128, D]) for i in range(4)]
        X6 = Ring([(xs6[i], P.buf(f"p6_x{i}")) for i in range(4)])
        ys6 = [sb(f"p6_y{i}", [128, D]) for i in range(8)]
        Y6 = Ring([(ys6[i], P.buf(f"p6_y{i}")) for i in range(8)])
        for i in range(8):
            P.op("dve", lambda e, i=i: e.memset(ys6[i][:], 0.0), writes=[Y6.items[i][1]])
        st6 = [(sb(f"p6_stat{i}", [128, 4, 6]), sb(f"p6_mv{i}", [128, 2]), sb(f"p6_rstd{i}", [128, 1])) for i in range(2)]
        ST6 = Ring([(st6[i], P.buf(f"p6_st{i}")) for i in range(2)])
        ntile6 = (dbg or {}).get("ntile6", NREAL // 128)
        def p6_prep(tt6):
            xa, Bx = X6.next()
            r0 = tt6 * 128
            P.op("sp", lambda e, xa=xa, r0=r0: e.dma_start(out=xa[:], in_=H1N[r0:r0 + 128, :]), writes=[Bx], dma=1)
            ya = []
            for kk in range(2):
                yt_, Byt = Y6.next()
                P.op("pool", lambda e, yt_=yt_, tt6=tt6, kk=kk: e.indirect_dma_start(
                    out=yt_[:, :], out_offset=None, in_=YEXP[:, :], in_offset=bass.IndirectOffsetOnAxis(ap=RK[:, tt6, kk:kk + 1], axis=0),
                    bounds_check=P.reg(e, RTOT - 1), oob_is_err=False), reads=[B_RK], writes=[Byt], dma=1)
                ya.append((yt_, Byt))
            P.op("pool", lambda e, xa=xa: e.tensor_tensor(out=xa[:], in0=xa[:], in1=gbt[0][:], op=ALU.mult), reads=[Bx, B_gb[0]], writes=[Bx])
            P.op("pool", lambda e, xa=xa: e.tensor_tensor(out=xa[:], in0=xa[:], in1=gbt[1][:], op=ALU.add), reads=[Bx, B_gb[1]], writes=[Bx])
            return xa, Bx, ya

        def p6_finish(tt6, xa, Bx, ya):
            r0 = tt6 * 128
            for kk in range(2):
                yt_, Byt = ya[kk]
                P.op("dve", lambda e, xa=xa, yt_=yt_, tt6=tt6, kk=kk: e.scalar_tensor_tensor(out=xa[:], in0=yt_[:], scalar=WK[:, tt6, kk:kk + 1], in1=xa[:],
                                                                                   op0=ALU.mult, op1=ALU.add), reads=[Bx, Byt, B_WK], writes=[Bx])
            (sa, ma, ra), Bs = ST6.next()
            for qq in range(4):
                P.op("dve", lambda e, qq=qq, sa=sa, xa=xa: e.bn_stats(out=sa[:, qq, :], in_=xa[:, qq * 512:(qq + 1) * 512]), reads=[Bx], writes=[Bs])
            P.op("dve", lambda e, sa=sa, ma=ma: e.bn_aggr(out=ma[:], in_=sa[:].rearrange("p a b -> p (a b)")), reads=[Bs], writes=[Bs])
            P.op("act", lambda e, ra=ra, ma=ma: e.activation(out=ra[:], in_=ma[:, 1:2], func=AF.Sqrt, bias=epst[:], scale=1.0), reads=[Bs, B_eps], writes=[Bs])
            P.op("dve", lambda e, ra=ra: e.reciprocal(out=ra[:], in_=ra[:]), reads=[Bs], writes=[Bs])
            P.op("dve", lambda e, xa=xa, ma=ma, ra=ra: e.tensor_scalar(out=xa[:], in0=xa[:], scalar1=ma[:, 0:1], scalar2=ra[:], op0=ALU.subtract, op1=ALU.mult),
                 reads=[Bx, Bs], writes=[Bx])
            P.op("dve", lambda e, xa=xa: e.tensor_tensor(out=xa[:], in0=xa[:], in1=gbt[2][:], op=ALU.mult), reads=[Bx, B_gb[2]], writes=[Bx])
            P.op("pool", lambda e, xa=xa: e.tensor_tensor(out=xa[:], in0=xa[:], in1=gbt[3][:], op=ALU.add), reads=[Bx, B_gb[3]], writes=[Bx])
            P.op("act", lambda e, xa=xa, r0=r0: e.dma_start(out=out[r0:r0 + 128, :], in_=xa[:]), reads=[Bx], dma=1)

        preps = {}
        DEPTH6 = 2
        for tt6 in range(min(DEPTH6, ntile6)):
            preps[tt6] = p6_prep(tt6)
        for tt6 in range(ntile6):
            if tt6 + DEPTH6 < ntile6:
                preps[tt6 + DEPTH6] = p6_prep(tt6 + DEPTH6)
            p6_finish(tt6, *preps.pop(tt6))
        end_phase()

    P.flush()
    es.close()
    return nc


def _prep_shared(inputs):
    sh = {}
    sh["meta"] = np.ascontiguousarray(inputs["meta_tokens"], dtype=np.float32)
    sh["ident_d"] = np.eye(128, dtype=np.float32)
    sh["ln_in_gT"] = np.ascontiguousarray(np.asarray(inputs["ln_in_g"], np.float32).reshape(KC, 128).T)
    sh["ln_in_bT"] = np.ascontiguousarray(np.asarray(inputs["ln_in_b"], np.float32).reshape(KC, 128).T)
    sh["w_in"] = np.ascontiguousarray(inputs["w_in"][0], dtype=np.float32)
    f = lambda k: np.asarray(inputs[k], np.float32).reshape(-1)
    sh["tri_d"] = np.triu(np.ones((128, 128), np.float32))
    sh["ecp1_d"] = np.ascontiguousarray(np.broadcast_to((np.arange(32, dtype=np.float32) * 512 + 1)[None, :], (128, 32)))
    rep = lambda a, n=128: np.ascontiguousarray(np.broadcast_to(np.asarray(a, np.float32).reshape(1, -1), (n, np.asarray(a).size)))
    sh["router_w"] = np.ascontiguousarray(np.concatenate([inputs["router_g_w"][0], inputs["router_e_w"][0]], axis=1), dtype=np.float32)
    sh["router_b_b"] = rep(np.concatenate([np.asarray(inputs["router_g_b"][0]).reshape(-1), np.asarray(inputs["router_e_b"][0]).reshape(-1)]))
    sh["exp_w_gate"] = np.ascontiguousarray(inputs["exp_w_gate"][0], dtype=np.float32)
    sh["exp_w_up"] = np.ascontiguousarray(inputs["exp_w_up"][0], dtype=np.float32)
    sh["exp_w_down"] = np.ascontiguousarray(inputs["exp_w_down"][0], dtype=np.float32)
    sh["ln1_g_b"] = rep(inputs["ln1_g"][0]); sh["ln1_b_b"] = rep(inputs["ln1_b"][0])
    sh["ln2_g_b"] = rep(inputs["ln2_g"][0]); sh["ln2_b_b"] = rep(inputs["ln2_b"][0])
    colT = lambda a: np.ascontiguousarray(np.asarray(a, np.float32).reshape(KC, 128).T)
    sh["ln1_gT"] = colT(inputs["ln1_g"][0]); sh["ln1_bT"] = colT(inputs["ln1_b"][0])
    sh["w_br_ssm"] = np.ascontiguousarray(inputs["w_br_ssm"][0], dtype=np.float32)
    sh["w_br_attn"] = np.ascontiguousarray(inputs["w_br_attn"][0], dtype=np.float32)
    sh["w_o"] = np.ascontiguousarray(inputs["w_o"][0], dtype=np.float32)
    sl = lambda a: np.ascontiguousarray(np.asarray(a, np.float32).reshape(16, 128).T)
    sh["ssm_are"] = sl(inputs["ssm_a_re"][0])
    sh["ssm_aim"] = sl(inputs["ssm_a_im"][0])
    sh["ssm_ldt"] = sl(np.repeat(np.asarray(inputs["ssm_log_dt"][0], np.float32).reshape(32, 1), 64, axis=1))
    sl3 = lambda a: np.ascontiguousarray(np.asarray(a, np.float32).reshape(16, 128, 16).transpose(1, 0, 2))
    sh["ssm_bre"] = sl3(inputs["ssm_b_re"][0])
    sh["ssm_bim"] = sl3(inputs["ssm_b_im"][0])
    sh["ssm_cre"] = sl3(np.asarray(inputs["ssm_c_re"][0]).transpose(0, 2, 1))
    sh["ssm_cim"] = sl3(np.asarray(inputs["ssm_c_im"][0]).transpose(0, 2, 1))
    sh["ssm_dT"] = np.ascontiguousarray(np.asarray(inputs["ssm_d"][0], np.float32).reshape(4, 128).T)
    sh["ssm_wglu"] = np.ascontiguousarray(inputs["ssm_w_glu"][0], dtype=np.float32)
    sh["tt_d"] = np.ascontiguousarray(np.broadcast_to(np.arange(1032, dtype=np.float32)[None, :], (128, 1032)))
    sh["lamv"] = np.concatenate([f("attn_lambda_q1"), f("attn_lambda_k1"), f("attn_lambda_q2"), f("attn_lambda_k2")]).reshape(1, 256)
    sh["gsub_b"] = np.ascontiguousarray(np.broadcast_to(f("attn_subln_g")[None, :], (128, 128)))
    return sh


def kernel(**inputs):
    x = np.asarray(inputs["x"], np.float32)
    sh = _prep_shared(inputs)
    nc = build_program()
    in_maps = []
    for c in range(NCORES):
        m = dict(sh)
        m["x"] = np.ascontiguousarray(x[c * NSEQ:(c + 1) * NSEQ].reshape(NREAL, D))
        in_maps.append(m)
    res = run_bass_kernel_spmd(nc, in_maps, core_ids=list(range(NCORES)))
    outs = [np.asarray(r["out"], np.float32).reshape(NSEQ, SEQ, D) for r in res.results]
    return np.concatenate(outs, axis=0)
```

```python
import math
from contextlib import ExitStack

import numpy as np
import concourse.bass as bass
import concourse.mybir as mybir
from concourse.bass_utils import run_bass_kernel_spmd

F32 = mybir.dt.float32
F32R = mybir.dt.float32r
BF16 = mybir.dt.bfloat16
U32 = mybir.dt.uint32
I32 = mybir.dt.int32
AF = mybir.ActivationFunctionType
ALU = mybir.AluOpType
AX = mybir.AxisListType

D = 2048
KC = 16
SEQ = 2048
NSEQ = 2
NREAL = NSEQ * SEQ
NTOK = NREAL + 128
META0 = NREAL
IN_COLS = 7680
LN_EPS = 1e-5
ALPHA = 2.0 ** 0.25
LAMBDA_INIT = 0.2
NCORES = 8


class Buf:
    __slots__ = ("name", "w", "r")

    def __init__(self, name):
        self.name = name
        self.w = None
        self.r = {}


class Prog:
    ENG = ("pe", "dve", "act", "pool", "sp")

    def __init__(self, nc, es):
        self.nc = nc
        self.es = es
        self.ops = {e: [] for e in self.ENG}
        self.sems = {}
        self.cnt = {}
        self.seen = {e: {} for e in self.ENG}
        self.pending = {e: {} for e in self.ENG}
        self.nbuf = 0

    def reg(self, eng, val):
        if not hasattr(self, "_regs"):
            self._regs = {}
        if val not in self._regs:
            self._regs[val] = eng.to_reg(val)
        return self._regs[val]

    def buf(self, name=None):
        self.nbuf += 1
        return Buf(name or f"b{self.nbuf}")

    def bufs(self, n, name="b"):
        return [self.buf(f"{name}{i}") for i in range(n)]

    def _mksem(self, key):
        self.sems[key] = self.es.enter_context(self.nc.semaphore("s_" + key))
        self.cnt[key] = 0

    def op(self, eng, emit, reads=(), writes=(), dma=None):
        if dma:
            dma = "d_" + (writes[0].name if writes else reads[0].name)
        key = dma if dma else eng
        if key not in self.sems:
            self._mksem(key)
        waits = dict(self.pending[eng])
        self.pending[eng] = {}

        def need(tok, kind):
            k, v = tok
            if dma is None and k == eng:
                if eng == "pe" or kind != "raw":
                    return
            if v > waits.get(k, 0):
                waits[k] = v

        for b in reads:
            if b.w:
                need(b.w, "raw")
        for b in writes:
            if b.w:
                need(b.w, "waw")
            for k, v in b.r.items():
                need((k, v), "war")
        wl = []
        for k, v in waits.items():
            if self.seen[eng].get(k, 0) >= v:
                continue
            self.seen[eng][k] = v
            wl.append((k, v))
        inc = 16 if dma else 1
        self.cnt[key] += inc
        tok = (key, self.cnt[key])
        self.ops[eng].append((wl, emit, key, inc))
        for b in writes:
            b.w = tok
            b.r = {}
        for b in reads:
            if b not in writes:
                if b.r.get(key, 0) < tok[1]:
                    b.r[key] = tok[1]
        return tok

    def barrier(self):
        for e in self.ENG:
            for k, v in self.cnt.items():
                if v > self.pending[e].get(k, 0):
                    self.pending[e][k] = v

    def flush(self):
        nc = self.nc
        self.barrier()
        for e in self.ENG:
            wl = []
            for k, v in self.pending[e].items():
                if self.seen[e].get(k, 0) < v:
                    self.seen[e][k] = v
                    wl.append((k, v))
            self.pending[e] = {}
            self.ops[e].append((wl, None, None, 0))
        sems = self.sems

        def mk(lst):
            def body(eng):
                for wl, emit, key, inc in lst:
                    for k, v in wl:
                        eng.wait_ge(sems[k], v)
                    if emit is not None:
                        ins = emit(eng)
                        ins.then_inc(sems[key], inc)
            return body

        with nc.Block() as block:
            block.tensor(mk(self.ops["pe"]))
            block.vector(mk(self.ops["dve"]))
            block.scalar(mk(self.ops["act"]))
            block.gpsimd(mk(self.ops["pool"]))
            block.sync(mk(self.ops["sp"]))
        self.ops = {e: [] for e in self.ENG}
        self._regs = {}

    emit_all = flush


class Ring:
    def __init__(self, items):
        self.items = items
        self.i = 0

    def next(self):
        it = self.items[self.i % len(self.items)]
        self.i += 1
        return it


def build_program(dbg=None):
    nc = bass.Bass("TRN2", target_bir_lowering=False)
    nc.dge_precook = False
    es = ExitStack()
    P = Prog(nc, es)

    def din(name, shape, dt=F32):
        return nc.dram_tensor(name, list(shape), dt, kind="ExternalInput").ap()

    def dscr(name, shape, dt=F32):
        kind = "ExternalOutput" if (dbg and name in dbg) else "Internal"
        return nc.dram_tensor(name, list(shape), dt, kind=kind).ap()

    cur = {"es": es, "n": 0}

    def sb(name, shape, dt=F32):
        return cur["es"].enter_context(nc.sbuf_tensor(name, list(shape), dt))

    def psum_banks(n=8, width=512):
        cur["n"] += 1
        return [cur["es"].enter_context(nc.psum_tensor(f"ps{cur['n']}_{i}", [128, width], F32)) for i in range(n)]

    def begin_phase():
        cur["es"] = ExitStack()

    def end_phase():
        P.flush()
        cur["es"].close()
        cur["es"] = es

    x = din("x", [NREAL, D])
    meta = din("meta", [16, D])
    ident_d = din("ident_d", [128, 128])
    lng_in = din("ln_in_gT", [128, KC])
    lnb_in = din("ln_in_bT", [128, KC])
    w_in = din("w_in", [D, IN_COLS], F32R)
    out = nc.dram_tensor("out", [NREAL, D], F32, kind="ExternalOutput").ap()

    HT = dscr("HT", [KC, 128, NTOK], F32R)
    Usc = dscr("Usc", [4, 128, NTOK], BF16)
    Qsc = dscr("Qsc", [8, 128, NTOK], BF16)
    Ksc = dscr("Ksc", [8, 128, NTOK], BF16)
    Vsc = dscr("Vsc", [8, NTOK, 128], BF16)

    ident = sb("ident", [128, 128])
    identb = sb("identb", [128, 128], BF16)
    g_in = sb("g_in", [128, KC])
    b_in = sb("b_in", [128, KC])
    epst = sb("epst", [128, 1])
    B_ident, B_identb, B_gin, B_bin, B_eps = P.bufs(5, "c")
    P.op("sp", lambda e: e.dma_start(out=ident[:], in_=ident_d), writes=[B_ident], dma="ldc")
    P.op("sp", lambda e: e.dma_start(out=g_in[:], in_=lng_in), writes=[B_gin], dma="ldc")
    P.op("sp", lambda e: e.dma_start(out=b_in[:], in_=lnb_in), writes=[B_bin], dma="ldc")
    P.op("dve", lambda e: e.tensor_copy(out=identb[:], in_=ident[:]), reads=[B_ident], writes=[B_identb])
    P.op("dve", lambda e: e.memset(epst[:], LN_EPS), writes=[B_eps])

    P.flush()

    begin_phase()
    psb = psum_banks(8)
    PS = Ring([(psb[i], P.buf(f"ps1_{i}")) for i in range(8)])
    xt = [sb(f"xt{i}", [128, D]) for i in range(2)]
    XT = Ring([(xt[i], P.buf(f"xt{i}")) for i in range(2)])
    hTs = [(sb(f"hT{i}", [128, KC, 512]), P.buf(f"hT{i}")) for i in range(2)]
    HC = {"t": hTs[0][0], "B": hTs[0][1]}
    wts = [sb(f"wt{i}", [128, 8, 512], F32R) for i in range(4)]
    WT = Ring([(wts[i], P.buf(f"wt{i}")) for i in range(4)])
    stat = [sb(f"stat{i}", [128, 4, 6]) for i in range(2)]
    mv = [sb(f"mv{i}", [128, 2]) for i in range(2)]
    rstd = [sb(f"rstd{i}", [128, 1]) for i in range(2)]
    ST = Ring([((stat[i], mv[i], rstd[i]), P.buf(f"st{i}")) for i in range(2)])
    ob = [sb(f"ob{i}", [128, 512], BF16) for i in range(3)]
    OB = Ring([(ob[i], P.buf(f"ob{i}")) for i in range(3)])
    vtm = [sb(f"vtm{i}", [128, 4, 128], BF16) for i in range(2)]
    VTM = Ring([(vtm[i], P.buf(f"vtm{i}")) for i in range(2)])

    w_in_v = w_in.rearrange("(k p) n -> p k n", p=128)

    def ln_tile_to_hT(src_ap, nrows, tcol, gT, bT, B_g, B_b):
        xa, Bx = XT.next()
        hT, B_hT = HC["wt"], HC["wB"]
        (sa, ma, ra), Bs = ST.next()
        if nrows < 128:
            P.op("dve", lambda e: e.memset(xa[:], 0.0), writes=[Bx])
        P.op("sp", lambda e: e.dma_start(out=xa[0:nrows, :], in_=src_ap), writes=[Bx], dma="ldx")
        sub = (dbg or {}).get('sub', 99)
        if sub < 2:
            return
        for q in range(4):
            P.op("dve", lambda e, q=q: e.bn_stats(out=sa[:, q, :], in_=xa[:, q * 512:(q + 1) * 512]),
                 reads=[Bx], writes=[Bs])
        P.op("dve", lambda e: e.bn_aggr(out=ma[:], in_=sa[:].rearrange("p a b -> p (a b)")), reads=[Bs], writes=[Bs])
        if sub < 3:
            return
        P.op("act", lambda e: e.activation(out=ra[:], in_=ma[:, 1:2], func=AF.Sqrt, bias=epst[:], scale=1.0),
             reads=[Bs, B_eps], writes=[Bs])
        P.op("dve", lambda e: e.reciprocal(out=ra[:], in_=ra[:]), reads=[Bs], writes=[Bs])
        if sub < 4:
            return
        P.op("dve", lambda e: e.tensor_scalar(out=xa[:], in0=xa[:], scalar1=ma[:, 0:1], scalar2=ra[:],
                                              op0=ALU.subtract, op1=ALU.mult), reads=[Bx, Bs], writes=[Bx])
        if sub < 5:
            return
        for b4 in range(4):
            pa, Bp = PS.next()
            for j in range(4):
                k = b4 * 4 + j
                P.op("pe", lambda e, k=k, j=j, pa=pa: e.transpose(out=pa[:, j * 128:(j + 1) * 128],
                                                           in_=xa[:, k * 128:(k + 1) * 128], identity=ident[:]),
                     reads=[Bx, B_ident], writes=[Bp])
            if sub < 6:
                continue
            if (dbg or {}).get('bar', 0):
                P.barrier()
            for j in range(4):
                k = b4 * 4 + j
                var = (dbg or {}).get('var', 0)
                if var == 0:
                    P.op("act", lambda e, k=k, j=j, pa=pa, hT=hT: e.activation(out=hT[:, k, tcol:tcol + 128].bitcast(F32R),
                                                                 in_=pa[:, j * 128:(j + 1) * 128], func=AF.Identity,
                                                                 scale=gT[:, k:k + 1], bias=bT[:, k:k + 1]),
                         reads=[Bp, B_g, B_b], writes=[B_hT])
                elif var == 3:
                    P.op("act", lambda e, k=k, j=j, pa=pa: e.activation(out=ident[:, :],
                                                                 in_=pa[:, j * 128:(j + 1) * 128], func=AF.Copy),
                         reads=[Bp, B_g, B_b], writes=[B_hT])
                elif var == 4:
                    P.op("act", lambda e, k=k, j=j, pa=pa: e.activation(out=hT[:, k, tcol:tcol + 128],
                                                                 in_=ident[:, :], func=AF.Copy),
                         reads=[Bp, B_g, B_b], writes=[B_hT])
                elif var == 1:
                    P.op("act", lambda e, k=k, j=j, pa=pa: e.activation(out=hT[:, k, tcol:tcol + 128],
                                                                 in_=pa[:, j * 128:(j + 1) * 128], func=AF.Copy),
                         reads=[Bp, B_g, B_b], writes=[B_hT])
                elif var == 2:
                    P.op("dve", lambda e, k=k, j=j, pa=pa: e.tensor_scalar(out=hT[:, k, tcol:tcol + 128],
                                                                 in0=pa[:, j * 128:(j + 1) * 128], scalar1=gT[:, k:k + 1],
                                                                 scalar2=bT[:, k:k + 1], op0=ALU.mult, op1=ALU.add),
                         reads=[Bp, B_g, B_b], writes=[B_hT])

    wcache = {}

    def proj_chunk(col0, ntok):
        hT, B_hT = HC["t"], HC["B"]
        base = (col0 // 512) * 512
        if wcache.get("base") != base:
            hv = []
            for hk in range(2):
                wa, Bw = WT.next()
                P.op("sp", lambda e, wa=wa, hk=hk: e.dma_start(out=wa[:], in_=w_in_v[:, hk * 8:(hk + 1) * 8, base:base + 512]), writes=[Bw], dma=1)
                hv.append((wa, Bw))
            wcache.update(base=base, hv=hv)
        hv = wcache["hv"]
        off = col0 - base
        pa, Bp = PS.next()
        for k in range(KC):
            wa, Bw = hv[k // 8]
            P.op("pe", lambda e, k=k, wa=wa, hT=hT: e.matmul(out=pa[:, 0:ntok], lhsT=wa[:, k % 8, off:off + 128],
                                                             rhs=hT[:, k, 0:ntok].bitcast(F32R), start=(k == 0), stop=(k == KC - 1)),
                 reads=[Bw, B_hT], writes=[Bp])
        return pa, Bp

    groups = [(g * 512, 512) for g in range(NREAL // 512)] + [(META0, 128)]
    if dbg and "ngroups" in dbg:
        groups = groups[:dbg["ngroups"]] + [groups[-1]]
    stage = (dbg or {}).get('stage', 99)
    def ln_group_closures(gi):
        tok0_, ntok_ = groups[gi]
        slot = gi % 2

        def mk(t):
            def run():
                HC["wt"], HC["wB"] = hTs[slot]
                if tok0_ == META0:
                    ln_tile_to_hT(meta, 16, 0, g_in, b_in, B_gin, B_bin)
                else:
                    ln_tile_to_hT(x[tok0_ + t * 128: tok0_ + (t + 1) * 128, :], 128, t * 128, g_in, b_in, B_gin, B_bin)
            return run
        return [mk(t) for t in range(ntok_ // 128)]

    if stage >= 1 and groups:
        for fn in ln_group_closures(0):
            fn()
    for gi, (tok0, ntok) in enumerate(groups):
        if stage < 1:
            break
        ntile = ntok // 128
        wcache.clear()
        HC["t"], HC["B"] = hTs[gi % 2]
        hT, B_hT = HC["t"], HC["B"]
        nxt = ln_group_closures(gi + 1) if gi + 1 < len(groups) else []
        if stage >= 2:
          P.op("pool", lambda e, tok0=tok0, ntok=ntok, hT=hT: e.dma_start(
            out=HT[:, :, tok0:tok0 + ntok].rearrange("k p t -> p k t"), in_=hT[:, :, 0:ntok].bitcast(F32R)),
            reads=[B_hT], dma="st1")
        if stage < 3:
            for fn in nxt:
                fn()
            continue
        plan = [("u", c, c * 128) for c in range(4)]
        if tok0 != META0:
            plan += [("q", c, 512 + c * 128) for c in range(8)]
        plan += [("k", c, 1536 + c * 128) for c in range(8)]
        plan += [("v", c, 2560 + c * 128) for c in range(8)]
        for ci, (kind, c, col0) in enumerate(plan):
            if nxt and ci % 6 == 5:
                nxt.pop(0)()
            pa, Bp = proj_chunk(col0, ntok)
            oa, Bo = OB.next()
            if kind == "q":
                P.op("act", lambda e, pa=pa, oa=oa, ntok=ntok: e.activation(
                    out=oa[:, 0:ntok], in_=pa[:, 0:ntok], func=AF.Copy, scale=0.125), reads=[Bp], writes=[Bo])
            else:
                P.op("dve", lambda e, pa=pa, oa=oa, ntok=ntok: e.tensor_copy(out=oa[:, 0:ntok], in_=pa[:, 0:ntok]),
                     reads=[Bp], writes=[Bo])
            if kind != "v":
                dst = {"u": Usc, "q": Qsc, "k": Ksc}[kind]
                P.op("pool", lambda e, dst=dst, c=c, oa=oa, tok0=tok0, ntok=ntok: e.dma_start(
                    out=dst[c, :, tok0:tok0 + ntok], in_=oa[:, 0:ntok]), reads=[Bo], dma="st1")
            else:
                pt, Bpt = PS.next()
                ptb = pt[:].bitcast(BF16)
                va, Bv = VTM.next()
                for t in range(ntile):
                    P.op("pe", lambda e, t=t, oa=oa, ptb=ptb: e.transpose(
                        out=ptb[:, t * 128:(t + 1) * 128], in_=oa[:, t * 128:(t + 1) * 128], identity=identb[:]),
                        reads=[Bo, B_identb], writes=[Bpt])
                P.op("dve", lambda e, va=va, ptb=ptb, ntile=ntile: e.tensor_copy(
                    out=va[:, 0:ntile, :], in_=ptb[:, 0:ntile * 128].rearrange("p (t e) -> p t e", e=128)),
                    reads=[Bpt], writes=[Bv])
                P.op("pool", lambda e, c=c, va=va, tok0=tok0, ntile=ntile, ntok=ntok: e.dma_start(
                    out=Vsc[c, tok0:tok0 + ntok, :].rearrange("(t p) e -> p t e", p=128), in_=va[:, 0:ntile, :]),
                    reads=[Bv], dma="st1")
        for fn in nxt:
            fn()
        nxt = []
    end_phase()

    Yattn = dscr("Yattn", [8, 128, NTOK], F32R)
    lamv_d = din("lamv", [1, 256])
    gsub_d = din("gsub_b", [128, 128])


    YG = dscr("YG", [4, 128, NTOK], F32R)
    Yssm = dscr("Yssm", [4, 128, NTOK], F32R)
    s_are_d = din("ssm_are", [128, 16]); s_aim_d = din("ssm_aim", [128, 16]); s_ldt_d = din("ssm_ldt", [128, 16])
    s_bre_d = din("ssm_bre", [128, 16, 16]); s_bim_d = din("ssm_bim", [128, 16, 16])
    s_cre_d = din("ssm_cre", [128, 16, 16]); s_cim_d = din("ssm_cim", [128, 16, 16])
    s_d_d = din("ssm_dT", [128, 4]); wglu_d = din("ssm_wglu", [512, 512], F32R)
    TS = 1032
    tt_d = din("tt_d", [128, TS])
    PI = math.pi
    ph3 = (dbg or {}).get("ph3", 1)
    ph2 = (dbg or {}).get("ph2", 1)
    nseq2 = (dbg or {}).get("nseq2", NSEQ)
    mid = ExitStack()

    def sbm(name, shape, dt=F32):
        return mid.enter_context(nc.sbuf_tensor(name, list(shape), dt))

    if ph2:
        hpi = sbm("s_hpi", [128, 1]); B_hpi = P.buf("s_hpi")
        prm = sbm("s_prm", [128, 24, 16])
        dT = sbm("s_dT", [128, 4])
        tt = sbm("s_tt", [128, TS])
        wbt = sbm("s_wb", [128, 16, 2, 128], BF16)
        cwt = sbm("s_cw", [128, 16, 2, 128], BF16)
        begin_phase()
        yb = psum_banks(2, 512)
        YPS = Ring([(yb[i], P.buf(f"p2y{i}")) for i in range(2)])
        P.op("dve", lambda e: e.memset(hpi[:], PI / 2), writes=[B_hpi])
        NPRM = 24
        B_prm = P.buf("s_prm")
        names = ["are", "aim", "ldt", "lre", "dt", "mag", "ang", "sn", "cs", "t1", "t2", "ar", "ai", "nr", "den", "zr", "zi", "phs"]
        V = {n: prm[:, i, :] for i, n in enumerate(names)}
        bc = sb("s_bc", [128, 4, 16, 16]); B_bc = P.buf("s_bc")
        bb = sb("s_bb", [128, 4, 16, 16]); B_bb = P.buf("s_bb")
        B_dT = P.buf("s_dT")
        B_tt = P.buf("s_tt")
        P.op("sp", lambda e: e.dma_start(out=prm[:, 0, :], in_=s_are_d), writes=[B_prm], dma=1)
        P.op("sp", lambda e: e.dma_start(out=prm[:, 1, :], in_=s_aim_d), writes=[B_prm], dma=1)
        P.op("sp", lambda e: e.dma_start(out=prm[:, 2, :], in_=s_ldt_d), writes=[B_prm], dma=1)
        for i, dd in enumerate((s_bre_d, s_bim_d, s_cre_d, s_cim_d)):
            P.op("sp", lambda e, i=i, dd=dd: e.dma_start(out=bc[:, i, :, :], in_=dd), writes=[B_bc], dma=1)
        P.op("sp", lambda e: e.dma_start(out=dT[:], in_=s_d_d), writes=[B_dT], dma=1)
        P.op("sp", lambda e: e.dma_start(out=tt[:], in_=tt_d), writes=[B_tt], dma=1)

        def dv(fn, rd=(), wr=None):
            P.op("dve", fn, reads=[B_prm] + list(rd), writes=[wr or B_prm])

        def tt_(o, a, b, op):
            dv(lambda e: e.tensor_tensor(out=V[o], in0=V[a], in1=V[b], op=op))

        def ts_(o, a, s1, op0, s2=None, op1=None):
            if op1 is None:
                dv(lambda e: e.tensor_scalar(out=V[o], in0=V[a], scalar1=s1, scalar2=None, op0=op0))
            else:
                dv(lambda e: e.tensor_scalar(out=V[o], in0=V[a], scalar1=s1, scalar2=s2, op0=op0, op1=op1))

        def act_(o, a, func, scale=1.0):
            P.op("act", lambda e: e.activation(out=V[o], in_=V[a], func=func, scale=scale), reads=[B_prm], writes=[B_prm])

        ts_("lre", "are", -1e-4, ALU.min)
        act_("dt", "ldt", AF.Exp)
        tt_("t1", "lre", "dt", ALU.mult)
        act_("mag", "t1", AF.Exp)
        tt_("ang", "aim", "dt", ALU.mult)
        dv(lambda e: e.tensor_scalar(out=V["t1"].bitcast(I32), in0=V["ang"], scalar1=1.0 / (2 * PI), scalar2=None, op0=ALU.mult))
        dv(lambda e: e.tensor_copy(out=V["t2"], in_=V["t1"].bitcast(I32)))
        dv(lambda e: e.scalar_tensor_tensor(out=V["ang"], in0=V["t2"], scalar=-2 * PI, in1=V["ang"], op0=ALU.mult, op1=ALU.add))
        dv(lambda e: e.tensor_scalar(out=V["t1"], in0=V["ang"], scalar1=0.0, scalar2=2 * PI, op0=ALU.is_lt, op1=ALU.mult))
        tt_("ang", "ang", "t1", ALU.add)
        dv(lambda e: e.tensor_scalar(out=V["t1"], in0=V["ang"], scalar1=PI, scalar2=-2 * PI, op0=ALU.is_gt, op1=ALU.mult))
        tt_("t1", "t1", "ang", ALU.add)
        act_("sn", "t1", AF.Sin)
        dv(lambda e: e.tensor_scalar(out=V["t1"], in0=V["ang"], scalar1=PI / 2, scalar2=-2 * PI, op0=ALU.is_gt, op1=ALU.mult))
        dv(lambda e: e.scalar_tensor_tensor(out=V["t1"], in0=V["ang"], scalar=PI / 2, in1=V["t1"], op0=ALU.add, op1=ALU.add))
        act_("cs", "t1", AF.Sin)
        tt_("ar", "mag", "cs", ALU.mult)
        tt_("ai", "mag", "sn", ALU.mult)
        ts_("nr", "ar", -1.0, ALU.add)
        tt_("t1", "lre", "lre", ALU.mult)
        tt_("t2", "aim", "aim", ALU.mult)
        tt_("den", "t1", "t2", ALU.add)
        dv(lambda e: e.reciprocal(out=V["den"], in_=V["den"]))
        tt_("t1", "nr", "lre", ALU.mult)
        tt_("t2", "ai", "aim", ALU.mult)
        tt_("zr", "t1", "t2", ALU.add)
        tt_("zr", "zr", "den", ALU.mult)
        tt_("t1", "ai", "lre", ALU.mult)
        tt_("t2", "nr", "aim", ALU.mult)
        tt_("zi", "t1", "t2", ALU.subtract)
        tt_("zi", "zi", "den", ALU.mult)
        ts_("phs", "ang", float(TS), ALU.mult)
        zrb = V["zr"].unsqueeze(2).to_broadcast([128, 16, 16])
        zib = V["zi"].unsqueeze(2).to_broadcast([128, 16, 16])
        P.op("dve", lambda e: e.tensor_tensor(out=bb[:, 2], in0=bc[:, 0], in1=zrb, op=ALU.mult), reads=[B_prm, B_bc], writes=[B_bb])
        P.op("dve", lambda e: e.tensor_tensor(out=bb[:, 3], in0=bc[:, 1], in1=zib, op=ALU.mult), reads=[B_prm, B_bc], writes=[B_bb])
        P.op("dve", lambda e: e.tensor_tensor(out=bb[:, 0], in0=bb[:, 2], in1=bb[:, 3], op=ALU.subtract), reads=[B_bb], writes=[B_bb])
        P.op("dve", lambda e: e.tensor_tensor(out=bb[:, 2], in0=bc[:, 1], in1=zrb, op=ALU.mult), reads=[B_prm, B_bc, B_bb], writes=[B_bb])
        P.op("dve", lambda e: e.tensor_tensor(out=bb[:, 3], in0=bc[:, 0], in1=zib, op=ALU.mult), reads=[B_prm, B_bc, B_bb], writes=[B_bb])
        P.op("dve", lambda e: e.tensor_tensor(out=bb[:, 1], in0=bb[:, 2], in1=bb[:, 3], op=ALU.add), reads=[B_bb], writes=[B_bb])
        bmw = sb("s_bmw", [128, 16, 2, 128], BF16); B_bmw = P.buf("s_bmw")
        B_wb = P.buf("s_wb")
        B_cw = P.buf("s_cw")
        P.op("pool", lambda e: e.memset(bmw[:], 0.0), writes=[B_bmw])
        P.op("pool", lambda e: e.memset(cwt[:], 0.0), writes=[B_cw])
        for jm in range(4):
            for gl in range(2):
                c0 = 32 * jm + 16 * gl
                for ri in range(2):
                    P.op("dve", lambda e, jm=jm, gl=gl, ri=ri, c0=c0: e.tensor_copy(
                        out=bmw[64 * gl:64 * gl + 64, jm::4, ri, c0:c0 + 16], in_=bb[64 * gl:64 * gl + 64, ri, jm::4, :]),
                        reads=[B_bb], writes=[B_bmw])
                P.op("dve", lambda e, jm=jm, gl=gl, c0=c0: e.tensor_copy(
                    out=cwt[64 * gl:64 * gl + 64, jm::4, 0, c0:c0 + 16], in_=bc[64 * gl:64 * gl + 64, 2, jm::4, :]),
                    reads=[B_bc], writes=[B_cw])
                P.op("dve", lambda e, jm=jm, gl=gl, c0=c0: e.tensor_scalar(
                    out=cwt[64 * gl:64 * gl + 64, jm::4, 1, c0:c0 + 16], in0=bc[64 * gl:64 * gl + 64, 3, jm::4, :],
                    scalar1=-1.0, scalar2=None, op0=ALU.mult), reads=[B_bc], writes=[B_cw])
        for g4 in range(8):
            pa, Bp = YPS.next()
            pab = pa[:].bitcast(BF16)
            for i4 in range(4):
                idx = g4 * 4 + i4
                j, ri = idx // 2, idx % 2
                P.op("pe", lambda e, pab=pab, i4=i4, j=j, ri=ri: e.transpose(out=pab[:, i4 * 128:(i4 + 1) * 128], in_=bmw[:, j, ri, :],
                                                                             identity=identb[:]), reads=[B_bmw, B_identb], writes=[Bp])
            j0 = (g4 * 4) // 2
            P.op("dve", lambda e, pab=pab, j0=j0: e.tensor_copy(out=wbt[:, j0:j0 + 2, :, :].rearrange("p a b c -> p (a b c)"),
                                                                 in_=pab[:, 0:512]), reads=[Bp], writes=[B_wb])

        end_phase()

    begin_phase()
    psb = psum_banks(8)
    bankA = psb[6]; B_bankA = P.buf("p23_bankA")
    bankB = psb[7]; B_bankBt = P.buf("p23_bankB"); B_bankBy = B_bankBt
    g3 = None
    g2 = None
    if ph3:
        PS = Ring([(psb[7], B_bankBt)])
        lamv = sb("lamv_s", [1, 256]); lamt = sb("lamt", [1, 8]); ones1 = sb("ones1", [1, 128])
        neglam = sb("neglam", [128, 1]); gsub = sb("gsub", [128, 128])
        B_lam, B_neglam, B_gsub, B_ones1 = P.bufs(4, "a3c")
        P.op("sp", lambda e: e.dma_start(out=lamv[:], in_=lamv_d), writes=[B_lam], dma=1)
        P.op("sp", lambda e: e.dma_start(out=gsub[:], in_=gsub_d), writes=[B_gsub], dma=1)
        P.op("dve", lambda e: e.memset(ones1[:], 1.0), writes=[B_ones1])
        P.op("dve", lambda e: e.tensor_scalar(out=gsub[:], in0=gsub[:], scalar1=1.0 - LAMBDA_INIT, scalar2=None, op0=ALU.mult),
             reads=[B_gsub], writes=[B_gsub])
        P.op("dve", lambda e: e.tensor_tensor(out=lamv[:, 0:64], in0=lamv[:, 0:64], in1=lamv[:, 64:128], op=ALU.mult), reads=[B_lam], writes=[B_lam])
        P.op("dve", lambda e: e.tensor_tensor(out=lamv[:, 128:192], in0=lamv[:, 128:192], in1=lamv[:, 192:256], op=ALU.mult), reads=[B_lam], writes=[B_lam])
        P.op("dve", lambda e: e.tensor_reduce(out=lamt[:, 0:1], in_=lamv[:, 0:64], axis=AX.X, op=ALU.add), reads=[B_lam], writes=[B_lam])
        P.op("dve", lambda e: e.tensor_reduce(out=lamt[:, 1:2], in_=lamv[:, 128:192], axis=AX.X, op=ALU.add), reads=[B_lam], writes=[B_lam])
        P.op("act", lambda e: e.activation(out=lamt[:, 2:4], in_=lamt[:, 0:2], func=AF.Exp), reads=[B_lam], writes=[B_lam])
        P.op("dve", lambda e: e.tensor_tensor(out=lamt[:, 4:5], in0=lamt[:, 3:4], in1=lamt[:, 2:3], op=ALU.subtract), reads=[B_lam], writes=[B_lam])
        P.op("dve", lambda e: e.tensor_scalar(out=lamt[:, 5:6], in0=lamt[:, 4:5], scalar1=-LAMBDA_INIT, scalar2=None, op0=ALU.add), reads=[B_lam], writes=[B_lam])
        pa, Bp = PS.next()
        P.op("pe", lambda e, pa=pa: e.matmul(out=pa[:, 0:1], lhsT=ones1[:, :], rhs=lamt[:, 5:6], start=True, stop=True),
             reads=[B_lam, B_ones1], writes=[Bp])
        P.op("dve", lambda e, pa=pa: e.tensor_copy(out=neglam[:], in_=pa[:, 0:1]), reads=[Bp], writes=[B_neglam])

        ORING = Ring([(psb[i], P.buf(f"pso{i}")) for i in range(0, 4)])
        SRING = Ring([(psb[i], P.buf(f"pss{i}")) for i in range(4, 6)])
        TRING = Ring([(psb[7], B_bankBt)])
        kts = [sb(f"a_kt{i}", [128, 2, 128 + SEQ], BF16) for i in range(2)]
        qts = [sb(f"a_qt{i}", [128, SEQ], BF16) for i in range(2)]
        vts = [sb(f"a_vt{i}", [128, 17, 129], BF16) for i in range(2)]
        HRING = Ring([((kts[i], qts[i], vts[i]), (P.buf(f"a_kt{i}"), P.buf(f"a_qt{i}"), P.buf(f"a_vt{i}"), P.buf(f"a_vm{i}"))) for i in range(2)])
        for i in range(2):
            P.op("pool", lambda e, i=i: e.memset(vts[i][:], 1.0), writes=[HRING.items[i][1][2], HRING.items[i][1][3]])
            P.op("pool", lambda e, i=i: e.memset(vts[i][:, 16, :], 0.0), writes=[HRING.items[i][1][3]])
            P.op("pool", lambda e, i=i: e.memset(vts[i][0:16, 16, 128:129], 1.0), writes=[HRING.items[i][1][3]])
            P.op("pool", lambda e, i=i: e.memset(kts[i][:], 0.0), writes=[HRING.items[i][1][0]])
        pts = [sb(f"a_pt{i}", [128, 512], BF16) for i in range(5)]
        PTR = Ring([(pts[i], P.buf(f"a_pt{i}")) for i in range(5)])
        yTs = [sb(f"a_yT{i}", [128, SEQ], F32R) for i in range(2)]
        YTR = Ring([(yTs[i], P.buf(f"a_yT{i}")) for i in range(2)])
        eps3 = [(sb(f"a_rc{i}", [128, 4]), sb(f"a_t1{i}", [128, 128]), sb(f"a_od{i}", [128, 128]), sb(f"a_yq{i}", [128, 128]),
                 sb(f"a_jk{i}", [128, 128])) for i in range(4)]
        EPR = Ring([(eps3[i], P.bufs(5, f"a_ep{i}_")) for i in range(4)])

        def gen3():
            nseq3 = (dbg or {}).get("nseq3", NSEQ)
            nhead3 = (dbg or {}).get("nhead3", 8)
            for s_ in range(nseq3):
                for h in range(nhead3):
                    (kt, qt_, vt), (Bk, Bq, Bv, Bvm) = HRING.next()
                    r0 = s_ * SEQ
                    for m in range(2):
                        P.op("sp", lambda e, kt=kt, h=h, m=m: e.dma_start(out=kt[m * 64:(m + 1) * 64, m, 0:16], in_=Ksc[h, m * 64:(m + 1) * 64, META0:META0 + 16]), writes=[Bk], dma=1)
                        P.op("sp", lambda e, kt=kt, h=h, r0=r0, m=m: e.dma_start(out=kt[m * 64:(m + 1) * 64, m, 128:128 + SEQ], in_=Ksc[h, m * 64:(m + 1) * 64, r0:r0 + SEQ]), writes=[Bk], dma=1)
                    P.op("sp", lambda e, qt_=qt_, h=h, r0=r0: e.dma_start(out=qt_[:, :], in_=Qsc[h, :, r0:r0 + SEQ]), writes=[Bq], dma=1)
                    P.op("sp", lambda e, vt=vt, h=h, r0=r0: e.dma_start(out=vt[:, 0:16, 0:128], in_=Vsc[h, r0:r0 + SEQ, :].rearrange("(t p) e -> p t e", p=128)), writes=[Bv], dma=1)
                    P.op("sp", lambda e, vt=vt, h=h: e.dma_start(out=vt[0:16, 16, 0:128], in_=Vsc[h, META0:META0 + 16, :]), writes=[Bvm], dma=1)
                    yT, ByT = YTR.next()
                    steps = []
                    for qi in range(SEQ // 128):
                        blks = [("m", 0)] + [("r", kb) for kb in range(qi + 1)]
                        pairs = [blks[i:i + 2] for i in range(0, len(blks), 2)]
                        for pi_, pr in enumerate(pairs):
                            steps.append((qi, pi_, pr, len(pairs)))
                    LA = 2
                    pend = {}
                    oacc = {}
                    later = []

                    def emit_S(st):
                        qi, pi_, pr, npair = st
                        sp_, Bs_ = SRING.next()
                        for bj, (typ, kb) in enumerate(pr):
                            kc0 = 0 if typ == "m" else 128 + kb * 128
                            for m in range(2):
                                c0 = bj * 256 + m * 128
                                P.op("pe", lambda e, sp_=sp_, m=m, kc0=kc0, kt=kt, qt_=qt_, qi=qi, c0=c0: e.matmul(
                                    out=sp_[:, c0:c0 + 128], lhsT=kt[:, m, kc0:kc0 + 128],
                                    rhs=qt_[:, qi * 128:(qi + 1) * 128], start=True, stop=True),
                                    reads=[Bk, Bq], writes=[Bs_])
                        pt, Bpt = PTR.next()
                        w_ = 256 * len(pr)
                        P.op("act", lambda e, pt=pt, sp_=sp_, w_=w_: e.activation(out=pt[:, 0:w_], in_=sp_[:, 0:w_], func=AF.Exp),
                             reads=[Bs_], writes=[Bpt])
                        for bj, (typ, kb) in enumerate(pr):
                            if typ == "r" and kb == qi:
                                P.op("pool", lambda e, pt=pt, bj=bj: e.memset(pt[64:128, bj * 256:(bj + 1) * 256].rearrange("p (m q) -> p m q", m=2)[:, :, 0:64], 0.0),
                                     reads=[Bpt], writes=[Bpt])
                        pend[(qi, pi_)] = (pt, Bpt)

                    def emit_AV(st, now):
                        qi, pi_, pr, npair = st
                        pt, Bpt = pend.pop((qi, pi_))
                        if pi_ == 0:
                            oacc[qi] = (ORING.next(), ORING.next())
                        (o0, Bo0), (o1, Bo1) = oacc[qi]
                        for bj, (typ, kb) in enumerate(pr):
                            vidx = 16 if typ == "m" else kb
                            first = (pi_ == 0 and bj == 0)
                            last = (pi_ == npair - 1 and bj == len(pr) - 1)
                            for m, (oo, Boo) in enumerate(((o0, Bo0), (o1, Bo1))):
                                c0 = bj * 256 + m * 128
                                P.op("pe", lambda e, oo=oo, pt=pt, c0=c0, vt=vt, vidx=vidx, first=first, last=last: e.matmul(
                                    out=oo[:, 0:129], lhsT=pt[:, c0:c0 + 128], rhs=vt[:, vidx, :], start=first, stop=last),
                                    reads=[Bpt, Bv, Bvm], writes=[Boo])
                        if pi_ != npair - 1:
                            return
                        del oacc[qi]
                        (rc, t1, od, yq, jk), (Brc, Bt1, Bod, Byq, Bjk) = EPR.next()
                        P.op("dve", lambda e, rc=rc, o0=o0: e.reciprocal(out=rc[:, 0:1], in_=o0[:, 128:129]), reads=[Bo0], writes=[Brc])
                        P.op("dve", lambda e, rc=rc, o1=o1: e.reciprocal(out=rc[:, 1:2], in_=o1[:, 128:129]), reads=[Bo1], writes=[Brc])
                        P.op("dve", lambda e, rc=rc: e.tensor_scalar(out=rc[:, 2:3], in0=rc[:, 1:2], scalar1=neglam[:, 0:1], scalar2=None, op0=ALU.mult),
                             reads=[Brc, B_neglam], writes=[Brc])
                        P.op("dve", lambda e, rc=rc, t1=t1, o1=o1: e.tensor_scalar(out=t1[:], in0=o1[:, 0:128], scalar1=rc[:, 2:3], scalar2=None, op0=ALU.mult),
                             reads=[Brc, Bo1], writes=[Bt1])
                        P.op("dve", lambda e, rc=rc, t1=t1, o0=o0, od=od: e.scalar_tensor_tensor(out=od[:], in0=o0[:, 0:128], scalar=rc[:, 0:1], in1=t1[:],
                                                                                           op0=ALU.mult, op1=ALU.add),
                             reads=[Brc, Bo0, Bt1], writes=[Bod])
                        P.op("dve", lambda e, od=od, jk=jk, rc=rc: e.scalar_tensor_tensor(out=jk[:], in0=od[:], scalar=1.0, in1=od[:], op0=ALU.mult, op1=ALU.mult,
                                                                                    accum_out=rc[:, 3:4]),
                             reads=[Bod, Brc], writes=[Bjk, Brc])

                        def stage2(rc=rc, od=od, yq=yq, Brc=Brc, Bod=Bod, Byq=Byq):
                            P.op("act", lambda e: e.activation(out=rc[:, 3:4], in_=rc[:, 3:4], func=AF.Sqrt, bias=epst[:], scale=1.0 / 128.0),
                                 reads=[Brc, B_eps], writes=[Brc])
                            P.op("dve", lambda e: e.reciprocal(out=rc[:, 3:4], in_=rc[:, 3:4]), reads=[Brc], writes=[Brc])
                            P.op("dve", lambda e: e.scalar_tensor_tensor(out=yq[:], in0=od[:], scalar=rc[:, 3:4], in1=gsub[:],
                                                                         op0=ALU.mult, op1=ALU.mult),
                                 reads=[Brc, Bod, B_gsub], writes=[Byq])

                        def stage3(yq=yq, Byq=Byq, qi=qi, yT=yT, ByT=ByT):
                            tp, Btp = TRING.next()
                            P.op("pe", lambda e: e.transpose(out=tp[:, 0:128], in_=yq[:], identity=ident[:]), reads=[Byq, B_ident], writes=[Btp])
                            P.op("dve", lambda e: e.tensor_copy(out=yT[:, qi * 128:(qi + 1) * 128], in_=tp[:, 0:128]),
                                 reads=[Btp], writes=[ByT])

                        later.append((now + 3, stage2))
                        later.append((now + 6, stage3))

                    def run_later(now):
                        keep = []
                        for due, fn in later:
                            if due <= now:
                                fn()
                            else:
                                keep.append((due, fn))
                        later[:] = keep

                    nst = len(steps)
                    for i in range(nst + LA):
                        if i < nst:
                            emit_S(steps[i])
                        if i >= LA:
                            emit_AV(steps[i - LA], i)
                        run_later(i)
                        yield
                    run_later(10 ** 9)
                    P.op("pool", lambda e, yT=yT, h=h, r0=r0: e.dma_start(out=Yattn[h, :, r0:r0 + SEQ], in_=yT[:, :]), reads=[ByT], dma=1)

        g3 = gen3()
    if ph2:
        uTs = [[sb(f"s_uT{s_}_{i}", [128, TS], BF16) for i in range(2)] for s_ in range(NSEQ)]
        UTR = [Ring([(uTs[s_][i], P.buf(f"s_uT{s_}_{i}")) for i in range(2)]) for s_ in range(NSEQ)]
        wk = {n: sb("s_" + n, [128, TS]) for n in ("A1", "A2", "M1", "M2", "M3", "M4", "Z1", "Z2", "W1", "W2", "N1", "N2", "N3", "N4",
                                                   "ST", "CT", "Rt")}
        Bw = {n: P.buf("s_" + n) for n in wk}
        ygs = [sb(f"s_yg{i}", [128, TS], F32R) for i in range(1)]
        YGR = Ring([(ygs[i], P.buf(f"s_yg{i}")) for i in range(1)])
        Xs = [sb(f"s_X{s_}", [128, 4, 2, TS], BF16) for s_ in range(NSEQ)]
        B_X = [[[P.buf(f"s_X{s_}{a}{b}") for b in range(2)] for a in range(4)] for s_ in range(NSEQ)]
        carry = sb("s_carry", [128, NSEQ, 16, 2]); B_carry = P.buf("s_carry")

        def gen2():
            CT6 = lambda ap: ap.rearrange("p (a b) -> p a b", b=172)
            for seg in range(2):
                for q in range(4):
                    uT_s = []
                    for s_ in range(nseq2):
                        r0 = s_ * SEQ
                        uT, BuT = UTR[s_].next()
                        if seg == 0:
                            P.op("sp", lambda e, uT=uT, q=q: e.dma_start(out=uT[:, 0:16], in_=Usc[q, :, META0:META0 + 16]), writes=[BuT], dma=1)
                            P.op("sp", lambda e, uT=uT, q=q, r0=r0: e.dma_start(out=uT[:, 16:TS], in_=Usc[q, :, r0:r0 + TS - 16]), writes=[BuT], dma=1)
                        else:
                            P.op("sp", lambda e, uT=uT, q=q, r0=r0: e.dma_start(out=uT[:, :], in_=Usc[q, :, r0 + TS - 16:r0 + SEQ]), writes=[BuT], dma=1)
                        uT_s.append((uT, BuT))
                    for jm in range(4):
                        j = 4 * q + jm
                        phj = V["ang"][:, j:j + 1]
                        if seg == 0:
                            P.op("dve", lambda e, phj=phj: e.tensor_scalar(out=wk["A1"][:], in0=tt[:], scalar1=phj, scalar2=None, op0=ALU.mult),
                                 reads=[B_tt, B_prm], writes=[Bw["A1"]])
                        else:
                            P.op("dve", lambda e, phj=phj, j=j: e.tensor_scalar(out=wk["A1"][:], in0=tt[:], scalar1=phj, scalar2=V["phs"][:, j:j + 1],
                                                                            op0=ALU.mult, op1=ALU.add), reads=[B_tt, B_prm], writes=[Bw["A1"]])
                        P.op("dve", lambda e: e.tensor_scalar(out=wk["A2"][:].bitcast(I32), in0=wk["A1"][:], scalar1=1.0 / (2 * PI), scalar2=None, op0=ALU.mult),
                             reads=[Bw["A1"]], writes=[Bw["A2"]])
                        P.op("dve", lambda e: e.scalar_tensor_tensor(out=wk["A1"][:], in0=wk["A2"][:].bitcast(I32), scalar=-2 * PI, in1=wk["A1"][:], op0=ALU.mult, op1=ALU.add),
                             reads=[Bw["A2"], Bw["A1"]], writes=[Bw["A1"]])
                        P.op("dve", lambda e: e.tensor_scalar(out=wk["A1"][:], in0=wk["A1"][:], scalar1=-PI, scalar2=PI, op0=ALU.max, op1=ALU.min),
                             reads=[Bw["A1"]], writes=[Bw["A1"]])
                        yield
                        P.op("act", lambda e: e.activation(out=wk["ST"][:], in_=wk["A1"][:], func=AF.Sin), reads=[Bw["A1"]], writes=[Bw["ST"]])
                        P.op("act", lambda e: e.activation(out=wk["A2"][:], in_=wk["A1"][:], func=AF.Abs), reads=[Bw["A1"]], writes=[Bw["A2"]])
                        P.op("act", lambda e: e.activation(out=wk["CT"][:], in_=wk["A2"][:], func=AF.Sin, scale=-1.0, bias=hpi[:]), reads=[Bw["A2"], B_hpi], writes=[Bw["CT"]])
                        P.op("pool", lambda e, j=j: e.tensor_scalar(out=wk["Rt"][:], in0=tt[:], scalar1=0.0, scalar2=V["mag"][:, j:j + 1], op0=ALU.mult, op1=ALU.add),
                             reads=[B_tt, B_prm], writes=[Bw["Rt"]])
                        yield
                        for s_ in range(nseq2):
                            uT, BuT = uT_s[s_]
                            for p6 in range(6):
                                c0 = p6 * 172
                                for ri in range(2):
                                    P.op("pe", lambda e, ri=ri, c0=c0, j=j, uT=uT: e.matmul(
                                        out=bankA[:, ri * 172:(ri + 1) * 172], lhsT=wbt[:, j, ri, :], rhs=uT[:, c0:c0 + 172],
                                        start=True, stop=True), reads=[B_wb, BuT], writes=[B_bankA])
                                bur, bui = bankA[:, 0:172], bankA[:, 172:344]
                                for (mn, src, tab) in (("M1", bur, "CT"), ("M2", bui, "ST"), ("M3", bui, "CT"), ("M4", bur, "ST")):
                                    P.op("dve", lambda e, mn=mn, src=src, tab=tab, c0=c0: e.tensor_tensor(out=wk[mn][:, c0:c0 + 172], in0=src, in1=wk[tab][:, c0:c0 + 172], op=ALU.mult),
                                         reads=[B_bankA, Bw[tab]], writes=[Bw[mn]])
                                yield
                            P.op("pool", lambda e: e.tensor_tensor(out=wk["Z1"][:], in0=wk["M1"][:], in1=wk["M2"][:], op=ALU.add),
                                 reads=[Bw["M1"], Bw["M2"]], writes=[Bw["Z1"]])
                            P.op("pool", lambda e: e.tensor_tensor(out=wk["Z2"][:], in0=wk["M3"][:], in1=wk["M4"][:], op=ALU.subtract),
                                 reads=[Bw["M3"], Bw["M4"]], writes=[Bw["Z2"]])
                            yield
                            for ri, (zn, wn) in enumerate((("Z1", "W1"), ("Z2", "W2"))):
                                if seg == 0:
                                    P.op("dve", lambda e, zn=zn, wn=wn: e.tensor_tensor_scan(out=wk[wn][:], data0=wk["Rt"][:], data1=wk[zn][:], initial=0.0,
                                                                                          op0=ALU.mult, op1=ALU.add),
                                         reads=[Bw["Rt"], Bw[zn]], writes=[Bw[wn]])
                                    P.op("dve", lambda e, wn=wn, j=j, ri=ri, s_=s_: e.tensor_copy(out=carry[:, s_, j, ri:ri + 1], in_=wk[wn][:, TS - 1:TS]),
                                         reads=[Bw[wn]], writes=[B_carry])
                                else:
                                    P.op("dve", lambda e, zn=zn, wn=wn, j=j, ri=ri, s_=s_: e.tensor_tensor_scan(out=wk[wn][:], data0=wk["Rt"][:], data1=wk[zn][:],
                                                                                                        initial=carry[:, s_, j, ri:ri + 1], op0=ALU.mult, op1=ALU.add),
                                         reads=[Bw["Rt"], Bw[zn], B_carry], writes=[Bw[wn]])
                            yield
                            P.op("dve", lambda e: e.tensor_tensor(out=wk["N1"][:], in0=wk["W1"][:], in1=wk["CT"][:], op=ALU.mult),
                                 reads=[Bw["W1"], Bw["CT"]], writes=[Bw["N1"]])
                            P.op("dve", lambda e: e.tensor_tensor(out=wk["N2"][:], in0=wk["W2"][:], in1=wk["ST"][:], op=ALU.mult),
                                 reads=[Bw["W2"], Bw["ST"]], writes=[Bw["N2"]])
                            P.op("dve", lambda e, jm=jm, s_=s_: e.tensor_tensor(out=Xs[s_][:, jm, 0, :], in0=wk["N1"][:], in1=wk["N2"][:], op=ALU.subtract),
                                 reads=[Bw["N1"], Bw["N2"]], writes=[B_X[s_][jm][0]])
                            P.op("pool", lambda e: e.tensor_tensor(out=wk["N3"][:], in0=wk["W1"][:], in1=wk["ST"][:], op=ALU.mult),
                                 reads=[Bw["W1"], Bw["ST"]], writes=[Bw["N3"]])
                            P.op("pool", lambda e: e.tensor_tensor(out=wk["N4"][:], in0=wk["W2"][:], in1=wk["CT"][:], op=ALU.mult),
                                 reads=[Bw["W2"], Bw["CT"]], writes=[Bw["N4"]])
                            P.op("pool", lambda e, jm=jm, s_=s_: e.tensor_tensor(out=Xs[s_][:, jm, 1, :], in0=wk["N3"][:], in1=wk["N4"][:], op=ALU.add),
                                 reads=[Bw["N3"], Bw["N4"]], writes=[B_X[s_][jm][1]])
                            yield
                    for _ in range(3):
                        yield
                    for s_ in range(nseq2):
                        r0 = s_ * SEQ
                        uT, BuT = uT_s[s_]
                        for p3 in range(3):
                            n = 0
                            for jm in range(4):
                                for ri in range(2):
                                    P.op("pe", lambda e, jm=jm, ri=ri, q=q, p3=p3, n=n, s_=s_: e.matmul(
                                        out=bankB[:, 128:472], lhsT=cwt[:, 4 * q + jm, ri, :], rhs=Xs[s_][:, jm, ri, p3 * 344:(p3 + 1) * 344],
                                        start=(n == 0), stop=(n == 7)), reads=[B_cw, B_X[s_][jm][ri]], writes=[B_bankBy])
                                    n += 1
                            P.op("dve", lambda e, uT=uT, q=q, p3=p3: e.scalar_tensor_tensor(
                                out=wk["M1"][:, p3 * 344:(p3 + 1) * 344], in0=uT[:, p3 * 344:(p3 + 1) * 344], scalar=dT[:, q:q + 1], in1=bankB[:, 128:472],
                                op0=ALU.mult, op1=ALU.add), reads=[B_bankBy, BuT, B_dT], writes=[Bw["M1"]])
                            yield
                        yg, Byg = YGR.next()
                        P.op("pool", lambda e: e.tensor_tensor(out=wk["M2"][:], in0=wk["M1"][:], in1=wk["M1"][:], op=ALU.mult), reads=[Bw["M1"]], writes=[Bw["M2"]])
                        P.op("pool", lambda e: e.tensor_scalar(out=wk["M2"][:], in0=wk["M2"][:], scalar1=0.044715, scalar2=1.0, op0=ALU.mult, op1=ALU.add),
                             reads=[Bw["M2"]], writes=[Bw["M2"]])
                        P.op("pool", lambda e: e.tensor_tensor(out=wk["M2"][:], in0=wk["M2"][:], in1=wk["M1"][:], op=ALU.mult), reads=[Bw["M2"], Bw["M1"]], writes=[Bw["M2"]])
                        yield
                        P.op("act", lambda e: e.activation(out=wk["M2"][:], in_=wk["M2"][:], func=AF.Sigmoid, scale=1.5957691216057308),
                             reads=[Bw["M2"]], writes=[Bw["M2"]])
                        P.op("pool", lambda e, yg=yg: e.tensor_tensor(out=yg[:], in0=wk["M2"][:], in1=wk["M1"][:], op=ALU.mult),
                             reads=[Bw["M2"], Bw["M1"]], writes=[Byg])
                        if seg == 0:
                            P.op("pool", lambda e, yg=yg, q=q, r0=r0: e.dma_start(out=YG[q, :, r0:r0 + TS - 16], in_=yg[:, 16:TS]), reads=[Byg], dma=1)
                        else:
                            P.op("pool", lambda e, yg=yg, q=q, r0=r0: e.dma_start(out=YG[q, :, r0 + TS - 16:r0 + SEQ], in_=yg[:, :]), reads=[Byg], dma=1)
                        yield

        g2 = gen2()
    alive3, alive2 = g3 is not None, g2 is not None
    while alive3 or alive2:
        for _ in range(2):
            if alive3:
                try:
                    next(g3)
                except StopIteration:
                    alive3 = False
        if alive2:
            try:
                next(g2)
            except StopIteration:
                alive2 = False
    end_phase()
    if ph2:

        begin_phase()
        psb = psum_banks(8)
        PS = Ring([(psb[i], P.buf(f"ps2b_{i}")) for i in range(8)])
        wgl = sb("g_w", [128, 4, 512], F32R); B_wgl = P.buf("g_w")
        P.op("sp", lambda e: e.dma_start(out=wgl[:], in_=wglu_d.rearrange("(k p) n -> p k n", p=128)), writes=[B_wgl], dma=1)
        gys = [sb(f"g_y{i}", [128, 4, 512], F32R) for i in range(2)]
        GYR = Ring([(gys[i], P.buf(f"g_y{i}")) for i in range(2)])
        gsg = [sb(f"g_s{i}", [128, 512]) for i in range(2)]
        GSR = Ring([(gsg[i], P.buf(f"g_s{i}")) for i in range(2)])
        gos = [sb(f"g_o{i}", [128, 512], F32R) for i in range(2)]
        GOR = Ring([(gos[i], P.buf(f"g_o{i}")) for i in range(2)])
        for tp in range(nseq2 * SEQ // 512):
            gy, Bgy = GYR.next()
            P.op("sp", lambda e, gy=gy, tp=tp: e.dma_start(out=gy[:], in_=YG[:, :, tp * 512:(tp + 1) * 512].rearrange("k p t -> p k t")), writes=[Bgy], dma=1)
            for c in range(4):
                pa, Bp = PS.next()
                for k in range(4):
                    P.op("pe", lambda e, pa=pa, k=k, c=c, gy=gy: e.matmul(out=pa[:, :], lhsT=wgl[:, k, c * 128:(c + 1) * 128], rhs=gy[:, k, :],
                                                                       start=(k == 0), stop=(k == 3)), reads=[B_wgl, Bgy], writes=[Bp])
                sg, Bsg = GSR.next()
                go, Bgo = GOR.next()
                P.op("act", lambda e, pa=pa, sg=sg: e.activation(out=sg[:], in_=pa[:, :], func=AF.Sigmoid), reads=[Bp], writes=[Bsg])
                P.op("dve", lambda e, sg=sg, go=go, gy=gy, c=c: e.tensor_tensor(out=go[:], in0=sg[:], in1=gy[:, c, :].bitcast(F32), op=ALU.mult),
                     reads=[Bsg, Bgy], writes=[Bgo])
                P.op("pool", lambda e, go=go, c=c, tp=tp: e.dma_start(out=Yssm[c, :, tp * 512:(tp + 1) * 512], in_=go[:]), reads=[Bgo], dma=1)
        end_phase()
    mid.close()


    H1T = dscr("H1T", [KC, 128, NREAL], F32R)
    H1N = dscr("H1N", [NREAL, D])
    wbs_d = din("w_br_ssm", [512, D], F32R)
    wba_d = din("w_br_attn", [1024, D], F32R)
    wo_d = din("w_o", [D, D], F32R)
    ln1g_d = din("ln1_gT", [128, KC]); ln1b_d = din("ln1_bT", [128, KC])
    ph4 = (dbg or {}).get("ph4", 1)
    if ph4:
        begin_phase()
        psb = psum_banks(8)
        PS = Ring([(psb[i], P.buf(f"ps4_{i}")) for i in range(8)])
        g1T = sb("p4_g1", [128, KC]); b1T = sb("p4_b1", [128, KC]); B_g1, B_b1 = P.bufs(2, "p4gb")
        P.op("sp", lambda e: e.dma_start(out=g1T[:], in_=ln1g_d), writes=[B_g1], dma=1)
        P.op("sp", lambda e: e.dma_start(out=b1T[:], in_=ln1b_d), writes=[B_b1], dma=1)
        hT4 = sb("p4_hT", [128, KC, 512], F32R); B_hT4 = P.buf("p4_hT")
        mT = sb("p4_mT", [128, KC, 512], F32R); B_mT = P.buf("p4_mT")
        ysT = sb("p4_ys", [128, 4, 512], F32R); B_ysT = P.buf("p4_ys")
        yaT = sb("p4_ya", [128, 8, 512], F32R); B_yaT = P.buf("p4_ya")
        w16 = [sb(f"p4_w16_{i}", [128, 8, 512], F32R) for i in range(5)]
        W16 = Ring([(w16[i], P.buf(f"p4_w16_{i}")) for i in range(5)])
        S1t = sb("p4_S1", [128, 4, 512]); B_S1 = [P.buf(f"p4_S1_{f}") for f in range(4)]
        sgs = [sb(f"p4_sg{i}", [128, 512]) for i in range(4)]
        SG = Ring([(sgs[i], P.buf(f"p4_sg{i}")) for i in range(4)])
        xt4 = [sb(f"p4_xt{i}", [128, D]) for i in range(2)]
        XT4 = Ring([(xt4[i], P.buf(f"p4_xt{i}")) for i in range(2)])
        st4 = [(sb(f"p4_stat{i}", [128, 4, 6]), sb(f"p4_mv{i}", [128, 2]), sb(f"p4_rstd{i}", [128, 1])) for i in range(2)]
        ST4 = Ring([(st4[i], P.buf(f"p4_st{i}")) for i in range(2)])
        w_in_v4 = w_in.rearrange("(k p) n -> p k n", p=128)
        wo_v = wo_d.rearrange("(k p) n -> p k n", p=128)
        wbs_v = wbs_d.rearrange("(k p) n -> p k n", p=128)
        wba_v = wba_d.rearrange("(k p) n -> p k n", p=128)

        def mm_chain(pa, Bp, wtile, Bw, off, nk, rhs_tile, B_rhs):
            for k in range(nk):
                P.op("pe", lambda e, k=k: e.matmul(out=pa[:, 0:512], lhsT=wtile[:, k, off:off + 128], rhs=rhs_tile[:, k, :],
                                                   start=(k == 0), stop=(k == nk - 1)), reads=[Bw, B_rhs], writes=[Bp])

        ngrp4 = (dbg or {}).get("ngrp4", NREAL // 512)
        for g in range(ngrp4):
            tok0 = g * 512
            P.op("sp", lambda e, tok0=tok0: e.dma_start(out=hT4[:], in_=HT[:, :, tok0:tok0 + 512].rearrange("k p t -> p k t")), writes=[B_hT4], dma=1)
            P.op("sp", lambda e, tok0=tok0: e.dma_start(out=ysT[:], in_=Yssm[:, :, tok0:tok0 + 512].rearrange("k p t -> p k t")), writes=[B_ysT], dma=1)
            P.op("sp", lambda e, tok0=tok0: e.dma_start(out=yaT[:], in_=Yattn[:, :, tok0:tok0 + 512].rearrange("k p t -> p k t")), writes=[B_yaT], dma=1)
            def load_half(src_v, col0, k0, nk):
                wt_, Bwt_ = W16.next()
                P.op("sp", lambda e, wt_=wt_: e.dma_start(out=wt_[:, 0:nk, :], in_=src_v[:, k0:k0 + nk, col0:col0 + 512]), writes=[Bwt_], dma=1)
                return wt_, Bwt_

            def mm16(pa, Bp, halves_, f, rhs_tile, B_rhs):
                for k in range(KC):
                    wt_, Bwt_ = halves_[k // 8]
                    P.op("pe", lambda e, k=k, wt_=wt_: e.matmul(out=pa[:, 0:512], lhsT=wt_[:, k % 8, f * 128:(f + 1) * 128], rhs=rhs_tile[:, k, :],
                                                            start=(k == 0), stop=(k == KC - 1)), reads=[Bwt_, B_rhs], writes=[Bp])

            def mmk(pa, Bp, wt_, Bwt_, nk, f, rhs_tile, B_rhs):
                for k in range(nk):
                    P.op("pe", lambda e, k=k: e.matmul(out=pa[:, 0:512], lhsT=wt_[:, k, f * 128:(f + 1) * 128], rhs=rhs_tile[:, k, :],
                                                       start=(k == 0), stop=(k == nk - 1)), reads=[Bwt_, B_rhs], writes=[Bp])

            for cb in range(4):
                gsh = [load_half(w_in_v4, 3584 + cb * 512, hk * 8, 8) for hk in range(2)]
                wsb, Bwsb = load_half(wbs_v, cb * 512, 0, 4)
                for f in range(4):
                    pgs, Bpgs = PS.next(); mm16(pgs, Bpgs, gsh, f, hT4, B_hT4)
                    pbs, Bpbs = PS.next(); mmk(pbs, Bpbs, wsb, Bwsb, 4, f, ysT, B_ysT)
                    s1, Bs1 = SG.next()
                    P.op("act", lambda e, s1=s1, pgs=pgs: e.activation(out=s1[:], in_=pgs[:, :], func=AF.Sigmoid), reads=[Bpgs], writes=[Bs1])
                    P.op("dve", lambda e, s1=s1, pbs=pbs, f=f: e.tensor_tensor(out=S1t[:, f, :], in0=s1[:], in1=pbs[:, :], op=ALU.mult), reads=[Bs1, Bpbs], writes=[B_S1[f]])
                gah = [load_half(w_in_v4, 5632 + cb * 512, hk * 8, 8) for hk in range(2)]
                wab, Bwab = load_half(wba_v, cb * 512, 0, 8)
                for f in range(4):
                    c = cb * 4 + f
                    pga, Bpga = PS.next(); mm16(pga, Bpga, gah, f, hT4, B_hT4)
                    pba, Bpba = PS.next(); mmk(pba, Bpba, wab, Bwab, 8, f, yaT, B_yaT)
                    s2, Bs2 = SG.next()
                    P.op("act", lambda e, s2=s2, pga=pga: e.activation(out=s2[:], in_=pga[:, :], func=AF.Sigmoid), reads=[Bpga], writes=[Bs2])
                    P.op("dve", lambda e, s2=s2, pba=pba: e.tensor_tensor(out=s2[:], in0=s2[:], in1=pba[:, :], op=ALU.mult), reads=[Bs2, Bpba], writes=[Bs2])
                    P.op("pool", lambda e, s2=s2, c=c, f=f: e.tensor_tensor(out=mT[:, c, :], in0=S1t[:, f, :], in1=s2[:], op=ALU.add),
                         reads=[B_S1[f], Bs2], writes=[B_mT])
            for cb in range(4):
                woh = [load_half(wo_v, cb * 512, hk * 8, 8) for hk in range(2)]
                for f in range(4):
                    c = cb * 4 + f
                    pm, Bpm = PS.next(); mm16(pm, Bpm, woh, f, mT, B_mT)
                    P.op("dve", lambda e, pm=pm, c=c: e.scalar_tensor_tensor(out=hT4[:, c, :], in0=hT4[:, c, :].bitcast(F32), scalar=ALPHA,
                                                                       in1=pm[:, :], op0=ALU.mult, op1=ALU.add),
                         reads=[Bpm, B_hT4], writes=[B_hT4])
            for t in range(4):
                xa, Bx = XT4.next()
                for b4 in range(4):
                    pa, Bp = PS.next()
                    for jj in range(4):
                        k = b4 * 4 + jj
                        P.op("pe", lambda e, pa=pa, jj=jj, k=k, t=t: e.transpose(out=pa[:, jj * 128:(jj + 1) * 128],
                                                                                 in_=hT4[:, k, t * 128:(t + 1) * 128].bitcast(F32), identity=ident[:]),
                             reads=[B_hT4, B_ident], writes=[Bp])
                    P.op("act", lambda e, pa=pa, xa=xa, b4=b4: e.activation(out=xa[:, b4 * 512:(b4 + 1) * 512], in_=pa[:, :], func=AF.Copy), reads=[Bp], writes=[Bx])
                (sa, ma, ra), Bs = ST4.next()
                for qq in range(4):
                    P.op("dve", lambda e, qq=qq, sa=sa, xa=xa: e.bn_stats(out=sa[:, qq, :], in_=xa[:, qq * 512:(qq + 1) * 512]), reads=[Bx], writes=[Bs])
                P.op("dve", lambda e, sa=sa, ma=ma: e.bn_aggr(out=ma[:], in_=sa[:].rearrange("p a b -> p (a b)")), reads=[Bs], writes=[Bs])
                P.op("act", lambda e, ra=ra, ma=ma: e.activation(out=ra[:], in_=ma[:, 1:2], func=AF.Sqrt, bias=epst[:], scale=1.0), reads=[Bs, B_eps], writes=[Bs])
                P.op("dve", lambda e, ra=ra: e.reciprocal(out=ra[:], in_=ra[:]), reads=[Bs], writes=[Bs])
                P.op("dve", lambda e, xa=xa, ma=ma, ra=ra: e.tensor_scalar(out=xa[:], in0=xa[:], scalar1=ma[:, 0:1], scalar2=ra[:], op0=ALU.subtract, op1=ALU.mult),
                     reads=[Bx, Bs], writes=[Bx])
                P.op("pool", lambda e, xa=xa, tok0=tok0, t=t: e.dma_start(out=H1N[tok0 + t * 128:tok0 + (t + 1) * 128, :], in_=xa[:]), reads=[Bx], dma=1)
                for b4 in range(4):
                    pa, Bp = PS.next()
                    for jj in range(4):
                        k = b4 * 4 + jj
                        P.op("pe", lambda e, pa=pa, jj=jj, k=k, xa=xa: e.transpose(out=pa[:, jj * 128:(jj + 1) * 128], in_=xa[:, k * 128:(k + 1) * 128], identity=ident[:]),
                             reads=[Bx, B_ident], writes=[Bp])
                    for jj in range(4):
                        k = b4 * 4 + jj
                        P.op("act", lambda e, pa=pa, jj=jj, k=k, t=t: e.activation(out=mT[:, k, t * 128:(t + 1) * 128], in_=pa[:, jj * 128:(jj + 1) * 128],
                                                                              func=AF.Identity, scale=g1T[:, k:k + 1], bias=b1T[:, k:k + 1]),
                             reads=[Bp, B_g1, B_b1], writes=[B_mT])
            P.op("pool", lambda e, tok0=tok0: e.dma_start(out=H1T[:, :, tok0:tok0 + 512].rearrange("k p t -> p k t"), in_=mT[:]), reads=[B_mT], dma=1)
        end_phase()


    CAP = 512
    RTOT = 32 * CAP
    BIG = float(RTOT + 64)
    YEXP = dscr("YEXP", [RTOT, D])
    LTOK = dscr("LTOK", [RTOT, 1], I32)
    wr_d = din("router_w", [D, 36], F32R)
    rb_d = din("router_b_b", [128, 36])
    wg_d = din("exp_w_gate", [32, D, 512], F32R)
    wu_d = din("exp_w_up", [32, D, 512], F32R)
    wd_d = din("exp_w_down", [32, 512, D], F32R)
    tri_d = din("tri_d", [128, 128])
    ecp1_d = din("ecp1_d", [128, 32])
    NT = NREAL // 128
    RK = sb("RK", [128, NT, 2], I32); WK = sb("WK", [128, NT, 2]); B_RK = P.buf("RK"); B_WK = P.buf("WK")
    ph5 = (dbg or {}).get("ph5", 1)
    ngrp5 = (dbg or {}).get("ngrp5", NREAL // 512)
    if ph5:
        begin_phase()
        psb = psum_banks(8)
        PS = Ring([(psb[i], P.buf(f"ps5_{i}")) for i in range(8)])
        wr = sb("p5_wr", [128, KC, 36], F32R); rbb = sb("p5_rb", [128, 36]); B_wr, B_rb = P.bufs(2, "p5r")
        P.op("sp", lambda e: e.dma_start(out=wr[:], in_=wr_d.rearrange("(k p) n -> p k n", p=128)), writes=[B_wr], dma=1)
        P.op("sp", lambda e: e.dma_start(out=rbb[:], in_=rb_d), writes=[B_rb], dma=1)
        trif = sb("p5_trif", [128, 128]); trib = sb("p5_trib", [128, 128], BF16); onesb = sb("p5_onesb", [128, 128], BF16)
        ecp1 = sb("p5_ecp1", [128, 32]); cnt = sb("p5_cnt", [128, 32]); zer = sb("p5_zer", [128, RTOT // 128], I32)
        B_tri, B_ones, B_ecp, B_cnt, B_zer, B_ltok = P.bufs(6, "p5c")
        P.op("sp", lambda e: e.dma_start(out=trif[:], in_=tri_d), writes=[B_tri], dma=1)
        P.op("sp", lambda e: e.dma_start(out=ecp1[:], in_=ecp1_d), writes=[B_ecp], dma=1)
        P.op("dve", lambda e: e.tensor_copy(out=trib[:], in_=trif[:]), reads=[B_tri], writes=[B_tri])
        P.op("dve", lambda e: e.memset(onesb[:], 1.0), writes=[B_ones])
        P.op("dve", lambda e: e.memset(cnt[:], 0.0), writes=[B_cnt])
        P.op("dve", lambda e: e.memset(zer[:], 0), writes=[B_zer])
        P.op("dve", lambda e: e.memset(RK[:], RTOT + 64), writes=[B_RK])
        P.op("dve", lambda e: e.memset(WK[:], 0.0), writes=[B_WK])
        P.op("sp", lambda e: e.dma_start(out=LTOK.rearrange("(p a) o -> p (a o)", p=128), in_=zer[:]), reads=[B_zer], writes=[B_ltok], dma=1)
        h1T = sb("p5_h1T", [128, KC, 512], F32R); B_h1T = P.buf("p5_h1T")
        W32 = sb("p5_W32", [128, 32]); B_W32 = P.buf("p5_W32")
        maskb = sb("p5_maskb", [128, 32], BF16); B_maskb = P.buf("p5_maskb")
        toks = [sb(f"p5_tok{i}", [128, 1], I32) for i in range(2)]
        TOK = Ring([(toks[i], P.buf(f"p5_tok{i}")) for i in range(2)])
        rt = sb("p5_rt", [128, 288]); B_rt = P.buf("p5_rt")
        L = rt[:, 0:36]; gmx = rt[:, 36:37]; gex = rt[:, 40:44]; gsum = rt[:, 44:45]; gmask = rt[:, 48:52]
        esel = rt[:, 56:64]; v8 = rt[:, 64:72]; sel = rt[:, 72:80]; ex8 = rt[:, 80:88]; den = rt[:, 88:89]
        tmp32 = rt[:, 96:128]; wsel = rt[:, 128:136]; slot = rt[:, 160:192]; key = rt[:, 192:224]; okm = rt[:, 224:256]
        kv8 = rt[:, 256:264]; zz = rt[:, 264:266]; rf = rt[:, 266:268]; jk32 = rt[:, 136:160]

        def rdv(fn, rd=(), wr_=()):
            P.op("dve", fn, reads=[B_rt] + list(rd), writes=[B_rt] + list(wr_))

        for g in range(ngrp5):
            tok0 = g * 512
            P.op("sp", lambda e, tok0=tok0: e.dma_start(out=h1T[:], in_=H1T[:, :, tok0:tok0 + 512].rearrange("k p t -> p k t")), writes=[B_h1T], dma=1)
            for t in range(4):
                ti = g * 4 + t
                pa, Bp = PS.next()
                for k in range(KC):
                    P.op("pe", lambda e, pa=pa, k=k, t=t: e.matmul(out=pa[:, 0:36], lhsT=h1T[:, k, t * 128:(t + 1) * 128], rhs=wr[:, k, :],
                                                             start=(k == 0), stop=(k == KC - 1)), reads=[B_h1T, B_wr], writes=[Bp])
                P.op("dve", lambda e, pa=pa: e.tensor_tensor(out=L, in0=pa[:, 0:36], in1=rbb[:], op=ALU.add), reads=[Bp, B_rb, B_rt], writes=[B_rt])
                rdv(lambda e: e.tensor_reduce(out=gmx, in_=L[:, 0:4], axis=AX.X, op=ALU.max))
                rdv(lambda e: e.tensor_scalar(out=gex, in0=L[:, 0:4], scalar1=gmx, scalar2=None, op0=ALU.subtract))
                P.op("act", lambda e: e.activation(out=gex, in_=gex, func=AF.Exp), reads=[B_rt], writes=[B_rt])
                rdv(lambda e: e.tensor_reduce(out=gsum, in_=gex, axis=AX.X, op=ALU.add))
                rdv(lambda e: e.reciprocal(out=gsum, in_=gsum))
                rdv(lambda e: e.tensor_scalar(out=gmask, in0=L[:, 0:4], scalar1=gmx, scalar2=None, op0=ALU.is_equal))
                rdv(lambda e: e.tensor_tensor(out=tmp32.rearrange("p (g e) -> p g e", g=4), in0=L[:, 4:36].rearrange("p (g e) -> p g e", g=4),
                                              in1=gmask.unsqueeze(2).to_broadcast([128, 4, 8]), op=ALU.mult))
                rdv(lambda e: e.tensor_reduce(out=esel, in_=tmp32.rearrange("p (g e) -> p e g", g=4), axis=AX.X, op=ALU.add))
                rdv(lambda e: e.max(out=v8, in_=esel))
                rdv(lambda e: e.tensor_scalar(out=sel, in0=esel, scalar1=v8[:, 1:2], scalar2=None, op0=ALU.is_ge))
                rdv(lambda e: e.tensor_scalar(out=ex8, in0=esel, scalar1=v8[:, 0:1], scalar2=None, op0=ALU.subtract))
                P.op("act", lambda e: e.activation(out=ex8, in_=ex8, func=AF.Exp), reads=[B_rt], writes=[B_rt])
                rdv(lambda e: e.tensor_tensor(out=ex8, in0=ex8, in1=sel, op=ALU.mult))
                rdv(lambda e: e.tensor_reduce(out=den, in_=ex8, axis=AX.X, op=ALU.add))
                rdv(lambda e: e.reciprocal(out=den, in_=den))
                rdv(lambda e: e.tensor_tensor(out=den, in0=den, in1=gsum, op=ALU.mult))
                rdv(lambda e: e.tensor_scalar(out=wsel, in0=ex8, scalar1=den, scalar2=None, op0=ALU.mult))
                P.op("dve", lambda e: e.tensor_tensor(out=W32[:, :].rearrange("p (g e) -> p g e", g=4),
                                                      in0=gmask.unsqueeze(2).to_broadcast([128, 4, 8]),
                                                      in1=wsel.unsqueeze(1).to_broadcast([128, 4, 8]), op=ALU.mult),
                     reads=[B_rt], writes=[B_W32])
                P.op("dve", lambda e: e.tensor_scalar(out=maskb[:], in0=W32[:], scalar1=0.0, scalar2=None, op0=ALU.is_gt), reads=[B_W32], writes=[B_maskb])
                pc, Bpc = PS.next()
                P.op("pe", lambda e, pc=pc: e.matmul(out=pc[:, 0:32], lhsT=trib[:], rhs=maskb[:], start=True, stop=True), reads=[B_tri, B_maskb], writes=[Bpc])
                ptot, Bpt = PS.next()
                P.op("pe", lambda e, ptot=ptot: e.matmul(out=ptot[:, 0:32], lhsT=onesb[:], rhs=maskb[:], start=True, stop=True), reads=[B_ones, B_maskb], writes=[Bpt])
                rdv(lambda e, pc=pc: e.scalar_tensor_tensor(out=slot, in0=pc[:, 0:32], scalar=-1.0, in1=cnt[:], op0=ALU.add, op1=ALU.add), rd=[Bpc, B_cnt])
                P.op("dve", lambda e, ptot=ptot: e.tensor_tensor(out=cnt[:], in0=cnt[:], in1=ptot[:, 0:32], op=ALU.add), reads=[Bpt, B_cnt, B_rt], writes=[B_cnt])
                rdv(lambda e: e.tensor_scalar(out=okm, in0=slot, scalar1=float(CAP), scalar2=None, op0=ALU.is_lt))
                rdv(lambda e: e.tensor_tensor(out=okm, in0=okm, in1=maskb[:], op=ALU.mult), rd=[B_maskb])
                rdv(lambda e: e.tensor_tensor(out=key, in0=slot, in1=ecp1[:], op=ALU.add), rd=[B_ecp])
                rdv(lambda e: e.tensor_tensor(out=key, in0=key, in1=okm, op=ALU.mult))
                rdv(lambda e: e.tensor_tensor(out=tmp32, in0=W32[:], in1=okm, op=ALU.mult), rd=[B_W32])
                rdv(lambda e: e.max(out=kv8, in_=key))
                rdv(lambda e: e.tensor_scalar(out=zz, in0=kv8[:, 0:2], scalar1=0.0, scalar2=BIG, op0=ALU.is_equal, op1=ALU.mult))
                rdv(lambda e: e.scalar_tensor_tensor(out=rf, in0=kv8[:, 0:2], scalar=-1.0, in1=zz, op0=ALU.add, op1=ALU.add))
                P.op("dve", lambda e, ti=ti: e.tensor_copy(out=RK[:, ti, :], in_=rf), reads=[B_rt], writes=[B_RK])
                for kk in range(2):
                    P.op("dve", lambda e, ti=ti, kk=kk: e.scalar_tensor_tensor(out=slot, in0=key, scalar=kv8[:, kk:kk + 1], in1=tmp32,
                                                                           op0=ALU.is_equal, op1=ALU.mult, accum_out=WK[:, ti, kk:kk + 1]),
                         reads=[B_rt], writes=[B_rt, B_WK])
                tk, Btk = TOK.next()
                P.op("pool", lambda e, tk=tk, ti=ti: e.iota(tk[:], pattern=[[0, 1]], base=ti * 128, channel_multiplier=1), writes=[Btk])
                for kk in range(2):
                    P.op("pool", lambda e, tk=tk, ti=ti, kk=kk: e.indirect_dma_start(
                        out=LTOK[:, :], out_offset=bass.IndirectOffsetOnAxis(ap=RK[:, ti, kk:kk + 1], axis=0), in_=tk[:, :], in_offset=None,
                        bounds_check=P.reg(e, RTOT - 1), oob_is_err=False), reads=[B_RK, Btk], writes=[B_ltok], dma=1)
        end_phase()

        begin_phase()
        psb = psum_banks(8)
        PS = Ring([(psb[i], P.buf(f"ps5e_{i}")) for i in range(8)])
        g1T = sb("p5_g1", [128, KC]); b1T = sb("p5_b1", [128, KC]); B_g1, B_b1 = P.bufs(2, "p5gb")
        P.op("sp", lambda e: e.dma_start(out=g1T[:], in_=ln1g_d), writes=[B_g1], dma=1)
        P.op("sp", lambda e: e.dma_start(out=b1T[:], in_=ln1b_d), writes=[B_b1], dma=1)
        XTl = [sb(f"p5_XT{i}", [128, KC, CAP], F32R) for i in range(2)]
        B_XTl = [P.buf(f"p5_XT{i}") for i in range(2)]
        NB = CAP // 128
        xgs = [sb(f"p5_xg{i}", [128, D]) for i in range(3)]
        XG = Ring([(xgs[i], P.buf(f"p5_xg{i}")) for i in range(3)])
        idxs = [sb(f"p5_idx{i}", [128, 4], I32) for i in range(2)]
        IDX = Ring([(idxs[i], P.buf(f"p5_idx{i}")) for i in range(2)])
        w16 = [sb(f"p5_w16_{i}", [128, 8, 512], F32R) for i in range(4)]
        W16 = Ring([(w16[i], P.buf(f"p5_w16_{i}")) for i in range(4)])
        wdt = [sb(f"p5_wd{i}", [128, 4, 512], F32R) for i in range(2)]
        WD = Ring([(wdt[i], P.buf(f"p5_wd{i}")) for i in range(2)])
        hb = sb("p5_hb", [128, 4, CAP], F32R); Bhb = P.buf("p5_hb")
        sg4 = sb("p5_sg4", [128, 4, CAP]); Bsg4 = [P.buf(f"p5_sg4_{f}") for f in range(4)]
        yrs = [sb(f"p5_yr{i}", [128, NB, 512]) for i in range(2)]
        YR = Ring([(yrs[i], P.buf(f"p5_yr{i}")) for i in range(2)])
        nexp5 = (dbg or {}).get("nexp5", 32)

        def prep_pieces(ex, slot):
            XT_, BXT_ = XTl[slot], B_XTl[slot]
            state = {}

            def p_fetch():
                idx, Bidx = IDX.next()
                for b in range(NB):
                    P.op("sp", lambda e, idx=idx, b=b: e.dma_start(out=idx[:, b:b + 1], in_=LTOK[ex * CAP + b * 128:ex * CAP + (b + 1) * 128, :]), writes=[Bidx], dma=1)
                state["idx"] = (idx, Bidx)

            def p_gather(b):
                idx, Bidx = state["idx"]
                xg, Bxg = XG.next()
                P.op("pool", lambda e, xg=xg, idx=idx, b=b: e.indirect_dma_start(
                    out=xg[:, :], out_offset=None, in_=H1N[:, :], in_offset=bass.IndirectOffsetOnAxis(ap=idx[:, b:b + 1], axis=0),
                    bounds_check=P.reg(e, NREAL - 1), oob_is_err=False), reads=[Bidx], writes=[Bxg], dma=1)
                state[("xg", b)] = (xg, Bxg)

            def p_tr(b, half):
                if half == 0:
                    p_gather(b)
                xg, Bxg = state[("xg", b)]
                for b4 in range(half * 2, half * 2 + 2):
                    pa, Bp = PS.next()
                    for jj in range(4):
                        k = b4 * 4 + jj
                        P.op("pe", lambda e, pa=pa, jj=jj, k=k, xg=xg: e.transpose(out=pa[:, jj * 128:(jj + 1) * 128], in_=xg[:, k * 128:(k + 1) * 128], identity=ident[:]),
                             reads=[Bxg, B_ident], writes=[Bp])
                    for jj in range(4):
                        k = b4 * 4 + jj
                        P.op("act", lambda e, pa=pa, jj=jj, k=k, b=b, XT_=XT_: e.activation(out=XT_[:, k, b * 128:(b + 1) * 128], in_=pa[:, jj * 128:(jj + 1) * 128],
                                                                                  func=AF.Identity, scale=g1T[:, k:k + 1], bias=b1T[:, k:k + 1]),
                             reads=[Bp, B_g1, B_b1], writes=[BXT_])

            return [p_fetch] + [(lambda b=b, half=half: p_tr(b, half)) for b in range(NB) for half in range(2)]

        pieces = prep_pieces(0, 0)
        for pc_ in pieces:
            pc_()
        for ex in range(nexp5):
            slot = ex % 2
            XT_, BXT_ = XTl[slot], B_XTl[slot]
            nxt = prep_pieces(ex + 1, 1 - slot) if ex + 1 < nexp5 else []
            if nxt:
                nxt.pop(0)()
            halves = {}

            def load_halves(nm, wsrc, ex=ex, halves=halves):
                for hk in range(2):
                    wt_, Bwt_ = W16.next()
                    P.op("sp", lambda e, wt_=wt_, hk=hk, wsrc=wsrc: e.dma_start(
                        out=wt_[:], in_=wsrc[ex].rearrange("(k p) n -> p k n", p=128)[:, hk * 8:(hk + 1) * 8, :]), writes=[Bwt_], dma=1)
                    halves[(nm, hk)] = (wt_, Bwt_)

            load_halves("g", wg_d)
            load_halves("u", wu_d)
            for nm in ("g", "u"):
                for f in range(4):
                    pp, Bpp = PS.next()
                    for k in range(KC):
                        wt_, Bwt_ = halves[(nm, k // 8)]
                        P.op("pe", lambda e, pp=pp, k=k, f=f, wt_=wt_, XT_=XT_: e.matmul(out=pp[:, 0:CAP], lhsT=wt_[:, k % 8, f * 128:(f + 1) * 128], rhs=XT_[:, k, :],
                                                                                  start=(k == 0), stop=(k == KC - 1)), reads=[Bwt_, BXT_], writes=[Bpp])
                    if nm == "g":
                        P.op("act", lambda e, pp=pp, f=f: e.activation(out=sg4[:, f, :], in_=pp[:, 0:CAP], func=AF.Silu), reads=[Bpp], writes=[Bsg4[f]])
                    else:
                        P.op("dve", lambda e, pp=pp, f=f: e.tensor_tensor(out=hb[:, f, :], in0=sg4[:, f, :], in1=pp[:, 0:CAP], op=ALU.mult),
                             reads=[Bsg4[f], Bpp], writes=[Bhb])
                    if nxt:
                        nxt.pop(0)()
            for cg in range(4):
                wd_, Bwd = WD.next()
                P.op("sp", lambda e, wd_=wd_, ex=ex, cg=cg: e.dma_start(out=wd_[:], in_=wd_d[ex].rearrange("(k p) n -> p k n", p=128)[:, :, cg * 512:(cg + 1) * 512]),
                     writes=[Bwd], dma=1)
                yr, Byr = YR.next()
                for b in range(NB):
                    pa, Bp = PS.next()
                    for k in range(4):
                        P.op("pe", lambda e, pa=pa, k=k, b=b, wd_=wd_: e.matmul(out=pa[:, :], lhsT=hb[:, k, b * 128:(b + 1) * 128], rhs=wd_[:, k, :],
                                                                        start=(k == 0), stop=(k == 3)), reads=[Bhb, Bwd], writes=[Bp])
                    P.op("dve", lambda e, pa=pa, b=b, yr=yr: e.tensor_copy(out=yr[:, b, :], in_=pa[:, :]), reads=[Bp], writes=[Byr])
                P.op("pool", lambda e, ex=ex, cg=cg, yr=yr: e.dma_start(
                    out=YEXP[ex * CAP:(ex + 1) * CAP, cg * 512:(cg + 1) * 512].rearrange("(b p) n -> p b n", p=128), in_=yr[:]), reads=[Byr], dma=1)
            while nxt:
                nxt.pop(0)()
        end_phase()

    gb_d = [din(n, [128, D]) for n in ("ln1_g_b", "ln1_b_b", "ln2_g_b", "ln2_b_b")]
    ph6 = (dbg or {}).get("ph6", 1)
    if ph6:
        begin_phase()
        gbt = [sb(f"p6_gb{i}", [128, D]) for i in range(4)]
        B_gb = P.bufs(4, "p6gb")
        for i in range(4):
            P.op("sp", lambda e, i=i: e.dma_start(out=gbt[i][:], in_=gb_d[i]), writes=[B_gb[i]], dma=1)
        for i in range(2):
            P.op("pool", lambda e, i=i: e.tensor_scalar(out=gbt[i][:], in0=gbt[i][:], scalar1=ALPHA, scalar2=None, op0=ALU.mult), reads=[B_gb[i]], writes=[B_gb[i]])
        xs6 = [sb(f"p6_x{i}", [128, D]) for i in range(4)]
        X6 = Ring([(xs6[i], P.buf(f"p6_x{i}")) for i in range(4)])
        ys6 = [sb(f"p6_y{i}", [128, D]) for i in range(8)]
        Y6 = Ring([(ys6[i], P.buf(f"p6_y{i}")) for i in range(8)])
        for i in range(8):
            P.op("dve", lambda e, i=i: e.memset(ys6[i][:], 0.0), writes=[Y6.items[i][1]])
        st6 = [(sb(f"p6_stat{i}", [128, 4, 6]), sb(f"p6_mv{i}", [128, 2]), sb(f"p6_rstd{i}", [128, 1])) for i in range(2)]
        ST6 = Ring([(st6[i], P.buf(f"p6_st{i}")) for i in range(2)])
        ntile6 = (dbg or {}).get("ntile6", NREAL // 128)
        def p6_prep(tt6):
            xa, Bx = X6.next()
            r0 = tt6 * 128
            P.op("sp", lambda e, xa=xa, r0=r0: e.dma_start(out=xa[:], in_=H1N[r0:r0 + 128, :]), writes=[Bx], dma=1)
            ya = []
            for kk in range(2):
                yt_, Byt = Y6.next()
                P.op("pool", lambda e, yt_=yt_, tt6=tt6, kk=kk: e.indirect_dma_start(
                    out=yt_[:, :], out_offset=None, in_=YEXP[:, :], in_offset=bass.IndirectOffsetOnAxis(ap=RK[:, tt6, kk:kk + 1], axis=0),
                    bounds_check=P.reg(e, RTOT - 1), oob_is_err=False), reads=[B_RK], writes=[Byt], dma=1)
                ya.append((yt_, Byt))
            P.op("pool", lambda e, xa=xa: e.tensor_tensor(out=xa[:], in0=xa[:], in1=gbt[0][:], op=ALU.mult), reads=[Bx, B_gb[0]], writes=[Bx])
            P.op("pool", lambda e, xa=xa: e.tensor_tensor(out=xa[:], in0=xa[:], in1=gbt[1][:], op=ALU.add), reads=[Bx, B_gb[1]], writes=[Bx])
            return xa, Bx, ya

        def p6_finish(tt6, xa, Bx, ya):
            r0 = tt6 * 128
            for kk in range(2):
                yt_, Byt = ya[kk]
                P.op("dve", lambda e, xa=xa, yt_=yt_, tt6=tt6, kk=kk: e.scalar_tensor_tensor(out=xa[:], in0=yt_[:], scalar=WK[:, tt6, kk:kk + 1], in1=xa[:],
                                                                                   op0=ALU.mult, op1=ALU.add), reads=[Bx, Byt, B_WK], writes=[Bx])
            (sa, ma, ra), Bs = ST6.next()
            for qq in range(4):
                P.op("dve", lambda e, qq=qq, sa=sa, xa=xa: e.bn_stats(out=sa[:, qq, :], in_=xa[:, qq * 512:(qq + 1) * 512]), reads=[Bx], writes=[Bs])
            P.op("dve", lambda e, sa=sa, ma=ma: e.bn_aggr(out=ma[:], in_=sa[:].rearrange("p a b -> p (a b)")), reads=[Bs], writes=[Bs])
            P.op("act", lambda e, ra=ra, ma=ma: e.activation(out=ra[:], in_=ma[:, 1:2], func=AF.Sqrt, bias=epst[:], scale=1.0), reads=[Bs, B_eps], writes=[Bs])
            P.op("dve", lambda e, ra=ra: e.reciprocal(out=ra[:], in_=ra[:]), reads=[Bs], writes=[Bs])
            P.op("dve", lambda e, xa=xa, ma=ma, ra=ra: e.tensor_scalar(out=xa[:], in0=xa[:], scalar1=ma[:, 0:1], scalar2=ra[:], op0=ALU.subtract, op1=ALU.mult),
                 reads=[Bx, Bs], writes=[Bx])
            P.op("dve", lambda e, xa=xa: e.tensor_tensor(out=xa[:], in0=xa[:], in1=gbt[2][:], op=ALU.mult), reads=[Bx, B_gb[2]], writes=[Bx])
            P.op("pool", lambda e, xa=xa: e.tensor_tensor(out=xa[:], in0=xa[:], in1=gbt[3][:], op=ALU.add), reads=[Bx, B_gb[3]], writes=[Bx])
            P.op("act", lambda e, xa=xa, r0=r0: e.dma_start(out=out[r0:r0 + 128, :], in_=xa[:]), reads=[Bx], dma=1)

        preps = {}
        DEPTH6 = 2
        for tt6 in range(min(DEPTH6, ntile6)):
            preps[tt6] = p6_prep(tt6)
        for tt6 in range(ntile6):
            if tt6 + DEPTH6 < ntile6:
                preps[tt6 + DEPTH6] = p6_prep(tt6 + DEPTH6)
            p6_finish(tt6, *preps.pop(tt6))
        end_phase()

    P.flush()
    es.close()
    return nc


def _prep_shared(inputs):
    sh = {}
    sh["meta"] = np.ascontiguousarray(inputs["meta_tokens"], dtype=np.float32)
    sh["ident_d"] = np.eye(128, dtype=np.float32)
    sh["ln_in_gT"] = np.ascontiguousarray(np.asarray(inputs["ln_in_g"], np.float32).reshape(KC, 128).T)
    sh["ln_in_bT"] = np.ascontiguousarray(np.asarray(inputs["ln_in_b"], np.float32).reshape(KC, 128).T)
    sh["w_in"] = np.ascontiguousarray(inputs["w_in"][0], dtype=np.float32)
    f = lambda k: np.asarray(inputs[k], np.float32).reshape(-1)
    sh["tri_d"] = np.triu(np.ones((128, 128), np.float32))
    sh["ecp1_d"] = np.ascontiguousarray(np.broadcast_to((np.arange(32, dtype=np.float32) * 512 + 1)[None, :], (128, 32)))
    rep = lambda a, n=128: np.ascontiguousarray(np.broadcast_to(np.asarray(a, np.float32).reshape(1, -1), (n, np.asarray(a).size)))
    sh["router_w"] = np.ascontiguousarray(np.concatenate([inputs["router_g_w"][0], inputs["router_e_w"][0]], axis=1), dtype=np.float32)
    sh["router_b_b"] = rep(np.concatenate([np.asarray(inputs["router_g_b"][0]).reshape(-1), np.asarray(inputs["router_e_b"][0]).reshape(-1)]))
    sh["exp_w_gate"] = np.ascontiguousarray(inputs["exp_w_gate"][0], dtype=np.float32)
    sh["exp_w_up"] = np.ascontiguousarray(inputs["exp_w_up"][0], dtype=np.float32)
    sh["exp_w_down"] = np.ascontiguousarray(inputs["exp_w_down"][0], dtype=np.float32)
    sh["ln1_g_b"] = rep(inputs["ln1_g"][0]); sh["ln1_b_b"] = rep(inputs["ln1_b"][0])
    sh["ln2_g_b"] = rep(inputs["ln2_g"][0]); sh["ln2_b_b"] = rep(inputs["ln2_b"][0])
    colT = lambda a: np.ascontiguousarray(np.asarray(a, np.float32).reshape(KC, 128).T)
    sh["ln1_gT"] = colT(inputs["ln1_g"][0]); sh["ln1_bT"] = colT(inputs["ln1_b"][0])
    sh["w_br_ssm"] = np.ascontiguousarray(inputs["w_br_ssm"][0], dtype=np.float32)
    sh["w_br_attn"] = np.ascontiguousarray(inputs["w_br_attn"][0], dtype=np.float32)
    sh["w_o"] = np.ascontiguousarray(inputs["w_o"][0], dtype=np.float32)
    sl = lambda a: np.ascontiguousarray(np.asarray(a, np.float32).reshape(16, 128).T)
    sh["ssm_are"] = sl(inputs["ssm_a_re"][0])
    sh["ssm_aim"] = sl(inputs["ssm_a_im"][0])
    sh["ssm_ldt"] = sl(np.repeat(np.asarray(inputs["ssm_log_dt"][0], np.float32).reshape(32, 1), 64, axis=1))
    sl3 = lambda a: np.ascontiguousarray(np.asarray(a, np.float32).reshape(16, 128, 16).transpose(1, 0, 2))
    sh["ssm_bre"] = sl3(inputs["ssm_b_re"][0])
    sh["ssm_bim"] = sl3(inputs["ssm_b_im"][0])
    sh["ssm_cre"] = sl3(np.asarray(inputs["ssm_c_re"][0]).transpose(0, 2, 1))
    sh["ssm_cim"] = sl3(np.asarray(inputs["ssm_c_im"][0]).transpose(0, 2, 1))
    sh["ssm_dT"] = np.ascontiguousarray(np.asarray(inputs["ssm_d"][0], np.float32).reshape(4, 128).T)
    sh["ssm_wglu"] = np.ascontiguousarray(inputs["ssm_w_glu"][0], dtype=np.float32)
    sh["tt_d"] = np.ascontiguousarray(np.broadcast_to(np.arange(1032, dtype=np.float32)[None, :], (128, 1032)))
    sh["lamv"] = np.concatenate([f("attn_lambda_q1"), f("attn_lambda_k1"), f("attn_lambda_q2"), f("attn_lambda_k2")]).reshape(1, 256)
    sh["gsub_b"] = np.ascontiguousarray(np.broadcast_to(f("attn_subln_g")[None, :], (128, 128)))
    return sh


def kernel(**inputs):
    x = np.asarray(inputs["x"], np.float32)
    sh = _prep_shared(inputs)
    nc = build_program()
    in_maps = []
    for c in range(NCORES):
        m = dict(sh)
        m["x"] = np.ascontiguousarray(x[c * NSEQ:(c + 1) * NSEQ].reshape(NREAL, D))
        in_maps.append(m)
    res = run_bass_kernel_spmd(nc, in_maps, core_ids=list(range(NCORES)))
    outs = [np.asarray(r["out"], np.float32).reshape(NSEQ, SEQ, D) for r in res.results]
    return np.concatenate(outs, axis=0)
```

```python
import math
from contextlib import ExitStack

import numpy as np
import concourse.bass as bass
import concourse.mybir as mybir
from concourse.bass_utils import run_bass_kernel_spmd

F32 = mybir.dt.float32
F32R = mybir.dt.float32r
BF16 = mybir.dt.bfloat16
U32 = mybir.dt.uint32
I32 = mybir.dt.int32
AF = mybir.ActivationFunctionType
ALU = mybir.AluOpType
AX = mybir.AxisListType

D = 2048
KC = 16
SEQ = 2048
NSEQ = 2
NREAL = NSEQ * SEQ
NTOK = NREAL + 128
META0 = NREAL
IN_COLS = 7680
LN_EPS = 1e-5
ALPHA = 2.0 ** 0.25
LAMBDA_INIT = 0.2
NCORES = 8


class Buf:
    __slots__ = ("name", "w", "r")

    def __init__(self, name):
        self.name = name
        self.w = None
        self.r = {}


class Prog:
    ENG = ("pe", "dve", "act", "pool", "sp")

    def __init__(self, nc, es):
        self.nc = nc
        self.es = es
        self.ops = {e: [] for e in self.ENG}
        self.sems = {}
        self.cnt = {}
        self.seen = {e: {} for e in self.ENG}
        self.pending = {e: {} for e in self.ENG}
        self.nbuf = 0

    def reg(self, eng, val):
        if not hasattr(self, "_regs"):
            self._regs = {}
        if val not in self._regs:
            self._regs[val] = eng.to_reg(val)
        return self._regs[val]

    def buf(self, name=None):
        self.nbuf += 1
        return Buf(name or f"b{self.nbuf}")

    def bufs(self, n, name="b"):
        return [self.buf(f"{name}{i}") for i in range(n)]

    def _mksem(self, key):
        self.sems[key] = self.es.enter_context(self.nc.semaphore("s_" + key))
        self.cnt[key] = 0

    def op(self, eng, emit, reads=(), writes=(), dma=None):
        if dma:
            dma = "d_" + (writes[0].name if writes else reads[0].name)
        key = dma if dma else eng
        if key not in self.sems:
            self._mksem(key)
        waits = dict(self.pending[eng])
        self.pending[eng] = {}

        def need(tok, kind):
            k, v = tok
            if dma is None and k == eng:
                if eng == "pe" or kind != "raw":
                    return
            if v > waits.get(k, 0):
                waits[k] = v

        for b in reads:
            if b.w:
                need(b.w, "raw")
        for b in writes:
            if b.w:
                need(b.w, "waw")
            for k, v in b.r.items():
                need((k, v), "war")
        wl = []
        for k, v in waits.items():
            if self.seen[eng].get(k, 0) >= v:
                continue
            self.seen[eng][k] = v
            wl.append((k, v))
        inc = 16 if dma else 1
        self.cnt[key] += inc
        tok = (key, self.cnt[key])
        self.ops[eng].append((wl, emit, key, inc))
        for b in writes:
            b.w = tok
            b.r = {}
        for b in reads:
            if b not in writes:
                if b.r.get(key, 0) < tok[1]:
                    b.r[key] = tok[1]
        return tok

    def barrier(self):
        for e in self.ENG:
            for k, v in self.cnt.items():
                if v > self.pending[e].get(k, 0):
                    self.pending[e][k] = v

    def flush(self):
        nc = self.nc
        self.barrier()
        for e in self.ENG:
            wl = []
            for k, v in self.pending[e].items():
                if self.seen[e].get(k, 0) < v:
                    self.seen[e][k] = v
                    wl.append((k, v))
            self.pending[e] = {}
            self.ops[e].append((wl, None, None, 0))
        sems = self.sems

        def mk(lst):
            def body(eng):
                for wl, emit, key, inc in lst:
                    for k, v in wl:
                        eng.wait_ge(sems[k], v)
                    if emit is not None:
                        ins = emit(eng)
                        ins.then_inc(sems[key], inc)
            return body

        with nc.Block() as block:
            block.tensor(mk(self.ops["pe"]))
            block.vector(mk(self.ops["dve"]))
            block.scalar(mk(self.ops["act"]))
            block.gpsimd(mk(self.ops["pool"]))
            block.sync(mk(self.ops["sp"]))
        self.ops = {e: [] for e in self.ENG}
        self._regs = {}

    emit_all = flush


class Ring:
    def __init__(self, items):
        self.items = items
        self.i = 0

    def next(self):
        it = self.items[self.i % len(self.items)]
        self.i += 1
        return it


def build_program(dbg=None):
    nc = bass.Bass("TRN2", target_bir_lowering=False)
    nc.dge_precook = False
    es = ExitStack()
    P = Prog(nc, es)

    def din(name, shape, dt=F32):
        return nc.dram_tensor(name, list(shape), dt, kind="ExternalInput").ap()

    def dscr(name, shape, dt=F32):
        kind = "ExternalOutput" if (dbg and name in dbg) else "Internal"
        return nc.dram_tensor(name, list(shape), dt, kind=kind).ap()

    cur = {"es": es, "n": 0}

    def sb(name, shape, dt=F32):
        return cur["es"].enter_context(nc.sbuf_tensor(name, list(shape), dt))

    def psum_banks(n=8, width=512):
        cur["n"] += 1
        return [cur["es"].enter_context(nc.psum_tensor(f"ps{cur['n']}_{i}", [128, width], F32)) for i in range(n)]

    def begin_phase():
        cur["es"] = ExitStack()

    def end_phase():
        P.flush()
        cur["es"].close()
        cur["es"] = es

    x = din("x", [NREAL, D])
    meta = din("meta", [16, D])
    ident_d = din("ident_d", [128, 128])
    lng_in = din("ln_in_gT", [128, KC])
    lnb_in = din("ln_in_bT", [128, KC])
    w_in = din("w_in", [D, IN_COLS], F32R)
    out = nc.dram_tensor("out", [NREAL, D], F32, kind="ExternalOutput").ap()

    HT = dscr("HT", [KC, 128, NTOK], F32R)
    Usc = dscr("Usc", [4, 128, NTOK], BF16)
    Qsc = dscr("Qsc", [8, 128, NTOK], BF16)
    Ksc = dscr("Ksc", [8, 128, NTOK], BF16)
    Vsc = dscr("Vsc", [8, NTOK, 128], BF16)

    ident = sb("ident", [128, 128])
    identb = sb("identb", [128, 128], BF16)
    g_in = sb("g_in", [128, KC])
    b_in = sb("b_in", [128, KC])
    epst = sb("epst", [128, 1])
    B_ident, B_identb, B_gin, B_bin, B_eps = P.bufs(5, "c")
    P.op("sp", lambda e: e.dma_start(out=ident[:], in_=ident_d), writes=[B_ident], dma="ldc")
    P.op("sp", lambda e: e.dma_start(out=g_in[:], in_=lng_in), writes=[B_gin], dma="ldc")
    P.op("sp", lambda e: e.dma_start(out=b_in[:], in_=lnb_in), writes=[B_bin], dma="ldc")
    P.op("dve", lambda e: e.tensor_copy(out=identb[:], in_=ident[:]), reads=[B_ident], writes=[B_identb])
    P.op("dve", lambda e: e.memset(epst[:], LN_EPS), writes=[B_eps])

    P.flush()

    begin_phase()
    psb = psum_banks(8)
    PS = Ring([(psb[i], P.buf(f"ps1_{i}")) for i in range(8)])
    xt = [sb(f"xt{i}", [128, D]) for i in range(2)]
    XT = Ring([(xt[i], P.buf(f"xt{i}")) for i in range(2)])
    hT = sb("hT", [128, KC, 512])
    B_hT = P.buf("hT")
    wts = [sb(f"wt{i}", [128, 8, 1024], F32R) for i in range(4)]
    WT = Ring([(wts[i], P.buf(f"wt{i}")) for i in range(4)])
    stat = [sb(f"stat{i}", [128, 4, 6]) for i in range(2)]
    mv = [sb(f"mv{i}", [128, 2]) for i in range(2)]
    rstd = [sb(f"rstd{i}", [128, 1]) for i in range(2)]
    ST = Ring([((stat[i], mv[i], rstd[i]), P.buf(f"st{i}")) for i in range(2)])
    ob = [sb(f"ob{i}", [128, 512], BF16) for i in range(3)]
    OB = Ring([(ob[i], P.buf(f"ob{i}")) for i in range(3)])
    vtm = [sb(f"vtm{i}", [128, 4, 128], BF16) for i in range(2)]
    VTM = Ring([(vtm[i], P.buf(f"vtm{i}")) for i in range(2)])

    w_in_v = w_in.rearrange("(k p) n -> p k n", p=128)

    def ln_tile_to_hT(src_ap, nrows, tcol, gT, bT, B_g, B_b):
        xa, Bx = XT.next()
        (sa, ma, ra), Bs = ST.next()
        if nrows < 128:
            P.op("dve", lambda e: e.memset(xa[:], 0.0), writes=[Bx])
        P.op("sp", lambda e: e.dma_start(out=xa[0:nrows, :], in_=src_ap), writes=[Bx], dma="ldx")
        sub = (dbg or {}).get('sub', 99)
        if sub < 2:
            return
        for q in range(4):
            P.op("dve", lambda e, q=q: e.bn_stats(out=sa[:, q, :], in_=xa[:, q * 512:(q + 1) * 512]),
                 reads=[Bx], writes=[Bs])
        P.op("dve", lambda e: e.bn_aggr(out=ma[:], in_=sa[:].rearrange("p a b -> p (a b)")), reads=[Bs], writes=[Bs])
        if sub < 3:
            return
        P.op("act", lambda e: e.activation(out=ra[:], in_=ma[:, 1:2], func=AF.Sqrt, bias=epst[:], scale=1.0),
             reads=[Bs, B_eps], writes=[Bs])
        P.op("dve", lambda e: e.reciprocal(out=ra[:], in_=ra[:]), reads=[Bs], writes=[Bs])
        if sub < 4:
            return
        P.op("dve", lambda e: e.tensor_scalar(out=xa[:], in0=xa[:], scalar1=ma[:, 0:1], scalar2=ra[:],
                                              op0=ALU.subtract, op1=ALU.mult), reads=[Bx, Bs], writes=[Bx])
        if sub < 5:
            return
        for b4 in range(4):
            pa, Bp = PS.next()
            for j in range(4):
                k = b4 * 4 + j
                P.op("pe", lambda e, k=k, j=j, pa=pa: e.transpose(out=pa[:, j * 128:(j + 1) * 128],
                                                           in_=xa[:, k * 128:(k + 1) * 128], identity=ident[:]),
                     reads=[Bx, B_ident], writes=[Bp])
            if sub < 6:
                continue
            if (dbg or {}).get('bar', 0):
                P.barrier()
            for j in range(4):
                k = b4 * 4 + j
                var = (dbg or {}).get('var', 0)
                if var == 0:
                    P.op("act", lambda e, k=k, j=j, pa=pa: e.activation(out=hT[:, k, tcol:tcol + 128].bitcast(F32R),
                                                                 in_=pa[:, j * 128:(j + 1) * 128], func=AF.Identity,
                                                                 scale=gT[:, k:k + 1], bias=bT[:, k:k + 1]),
                         reads=[Bp, B_g, B_b], writes=[B_hT])
                elif var == 3:
                    P.op("act", lambda e, k=k, j=j, pa=pa: e.activation(out=ident[:, :],
                                                                 in_=pa[:, j * 128:(j + 1) * 128], func=AF.Copy),
                         reads=[Bp, B_g, B_b], writes=[B_hT])
                elif var == 4:
                    P.op("act", lambda e, k=k, j=j, pa=pa: e.activation(out=hT[:, k, tcol:tcol + 128],
                                                                 in_=ident[:, :], func=AF.Copy),
                         reads=[Bp, B_g, B_b], writes=[B_hT])
                elif var == 1:
                    P.op("act", lambda e, k=k, j=j, pa=pa: e.activation(out=hT[:, k, tcol:tcol + 128],
                                                                 in_=pa[:, j * 128:(j + 1) * 128], func=AF.Copy),
                         reads=[Bp, B_g, B_b], writes=[B_hT])
                elif var == 2:
                    P.op("dve", lambda e, k=k, j=j, pa=pa: e.tensor_scalar(out=hT[:, k, tcol:tcol + 128],
                                                                 in0=pa[:, j * 128:(j + 1) * 128], scalar1=gT[:, k:k + 1],
                                                                 scalar2=bT[:, k:k + 1], op0=ALU.mult, op1=ALU.add),
                         reads=[Bp, B_g, B_b], writes=[B_hT])

    wcache = {}

    def proj_chunk(col0, ntok):
        base = ((col0 - 512) // 1024) * 1024 + 512 if col0 >= 512 else 0
        width = 1024 if col0 >= 512 else 512
        if wcache.get("base") != base:
            hv = []
            for hk in range(2):
                wa, Bw = WT.next()
                P.op("sp", lambda e, wa=wa, hk=hk: e.dma_start(out=wa[:, :, 0:width], in_=w_in_v[:, hk * 8:(hk + 1) * 8, base:base + width]), writes=[Bw], dma=1)
                hv.append((wa, Bw))
            wcache.update(base=base, hv=hv)
        hv = wcache["hv"]
        off = col0 - base
        pa, Bp = PS.next()
        for k in range(KC):
            wa, Bw = hv[k // 8]
            P.op("pe", lambda e, k=k, wa=wa: e.matmul(out=pa[:, 0:ntok], lhsT=wa[:, k % 8, off:off + 128],
                                                      rhs=hT[:, k, 0:ntok].bitcast(F32R), start=(k == 0), stop=(k == KC - 1)),
                 reads=[Bw, B_hT], writes=[Bp])
        return pa, Bp

    groups = [(g * 512, 512) for g in range(NREAL // 512)] + [(META0, 128)]
    if dbg and "ngroups" in dbg:
        groups = groups[:dbg["ngroups"]] + [groups[-1]]
    stage = (dbg or {}).get('stage', 99)
    for (tok0, ntok) in groups:
        if stage < 1:
            break
        ntile = ntok // 128
        wcache.clear()
        for t in range(ntile):
            if tok0 == META0:
                ln_tile_to_hT(meta, 16, 0, g_in, b_in, B_gin, B_bin)
            else:
                ln_tile_to_hT(x[tok0 + t * 128: tok0 + (t + 1) * 128, :], 128, t * 128, g_in, b_in, B_gin, B_bin)
        if stage >= 2:
          P.op("pool", lambda e, tok0=tok0, ntok=ntok: e.dma_start(
            out=HT[:, :, tok0:tok0 + ntok].rearrange("k p t -> p k t"), in_=hT[:, :, 0:ntok].bitcast(F32R)),
            reads=[B_hT], dma="st1")
        if stage < 3:
            continue
        plan = [("u", c, c * 128) for c in range(4)]
        if tok0 != META0:
            plan += [("q", c, 512 + c * 128) for c in range(8)]
        plan += [("k", c, 1536 + c * 128) for c in range(8)]
        plan += [("v", c, 2560 + c * 128) for c in range(8)]
        for (kind, c, col0) in plan:
            pa, Bp = proj_chunk(col0, ntok)
            oa, Bo = OB.next()
            if kind == "q":
                P.op("act", lambda e, pa=pa, oa=oa, ntok=ntok: e.activation(
                    out=oa[:, 0:ntok], in_=pa[:, 0:ntok], func=AF.Copy, scale=0.125), reads=[Bp], writes=[Bo])
            else:
                P.op("dve", lambda e, pa=pa, oa=oa, ntok=ntok: e.tensor_copy(out=oa[:, 0:ntok], in_=pa[:, 0:ntok]),
                     reads=[Bp], writes=[Bo])
            if kind != "v":
                dst = {"u": Usc, "q": Qsc, "k": Ksc}[kind]
                P.op("pool", lambda e, dst=dst, c=c, oa=oa, tok0=tok0, ntok=ntok: e.dma_start(
                    out=dst[c, :, tok0:tok0 + ntok], in_=oa[:, 0:ntok]), reads=[Bo], dma="st1")
            else:
                pt, Bpt = PS.next()
                ptb = pt[:].bitcast(BF16)
                va, Bv = VTM.next()
                for t in range(ntile):
                    P.op("pe", lambda e, t=t, oa=oa, ptb=ptb: e.transpose(
                        out=ptb[:, t * 128:(t + 1) * 128], in_=oa[:, t * 128:(t + 1) * 128], identity=identb[:]),
                        reads=[Bo, B_identb], writes=[Bpt])
                P.op("dve", lambda e, va=va, ptb=ptb, ntile=ntile: e.tensor_copy(
                    out=va[:, 0:ntile, :], in_=ptb[:, 0:ntile * 128].rearrange("p (t e) -> p t e", e=128)),
                    reads=[Bpt], writes=[Bv])
                P.op("pool", lambda e, c=c, va=va, tok0=tok0, ntile=ntile, ntok=ntok: e.dma_start(
                    out=Vsc[c, tok0:tok0 + ntok, :].rearrange("(t p) e -> p t e", p=128), in_=va[:, 0:ntile, :]),
                    reads=[Bv], dma="st1")
    end_phase()

    Yattn = dscr("Yattn", [8, 128, NTOK], F32R)
    lamv_d = din("lamv", [1, 256])
    gsub_d = din("gsub_b", [128, 128])


    YG = dscr("YG", [4, 128, NTOK], F32R)
    Yssm = dscr("Yssm", [4, 128, NTOK], F32R)
    s_are_d = din("ssm_are", [128, 16]); s_aim_d = din("ssm_aim", [128, 16]); s_ldt_d = din("ssm_ldt", [128, 16])
    s_bre_d = din("ssm_bre", [128, 16, 16]); s_bim_d = din("ssm_bim", [128, 16, 16])
    s_cre_d = din("ssm_cre", [128, 16, 16]); s_cim_d = din("ssm_cim", [128, 16, 16])
    s_d_d = din("ssm_dT", [128, 4]); wglu_d = din("ssm_wglu", [512, 512], F32R)
    TS = 1032
    tt_d = din("tt_d", [128, TS])
    PI = math.pi
    ph3 = (dbg or {}).get("ph3", 1)
    ph2 = (dbg or {}).get("ph2", 1)
    nseq2 = (dbg or {}).get("nseq2", NSEQ)
    mid = ExitStack()

    def sbm(name, shape, dt=F32):
        return mid.enter_context(nc.sbuf_tensor(name, list(shape), dt))

    if ph2:
        hpi = sbm("s_hpi", [128, 1]); B_hpi = P.buf("s_hpi")
        prm = sbm("s_prm", [128, 24, 16])
        dT = sbm("s_dT", [128, 4])
        tt = sbm("s_tt", [128, TS])
        wbt = sbm("s_wb", [128, 16, 2, 128], BF16)
        cwt = sbm("s_cw", [128, 16, 2, 128], BF16)
        begin_phase()
        yb = psum_banks(2, 512)
        YPS = Ring([(yb[i], P.buf(f"p2y{i}")) for i in range(2)])
        P.op("dve", lambda e: e.memset(hpi[:], PI / 2), writes=[B_hpi])
        NPRM = 24
        B_prm = P.buf("s_prm")
        names = ["are", "aim", "ldt", "lre", "dt", "mag", "ang", "sn", "cs", "t1", "t2", "ar", "ai", "nr", "den", "zr", "zi", "phs"]
        V = {n: prm[:, i, :] for i, n in enumerate(names)}
        bc = sb("s_bc", [128, 4, 16, 16]); B_bc = P.buf("s_bc")
        bb = sb("s_bb", [128, 4, 16, 16]); B_bb = P.buf("s_bb")
        B_dT = P.buf("s_dT")
        B_tt = P.buf("s_tt")
        P.op("sp", lambda e: e.dma_start(out=prm[:, 0, :], in_=s_are_d), writes=[B_prm], dma=1)
        P.op("sp", lambda e: e.dma_start(out=prm[:, 1, :], in_=s_aim_d), writes=[B_prm], dma=1)
        P.op("sp", lambda e: e.dma_start(out=prm[:, 2, :], in_=s_ldt_d), writes=[B_prm], dma=1)
        for i, dd in enumerate((s_bre_d, s_bim_d, s_cre_d, s_cim_d)):
            P.op("sp", lambda e, i=i, dd=dd: e.dma_start(out=bc[:, i, :, :], in_=dd), writes=[B_bc], dma=1)
        P.op("sp", lambda e: e.dma_start(out=dT[:], in_=s_d_d), writes=[B_dT], dma=1)
        P.op("sp", lambda e: e.dma_start(out=tt[:], in_=tt_d), writes=[B_tt], dma=1)

        def dv(fn, rd=(), wr=None):
            P.op("dve", fn, reads=[B_prm] + list(rd), writes=[wr or B_prm])

        def tt_(o, a, b, op):
            dv(lambda e: e.tensor_tensor(out=V[o], in0=V[a], in1=V[b], op=op))

        def ts_(o, a, s1, op0, s2=None, op1=None):
            if op1 is None:
                dv(lambda e: e.tensor_scalar(out=V[o], in0=V[a], scalar1=s1, scalar2=None, op0=op0))
            else:
                dv(lambda e: e.tensor_scalar(out=V[o], in0=V[a], scalar1=s1, scalar2=s2, op0=op0, op1=op1))

        def act_(o, a, func, scale=1.0):
            P.op("act", lambda e: e.activation(out=V[o], in_=V[a], func=func, scale=scale), reads=[B_prm], writes=[B_prm])

        ts_("lre", "are", -1e-4, ALU.min)
        act_("dt", "ldt", AF.Exp)
        tt_("t1", "lre", "dt", ALU.mult)
        act_("mag", "t1", AF.Exp)
        tt_("ang", "aim", "dt", ALU.mult)
        dv(lambda e: e.tensor_scalar(out=V["t1"].bitcast(I32), in0=V["ang"], scalar1=1.0 / (2 * PI), scalar2=None, op0=ALU.mult))
        dv(lambda e: e.tensor_copy(out=V["t2"], in_=V["t1"].bitcast(I32)))
        dv(lambda e: e.scalar_tensor_tensor(out=V["ang"], in0=V["t2"], scalar=-2 * PI, in1=V["ang"], op0=ALU.mult, op1=ALU.add))
        dv(lambda e: e.tensor_scalar(out=V["t1"], in0=V["ang"], scalar1=0.0, scalar2=2 * PI, op0=ALU.is_lt, op1=ALU.mult))
        tt_("ang", "ang", "t1", ALU.add)
        dv(lambda e: e.tensor_scalar(out=V["t1"], in0=V["ang"], scalar1=PI, scalar2=-2 * PI, op0=ALU.is_gt, op1=ALU.mult))
        tt_("t1", "t1", "ang", ALU.add)
        act_("sn", "t1", AF.Sin)
        dv(lambda e: e.tensor_scalar(out=V["t1"], in0=V["ang"], scalar1=PI / 2, scalar2=-2 * PI, op0=ALU.is_gt, op1=ALU.mult))
        dv(lambda e: e.scalar_tensor_tensor(out=V["t1"], in0=V["ang"], scalar=PI / 2, in1=V["t1"], op0=ALU.add, op1=ALU.add))
        act_("cs", "t1", AF.Sin)
        tt_("ar", "mag", "cs", ALU.mult)
        tt_("ai", "mag", "sn", ALU.mult)
        ts_("nr", "ar", -1.0, ALU.add)
        tt_("t1", "lre", "lre", ALU.mult)
        tt_("t2", "aim", "aim", ALU.mult)
        tt_("den", "t1", "t2", ALU.add)
        dv(lambda e: e.reciprocal(out=V["den"], in_=V["den"]))
        tt_("t1", "nr", "lre", ALU.mult)
        tt_("t2", "ai", "aim", ALU.mult)
        tt_("zr", "t1", "t2", ALU.add)
        tt_("zr", "zr", "den", ALU.mult)
        tt_("t1", "ai", "lre", ALU.mult)
        tt_("t2", "nr", "aim", ALU.mult)
        tt_("zi", "t1", "t2", ALU.subtract)
        tt_("zi", "zi", "den", ALU.mult)
        ts_("phs", "ang", float(TS), ALU.mult)
        zrb = V["zr"].unsqueeze(2).to_broadcast([128, 16, 16])
        zib = V["zi"].unsqueeze(2).to_broadcast([128, 16, 16])
        P.op("dve", lambda e: e.tensor_tensor(out=bb[:, 2], in0=bc[:, 0], in1=zrb, op=ALU.mult), reads=[B_prm, B_bc], writes=[B_bb])
        P.op("dve", lambda e: e.tensor_tensor(out=bb[:, 3], in0=bc[:, 1], in1=zib, op=ALU.mult), reads=[B_prm, B_bc], writes=[B_bb])
        P.op("dve", lambda e: e.tensor_tensor(out=bb[:, 0], in0=bb[:, 2], in1=bb[:, 3], op=ALU.subtract), reads=[B_bb], writes=[B_bb])
        P.op("dve", lambda e: e.tensor_tensor(out=bb[:, 2], in0=bc[:, 1], in1=zrb, op=ALU.mult), reads=[B_prm, B_bc, B_bb], writes=[B_bb])
        P.op("dve", lambda e: e.tensor_tensor(out=bb[:, 3], in0=bc[:, 0], in1=zib, op=ALU.mult), reads=[B_prm, B_bc, B_bb], writes=[B_bb])
        P.op("dve", lambda e: e.tensor_tensor(out=bb[:, 1], in0=bb[:, 2], in1=bb[:, 3], op=ALU.add), reads=[B_bb], writes=[B_bb])
        bmw = sb("s_bmw", [128, 16, 2, 128], BF16); B_bmw = P.buf("s_bmw")
        B_wb = P.buf("s_wb")
        B_cw = P.buf("s_cw")
        P.op("pool", lambda e: e.memset(bmw[:], 0.0), writes=[B_bmw])
        P.op("pool", lambda e: e.memset(cwt[:], 0.0), writes=[B_cw])
        for jm in range(4):
            for gl in range(2):
                c0 = 32 * jm + 16 * gl
                for ri in range(2):
                    P.op("dve", lambda e, jm=jm, gl=gl, ri=ri, c0=c0: e.tensor_copy(
                        out=bmw[64 * gl:64 * gl + 64, jm::4, ri, c0:c0 + 16], in_=bb[64 * gl:64 * gl + 64, ri, jm::4, :]),
                        reads=[B_bb], writes=[B_bmw])
                P.op("dve", lambda e, jm=jm, gl=gl, c0=c0: e.tensor_copy(
                    out=cwt[64 * gl:64 * gl + 64, jm::4, 0, c0:c0 + 16], in_=bc[64 * gl:64 * gl + 64, 2, jm::4, :]),
                    reads=[B_bc], writes=[B_cw])
                P.op("dve", lambda e, jm=jm, gl=gl, c0=c0: e.tensor_scalar(
                    out=cwt[64 * gl:64 * gl + 64, jm::4, 1, c0:c0 + 16], in0=bc[64 * gl:64 * gl + 64, 3, jm::4, :],
                    scalar1=-1.0, scalar2=None, op0=ALU.mult), reads=[B_bc], writes=[B_cw])
        for g4 in range(8):
            pa, Bp = YPS.next()
            pab = pa[:].bitcast(BF16)
            for i4 in range(4):
                idx = g4 * 4 + i4
                j, ri = idx // 2, idx % 2
                P.op("pe", lambda e, pab=pab, i4=i4, j=j, ri=ri: e.transpose(out=pab[:, i4 * 128:(i4 + 1) * 128], in_=bmw[:, j, ri, :],
                                                                             identity=identb[:]), reads=[B_bmw, B_identb], writes=[Bp])
            j0 = (g4 * 4) // 2
            P.op("dve", lambda e, pab=pab, j0=j0: e.tensor_copy(out=wbt[:, j0:j0 + 2, :, :].rearrange("p a b c -> p (a b c)"),
                                                                 in_=pab[:, 0:512]), reads=[Bp], writes=[B_wb])

        end_phase()

    begin_phase()
    psb = psum_banks(8)
    bankA = psb[6]; B_bankA = P.buf("p23_bankA")
    bankB = psb[7]; B_bankBt = P.buf("p23_bankB"); B_bankBy = B_bankBt
    g3 = None
    g2 = None
    if ph3:
        PS = Ring([(psb[7], B_bankBt)])
        lamv = sb("lamv_s", [1, 256]); lamt = sb("lamt", [1, 8]); ones1 = sb("ones1", [1, 128])
        neglam = sb("neglam", [128, 1]); gsub = sb("gsub", [128, 128])
        B_lam, B_neglam, B_gsub, B_ones1 = P.bufs(4, "a3c")
        P.op("sp", lambda e: e.dma_start(out=lamv[:], in_=lamv_d), writes=[B_lam], dma=1)
        P.op("sp", lambda e: e.dma_start(out=gsub[:], in_=gsub_d), writes=[B_gsub], dma=1)
        P.op("dve", lambda e: e.memset(ones1[:], 1.0), writes=[B_ones1])
        P.op("dve", lambda e: e.tensor_scalar(out=gsub[:], in0=gsub[:], scalar1=1.0 - LAMBDA_INIT, scalar2=None, op0=ALU.mult),
             reads=[B_gsub], writes=[B_gsub])
        P.op("dve", lambda e: e.tensor_tensor(out=lamv[:, 0:64], in0=lamv[:, 0:64], in1=lamv[:, 64:128], op=ALU.mult), reads=[B_lam], writes=[B_lam])
        P.op("dve", lambda e: e.tensor_tensor(out=lamv[:, 128:192], in0=lamv[:, 128:192], in1=lamv[:, 192:256], op=ALU.mult), reads=[B_lam], writes=[B_lam])
        P.op("dve", lambda e: e.tensor_reduce(out=lamt[:, 0:1], in_=lamv[:, 0:64], axis=AX.X, op=ALU.add), reads=[B_lam], writes=[B_lam])
        P.op("dve", lambda e: e.tensor_reduce(out=lamt[:, 1:2], in_=lamv[:, 128:192], axis=AX.X, op=ALU.add), reads=[B_lam], writes=[B_lam])
        P.op("act", lambda e: e.activation(out=lamt[:, 2:4], in_=lamt[:, 0:2], func=AF.Exp), reads=[B_lam], writes=[B_lam])
        P.op("dve", lambda e: e.tensor_tensor(out=lamt[:, 4:5], in0=lamt[:, 3:4], in1=lamt[:, 2:3], op=ALU.subtract), reads=[B_lam], writes=[B_lam])
        P.op("dve", lambda e: e.tensor_scalar(out=lamt[:, 5:6], in0=lamt[:, 4:5], scalar1=-LAMBDA_INIT, scalar2=None, op0=ALU.add), reads=[B_lam], writes=[B_lam])
        pa, Bp = PS.next()
        P.op("pe", lambda e, pa=pa: e.matmul(out=pa[:, 0:1], lhsT=ones1[:, :], rhs=lamt[:, 5:6], start=True, stop=True),
             reads=[B_lam, B_ones1], writes=[Bp])
        P.op("dve", lambda e, pa=pa: e.tensor_copy(out=neglam[:], in_=pa[:, 0:1]), reads=[Bp], writes=[B_neglam])

        ORING = Ring([(psb[i], P.buf(f"pso{i}")) for i in range(0, 4)])
        SRING = Ring([(psb[i], P.buf(f"pss{i}")) for i in range(4, 6)])
        TRING = Ring([(psb[7], B_bankBt)])
        kts = [sb(f"a_kt{i}", [128, 2, 128 + SEQ], BF16) for i in range(2)]
        qts = [sb(f"a_qt{i}", [128, SEQ], BF16) for i in range(2)]
        vts = [sb(f"a_vt{i}", [128, 17, 129], BF16) for i in range(2)]
        HRING = Ring([((kts[i], qts[i], vts[i]), (P.buf(f"a_kt{i}"), P.buf(f"a_qt{i}"), P.buf(f"a_vt{i}"), P.buf(f"a_vm{i}"))) for i in range(2)])
        for i in range(2):
            P.op("pool", lambda e, i=i: e.memset(vts[i][:], 1.0), writes=[HRING.items[i][1][2], HRING.items[i][1][3]])
            P.op("pool", lambda e, i=i: e.memset(vts[i][:, 16, :], 0.0), writes=[HRING.items[i][1][3]])
            P.op("pool", lambda e, i=i: e.memset(vts[i][0:16, 16, 128:129], 1.0), writes=[HRING.items[i][1][3]])
            P.op("pool", lambda e, i=i: e.memset(kts[i][:], 0.0), writes=[HRING.items[i][1][0]])
        pts = [sb(f"a_pt{i}", [128, 512], BF16) for i in range(5)]
        PTR = Ring([(pts[i], P.buf(f"a_pt{i}")) for i in range(5)])
        yTs = [sb(f"a_yT{i}", [128, SEQ], F32R) for i in range(2)]
        YTR = Ring([(yTs[i], P.buf(f"a_yT{i}")) for i in range(2)])
        eps3 = [(sb(f"a_rc{i}", [128, 4]), sb(f"a_t1{i}", [128, 128]), sb(f"a_od{i}", [128, 128]), sb(f"a_yq{i}", [128, 128]),
                 sb(f"a_jk{i}", [128, 128])) for i in range(4)]
        EPR = Ring([(eps3[i], P.bufs(5, f"a_ep{i}_")) for i in range(4)])

        def gen3():
            nseq3 = (dbg or {}).get("nseq3", NSEQ)
            nhead3 = (dbg or {}).get("nhead3", 8)
            for s_ in range(nseq3):
                for h in range(nhead3):
                    (kt, qt_, vt), (Bk, Bq, Bv, Bvm) = HRING.next()
                    r0 = s_ * SEQ
                    for m in range(2):
                        P.op("sp", lambda e, kt=kt, h=h, m=m: e.dma_start(out=kt[m * 64:(m + 1) * 64, m, 0:16], in_=Ksc[h, m * 64:(m + 1) * 64, META0:META0 + 16]), writes=[Bk], dma=1)
                        P.op("sp", lambda e, kt=kt, h=h, r0=r0, m=m: e.dma_start(out=kt[m * 64:(m + 1) * 64, m, 128:128 + SEQ], in_=Ksc[h, m * 64:(m + 1) * 64, r0:r0 + SEQ]), writes=[Bk], dma=1)
                    P.op("sp", lambda e, qt_=qt_, h=h, r0=r0: e.dma_start(out=qt_[:, :], in_=Qsc[h, :, r0:r0 + SEQ]), writes=[Bq], dma=1)
                    P.op("sp", lambda e, vt=vt, h=h, r0=r0: e.dma_start(out=vt[:, 0:16, 0:128], in_=Vsc[h, r0:r0 + SEQ, :].rearrange("(t p) e -> p t e", p=128)), writes=[Bv], dma=1)
                    P.op("sp", lambda e, vt=vt, h=h: e.dma_start(out=vt[0:16, 16, 0:128], in_=Vsc[h, META0:META0 + 16, :]), writes=[Bvm], dma=1)
                    yT, ByT = YTR.next()
                    steps = []
                    for qi in range(SEQ // 128):
                        blks = [("m", 0)] + [("r", kb) for kb in range(qi + 1)]
                        pairs = [blks[i:i + 2] for i in range(0, len(blks), 2)]
                        for pi_, pr in enumerate(pairs):
                            steps.append((qi, pi_, pr, len(pairs)))
                    LA = 2
                    pend = {}
                    oacc = {}
                    later = []

                    def emit_S(st):
                        qi, pi_, pr, npair = st
                        sp_, Bs_ = SRING.next()
                        for bj, (typ, kb) in enumerate(pr):
                            kc0 = 0 if typ == "m" else 128 + kb * 128
                            for m in range(2):
                                c0 = bj * 256 + m * 128
                                P.op("pe", lambda e, sp_=sp_, m=m, kc0=kc0, kt=kt, qt_=qt_, qi=qi, c0=c0: e.matmul(
                                    out=sp_[:, c0:c0 + 128], lhsT=kt[:, m, kc0:kc0 + 128],
                                    rhs=qt_[:, qi * 128:(qi + 1) * 128], start=True, stop=True),
                                    reads=[Bk, Bq], writes=[Bs_])
                        pt, Bpt = PTR.next()
                        w_ = 256 * len(pr)
                        P.op("act", lambda e, pt=pt, sp_=sp_, w_=w_: e.activation(out=pt[:, 0:w_], in_=sp_[:, 0:w_], func=AF.Exp),
                             reads=[Bs_], writes=[Bpt])
                        for bj, (typ, kb) in enumerate(pr):
                            if typ == "r" and kb == qi:
                                P.op("pool", lambda e, pt=pt, bj=bj: e.memset(pt[64:128, bj * 256:(bj + 1) * 256].rearrange("p (m q) -> p m q", m=2)[:, :, 0:64], 0.0),
                                     reads=[Bpt], writes=[Bpt])
                        pend[(qi, pi_)] = (pt, Bpt)

                    def emit_AV(st, now):
                        qi, pi_, pr, npair = st
                        pt, Bpt = pend.pop((qi, pi_))
                        if pi_ == 0:
                            oacc[qi] = (ORING.next(), ORING.next())
                        (o0, Bo0), (o1, Bo1) = oacc[qi]
                        for bj, (typ, kb) in enumerate(pr):
                            vidx = 16 if typ == "m" else kb
                            first = (pi_ == 0 and bj == 0)
                            last = (pi_ == npair - 1 and bj == len(pr) - 1)
                            for m, (oo, Boo) in enumerate(((o0, Bo0), (o1, Bo1))):
                                c0 = bj * 256 + m * 128
                                P.op("pe", lambda e, oo=oo, pt=pt, c0=c0, vt=vt, vidx=vidx, first=first, last=last: e.matmul(
                                    out=oo[:, 0:129], lhsT=pt[:, c0:c0 + 128], rhs=vt[:, vidx, :], start=first, stop=last),
                                    reads=[Bpt, Bv, Bvm], writes=[Boo])
                        if pi_ != npair - 1:
                            return
                        del oacc[qi]
                        (rc, t1, od, yq, jk), (Brc, Bt1, Bod, Byq, Bjk) = EPR.next()
                        P.op("dve", lambda e, rc=rc, o0=o0: e.reciprocal(out=rc[:, 0:1], in_=o0[:, 128:129]), reads=[Bo0], writes=[Brc])
                        P.op("dve", lambda e, rc=rc, o1=o1: e.reciprocal(out=rc[:, 1:2], in_=o1[:, 128:129]), reads=[Bo1], writes=[Brc])
                        P.op("dve", lambda e, rc=rc: e.tensor_scalar(out=rc[:, 2:3], in0=rc[:, 1:2], scalar1=neglam[:, 0:1], scalar2=None, op0=ALU.mult),
                             reads=[Brc, B_neglam], writes=[Brc])
                        P.op("dve", lambda e, rc=rc, t1=t1, o1=o1: e.tensor_scalar(out=t1[:], in0=o1[:, 0:128], scalar1=rc[:, 2:3], scalar2=None, op0=ALU.mult),
                             reads=[Brc, Bo1], writes=[Bt1])
                        P.op("dve", lambda e, rc=rc, t1=t1, o0=o0, od=od: e.scalar_tensor_tensor(out=od[:], in0=o0[:, 0:128], scalar=rc[:, 0:1], in1=t1[:],
                                                                                           op0=ALU.mult, op1=ALU.add),
                             reads=[Brc, Bo0, Bt1], writes=[Bod])
                        P.op("dve", lambda e, od=od, jk=jk, rc=rc: e.scalar_tensor_tensor(out=jk[:], in0=od[:], scalar=1.0, in1=od[:], op0=ALU.mult, op1=ALU.mult,
                                                                                    accum_out=rc[:, 3:4]),
                             reads=[Bod, Brc], writes=[Bjk, Brc])

                        def stage2(rc=rc, od=od, yq=yq, Brc=Brc, Bod=Bod, Byq=Byq):
                            P.op("act", lambda e: e.activation(out=rc[:, 3:4], in_=rc[:, 3:4], func=AF.Sqrt, bias=epst[:], scale=1.0 / 128.0),
                                 reads=[Brc, B_eps], writes=[Brc])
                            P.op("dve", lambda e: e.reciprocal(out=rc[:, 3:4], in_=rc[:, 3:4]), reads=[Brc], writes=[Brc])
                            P.op("dve", lambda e: e.scalar_tensor_tensor(out=yq[:], in0=od[:], scalar=rc[:, 3:4], in1=gsub[:],
                                                                         op0=ALU.mult, op1=ALU.mult),
                                 reads=[Brc, Bod, B_gsub], writes=[Byq])

                        def stage3(yq=yq, Byq=Byq, qi=qi, yT=yT, ByT=ByT):
                            tp, Btp = TRING.next()
                            P.op("pe", lambda e: e.transpose(out=tp[:, 0:128], in_=yq[:], identity=ident[:]), reads=[Byq, B_ident], writes=[Btp])
                            P.op("dve", lambda e: e.tensor_copy(out=yT[:, qi * 128:(qi + 1) * 128], in_=tp[:, 0:128]),
                                 reads=[Btp], writes=[ByT])

                        later.append((now + 3, stage2))
                        later.append((now + 6, stage3))

                    def run_later(now):
                        keep = []
                        for due, fn in later:
                            if due <= now:
                                fn()
                            else:
                                keep.append((due, fn))
                        later[:] = keep

                    nst = len(steps)
                    for i in range(nst + LA):
                        if i < nst:
                            emit_S(steps[i])
                        if i >= LA:
                            emit_AV(steps[i - LA], i)
                        run_later(i)
                        yield
                    run_later(10 ** 9)
                    P.op("pool", lambda e, yT=yT, h=h, r0=r0: e.dma_start(out=Yattn[h, :, r0:r0 + SEQ], in_=yT[:, :]), reads=[ByT], dma=1)

        g3 = gen3()
    if ph2:
        uTs = [[sb(f"s_uT{s_}_{i}", [128, TS], BF16) for i in range(2)] for s_ in range(NSEQ)]
        UTR = [Ring([(uTs[s_][i], P.buf(f"s_uT{s_}_{i}")) for i in range(2)]) for s_ in range(NSEQ)]
        wk = {n: sb("s_" + n, [128, TS]) for n in ("A1", "A2", "M1", "M2", "M3", "M4", "Z1", "Z2", "W1", "W2", "N1", "N2", "N3", "N4",
                                                   "ST", "CT", "Rt")}
        Bw = {n: P.buf("s_" + n) for n in wk}
        ygs = [sb(f"s_yg{i}", [128, TS], F32R) for i in range(1)]
        YGR = Ring([(ygs[i], P.buf(f"s_yg{i}")) for i in range(1)])
        Xs = [sb(f"s_X{s_}", [128, 4, 2, TS], BF16) for s_ in range(NSEQ)]
        B_X = [[[P.buf(f"s_X{s_}{a}{b}") for b in range(2)] for a in range(4)] for s_ in range(NSEQ)]
        carry = sb("s_carry", [128, NSEQ, 16, 2]); B_carry = P.buf("s_carry")

        def gen2():
            CT6 = lambda ap: ap.rearrange("p (a b) -> p a b", b=172)
            for seg in range(2):
                for q in range(4):
                    uT_s = []
                    for s_ in range(nseq2):
                        r0 = s_ * SEQ
                        uT, BuT = UTR[s_].next()
                        if seg == 0:
                            P.op("sp", lambda e, uT=uT, q=q: e.dma_start(out=uT[:, 0:16], in_=Usc[q, :, META0:META0 + 16]), writes=[BuT], dma=1)
                            P.op("sp", lambda e, uT=uT, q=q, r0=r0: e.dma_start(out=uT[:, 16:TS], in_=Usc[q, :, r0:r0 + TS - 16]), writes=[BuT], dma=1)
                        else:
                            P.op("sp", lambda e, uT=uT, q=q, r0=r0: e.dma_start(out=uT[:, :], in_=Usc[q, :, r0 + TS - 16:r0 + SEQ]), writes=[BuT], dma=1)
                        uT_s.append((uT, BuT))
                    for jm in range(4):
                        j = 4 * q + jm
                        phj = V["ang"][:, j:j + 1]
                        if seg == 0:
                            P.op("dve", lambda e, phj=phj: e.tensor_scalar(out=wk["A1"][:], in0=tt[:], scalar1=phj, scalar2=None, op0=ALU.mult),
                                 reads=[B_tt, B_prm], writes=[Bw["A1"]])
                        else:
                            P.op("dve", lambda e, phj=phj, j=j: e.tensor_scalar(out=wk["A1"][:], in0=tt[:], scalar1=phj, scalar2=V["phs"][:, j:j + 1],
                                                                            op0=ALU.mult, op1=ALU.add), reads=[B_tt, B_prm], writes=[Bw["A1"]])
                        P.op("dve", lambda e: e.tensor_scalar(out=wk["A2"][:].bitcast(I32), in0=wk["A1"][:], scalar1=1.0 / (2 * PI), scalar2=None, op0=ALU.mult),
                             reads=[Bw["A1"]], writes=[Bw["A2"]])
                        P.op("dve", lambda e: e.scalar_tensor_tensor(out=wk["A1"][:], in0=wk["A2"][:].bitcast(I32), scalar=-2 * PI, in1=wk["A1"][:], op0=ALU.mult, op1=ALU.add),
                             reads=[Bw["A2"], Bw["A1"]], writes=[Bw["A1"]])
                        P.op("dve", lambda e: e.tensor_scalar(out=wk["A1"][:], in0=wk["A1"][:], scalar1=-PI, scalar2=PI, op0=ALU.max, op1=ALU.min),
                             reads=[Bw["A1"]], writes=[Bw["A1"]])
                        yield
                        P.op("act", lambda e: e.activation(out=wk["ST"][:], in_=wk["A1"][:], func=AF.Sin), reads=[Bw["A1"]], writes=[Bw["ST"]])
                        P.op("act", lambda e: e.activation(out=wk["A2"][:], in_=wk["A1"][:], func=AF.Abs), reads=[Bw["A1"]], writes=[Bw["A2"]])
                        P.op("act", lambda e: e.activation(out=wk["CT"][:], in_=wk["A2"][:], func=AF.Sin, scale=-1.0, bias=hpi[:]), reads=[Bw["A2"], B_hpi], writes=[Bw["CT"]])
                        P.op("pool", lambda e, j=j: e.tensor_scalar(out=wk["Rt"][:], in0=tt[:], scalar1=0.0, scalar2=V["mag"][:, j:j + 1], op0=ALU.mult, op1=ALU.add),
                             reads=[B_tt, B_prm], writes=[Bw["Rt"]])
                        yield
                        for s_ in range(nseq2):
                            uT, BuT = uT_s[s_]
                            for p6 in range(6):
                                c0 = p6 * 172
                                for ri in range(2):
                                    P.op("pe", lambda e, ri=ri, c0=c0, j=j, uT=uT: e.matmul(
                                        out=bankA[:, ri * 172:(ri + 1) * 172], lhsT=wbt[:, j, ri, :], rhs=uT[:, c0:c0 + 172],
                                        start=True, stop=True), reads=[B_wb, BuT], writes=[B_bankA])
                                bur, bui = bankA[:, 0:172], bankA[:, 172:344]
                                for (mn, src, tab) in (("M1", bur, "CT"), ("M2", bui, "ST"), ("M3", bui, "CT"), ("M4", bur, "ST")):
                                    P.op("dve", lambda e, mn=mn, src=src, tab=tab, c0=c0: e.tensor_tensor(out=wk[mn][:, c0:c0 + 172], in0=src, in1=wk[tab][:, c0:c0 + 172], op=ALU.mult),
                                         reads=[B_bankA, Bw[tab]], writes=[Bw[mn]])
                                yield
                            P.op("pool", lambda e: e.tensor_tensor(out=wk["Z1"][:], in0=wk["M1"][:], in1=wk["M2"][:], op=ALU.add),
                                 reads=[Bw["M1"], Bw["M2"]], writes=[Bw["Z1"]])
                            P.op("pool", lambda e: e.tensor_tensor(out=wk["Z2"][:], in0=wk["M3"][:], in1=wk["M4"][:], op=ALU.subtract),
                                 reads=[Bw["M3"], Bw["M4"]], writes=[Bw["Z2"]])
                            yield
                            for ri, (zn, wn) in enumerate((("Z1", "W1"), ("Z2", "W2"))):
                                if seg == 0:
                                    P.op("dve", lambda e, zn=zn, wn=wn: e.tensor_tensor_scan(out=wk[wn][:], data0=wk["Rt"][:], data1=wk[zn][:], initial=0.0,
                                                                                          op0=ALU.mult, op1=ALU.add),
                                         reads=[Bw["Rt"], Bw[zn]], writes=[Bw[wn]])
                                    P.op("dve", lambda e, wn=wn, j=j, ri=ri, s_=s_: e.tensor_copy(out=carry[:, s_, j, ri:ri + 1], in_=wk[wn][:, TS - 1:TS]),
                                         reads=[Bw[wn]], writes=[B_carry])
                                else:
                                    P.op("dve", lambda e, zn=zn, wn=wn, j=j, ri=ri, s_=s_: e.tensor_tensor_scan(out=wk[wn][:], data0=wk["Rt"][:], data1=wk[zn][:],
                                                                                                        initial=carry[:, s_, j, ri:ri + 1], op0=ALU.mult, op1=ALU.add),
                                         reads=[Bw["Rt"], Bw[zn], B_carry], writes=[Bw[wn]])
                            yield
                            P.op("dve", lambda e: e.tensor_tensor(out=wk["N1"][:], in0=wk["W1"][:], in1=wk["CT"][:], op=ALU.mult),
                                 reads=[Bw["W1"], Bw["CT"]], writes=[Bw["N1"]])
                            P.op("dve", lambda e: e.tensor_tensor(out=wk["N2"][:], in0=wk["W2"][:], in1=wk["ST"][:], op=ALU.mult),
                                 reads=[Bw["W2"], Bw["ST"]], writes=[Bw["N2"]])
                            P.op("dve", lambda e, jm=jm, s_=s_: e.tensor_tensor(out=Xs[s_][:, jm, 0, :], in0=wk["N1"][:], in1=wk["N2"][:], op=ALU.subtract),
                                 reads=[Bw["N1"], Bw["N2"]], writes=[B_X[s_][jm][0]])
                            P.op("pool", lambda e: e.tensor_tensor(out=wk["N3"][:], in0=wk["W1"][:], in1=wk["ST"][:], op=ALU.mult),
                                 reads=[Bw["W1"], Bw["ST"]], writes=[Bw["N3"]])
                            P.op("pool", lambda e: e.tensor_tensor(out=wk["N4"][:], in0=wk["W2"][:], in1=wk["CT"][:], op=ALU.mult),
                                 reads=[Bw["W2"], Bw["CT"]], writes=[Bw["N4"]])
                            P.op("pool", lambda e, jm=jm, s_=s_: e.tensor_tensor(out=Xs[s_][:, jm, 1, :], in0=wk["N3"][:], in1=wk["N4"][:], op=ALU.add),
                                 reads=[Bw["N3"], Bw["N4"]], writes=[B_X[s_][jm][1]])
                            yield
                    for _ in range(3):
                        yield
                    for s_ in range(nseq2):
                        r0 = s_ * SEQ
                        uT, BuT = uT_s[s_]
                        for p3 in range(3):
                            n = 0
                            for jm in range(4):
                                for ri in range(2):
                                    P.op("pe", lambda e, jm=jm, ri=ri, q=q, p3=p3, n=n, s_=s_: e.matmul(
                                        out=bankB[:, 128:472], lhsT=cwt[:, 4 * q + jm, ri, :], rhs=Xs[s_][:, jm, ri, p3 * 344:(p3 + 1) * 344],
                                        start=(n == 0), stop=(n == 7)), reads=[B_cw, B_X[s_][jm][ri]], writes=[B_bankBy])
                                    n += 1
                            P.op("dve", lambda e, uT=uT, q=q, p3=p3: e.scalar_tensor_tensor(
                                out=wk["M1"][:, p3 * 344:(p3 + 1) * 344], in0=uT[:, p3 * 344:(p3 + 1) * 344], scalar=dT[:, q:q + 1], in1=bankB[:, 128:472],
                                op0=ALU.mult, op1=ALU.add), reads=[B_bankBy, BuT, B_dT], writes=[Bw["M1"]])
                            yield
                        yg, Byg = YGR.next()
                        P.op("pool", lambda e: e.tensor_tensor(out=wk["M2"][:], in0=wk["M1"][:], in1=wk["M1"][:], op=ALU.mult), reads=[Bw["M1"]], writes=[Bw["M2"]])
                        P.op("pool", lambda e: e.tensor_scalar(out=wk["M2"][:], in0=wk["M2"][:], scalar1=0.044715, scalar2=1.0, op0=ALU.mult, op1=ALU.add),
                             reads=[Bw["M2"]], writes=[Bw["M2"]])
                        P.op("pool", lambda e: e.tensor_tensor(out=wk["M2"][:], in0=wk["M2"][:], in1=wk["M1"][:], op=ALU.mult), reads=[Bw["M2"], Bw["M1"]], writes=[Bw["M2"]])
                        yield
                        P.op("act", lambda e: e.activation(out=wk["M2"][:], in_=wk["M2"][:], func=AF.Sigmoid, scale=1.5957691216057308),
                             reads=[Bw["M2"]], writes=[Bw["M2"]])
                        P.op("pool", lambda e, yg=yg: e.tensor_tensor(out=yg[:], in0=wk["M2"][:], in1=wk["M1"][:], op=ALU.mult),
                             reads=[Bw["M2"], Bw["M1"]], writes=[Byg])
                        if seg == 0:
                            P.op("pool", lambda e, yg=yg, q=q, r0=r0: e.dma_start(out=YG[q, :, r0:r0 + TS - 16], in_=yg[:, 16:TS]), reads=[Byg], dma=1)
                        else:
                            P.op("pool", lambda e, yg=yg, q=q, r0=r0: e.dma_start(out=YG[q, :, r0 + TS - 16:r0 + SEQ], in_=yg[:, :]), reads=[Byg], dma=1)
                        yield

        g2 = gen2()
    alive3, alive2 = g3 is not None, g2 is not None
    while alive3 or alive2:
        for _ in range(2):
            if alive3:
                try:
                    next(g3)
                except StopIteration:
                    alive3 = False
        if alive2:
            try:
                next(g2)
            except StopIteration:
                alive2 = False
    end_phase()
    if ph2:

        begin_phase()
        psb = psum_banks(8)
        PS = Ring([(psb[i], P.buf(f"ps2b_{i}")) for i in range(8)])
        wgl = sb("g_w", [128, 4, 512], F32R); B_wgl = P.buf("g_w")
        P.op("sp", lambda e: e.dma_start(out=wgl[:], in_=wglu_d.rearrange("(k p) n -> p k n", p=128)), writes=[B_wgl], dma=1)
        gys = [sb(f"g_y{i}", [128, 4, 512], F32R) for i in range(2)]
        GYR = Ring([(gys[i], P.buf(f"g_y{i}")) for i in range(2)])
        gsg = [sb(f"g_s{i}", [128, 512]) for i in range(2)]
        GSR = Ring([(gsg[i], P.buf(f"g_s{i}")) for i in range(2)])
        gos = [sb(f"g_o{i}", [128, 512], F32R) for i in range(2)]
        GOR = Ring([(gos[i], P.buf(f"g_o{i}")) for i in range(2)])
        for tp in range(nseq2 * SEQ // 512):
            gy, Bgy = GYR.next()
            P.op("sp", lambda e, gy=gy, tp=tp: e.dma_start(out=gy[:], in_=YG[:, :, tp * 512:(tp + 1) * 512].rearrange("k p t -> p k t")), writes=[Bgy], dma=1)
            for c in range(4):
                pa, Bp = PS.next()
                for k in range(4):
                    P.op("pe", lambda e, pa=pa, k=k, c=c, gy=gy: e.matmul(out=pa[:, :], lhsT=wgl[:, k, c * 128:(c + 1) * 128], rhs=gy[:, k, :],
                                                                       start=(k == 0), stop=(k == 3)), reads=[B_wgl, Bgy], writes=[Bp])
                sg, Bsg = GSR.next()
                go, Bgo = GOR.next()
                P.op("act", lambda e, pa=pa, sg=sg: e.activation(out=sg[:], in_=pa[:, :], func=AF.Sigmoid), reads=[Bp], writes=[Bsg])
                P.op("dve", lambda e, sg=sg, go=go, gy=gy, c=c: e.tensor_tensor(out=go[:], in0=sg[:], in1=gy[:, c, :].bitcast(F32), op=ALU.mult),
                     reads=[Bsg, Bgy], writes=[Bgo])
                P.op("pool", lambda e, go=go, c=c, tp=tp: e.dma_start(out=Yssm[c, :, tp * 512:(tp + 1) * 512], in_=go[:]), reads=[Bgo], dma=1)
        end_phase()
    mid.close()


    H1T = dscr("H1T", [KC, 128, NREAL], F32R)
    H1N = dscr("H1N", [NREAL, D])
    wbs_d = din("w_br_ssm", [512, D], F32R)
    wba_d = din("w_br_attn", [1024, D], F32R)
    wo_d = din("w_o", [D, D], F32R)
    ln1g_d = din("ln1_gT", [128, KC]); ln1b_d = din("ln1_bT", [128, KC])
    ph4 = (dbg or {}).get("ph4", 1)
    if ph4:
        begin_phase()
        psb = psum_banks(8)
        PS = Ring([(psb[i], P.buf(f"ps4_{i}")) for i in range(8)])
        g1T = sb("p4_g1", [128, KC]); b1T = sb("p4_b1", [128, KC]); B_g1, B_b1 = P.bufs(2, "p4gb")
        P.op("sp", lambda e: e.dma_start(out=g1T[:], in_=ln1g_d), writes=[B_g1], dma=1)
        P.op("sp", lambda e: e.dma_start(out=b1T[:], in_=ln1b_d), writes=[B_b1], dma=1)
        hT4 = sb("p4_hT", [128, KC, 512], F32R); B_hT4 = P.buf("p4_hT")
        mT = sb("p4_mT", [128, KC, 512], F32R); B_mT = P.buf("p4_mT")
        ysT = sb("p4_ys", [128, 4, 512], F32R); B_ysT = P.buf("p4_ys")
        yaT = sb("p4_ya", [128, 8, 512], F32R); B_yaT = P.buf("p4_ya")
        w16 = [sb(f"p4_w16_{i}", [128, 8, 512], F32R) for i in range(5)]
        W16 = Ring([(w16[i], P.buf(f"p4_w16_{i}")) for i in range(5)])
        S1t = sb("p4_S1", [128, 4, 512]); B_S1 = [P.buf(f"p4_S1_{f}") for f in range(4)]
        sgs = [sb(f"p4_sg{i}", [128, 512]) for i in range(4)]
        SG = Ring([(sgs[i], P.buf(f"p4_sg{i}")) for i in range(4)])
        xt4 = [sb(f"p4_xt{i}", [128, D]) for i in range(2)]
        XT4 = Ring([(xt4[i], P.buf(f"p4_xt{i}")) for i in range(2)])
        st4 = [(sb(f"p4_stat{i}", [128, 4, 6]), sb(f"p4_mv{i}", [128, 2]), sb(f"p4_rstd{i}", [128, 1])) for i in range(2)]
        ST4 = Ring([(st4[i], P.buf(f"p4_st{i}")) for i in range(2)])
        w_in_v4 = w_in.rearrange("(k p) n -> p k n", p=128)
        wo_v = wo_d.rearrange("(k p) n -> p k n", p=128)
        wbs_v = wbs_d.rearrange("(k p) n -> p k n", p=128)
        wba_v = wba_d.rearrange("(k p) n -> p k n", p=128)

        def mm_chain(pa, Bp, wtile, Bw, off, nk, rhs_tile, B_rhs):
            for k in range(nk):
                P.op("pe", lambda e, k=k: e.matmul(out=pa[:, 0:512], lhsT=wtile[:, k, off:off + 128], rhs=rhs_tile[:, k, :],
                                                   start=(k == 0), stop=(k == nk - 1)), reads=[Bw, B_rhs], writes=[Bp])

        ngrp4 = (dbg or {}).get("ngrp4", NREAL // 512)
        for g in range(ngrp4):
            tok0 = g * 512
            P.op("sp", lambda e, tok0=tok0: e.dma_start(out=hT4[:], in_=HT[:, :, tok0:tok0 + 512].rearrange("k p t -> p k t")), writes=[B_hT4], dma=1)
            P.op("sp", lambda e, tok0=tok0: e.dma_start(out=ysT[:], in_=Yssm[:, :, tok0:tok0 + 512].rearrange("k p t -> p k t")), writes=[B_ysT], dma=1)
            P.op("sp", lambda e, tok0=tok0: e.dma_start(out=yaT[:], in_=Yattn[:, :, tok0:tok0 + 512].rearrange("k p t -> p k t")), writes=[B_yaT], dma=1)
            def load_half(src_v, col0, k0, nk):
                wt_, Bwt_ = W16.next()
                P.op("sp", lambda e, wt_=wt_: e.dma_start(out=wt_[:, 0:nk, :], in_=src_v[:, k0:k0 + nk, col0:col0 + 512]), writes=[Bwt_], dma=1)
                return wt_, Bwt_

            def mm16(pa, Bp, halves_, f, rhs_tile, B_rhs):
                for k in range(KC):
                    wt_, Bwt_ = halves_[k // 8]
                    P.op("pe", lambda e, k=k, wt_=wt_: e.matmul(out=pa[:, 0:512], lhsT=wt_[:, k % 8, f * 128:(f + 1) * 128], rhs=rhs_tile[:, k, :],
                                                            start=(k == 0), stop=(k == KC - 1)), reads=[Bwt_, B_rhs], writes=[Bp])

            def mmk(pa, Bp, wt_, Bwt_, nk, f, rhs_tile, B_rhs):
                for k in range(nk):
                    P.op("pe", lambda e, k=k: e.matmul(out=pa[:, 0:512], lhsT=wt_[:, k, f * 128:(f + 1) * 128], rhs=rhs_tile[:, k, :],
                                                       start=(k == 0), stop=(k == nk - 1)), reads=[Bwt_, B_rhs], writes=[Bp])

            for cb in range(4):
                gsh = [load_half(w_in_v4, 3584 + cb * 512, hk * 8, 8) for hk in range(2)]
                wsb, Bwsb = load_half(wbs_v, cb * 512, 0, 4)
                for f in range(4):
                    pgs, Bpgs = PS.next(); mm16(pgs, Bpgs, gsh, f, hT4, B_hT4)
                    pbs, Bpbs = PS.next(); mmk(pbs, Bpbs, wsb, Bwsb, 4, f, ysT, B_ysT)
                    s1, Bs1 = SG.next()
                    P.op("act", lambda e, s1=s1, pgs=pgs: e.activation(out=s1[:], in_=pgs[:, :], func=AF.Sigmoid), reads=[Bpgs], writes=[Bs1])
                    P.op("dve", lambda e, s1=s1, pbs=pbs, f=f: e.tensor_tensor(out=S1t[:, f, :], in0=s1[:], in1=pbs[:, :], op=ALU.mult), reads=[Bs1, Bpbs], writes=[B_S1[f]])
                gah = [load_half(w_in_v4, 5632 + cb * 512, hk * 8, 8) for hk in range(2)]
                wab, Bwab = load_half(wba_v, cb * 512, 0, 8)
                for f in range(4):
                    c = cb * 4 + f
                    pga, Bpga = PS.next(); mm16(pga, Bpga, gah, f, hT4, B_hT4)
                    pba, Bpba = PS.next(); mmk(pba, Bpba, wab, Bwab, 8, f, yaT, B_yaT)
                    s2, Bs2 = SG.next()
                    P.op("act", lambda e, s2=s2, pga=pga: e.activation(out=s2[:], in_=pga[:, :], func=AF.Sigmoid), reads=[Bpga], writes=[Bs2])
                    P.op("dve", lambda e, s2=s2, pba=pba: e.tensor_tensor(out=s2[:], in0=s2[:], in1=pba[:, :], op=ALU.mult), reads=[Bs2, Bpba], writes=[Bs2])
                    P.op("pool", lambda e, s2=s2, c=c, f=f: e.tensor_tensor(out=mT[:, c, :], in0=S1t[:, f, :], in1=s2[:], op=ALU.add),
                         reads=[B_S1[f], Bs2], writes=[B_mT])
            for cb in range(4):
                woh = [load_half(wo_v, cb * 512, hk * 8, 8) for hk in range(2)]
                for f in range(4):
                    c = cb * 4 + f
                    pm, Bpm = PS.next(); mm16(pm, Bpm, woh, f, mT, B_mT)
                    P.op("dve", lambda e, pm=pm, c=c: e.scalar_tensor_tensor(out=hT4[:, c, :], in0=hT4[:, c, :].bitcast(F32), scalar=ALPHA,
                                                                       in1=pm[:, :], op0=ALU.mult, op1=ALU.add),
                         reads=[Bpm, B_hT4], writes=[B_hT4])
            for t in range(4):
                xa, Bx = XT4.next()
                for b4 in range(4):
                    pa, Bp = PS.next()
                    for jj in range(4):
                        k = b4 * 4 + jj
                        P.op("pe", lambda e, pa=pa, jj=jj, k=k, t=t: e.transpose(out=pa[:, jj * 128:(jj + 1) * 128],
                                                                                 in_=hT4[:, k, t * 128:(t + 1) * 128].bitcast(F32), identity=ident[:]),
                             reads=[B_hT4, B_ident], writes=[Bp])
                    P.op("act", lambda e, pa=pa, xa=xa, b4=b4: e.activation(out=xa[:, b4 * 512:(b4 + 1) * 512], in_=pa[:, :], func=AF.Copy), reads=[Bp], writes=[Bx])
                (sa, ma, ra), Bs = ST4.next()
                for qq in range(4):
                    P.op("dve", lambda e, qq=qq, sa=sa, xa=xa: e.bn_stats(out=sa[:, qq, :], in_=xa[:, qq * 512:(qq + 1) * 512]), reads=[Bx], writes=[Bs])
                P.op("dve", lambda e, sa=sa, ma=ma: e.bn_aggr(out=ma[:], in_=sa[:].rearrange("p a b -> p (a b)")), reads=[Bs], writes=[Bs])
                P.op("act", lambda e, ra=ra, ma=ma: e.activation(out=ra[:], in_=ma[:, 1:2], func=AF.Sqrt, bias=epst[:], scale=1.0), reads=[Bs, B_eps], writes=[Bs])
                P.op("dve", lambda e, ra=ra: e.reciprocal(out=ra[:], in_=ra[:]), reads=[Bs], writes=[Bs])
                P.op("dve", lambda e, xa=xa, ma=ma, ra=ra: e.tensor_scalar(out=xa[:], in0=xa[:], scalar1=ma[:, 0:1], scalar2=ra[:], op0=ALU.subtract, op1=ALU.mult),
                     reads=[Bx, Bs], writes=[Bx])
                P.op("pool", lambda e, xa=xa, tok0=tok0, t=t: e.dma_start(out=H1N[tok0 + t * 128:tok0 + (t + 1) * 128, :], in_=xa[:]), reads=[Bx], dma=1)
                for b4 in range(4):
                    pa, Bp = PS.next()
                    for jj in range(4):
                        k = b4 * 4 + jj
                        P.op("pe", lambda e, pa=pa, jj=jj, k=k, xa=xa: e.transpose(out=pa[:, jj * 128:(jj + 1) * 128], in_=xa[:, k * 128:(k + 1) * 128], identity=ident[:]),
                             reads=[Bx, B_ident], writes=[Bp])
                    for jj in range(4):
                        k = b4 * 4 + jj
                        P.op("act", lambda e, pa=pa, jj=jj, k=k, t=t: e.activation(out=mT[:, k, t * 128:(t + 1) * 128], in_=pa[:, jj * 128:(jj + 1) * 128],
                                                                              func=AF.Identity, scale=g1T[:, k:k + 1], bias=b1T[:, k:k + 1]),
                             reads=[Bp, B_g1, B_b1], writes=[B_mT])
            P.op("pool", lambda e, tok0=tok0: e.dma_start(out=H1T[:, :, tok0:tok0 + 512].rearrange("k p t -> p k t"), in_=mT[:]), reads=[B_mT], dma=1)
        end_phase()


    CAP = 512
    RTOT = 32 * CAP
    BIG = float(RTOT + 64)
    YEXP = dscr("YEXP", [RTOT, D])
    LTOK = dscr("LTOK", [RTOT, 1], I32)
    wr_d = din("router_w", [D, 36], F32R)
    rb_d = din("router_b_b", [128, 36])
    wg_d = din("exp_w_gate", [32, D, 512], F32R)
    wu_d = din("exp_w_up", [32, D, 512], F32R)
    wd_d = din("exp_w_down", [32, 512, D], F32R)
    tri_d = din("tri_d", [128, 128])
    ecp1_d = din("ecp1_d", [128, 32])
    NT = NREAL // 128
    RK = sb("RK", [128, NT, 2], I32); WK = sb("WK", [128, NT, 2]); B_RK = P.buf("RK"); B_WK = P.buf("WK")
    ph5 = (dbg or {}).get("ph5", 1)
    ngrp5 = (dbg or {}).get("ngrp5", NREAL // 512)
    if ph5:
        begin_phase()
        psb = psum_banks(8)
        PS = Ring([(psb[i], P.buf(f"ps5_{i}")) for i in range(8)])
        wr = sb("p5_wr", [128, KC, 36], F32R); rbb = sb("p5_rb", [128, 36]); B_wr, B_rb = P.bufs(2, "p5r")
        P.op("sp", lambda e: e.dma_start(out=wr[:], in_=wr_d.rearrange("(k p) n -> p k n", p=128)), writes=[B_wr], dma=1)
        P.op("sp", lambda e: e.dma_start(out=rbb[:], in_=rb_d), writes=[B_rb], dma=1)
        trif = sb("p5_trif", [128, 128]); trib = sb("p5_trib", [128, 128], BF16); onesb = sb("p5_onesb", [128, 128], BF16)
        ecp1 = sb("p5_ecp1", [128, 32]); cnt = sb("p5_cnt", [128, 32]); zer = sb("p5_zer", [128, RTOT // 128], I32)
        B_tri, B_ones, B_ecp, B_cnt, B_zer, B_ltok = P.bufs(6, "p5c")
        P.op("sp", lambda e: e.dma_start(out=trif[:], in_=tri_d), writes=[B_tri], dma=1)
        P.op("sp", lambda e: e.dma_start(out=ecp1[:], in_=ecp1_d), writes=[B_ecp], dma=1)
        P.op("dve", lambda e: e.tensor_copy(out=trib[:], in_=trif[:]), reads=[B_tri], writes=[B_tri])
        P.op("dve", lambda e: e.memset(onesb[:], 1.0), writes=[B_ones])
        P.op("dve", lambda e: e.memset(cnt[:], 0.0), writes=[B_cnt])
        P.op("dve", lambda e: e.memset(zer[:], 0), writes=[B_zer])
        P.op("dve", lambda e: e.memset(RK[:], RTOT + 64), writes=[B_RK])
        P.op("dve", lambda e: e.memset(WK[:], 0.0), writes=[B_WK])
        P.op("sp", lambda e: e.dma_start(out=LTOK.rearrange("(p a) o -> p (a o)", p=128), in_=zer[:]), reads=[B_zer], writes=[B_ltok], dma=1)
        h1T = sb("p5_h1T", [128, KC, 512], F32R); B_h1T = P.buf("p5_h1T")
        NI = 2
        W32s = [sb(f"p5_W32_{i}", [128, 32]) for i in range(NI)]; B_W32s = [P.buf(f"p5_W32_{i}") for i in range(NI)]
        maskbs = [sb(f"p5_maskb{i}", [128, 32], BF16) for i in range(NI)]; B_maskbs = [P.buf(f"p5_maskb{i}") for i in range(NI)]
        toks = [sb(f"p5_tok{i}", [128, 1], I32) for i in range(2)]
        TOK = Ring([(toks[i], P.buf(f"p5_tok{i}")) for i in range(2)])
        rts = [sb(f"p5_rt{i}", [128, 288]) for i in range(NI)]; B_rts = [P.buf(f"p5_rt{i}") for i in range(NI)]

        def route_tile(g, t, si):
            ti = g * 4 + t
            rt = rts[si]; B_rt = B_rts[si]; W32 = W32s[si]; B_W32 = B_W32s[si]; maskb = maskbs[si]; B_maskb = B_maskbs[si]
            L = rt[:, 0:36]; gmx = rt[:, 36:37]; gex = rt[:, 40:44]; gsum = rt[:, 44:45]; gmask = rt[:, 48:52]
            esel = rt[:, 56:64]; v8 = rt[:, 64:72]; sel = rt[:, 72:80]; ex8 = rt[:, 80:88]; den = rt[:, 88:89]
            tmp32 = rt[:, 96:128]; wsel = rt[:, 128:136]; slot = rt[:, 160:192]; key = rt[:, 192:224]; okm = rt[:, 224:256]
            kv8 = rt[:, 256:264]; zz = rt[:, 264:266]; rf = rt[:, 266:268]

            def rdv(fn, rd=(), wr_=()):
                P.op("dve", fn, reads=[B_rt] + list(rd), writes=[B_rt] + list(wr_))

            if True:
                pa, Bp = PS.next()
                for k in range(KC):
                    P.op("pe", lambda e, pa=pa, k=k, t=t: e.matmul(out=pa[:, 0:36], lhsT=h1T[:, k, t * 128:(t + 1) * 128], rhs=wr[:, k, :],
                                                             start=(k == 0), stop=(k == KC - 1)), reads=[B_h1T, B_wr], writes=[Bp])
                P.op("dve", lambda e, pa=pa: e.tensor_tensor(out=L, in0=pa[:, 0:36], in1=rbb[:], op=ALU.add), reads=[Bp, B_rb, B_rt], writes=[B_rt])
                yield
                rdv(lambda e: e.tensor_reduce(out=gmx, in_=L[:, 0:4], axis=AX.X, op=ALU.max))
                yield
                rdv(lambda e: e.tensor_scalar(out=gex, in0=L[:, 0:4], scalar1=gmx, scalar2=None, op0=ALU.subtract))
                yield
                P.op("act", lambda e: e.activation(out=gex, in_=gex, func=AF.Exp), reads=[B_rt], writes=[B_rt])
                yield
                rdv(lambda e: e.tensor_reduce(out=gsum, in_=gex, axis=AX.X, op=ALU.add))
                yield
                rdv(lambda e: e.reciprocal(out=gsum, in_=gsum))
                yield
                rdv(lambda e: e.tensor_scalar(out=gmask, in0=L[:, 0:4], scalar1=gmx, scalar2=None, op0=ALU.is_equal))
                yield
                rdv(lambda e: e.tensor_tensor(out=tmp32.rearrange("p (g e) -> p g e", g=4), in0=L[:, 4:36].rearrange("p (g e) -> p g e", g=4),
                                              in1=gmask.unsqueeze(2).to_broadcast([128, 4, 8]), op=ALU.mult))
                rdv(lambda e: e.tensor_reduce(out=esel, in_=tmp32.rearrange("p (g e) -> p e g", g=4), axis=AX.X, op=ALU.add))
                yield
                rdv(lambda e: e.max(out=v8, in_=esel))
                yield
                rdv(lambda e: e.tensor_scalar(out=sel, in0=esel, scalar1=v8[:, 1:2], scalar2=None, op0=ALU.is_ge))
                yield
                rdv(lambda e: e.tensor_scalar(out=ex8, in0=esel, scalar1=v8[:, 0:1], scalar2=None, op0=ALU.subtract))
                yield
                P.op("act", lambda e: e.activation(out=ex8, in_=ex8, func=AF.Exp), reads=[B_rt], writes=[B_rt])
                yield
                rdv(lambda e: e.tensor_tensor(out=ex8, in0=ex8, in1=sel, op=ALU.mult))
                yield
                rdv(lambda e: e.tensor_reduce(out=den, in_=ex8, axis=AX.X, op=ALU.add))
                yield
                rdv(lambda e: e.reciprocal(out=den, in_=den))
                yield
                rdv(lambda e: e.tensor_tensor(out=den, in0=den, in1=gsum, op=ALU.mult))
                yield
                rdv(lambda e: e.tensor_scalar(out=wsel, in0=ex8, scalar1=den, scalar2=None, op0=ALU.mult))
                yield
                P.op("dve", lambda e: e.tensor_tensor(out=W32[:, :].rearrange("p (g e) -> p g e", g=4),
                                                      in0=gmask.unsqueeze(2).to_broadcast([128, 4, 8]),
                                                      in1=wsel.unsqueeze(1).to_broadcast([128, 4, 8]), op=ALU.mult),
                     reads=[B_rt], writes=[B_W32])
                P.op("dve", lambda e: e.tensor_scalar(out=maskb[:], in0=W32[:], scalar1=0.0, scalar2=None, op0=ALU.is_gt), reads=[B_W32], writes=[B_maskb])
                yield
                pc, Bpc = PS.next()
                P.op("pe", lambda e, pc=pc: e.matmul(out=pc[:, 0:32], lhsT=trib[:], rhs=maskb[:], start=True, stop=True), reads=[B_tri, B_maskb], writes=[Bpc])
                yield
                ptot, Bpt = PS.next()
                P.op("pe", lambda e, ptot=ptot: e.matmul(out=ptot[:, 0:32], lhsT=onesb[:], rhs=maskb[:], start=True, stop=True), reads=[B_ones, B_maskb], writes=[Bpt])
                yield
                rdv(lambda e, pc=pc: e.scalar_tensor_tensor(out=slot, in0=pc[:, 0:32], scalar=-1.0, in1=cnt[:], op0=ALU.add, op1=ALU.add), rd=[Bpc, B_cnt])
                P.op("dve", lambda e, ptot=ptot: e.tensor_tensor(out=cnt[:], in0=cnt[:], in1=ptot[:, 0:32], op=ALU.add), reads=[Bpt, B_cnt, B_rt], writes=[B_cnt])
                yield
                rdv(lambda e: e.tensor_scalar(out=okm, in0=slot, scalar1=float(CAP), scalar2=None, op0=ALU.is_lt))
                yield
                rdv(lambda e: e.tensor_tensor(out=okm, in0=okm, in1=maskb[:], op=ALU.mult), rd=[B_maskb])
                yield
                rdv(lambda e: e.tensor_tensor(out=key, in0=slot, in1=ecp1[:], op=ALU.add), rd=[B_ecp])
                yield
                rdv(lambda e: e.tensor_tensor(out=key, in0=key, in1=okm, op=ALU.mult))
                yield
                rdv(lambda e: e.tensor_tensor(out=tmp32, in0=W32[:], in1=okm, op=ALU.mult), rd=[B_W32])
                yield
                rdv(lambda e: e.max(out=kv8, in_=key))
                yield
                rdv(lambda e: e.tensor_scalar(out=zz, in0=kv8[:, 0:2], scalar1=0.0, scalar2=BIG, op0=ALU.is_equal, op1=ALU.mult))
                yield
                rdv(lambda e: e.scalar_tensor_tensor(out=rf, in0=kv8[:, 0:2], scalar=-1.0, in1=zz, op0=ALU.add, op1=ALU.add))
                yield
                P.op("dve", lambda e, ti=ti: e.tensor_copy(out=RK[:, ti, :], in_=rf), reads=[B_rt], writes=[B_RK])
                yield
                for kk in range(2):
                    P.op("dve", lambda e, ti=ti, kk=kk: e.scalar_tensor_tensor(out=slot, in0=key, scalar=kv8[:, kk:kk + 1], in1=tmp32,
                                                                           op0=ALU.is_equal, op1=ALU.mult, accum_out=WK[:, ti, kk:kk + 1]),
                         reads=[B_rt], writes=[B_rt, B_WK])
                tk, Btk = TOK.next()
                P.op("pool", lambda e, tk=tk, ti=ti: e.iota(tk[:], pattern=[[0, 1]], base=ti * 128, channel_multiplier=1), writes=[Btk])
                yield
                for kk in range(2):
                    P.op("pool", lambda e, tk=tk, ti=ti, kk=kk: e.indirect_dma_start(
                        out=LTOK[:, :], out_offset=bass.IndirectOffsetOnAxis(ap=RK[:, ti, kk:kk + 1], axis=0), in_=tk[:, :], in_offset=None,
                        bounds_check=P.reg(e, RTOT - 1), oob_is_err=False), reads=[B_RK, Btk], writes=[B_ltok], dma=1)


        for g in range(ngrp5):
            tok0 = g * 512
            P.op("sp", lambda e, tok0=tok0: e.dma_start(out=h1T[:], in_=H1T[:, :, tok0:tok0 + 512].rearrange("k p t -> p k t")), writes=[B_h1T], dma=1)
            for t2 in range(0, 4, NI):
                gens = [route_tile(g, t2 + i, i) for i in range(NI)]
                live = [True] * NI
                while any(live):
                    for i in range(NI):
                        if live[i]:
                            try:
                                next(gens[i])
                            except StopIteration:
                                live[i] = False
        end_phase()

        begin_phase()
        psb = psum_banks(8)
        PS = Ring([(psb[i], P.buf(f"ps5e_{i}")) for i in range(8)])
        g1T = sb("p5_g1", [128, KC]); b1T = sb("p5_b1", [128, KC]); B_g1, B_b1 = P.bufs(2, "p5gb")
        P.op("sp", lambda e: e.dma_start(out=g1T[:], in_=ln1g_d), writes=[B_g1], dma=1)
        P.op("sp", lambda e: e.dma_start(out=b1T[:], in_=ln1b_d), writes=[B_b1], dma=1)
        XTl = [sb(f"p5_XT{i}", [128, KC, CAP], F32R) for i in range(2)]
        B_XTl = [P.buf(f"p5_XT{i}") for i in range(2)]
        NB = CAP // 128
        xgs = [sb(f"p5_xg{i}", [128, D]) for i in range(3)]
        XG = Ring([(xgs[i], P.buf(f"p5_xg{i}")) for i in range(3)])
        idxs = [sb(f"p5_idx{i}", [128, 4], I32) for i in range(2)]
        IDX = Ring([(idxs[i], P.buf(f"p5_idx{i}")) for i in range(2)])
        w16 = [sb(f"p5_w16_{i}", [128, 8, 512], F32R) for i in range(4)]
        W16 = Ring([(w16[i], P.buf(f"p5_w16_{i}")) for i in range(4)])
        wdt = [sb(f"p5_wd{i}", [128, 4, 512], F32R) for i in range(2)]
        WD = Ring([(wdt[i], P.buf(f"p5_wd{i}")) for i in range(2)])
        hb = sb("p5_hb", [128, 4, CAP], F32R); Bhb = P.buf("p5_hb")
        sg4 = sb("p5_sg4", [128, 4, CAP]); Bsg4 = [P.buf(f"p5_sg4_{f}") for f in range(4)]
        yrs = [sb(f"p5_yr{i}", [128, NB, 512]) for i in range(2)]
        YR = Ring([(yrs[i], P.buf(f"p5_yr{i}")) for i in range(2)])
        nexp5 = (dbg or {}).get("nexp5", 32)

        def prep_pieces(ex, slot):
            XT_, BXT_ = XTl[slot], B_XTl[slot]
            state = {}

            def p_fetch():
                idx, Bidx = IDX.next()
                for b in range(NB):
                    P.op("sp", lambda e, idx=idx, b=b: e.dma_start(out=idx[:, b:b + 1], in_=LTOK[ex * CAP + b * 128:ex * CAP + (b + 1) * 128, :]), writes=[Bidx], dma=1)
                state["idx"] = (idx, Bidx)

            def p_gather(b):
                idx, Bidx = state["idx"]
                xg, Bxg = XG.next()
                P.op("pool", lambda e, xg=xg, idx=idx, b=b: e.indirect_dma_start(
                    out=xg[:, :], out_offset=None, in_=H1N[:, :], in_offset=bass.IndirectOffsetOnAxis(ap=idx[:, b:b + 1], axis=0),
                    bounds_check=P.reg(e, NREAL - 1), oob_is_err=False), reads=[Bidx], writes=[Bxg], dma=1)
                state[("xg", b)] = (xg, Bxg)

            def p_tr(b, half):
                if half == 0:
                    p_gather(b)
                xg, Bxg = state[("xg", b)]
                for b4 in range(half * 2, half * 2 + 2):
                    pa, Bp = PS.next()
                    for jj in range(4):
                        k = b4 * 4 + jj
                        P.op("pe", lambda e, pa=pa, jj=jj, k=k, xg=xg: e.transpose(out=pa[:, jj * 128:(jj + 1) * 128], in_=xg[:, k * 128:(k + 1) * 128], identity=ident[:]),
                             reads=[Bxg, B_ident], writes=[Bp])
                    for jj in range(4):
                        k = b4 * 4 + jj
                        P.op("act", lambda e, pa=pa, jj=jj, k=k, b=b, XT_=XT_: e.activation(out=XT_[:, k, b * 128:(b + 1) * 128], in_=pa[:, jj * 128:(jj + 1) * 128],
                                                                                  func=AF.Identity, scale=g1T[:, k:k + 1], bias=b1T[:, k:k + 1]),
                             reads=[Bp, B_g1, B_b1], writes=[BXT_])

            return [p_fetch] + [(lambda b=b, half=half: p_tr(b, half)) for b in range(NB) for half in range(2)]

        pieces = prep_pieces(0, 0)
        for pc_ in pieces:
            pc_()
        for ex in range(nexp5):
            slot = ex % 2
            XT_, BXT_ = XTl[slot], B_XTl[slot]
            nxt = prep_pieces(ex + 1, 1 - slot) if ex + 1 < nexp5 else []
            if nxt:
                nxt.pop(0)()
            halves = {}

            def load_halves(nm, wsrc, ex=ex, halves=halves):
                for hk in range(2):
                    wt_, Bwt_ = W16.next()
                    P.op("sp", lambda e, wt_=wt_, hk=hk, wsrc=wsrc: e.dma_start(
                        out=wt_[:], in_=wsrc[ex].rearrange("(k p) n -> p k n", p=128)[:, hk * 8:(hk + 1) * 8, :]), writes=[Bwt_], dma=1)
                    halves[(nm, hk)] = (wt_, Bwt_)

            load_halves("g", wg_d)
            load_halves("u", wu_d)
            for nm in ("g", "u"):
                for f in range(4):
                    pp, Bpp = PS.next()
                    for k in range(KC):
                        wt_, Bwt_ = halves[(nm, k // 8)]
                        P.op("pe", lambda e, pp=pp, k=k, f=f, wt_=wt_, XT_=XT_: e.matmul(out=pp[:, 0:CAP], lhsT=wt_[:, k % 8, f * 128:(f + 1) * 128], rhs=XT_[:, k, :],
                                                                                  start=(k == 0), stop=(k == KC - 1)), reads=[Bwt_, BXT_], writes=[Bpp])
                    if nm == "g":
                        P.op("act", lambda e, pp=pp, f=f: e.activation(out=sg4[:, f, :], in_=pp[:, 0:CAP], func=AF.Silu), reads=[Bpp], writes=[Bsg4[f]])
                    else:
                        P.op("dve", lambda e, pp=pp, f=f: e.tensor_tensor(out=hb[:, f, :], in0=sg4[:, f, :], in1=pp[:, 0:CAP], op=ALU.mult),
                             reads=[Bsg4[f], Bpp], writes=[Bhb])
                    if nxt:
                        nxt.pop(0)()
            for cg in range(4):
                wd_, Bwd = WD.next()
                P.op("sp", lambda e, wd_=wd_, ex=ex, cg=cg: e.dma_start(out=wd_[:], in_=wd_d[ex].rearrange("(k p) n -> p k n", p=128)[:, :, cg * 512:(cg + 1) * 512]),
                     writes=[Bwd], dma=1)
                yr, Byr = YR.next()
                for b in range(NB):
                    pa, Bp = PS.next()
                    for k in range(4):
                        P.op("pe", lambda e, pa=pa, k=k, b=b, wd_=wd_: e.matmul(out=pa[:, :], lhsT=hb[:, k, b * 128:(b + 1) * 128], rhs=wd_[:, k, :],
                                                                        start=(k == 0), stop=(k == 3)), reads=[Bhb, Bwd], writes=[Bp])
                    P.op("dve", lambda e, pa=pa, b=b, yr=yr: e.tensor_copy(out=yr[:, b, :], in_=pa[:, :]), reads=[Bp], writes=[Byr])
                P.op("pool", lambda e, ex=ex, cg=cg, yr=yr: e.dma_start(
                    out=YEXP[ex * CAP:(ex + 1) * CAP, cg * 512:(cg + 1) * 512].rearrange("(b p) n -> p b n", p=128), in_=yr[:]), reads=[Byr], dma=1)
            while nxt:
                nxt.pop(0)()
        end_phase()

    gb_d = [din(n, [128, D]) for n in ("ln1_g_b", "ln1_b_b", "ln2_g_b", "ln2_b_b")]
    ph6 = (dbg or {}).get("ph6", 1)
    if ph6:
        begin_phase()
        gbt = [sb(f"p6_gb{i}", [128, D]) for i in range(4)]
        B_gb = P.bufs(4, "p6gb")
        for i in range(4):
            P.op("sp", lambda e, i=i: e.dma_start(out=gbt[i][:], in_=gb_d[i]), writes=[B_gb[i]], dma=1)
        for i in range(2):
            P.op("pool", lambda e, i=i: e.tensor_scalar(out=gbt[i][:], in0=gbt[i][:], scalar1=ALPHA, scalar2=None, op0=ALU.mult), reads=[B_gb[i]], writes=[B_gb[i]])
        xs6 = [sb(f"p6_x{i}", [128, D]) for i in range(4)]
        X6 = Ring([(xs6[i], P.buf(f"p6_x{i}")) for i in range(4)])
        ys6 = [sb(f"p6_y{i}", [128, D]) for i in range(8)]
        Y6 = Ring([(ys6[i], P.buf(f"p6_y{i}")) for i in range(8)])
        for i in range(8):
            P.op("dve", lambda e, i=i: e.memset(ys6[i][:], 0.0), writes=[Y6.items[i][1]])
        st6 = [(sb(f"p6_stat{i}", [128, 4, 6]), sb(f"p6_mv{i}", [128, 2]), sb(f"p6_rstd{i}", [128, 1])) for i in range(2)]
        ST6 = Ring([(st6[i], P.buf(f"p6_st{i}")) for i in range(2)])
        ntile6 = (dbg or {}).get("ntile6", NREAL // 128)
        def p6_prep(tt6):
            xa, Bx = X6.next()
            r0 = tt6 * 128
            P.op("sp", lambda e, xa=xa, r0=r0: e.dma_start(out=xa[:], in_=H1N[r0:r0 + 128, :]), writes=[Bx], dma=1)
            ya = []
            for kk in range(2):
                yt_, Byt = Y6.next()
                P.op("pool", lambda e, yt_=yt_, tt6=tt6, kk=kk: e.indirect_dma_start(
                    out=yt_[:, :], out_offset=None, in_=YEXP[:, :], in_offset=bass.IndirectOffsetOnAxis(ap=RK[:, tt6, kk:kk + 1], axis=0),
                    bounds_check=P.reg(e, RTOT - 1), oob_is_err=False), reads=[B_RK], writes=[Byt], dma=1)
                ya.append((yt_, Byt))
            P.op("pool", lambda e, xa=xa: e.tensor_tensor(out=xa[:], in0=xa[:], in1=gbt[0][:], op=ALU.mult), reads=[Bx, B_gb[0]], writes=[Bx])
            P.op("pool", lambda e, xa=xa: e.tensor_tensor(out=xa[:], in0=xa[:], in1=gbt[1][:], op=ALU.add), reads=[Bx, B_gb[1]], writes=[Bx])
            return xa, Bx, ya

        def p6_finish(tt6, xa, Bx, ya):
            r0 = tt6 * 128
            for kk in range(2):
                yt_, Byt = ya[kk]
                P.op("dve", lambda e, xa=xa, yt_=yt_, tt6=tt6, kk=kk: e.scalar_tensor_tensor(out=xa[:], in0=yt_[:], scalar=WK[:, tt6, kk:kk + 1], in1=xa[:],
                                                                                   op0=ALU.mult, op1=ALU.add), reads=[Bx, Byt, B_WK], writes=[Bx])
            (sa, ma, ra), Bs = ST6.next()
            for qq in range(4):
                P.op("dve", lambda e, qq=qq, sa=sa, xa=xa: e.bn_stats(out=sa[:, qq, :], in_=xa[:, qq * 512:(qq + 1) * 512]), reads=[Bx], writes=[Bs])
            P.op("dve", lambda e, sa=sa, ma=ma: e.bn_aggr(out=ma[:], in_=sa[:].rearrange("p a b -> p (a b)")), reads=[Bs], writes=[Bs])
            P.op("act", lambda e, ra=ra, ma=ma: e.activation(out=ra[:], in_=ma[:, 1:2], func=AF.Sqrt, bias=epst[:], scale=1.0), reads=[Bs, B_eps], writes=[Bs])
            P.op("dve", lambda e, ra=ra: e.reciprocal(out=ra[:], in_=ra[:]), reads=[Bs], writes=[Bs])
            P.op("dve", lambda e, xa=xa, ma=ma, ra=ra: e.tensor_scalar(out=xa[:], in0=xa[:], scalar1=ma[:, 0:1], scalar2=ra[:], op0=ALU.subtract, op1=ALU.mult),
                 reads=[Bx, Bs], writes=[Bx])
            P.op("dve", lambda e, xa=xa: e.tensor_tensor(out=xa[:], in0=xa[:], in1=gbt[2][:], op=ALU.mult), reads=[Bx, B_gb[2]], writes=[Bx])
            P.op("pool", lambda e, xa=xa: e.tensor_tensor(out=xa[:], in0=xa[:], in1=gbt[3][:], op=ALU.add), reads=[Bx, B_gb[3]], writes=[Bx])
            P.op("act", lambda e, xa=xa, r0=r0: e.dma_start(out=out[r0:r0 + 128, :], in_=xa[:]), reads=[Bx], dma=1)

        preps = {}
        DEPTH6 = 2
        for tt6 in range(min(DEPTH6, ntile6)):
            preps[tt6] = p6_prep(tt6)
        for tt6 in range(ntile6):
            if tt6 + DEPTH6 < ntile6:
                preps[tt6 + DEPTH6] = p6_prep(tt6 + DEPTH6)
            p6_finish(tt6, *preps.pop(tt6))
        end_phase()

    P.flush()
    es.close()
    return nc


def _prep_shared(inputs):
    sh = {}
    sh["meta"] = np.ascontiguousarray(inputs["meta_tokens"], dtype=np.float32)
    sh["ident_d"] = np.eye(128, dtype=np.float32)
    sh["ln_in_gT"] = np.ascontiguousarray(np.asarray(inputs["ln_in_g"], np.float32).reshape(KC, 128).T)
    sh["ln_in_bT"] = np.ascontiguousarray(np.asarray(inputs["ln_in_b"], np.float32).reshape(KC, 128).T)
    sh["w_in"] = np.ascontiguousarray(inputs["w_in"][0], dtype=np.float32)
    f = lambda k: np.asarray(inputs[k], np.float32).reshape(-1)
    sh["tri_d"] = np.triu(np.ones((128, 128), np.float32))
    sh["ecp1_d"] = np.ascontiguousarray(np.broadcast_to((np.arange(32, dtype=np.float32) * 512 + 1)[None, :], (128, 32)))
    rep = lambda a, n=128: np.ascontiguousarray(np.broadcast_to(np.asarray(a, np.float32).reshape(1, -1), (n, np.asarray(a).size)))
    sh["router_w"] = np.ascontiguousarray(np.concatenate([inputs["router_g_w"][0], inputs["router_e_w"][0]], axis=1), dtype=np.float32)
    sh["router_b_b"] = rep(np.concatenate([np.asarray(inputs["router_g_b"][0]).reshape(-1), np.asarray(inputs["router_e_b"][0]).reshape(-1)]))
    sh["exp_w_gate"] = np.ascontiguousarray(inputs["exp_w_gate"][0], dtype=np.float32)
    sh["exp_w_up"] = np.ascontiguousarray(inputs["exp_w_up"][0], dtype=np.float32)
    sh["exp_w_down"] = np.ascontiguousarray(inputs["exp_w_down"][0], dtype=np.float32)
    sh["ln1_g_b"] = rep(inputs["ln1_g"][0]); sh["ln1_b_b"] = rep(inputs["ln1_b"][0])
    sh["ln2_g_b"] = rep(inputs["ln2_g"][0]); sh["ln2_b_b"] = rep(inputs["ln2_b"][0])
    colT = lambda a: np.ascontiguousarray(np.asarray(a, np.float32).reshape(KC, 128).T)
    sh["ln1_gT"] = colT(inputs["ln1_g"][0]); sh["ln1_bT"] = colT(inputs["ln1_b"][0])
    sh["w_br_ssm"] = np.ascontiguousarray(inputs["w_br_ssm"][0], dtype=np.float32)
    sh["w_br_attn"] = np.ascontiguousarray(inputs["w_br_attn"][0], dtype=np.float32)
    sh["w_o"] = np.ascontiguousarray(inputs["w_o"][0], dtype=np.float32)
    sl = lambda a: np.ascontiguousarray(np.asarray(a, np.float32).reshape(16, 128).T)
    sh["ssm_are"] = sl(inputs["ssm_a_re"][0])
    sh["ssm_aim"] = sl(inputs["ssm_a_im"][0])
    sh["ssm_ldt"] = sl(np.repeat(np.asarray(inputs["ssm_log_dt"][0], np.float32).reshape(32, 1), 64, axis=1))
    sl3 = lambda a: np.ascontiguousarray(np.asarray(a, np.float32).reshape(16, 128, 16).transpose(1, 0, 2))
    sh["ssm_bre"] = sl3(inputs["ssm_b_re"][0])
    sh["ssm_bim"] = sl3(inputs["ssm_b_im"][0])
    sh["ssm_cre"] = sl3(np.asarray(inputs["ssm_c_re"][0]).transpose(0, 2, 1))
    sh["ssm_cim"] = sl3(np.asarray(inputs["ssm_c_im"][0]).transpose(0, 2, 1))
    sh["ssm_dT"] = np.ascontiguousarray(np.asarray(inputs["ssm_d"][0], np.float32).reshape(4, 128).T)
    sh["ssm_wglu"] = np.ascontiguousarray(inputs["ssm_w_glu"][0], dtype=np.float32)
    sh["tt_d"] = np.ascontiguousarray(np.broadcast_to(np.arange(1032, dtype=np.float32)[None, :], (128, 1032)))
    sh["lamv"] = np.concatenate([f("attn_lambda_q1"), f("attn_lambda_k1"), f("attn_lambda_q2"), f("attn_lambda_k2")]).reshape(1, 256)
    sh["gsub_b"] = np.ascontiguousarray(np.broadcast_to(f("attn_subln_g")[None, :], (128, 128)))
    return sh


def kernel(**inputs):
    x = np.asarray(inputs["x"], np.float32)
    sh = _prep_shared(inputs)
    nc = build_program()
    in_maps = []
    for c in range(NCORES):
        m = dict(sh)
        m["x"] = np.ascontiguousarray(x[c * NSEQ:(c + 1) * NSEQ].reshape(NREAL, D))
        in_maps.append(m)
    res = run_bass_kernel_spmd(nc, in_maps, core_ids=list(range(NCORES)))
    outs = [np.asarray(r["out"], np.float32).reshape(NSEQ, SEQ, D) for r in res.results]
    return np.concatenate(outs, axis=0)
```
